# Optimizing a Trainium2 kernel written in Bass

```python
import jax, jax.numpy as jnp
from jax import lax
import numpy as np

D_MODEL = 1024
BATCH = 4
SEQ = 8192
DEPTH = 2

HEAD_DIM = 64
ROPE_THETA = 500000.0
ROT_DIM = HEAD_DIM // 4
GRID_W = 64
EPS = 1e-6
NEG_INF = -1e30
QBLK = 128

A_HEADS = 3 * D_MODEL // (4 * HEAD_DIM)
A_WIDTH = A_HEADS * HEAD_DIM
A_PATTERNS = ((128, 1), (512, 4), (2048, 16))
B_GROUP_DIM = 64
B_GROUPS = D_MODEL // (4 * B_GROUP_DIM)
B_WIDTH = B_GROUPS * B_GROUP_DIM
B_CHUNK = 128
EVEN_IN = 3 * A_WIDTH + 2 * B_WIDTH
C_HEADS = D_MODEL // (2 * HEAD_DIM)
C_Q_RANK = D_MODEL // 4
C_KV_RANK = D_MODEL // 8
C_NOPE = HEAD_DIM
C_ROPE = HEAD_DIM // 2
C_V = HEAD_DIM
D_HEADS = D_MODEL // (2 * HEAD_DIM)
D_KV_HEADS = 2
D_GROUP = D_HEADS // D_KV_HEADS
D_THETA = 10000.0
ODD_IN = C_Q_RANK + C_KV_RANK + C_ROPE + (D_HEADS + 2 * D_KV_HEADS) * HEAD_DIM
N_EXPERTS = 16
EC_FACTOR = 2
EXPERT_FF = 512

kernel_name = "hybrid_dilated_gmlp_mla_axialgqa_ecmoe_encoder"


def rmsnorm(x, g):
    xf = x.astype(jnp.float32)
    y = xf * lax.rsqrt(jnp.mean(xf * xf, axis=-1, keepdims=True) + EPS)
    return (y * g.astype(jnp.float32)).astype(x.dtype)


def rope(x, pos, theta):
    r = x.shape[-1]
    half = r // 2
    inv = jnp.power(jnp.float32(theta), -jnp.arange(half, dtype=jnp.float32) * (2.0 / r))
    ang = pos.astype(jnp.float32)[:, None] * inv[None, :]
    shape = (pos.shape[0],) + (1,) * (x.ndim - 3) + (half,)
    cos = jnp.cos(ang).reshape(shape)
    sin = jnp.sin(ang).reshape(shape)
    xf = x.astype(jnp.float32)
    x1, x2 = xf[..., :half], xf[..., half:]
    return jnp.concatenate([x1 * cos - x2 * sin, x1 * sin + x2 * cos], axis=-1).astype(x.dtype)


def partial_rope(x, pos):
    return jnp.concatenate([rope(x[..., :ROT_DIM], pos, ROPE_THETA), x[..., ROT_DIM:]], axis=-1)


def axial_rope(x, row, col):
    half = x.shape[-1] // 2
    return jnp.concatenate([rope(x[..., :half], row, D_THETA), rope(x[..., half:], col, D_THETA)], axis=-1)


def dilated_band_attention(q, k, v, window, dilation):
    b, s, h, dh = q.shape
    r = window // (2 * dilation)
    blk = r
    L = s // dilation
    nb = -(-L // blk)
    lp = nb * blk

    def residue(t):
        return t.reshape(b, L, dilation, h, dh).transpose(0, 2, 1, 3, 4)

    qr = jnp.pad(residue(q), ((0, 0), (0, 0), (0, lp - L), (0, 0), (0, 0)))
    qr = qr.reshape(b, dilation, nb, blk, h, dh)

    def band(t):
        tp = jnp.pad(residue(t), ((0, 0), (0, 0), (blk, lp - L + blk), (0, 0), (0, 0)))
        tp = tp.reshape(b, dilation, nb + 2, blk, h, dh)
        return jnp.concatenate([tp[:, :, 0:nb], tp[:, :, 1:nb + 1], tp[:, :, 2:nb + 2]], axis=3)

    kb, vb = band(k), band(v)
    scores = jnp.einsum('bdnqhc,bdnkhc->bdnhqk', qr, kb).astype(jnp.float32) * (dh ** -0.5)
    qi = jnp.arange(blk)[:, None]
    ki = jnp.arange(3 * blk)[None, :]
    in_band = jnp.abs(ki - blk - qi) <= r
    kpos = jnp.arange(nb)[:, None] * blk - blk + jnp.arange(3 * blk)[None, :]
    valid = in_band[None] & ((kpos >= 0) & (kpos < L))[:, None, :]
    scores = jnp.where(valid[None, None, :, None], scores, NEG_INF)
    lse = jax.nn.logsumexp(scores, axis=-1)
    p = jnp.exp(scores - lse[..., None])
    out = jnp.einsum('bdnhqk,bdnkhc->bdnqhc', p.astype(v.dtype), vb)
    out = out.reshape(b, dilation, lp, h, dh)[:, :, :L].transpose(0, 2, 1, 3, 4).reshape(b, s, h, dh)
    lse = lse.transpose(0, 1, 2, 4, 3).reshape(b, dilation, lp, h)[:, :, :L]
    lse = lse.transpose(0, 2, 1, 3).reshape(b, s, h)
    return out, lse


def dilated_mixture(q, k, v):
    results = [dilated_band_attention(q, k, v, w, d) for (w, d) in A_PATTERNS]
    outs = jnp.stack([o for (o, _) in results], axis=0)
    lses = jnp.stack([l for (_, l) in results], axis=0)
    wts = jax.nn.softmax(lses, axis=0)
    return jnp.einsum('pbsh,pbshc->bshc', wts.astype(q.dtype), outs)


def spatial_gating(z, v_norm, w_s, b_s):
    b, s, _ = z.shape
    z = jax.nn.gelu(z)
    u, vv = jnp.split(z, 2, axis=-1)
    u = u.reshape(b, s, B_GROUPS, B_GROUP_DIM)
    vv = rmsnorm(vv.reshape(b, s, B_GROUPS, B_GROUP_DIM), v_norm)
    vv = vv.reshape(b, s // B_CHUNK, B_CHUNK, B_GROUPS, B_GROUP_DIM)
    mixed = jnp.einsum('gpq,bnqgc->bnpgc', w_s, vv) + b_s.T[None, None, :, :, None]
    return (u * mixed.reshape(b, s, B_GROUPS, B_GROUP_DIM)).reshape(b, s, B_WIDTH)


def block_attention(q, k, v):
    b, s, kvh, g, dk = q.shape
    nq = s // QBLK
    scale = dk ** -0.5
    qb = q.reshape(b, nq, QBLK, kvh, g, dk).transpose(1, 0, 2, 3, 4, 5)

    def one(qblk):
        sc = jnp.einsum('bqkgc,bskc->bkgqs', qblk, k).astype(jnp.float32) * scale
        p = jax.nn.softmax(sc, axis=-1)
        return jnp.einsum('bkgqs,bskc->bqkgc', p.astype(v.dtype), v)

    out = lax.map(one, qb)
    return out.transpose(1, 0, 2, 3, 4, 5).reshape(b, s, kvh * g * v.shape[-1])


def mixer_even(h, pos, w_in, gmlp_norm, w_s, b_s, w_out):
    b, s, _ = h.shape
    proj = h @ w_in
    q, k, v, z = jnp.split(proj, [A_WIDTH, 2 * A_WIDTH, 3 * A_WIDTH], axis=-1)
    q = partial_rope(q.reshape(b, s, A_HEADS, HEAD_DIM), pos)
    k = partial_rope(k.reshape(b, s, A_HEADS, HEAD_DIM), pos)
    v = v.reshape(b, s, A_HEADS, HEAD_DIM)
    a_out = dilated_mixture(q, k, v).reshape(b, s, A_WIDTH)
    g_out = spatial_gating(z, gmlp_norm, w_s, b_s)
    return jnp.concatenate([a_out, g_out], axis=-1) @ w_out


def mixer_odd(h, pos, row, col, w_in, cq_norm, w_cq_up, ckv_norm, w_ckv_up, dq_norm, dk_norm, w_out):
    b, s, _ = h.shape
    proj = h @ w_in
    o1 = C_Q_RANK
    o2 = o1 + C_KV_RANK
    o3 = o2 + C_ROPE
    o4 = o3 + D_HEADS * HEAD_DIM
    o5 = o4 + D_KV_HEADS * HEAD_DIM
    cq, ckv, ck_rope, dq, dk, dv = jnp.split(proj, [o1, o2, o3, o4, o5], axis=-1)
    cq = (rmsnorm(cq, cq_norm) @ w_cq_up).reshape(b, s, C_HEADS, C_NOPE + C_ROPE)
    q_nope, q_rope = cq[..., :C_NOPE], rope(cq[..., C_NOPE:], pos, ROPE_THETA)
    kv = (rmsnorm(ckv, ckv_norm) @ w_ckv_up).reshape(b, s, C_HEADS, C_NOPE + C_V)
    k_nope, v_c = kv[..., :C_NOPE], kv[..., C_NOPE:]
    k_rope = rope(ck_rope, pos, ROPE_THETA)
    q_c = jnp.concatenate([q_nope, q_rope], axis=-1)[:, :, :, None]
    k_c = jnp.concatenate([k_nope, jnp.broadcast_to(k_rope[:, :, None], (b, s, C_HEADS, C_ROPE))], axis=-1)
    c_out = block_attention(q_c, k_c, v_c)
    dq = axial_rope(rmsnorm(dq.reshape(b, s, D_KV_HEADS, D_GROUP, HEAD_DIM), dq_norm), row, col)
    dk = axial_rope(rmsnorm(dk.reshape(b, s, D_KV_HEADS, HEAD_DIM), dk_norm), row, col)
    dv = dv.reshape(b, s, D_KV_HEADS, HEAD_DIM)
    d_out = block_attention(dq, dk, dv)
    return jnp.concatenate([c_out, d_out], axis=-1) @ w_out


def ec_moe(h, w_router, w_gate, w_up, w_down):
    b, s, d = h.shape
    cap = EC_FACTOR * s // N_EXPERTS
    logits = jnp.einsum('bsd,de->bse', h, w_router).astype(jnp.float32)
    aff = jax.nn.softmax(logits, axis=-1)
    gates, idx = lax.top_k(aff.transpose(0, 2, 1), cap)
    xs = jax.vmap(lambda hb, ib: hb[ib])(h, idx)
    g = jnp.einsum('becd,edf->becf', xs, w_gate)
    u = jnp.einsum('becd,edf->becf', xs, w_up)
    y = jnp.einsum('becf,efd->becd', jax.nn.silu(g) * u, w_down)
    y = y * gates[..., None].astype(y.dtype)
    return jax.vmap(lambda ib, yb: jnp.zeros((s, d), yb.dtype).at[ib.reshape(-1)].add(yb.reshape(-1, d)))(idx, y)


def setup_inputs(seed: int = 0) -> dict:
    key = jax.random.key(seed)
    ks = jax.random.split(key, 21)
    n_even = (DEPTH + 1) // 2
    n_odd = DEPTH // 2

    def normal(k, shape, scale):
        return jax.random.normal(k, shape, jnp.float32) * scale

    def gain(k, shape):
        return 1.0 + 0.02 * jax.random.normal(k, shape, jnp.float32)

    return {
        "x": normal(ks[0], (BATCH, SEQ, D_MODEL), 1.0),
        "norm_mix": gain(ks[1], (DEPTH, D_MODEL)),
        "norm_ffn": gain(ks[2], (DEPTH, D_MODEL)),
        "even_w_in": normal(ks[3], (n_even, D_MODEL, EVEN_IN), D_MODEL ** -0.5),
        "even_gmlp_norm": gain(ks[4], (n_even, B_GROUPS, B_GROUP_DIM)),
        "even_w_spatial": normal(ks[5], (n_even, B_GROUPS, B_CHUNK, B_CHUNK), B_CHUNK ** -0.5),
        "even_b_spatial": normal(ks[6], (n_even, B_GROUPS, B_CHUNK), 0.02),
        "even_w_out": normal(ks[7], (n_even, A_WIDTH + B_WIDTH, D_MODEL), (A_WIDTH + B_WIDTH) ** -0.5),
        "odd_w_in": normal(ks[8], (n_odd, D_MODEL, ODD_IN), D_MODEL ** -0.5),
        "odd_cq_norm": gain(ks[9], (n_odd, C_Q_RANK)),
        "odd_w_cq_up": normal(ks[10], (n_odd, C_Q_RANK, C_HEADS * (C_NOPE + C_ROPE)), C_Q_RANK ** -0.5),
        "odd_ckv_norm": gain(ks[11], (n_odd, C_KV_RANK)),
        "odd_w_ckv_up": normal(ks[12], (n_odd, C_KV_RANK, C_HEADS * (C_NOPE + C_V)), C_KV_RANK ** -0.5),
        "odd_dq_norm": gain(ks[13], (n_odd, HEAD_DIM)),
        "odd_dk_norm": gain(ks[14], (n_odd, HEAD_DIM)),
        "odd_w_out": normal(ks[15], (n_odd, C_HEADS * C_V + D_HEADS * HEAD_DIM, D_MODEL), (C_HEADS * C_V + D_HEADS * HEAD_DIM) ** -0.5),
        "moe_w_router": normal(ks[16], (DEPTH, D_MODEL, N_EXPERTS), D_MODEL ** -0.5),
        "moe_w_gate": normal(ks[17], (DEPTH, N_EXPERTS, D_MODEL, EXPERT_FF), D_MODEL ** -0.5),
        "moe_w_up": normal(ks[18], (DEPTH, N_EXPERTS, D_MODEL, EXPERT_FF), D_MODEL ** -0.5),
        "moe_w_down": normal(ks[19], (DEPTH, N_EXPERTS, EXPERT_FF, D_MODEL), EXPERT_FF ** -0.5),
        "final_norm": gain(ks[20], (D_MODEL,)),
    }


def reference(x, norm_mix, norm_ffn, even_w_in, even_gmlp_norm, even_w_spatial, even_b_spatial, even_w_out,
              odd_w_in, odd_cq_norm, odd_w_cq_up, odd_ckv_norm, odd_w_ckv_up, odd_dq_norm, odd_dk_norm, odd_w_out,
              moe_w_router, moe_w_gate, moe_w_up, moe_w_down, final_norm):
    b, s, _ = x.shape
    pos = jnp.arange(s, dtype=jnp.int32)
    rows = s // GRID_W
    row = jnp.repeat(jnp.arange(rows, dtype=jnp.int32), GRID_W)
    col = jnp.tile(jnp.arange(GRID_W, dtype=jnp.int32), rows)
    for i in range(DEPTH):
        j = i // 2
        h = rmsnorm(x, norm_mix[i])
        if i % 2 == 0:
            mix = mixer_even(h, pos, even_w_in[j], even_gmlp_norm[j], even_w_spatial[j], even_b_spatial[j], even_w_out[j])
        else:
            mix = mixer_odd(h, pos, row, col, odd_w_in[j], odd_cq_norm[j], odd_w_cq_up[j], odd_ckv_norm[j],
                            odd_w_ckv_up[j], odd_dq_norm[j], odd_dk_norm[j], odd_w_out[j])
        x = x + mix
        x = x + ec_moe(rmsnorm(x, norm_ffn[i]), moe_w_router[i], moe_w_gate[i], moe_w_up[i], moe_w_down[i])
    return rmsnorm(x, final_norm)
```

```python
import numpy as np
import ml_dtypes
import concourse.bass as bass
import concourse.mybir as mybir
from concourse.bass_utils import run_bass_kernel_spmd
from contextlib import ExitStack

F32 = mybir.dt.float32
BF16 = mybir.dt.bfloat16
ALU = mybir.AluOpType
AF = mybir.ActivationFunctionType
AX = mybir.AxisListType

S_LEN = 8192
D = 1024
PAD = 1024
NT = S_LEN // 128
CH = 16000
EPS = 1e-6
NEXP = 16
CAP = 2 * S_LEN // NEXP
FUSE_WAIT = True


class Res:
    __slots__ = ("name", "writers", "readers", "stream")

    def __init__(self, name):
        self.name = name
        self.writers = {}
        self.readers = {}
        self.stream = None


class Stream:
    __slots__ = ("sem", "count", "key")

    def __init__(self, sem, key):
        self.sem = sem
        self.count = 0
        self.key = key


class Buf:
    __slots__ = ("t", "r", "track")

    def __init__(self, t, name, track=True):
        self.t = t
        self.r = Res(name)
        self.track = track


class Sched:
    ENG = ("pe", "act", "dve", "pool", "sp")
    HANDLE = {"pe": "tensor", "act": "scalar", "dve": "vector", "pool": "gpsimd", "sp": "sync"}

    def __init__(self, nc, stack):
        self.nc = nc
        self.stack = stack
        self.prog = {e: [] for e in self.ENG}
        self.cnt = {e: 0 for e in self.ENG}
        self.seen = {e: {} for e in self.ENG}
        self.esems = {e: [] for e in self.ENG}
        self.streams = []
        self.free_sems = []
        self.live = []
        self.bg = set()
        self.nsem = 0
        self.total = 0

    def _new_sem(self, name):
        self.nsem += 1
        return self.stack.enter_context(self.nc.semaphore(name))

    def _esem(self, e, chunk):
        lst = self.esems[e]
        while len(lst) <= chunk:
            lst.append(self._new_sem(f"s_{e}_{len(lst)}"))
        return lst[chunk]

    def stream_of(self, res):
        if res.stream is None:
            if self.free_sems:
                self.free_sems.sort(key=lambda x: x[1])
                sem, base = self.free_sems.pop(0)
            else:
                sem, base = self._new_sem(f"d{len(self.streams)}"), 0
            res.stream = Stream(sem, ("d", len(self.streams)))
            res.stream.count = base
            self.streams.append(res.stream)
            self.live.append(res)
        return res.stream

    def mark_bg(self, res):
        self.bg.add(self.stream_of(res).key)

    def bg_done(self):
        self.bg = set()

    def release_streams(self):
        keep = []
        for res in self.live:
            st = res.stream
            if st.key in self.bg:
                keep.append(res)
                continue
            self.free_sems.append((st.sem, st.count))
            st.count = 0
            res.stream = None
        self.live = keep

    def _wait(self, e, key, val, payload):
        seen = self.seen[e]
        if seen.get(key, 0) >= val:
            return
        seen[key] = val
        self.prog[e].append(("wait", payload))

    def _wait_tok(self, e, key, val):
        if key[0] == "e":
            eng = key[1]
            if eng == e and e in ("pe", "sp"):
                return
            chunk, v = (val - 1) // CH, (val - 1) % CH + 1
            self._wait(e, key, val, (self._esem(eng, chunk), v))
        else:
            st = self.streams[key[1]]
            self._wait(e, key, val, (st.sem, val))

    def op(self, e, fn, reads=(), writes=(), stream=None):
        for r in reads:
            for k, v in r.writers.items():
                self._wait_tok(e, k, v)
        for w in writes:
            for k, v in w.writers.items():
                self._wait_tok(e, k, v)
            for k, v in w.readers.items():
                self._wait_tok(e, k, v)
        if stream is not None:
            st = self.stream_of(stream)
            st.count += 16
            key, val = st.key, st.count
            self.prog[e].append(("dma", fn, st.sem))
        else:
            self.cnt[e] += 1
            n = self.cnt[e]
            key, val = ("e", e), n
            self.prog[e].append(("ins", fn, self._esem(e, (n - 1) // CH)))
        for r in reads:
            r.readers[key] = val
        for w in writes:
            w.writers = {key: val}
            w.readers = {}

    def barrier(self, engines=None):
        for e in (engines or self.ENG):
            for st in self.streams:
                if st.count and st.key not in self.bg:
                    self._wait(e, st.key, st.count, (st.sem, st.count))
            for eng in self.ENG:
                if eng != e and self.cnt[eng]:
                    self._wait_tok(e, ("e", eng), self.cnt[eng])

    def emit(self):
        nc = self.nc
        import os
        if os.environ.get('DUMP'):
            for e in self.ENG:
                print('ENGINE', e)
                for it in self.prog[e][:int(os.environ['DUMP'])]:
                    if it[0] == 'wait':
                        print('   wait', it[1][0].name if hasattr(it[1][0], 'name') else it[1][0], it[1][1])
                    else:
                        print('  ', it[0], (it[2].name if hasattr(it[2], 'name') else it[2]), it[1].__code__.co_firstlineno)
        with nc.Block() as block:
            def mk(e):
                items = self.prog[e]

                def body(eng):
                    n = len(items)
                    for i, it in enumerate(items):
                        if it[0] == "wait":
                            if FUSE_WAIT and i + 1 < n and items[i + 1][0] != "wait":
                                continue
                            eng.wait_ge(it[1][0], it[1][1])
                        else:
                            ins = it[1](eng)
                            if FUSE_WAIT and i > 0 and items[i - 1][0] == "wait":
                                ins._wait_ge(items[i - 1][1][0], items[i - 1][1][1])
                            ins.then_inc(it[2], 16 if it[0] == "dma" else 1)
                return body

            for e in self.ENG:
                if self.prog[e]:
                    getattr(block, self.HANDLE[e])(mk(e))
        for e in self.ENG:
            self.total += len(self.prog[e])
            self.prog[e] = []


def _rope_table(pos, r, theta):
    half = r // 2
    inv = np.power(np.float32(theta), -np.arange(half, dtype=np.float32) * np.float32(2.0 / r)).astype(np.float32)
    ang = pos.astype(np.float32)[:, None] * inv[None, :]
    return np.concatenate([np.cos(ang), np.sin(ang)], axis=1).astype(np.float32)


def make_consts():
    pos = np.arange(S_LEN)
    c = {}
    c["c_ident_bf"] = np.eye(128, dtype=np.float32).astype(ml_dtypes.bfloat16)
    c["c_ident_f"] = np.eye(128, dtype=np.float32)
    sel = np.zeros((65, 64), np.float32)
    sel[64, :] = 1.0
    c["c_sel"] = sel
    c["c_rope0"] = _rope_table(pos, 16, 500000.0)
    c["c_ropec"] = _rope_table(pos, 32, 500000.0)
    c["c_roperow"] = _rope_table(pos // 64, 32, 10000.0)
    c["c_ropecol"] = _rope_table(pos % 64, 32, 10000.0)
    a = np.arange(128)[:, None]
    q = np.arange(128)[None, :]
    A = (a >= q).astype(np.float32)
    B = (a <= q).astype(np.float32)
    Ae = A * (a >= 64)
    Be = B * (a < 64)
    m = np.zeros((3, 128, 512), np.float32)
    m[0] = np.concatenate([A, B, A, B], 1)
    m[1] = np.concatenate([Ae, B, A, B], 1)
    m[2] = np.concatenate([A, B, A, Be], 1)
    c["c_mask"] = np.ascontiguousarray(m.transpose(1, 0, 2)).astype(ml_dtypes.bfloat16)
    return c


CONST_SPECS = {
    "c_ident_bf": ([128, 128], BF16), "c_ident_f": ([128, 128], F32), "c_sel": ([65, 64], F32),
    "c_rope0": ([S_LEN, 16], F32), "c_ropec": ([S_LEN, 32], F32), "c_roperow": ([S_LEN, 32], F32),
    "c_ropecol": ([S_LEN, 32], F32), "c_mask": ([128, 3, 512], BF16),
}

INPUT_SPECS = {
    "x": [S_LEN, D], "norm_mix": [2, D], "norm_ffn": [2, D], "even_w_in": [D, 2816],
    "even_gmlp_norm": [1, 256], "even_w_spatial": [4, 128, 128], "even_b_spatial": [4, 128],
    "even_w_out": [D, D], "odd_w_in": [D, 1184], "odd_cq_norm": [1, 256], "odd_w_cq_up": [256, 768],
    "odd_ckv_norm": [1, 128], "odd_w_ckv_up": [128, 1024], "odd_dq_norm": [1, 64], "odd_dk_norm": [1, 64],
    "odd_w_out": [D, D], "moe_w_router": [2, D, 16], "moe_w_gate": [2, 16, D, 512],
    "moe_w_up": [2, 16, D, 512], "moe_w_down": [2, 16, 512, D], "final_norm": [1, D],
}


class Builder:
    def __init__(self, dbg=(), upto="all"):
        self.nc = bass.Bass("TRN2", target_bir_lowering=False)
        self.dbg = set(dbg)
        self.upto = upto
        self.root = ExitStack()
        self.S = Sched(self.nc, self.root)
        self.inp = {}
        for k, shp in INPUT_SPECS.items():
            self.inp[k] = Buf(self.nc.dram_tensor(k, shp, F32, kind="ExternalInput").ap(), k, False)
        for k, (shp, dt) in CONST_SPECS.items():
            self.inp[k] = Buf(self.nc.dram_tensor(k, shp, dt, kind="ExternalInput").ap(), k, False)
        self.out = Buf(self.nc.dram_tensor("y", [S_LEN, D], F32, kind="ExternalOutput").ap(), "y", False)
        self.scr = {}
        self.phn = 0
        self.affall = self.sb(self.root, "affall", [128, NT, NEXP], F32)
        self.gw = self.sb(self.root, "gw", [128, NT, NEXP], F32)
        self.xa = self.dram("xa", [S_LEN, D], F32)
        self.xb = self.dram("xb", [S_LEN, D], F32)
        self.hTd = self.dram("hTd", [D, S_LEN], BF16)

    def dram(self, name, shape, dt):
        kind = "ExternalOutput" if name in self.dbg else "Internal"
        b = Buf(self.nc.dram_tensor(name, shape, dt, kind=kind).ap(), name, False)
        self.scr[name] = b
        return b

    def sb(self, ph, name, shape, dt):
        name = f"p{self.phn}_{name}"
        return Buf(ph.enter_context(self.nc.sbuf_tensor(name, shape, dt)), name)

    def ps(self, ph, name, shape, dt):
        name = f"p{self.phn}_{name}"
        return Buf(ph.enter_context(self.nc.psum_tensor(name, shape, dt)), name)

    def _op(self, e, fn, r, w):
        self.S.op(e, fn, reads=[b.r for b in r], writes=[b.r for b in w])

    def pe(self, fn, r, w):
        self._op("pe", fn, r, w)

    def act(self, fn, r, w):
        self._op("act", fn, r, w)

    def dve(self, fn, r, w):
        self._op("dve", fn, r, w)

    def pool(self, fn, r, w):
        self._op("pool", fn, r, w)

    def dma(self, q, out, in_, r, w, stream, **kw):
        self.S.op(q, lambda e: e.dma_start(out=out, in_=in_, **kw), reads=[b.r for b in r if b.track],
                  writes=[b.r for b in w if b.track], stream=stream.r)

    def phase_begin(self):
        self.phn += 1
        self.S.barrier()
        self.S.release_streams()
        return ExitStack()

    def phase_end(self, ph):
        self.S.emit()
        ph.close()

    def rmsnorm_rstd(self, src, srcbufs, junk, ssq, rstd, n, width=None):
        sc = float(n) ** -0.5
        self.pool(lambda e: e.memset(ssq.t[:, 0:1], 0.0), [], [ssq])
        self.act(lambda e: e.activation(out=junk, in_=src, func=AF.Square, scale=sc, accum_out=ssq.t[:, 0:1]),
                 srcbufs, [ssq])
        self.dve(lambda e: e.tensor_scalar_add(out=rstd.t[:, 0:1], in0=ssq.t[:, 0:1], scalar1=EPS), [ssq], [rstd])
        self.act(lambda e: e.activation(out=rstd.t[:, 0:1], in_=rstd.t[:, 0:1], func=AF.Sqrt), [rstd], [rstd])
        self.dve(lambda e: e.reciprocal(out=rstd.t[:, 0:1], in_=rstd.t[:, 0:1]), [rstd], [rstd])

    def build(self):
        with self.root:
            if self.upto in ("B1only", "B0only", "Eonly"):
                self.catT = self.dram("catT", [1024, S_LEN], BF16)
                if self.upto == "B1only":
                    self.QcT = self.dram("QcT", [768, S_LEN], BF16)
                    self.KcT = self.dram("KcT", [768, S_LEN], BF16)
                    self.Vc = self.dram("Vc", [S_LEN, 512], BF16)
                    self.QdT = self.dram("QdT", [512, S_LEN], BF16)
                    self.KdT = self.dram("KdT", [128, S_LEN], BF16)
                    self.Vd = self.dram("Vd", [S_LEN, 128], BF16)
                    self.layer1_B()
                elif self.upto == "B0only":
                    self.V0 = self.dram("V0", [PAD + S_LEN + PAD, 768], BF16)
                    self.QT0 = self.dram("QT0", [768, S_LEN], BF16)
                    self.KT0 = self.dram("KT0", [768, S_LEN], BF16)
                    self.layer0_B()
                else:
                    self.wg_bf = self.dram("wg_bf", [2, NEXP, D, 512], BF16)
                    self.wu_bf = self.dram("wu_bf", [2, NEXP, D, 512], BF16)
                    self.wd_bf = self.dram("wd_bf", [2, NEXP, 512, D], BF16)
                    self.phase_E(0, last=False)
                return self.finish()
            self.prologue()
            if self.upto == "pro":
                return self.finish()
            self.layer0_A()
            if self.upto == "A0":
                return self.finish()
            self.layer0_B()
            if self.upto == "B0":
                return self.finish()
            self.phase_C(0, self.inp["x"], self.inp["even_w_out"])
            if self.upto == "C0":
                return self.finish()
            self.phase_D()
            if self.upto == "D0":
                return self.finish()
            self.phase_E(0, last=False)
            if self.upto == "E0":
                return self.finish()
            self.layer1_A()
            if self.upto == "A1":
                return self.finish()
            self.layer1_B()
            if self.upto == "B1":
                return self.finish()
            self.phase_C(1, self.xb, self.inp["odd_w_out"])
            if self.upto == "C1":
                return self.finish()
            self.phase_D()
            self.phase_E(1, last=True)
            return self.finish()

    def finish(self):
        ph = self.phase_begin()
        self.phase_end(ph)
        return self.nc

    def prologue(self):
        self.wg_bf = self.dram("wg_bf", [2, NEXP, D, 512], BF16)
        self.wu_bf = self.dram("wu_bf", [2, NEXP, D, 512], BF16)
        self.wd_bf = self.dram("wd_bf", [2, NEXP, 512, D], BF16)

    def prologue_issue(self):
        for l in range(2):
            for e_ in range(NEXP):
                for src, dst in ((self.inp["moe_w_gate"], self.wg_bf), (self.inp["moe_w_up"], self.wu_bf),
                                 (self.inp["moe_w_down"], self.wd_bf)):
                    self.dma("pool", dst.t[l, e_], src.t[l, e_], [], [], dst)
        for dst in (self.wg_bf, self.wu_bf, self.wd_bf):
            self.S.mark_bg(dst.r)

    def layer0_A(self):
        nc = self.nc
        I = self.inp
        self.V0 = self.dram("V0", [PAD + S_LEN + PAD, 768], BF16)
        self.QT0 = self.dram("QT0", [768, S_LEN], BF16)
        self.KT0 = self.dram("KT0", [768, S_LEN], BF16)
        self.catT = self.dram("catT", [1024, S_LEN], BF16)
        ph = self.phase_begin()
        sb = lambda n, s, d: self.sb(ph, n, s, d)
        ps = lambda n, s, d: self.ps(ph, n, s, d)
        ident = sb("ident", [128, 128], BF16)
        win = sb("win", [128, 8, 2816], BF16)
        gmix = sb("gmix", [128, D], F32)
        gmn = sb("gmn", [128, 256], F32)
        wsf = sb("wsf", [128, 4, 128], F32)
        wsb = sb("wsb", [128, 4, 128], BF16)
        wsT = sb("wsT", [128, 4, 128], BF16)
        bsT = sb("bsT", [128, 4], F32)
        zero = sb("zero", [128, 768], BF16)
        xt = [sb(f"xt{i}", [128, D], F32) for i in range(2)]
        junk = sb("junk", [128, D], BF16)
        ssq = [sb(f"ssq{i}", [128, 1], F32) for i in range(2)]
        rstd = [sb(f"rstd{i}", [128, 1], F32) for i in range(2)]
        hn = [sb(f"hn{i}", [128, D], BF16) for i in range(2)]
        hT = [sb(f"hT{i}", [128, 8, 128], BF16) for i in range(2)]
        cs = [sb(f"cs{i}", [128, 16], F32) for i in range(2)]
        qk = [sb(f"qk{i}", [128, 1536], BF16) for i in range(2)]
        rt = [sb(f"rt{i}", [128, 24, 8], F32) for i in range(4)]
        vb = [sb(f"vb{i}", [128, 768], BF16) for i in range(2)]
        zg = sb("zg", [128, 512], F32)
        qkf = sb("qkf", [128, 1536], F32)
        sqv = sb("sqv", [128, 256], F32)
        ss4 = sb("ss4", [128, 4], F32)
        r4 = sb("r4", [128, 4], F32)
        vvn = sb("vvn", [128, 256], F32)
        vvb = sb("vvb", [128, 256], BF16)
        mxb = sb("mxb", [128, 256], F32)
        gout = sb("gout", [128, 256], BF16)
        stq = [sb(f"stq{i}", [128, 6, 512], BF16) for i in range(2)]
        stk = [sb(f"stk{i}", [128, 6, 512], BF16) for i in range(2)]
        stg = [sb(f"stg{i}", [64, 4, 512], BF16) for i in range(2)]
        T0 = ps("T0", [128, 1024], BF16)
        T1 = ps("T1", [128, 1024], BF16)
        pqk = ps("pqk", [128, 1536], F32)
        pvz = ps("pvz", [128, 1536], F32)

        self.dma("sp", ident.t[:], I["c_ident_bf"].t[:, :], [], [ident], ident)
        self.dma("pool", win.t[:], I["even_w_in"].t.rearrange("(c p) n -> p c n", p=128), [], [win], win)
        self.dma("sp", gmix.t[:], I["norm_mix"].t[0:1, :].partition_broadcast(128), [], [gmix], gmix)
        self.dma("sp", gmn.t[:], I["even_gmlp_norm"].t[0:1, :].partition_broadcast(128), [], [gmn], gmn)
        self.dma("sp", wsf.t[:], I["even_w_spatial"].t.rearrange("g p q -> p g q"), [], [wsf], wsf)
        self.dma("sp", bsT.t[:], I["even_b_spatial"].t.rearrange("g p -> p g"), [], [bsT], bsT,
                 allow_slow_non_contiguous=True)
        self.dve(lambda e: e.tensor_copy(out=wsb.t[:], in_=wsf.t[:]), [wsf], [wsb])
        for g in range(4):
            self.pe(lambda e, g=g: e.transpose(out=T0.t[:, g * 128:(g + 1) * 128], in_=wsb.t[:, g, :], identity=ident.t[:]),
                    [wsb, ident], [T0])
        self.act(lambda e: e.copy(out=wsT.t[:].rearrange("p g q -> p (g q)"), in_=T0.t[:, 0:512]), [T0], [wsT])
        self.pool(lambda e: e.memset(zero.t[:], 0.0), [], [zero])
        for side in range(2):
            for k in range(PAD // 128):
                r0 = (0 if side == 0 else PAD + S_LEN) + k * 128
                self.dma("sp", self.V0.t[r0:r0 + 128, :], zero.t[:], [zero], [self.V0], zero)

        import os
        for grp in range(int(os.environ.get('NGRP', NT // 4))):
            sq_, sk_, sg_ = stq[grp % 2], stk[grp % 2], stg[grp % 2]
            for tt in range(4):
                ti = grp * 4 + tt
                b = ti % 2
                x_, ssq_, rstd_, hn_, hT_, cs_, qk_, vb_ = xt[b], ssq[b], rstd[b], hn[b], hT[b], cs[b], qk[b], vb[b]
                tok = slice(ti * 128, (ti + 1) * 128)
                CUT = int(os.environ.get('A0CUT', 99))
                if CUT < 1: continue
                self.dma("sp", x_.t[:], I["x"].t[tok, :], [I["x"]], [x_], x_)
                self.dma("sp", cs_.t[:], I["c_rope0"].t[tok, :], [], [cs_], cs_)
                self.rmsnorm_rstd(x_.t[:], [x_], junk.t[:], ssq_, rstd_, D)
                self.dve(lambda e, x_=x_, rstd_=rstd_, hn_=hn_: e.scalar_tensor_tensor(
                    out=hn_.t[:], in0=x_.t[:], scalar=rstd_.t[:, 0:1], in1=gmix.t[:], op0=ALU.mult, op1=ALU.mult),
                    [x_, rstd_, gmix], [hn_])
                if CUT < 2: continue
                for j in range(8):
                    self.pe(lambda e, j=j, hn_=hn_: e.transpose(out=T0.t[:, j * 128:(j + 1) * 128],
                                                               in_=hn_.t[:, j * 128:(j + 1) * 128], identity=ident.t[:]),
                            [hn_, ident], [T0])
                self.act(lambda e, hT_=hT_: e.copy(out=hT_.t[:].rearrange("p c n -> p (c n)"), in_=T0.t[:]), [T0], [hT_])
                if CUT < 3: continue
                for bank in range(3):
                    for j in range(8):
                        self.pe(lambda e, j=j, bank=bank, hT_=hT_: e.matmul(
                            pqk.t[:, bank * 512:(bank + 1) * 512], lhsT=hT_.t[:, j, :],
                            rhs=win.t[:, j, bank * 512:(bank + 1) * 512], start=(j == 0), stop=(j == 7)),
                            [hT_, win], [pqk])
                for (o0, n_, c0) in ((0, 512, 1536), (512, 256, 2048), (1024, 512, 2304)):
                    for j in range(8):
                        self.pe(lambda e, j=j, o0=o0, n_=n_, c0=c0, hT_=hT_: e.matmul(
                            pvz.t[:, o0:o0 + n_], lhsT=hT_.t[:, j, :], rhs=win.t[:, j, c0:c0 + n_],
                            start=(j == 0), stop=(j == 7)), [hT_, win], [pvz])
                if CUT < 4: continue
                self.act(lambda e: e.copy(out=qkf.t[:], in_=pqk.t[:]), [pqk], [qkf])
                self.pool(lambda e, qk_=qk_: e.tensor_copy(out=qk_.t[:], in_=qkf.t[:]), [qkf], [qk_])
                pv = qkf.t[:].rearrange("p (h c) -> p h c", c=64)
                qv = qk_.t[:].rearrange("p (h c) -> p h c", c=64)
                cosb = cs_.t[:, 0:8].unsqueeze(1).to_broadcast([128, 24, 8])
                sinb = cs_.t[:, 8:16].unsqueeze(1).to_broadcast([128, 24, 8])
                x1 = pv[:, :, 0:8]
                x2 = pv[:, :, 8:16]
                self.dve(lambda e, x1=x1, cosb=cosb: e.tensor_tensor(out=rt[0].t[:], in0=x1, in1=cosb, op=ALU.mult), [qkf, cs_], [rt[0]])
                self.dve(lambda e, x2=x2, sinb=sinb: e.tensor_tensor(out=rt[1].t[:], in0=x2, in1=sinb, op=ALU.mult), [qkf, cs_], [rt[1]])
                self.dve(lambda e, x1=x1, sinb=sinb: e.tensor_tensor(out=rt[2].t[:], in0=x1, in1=sinb, op=ALU.mult), [qkf, cs_], [rt[2]])
                self.dve(lambda e, x2=x2, cosb=cosb: e.tensor_tensor(out=rt[3].t[:], in0=x2, in1=cosb, op=ALU.mult), [qkf, cs_], [rt[3]])
                self.pool(lambda e, qv=qv: e.tensor_tensor(out=qv[:, :, 0:8], in0=rt[0].t[:], in1=rt[1].t[:], op=ALU.subtract),
                          [rt[0], rt[1]], [qk_])
                self.pool(lambda e, qv=qv: e.tensor_tensor(out=qv[:, :, 8:16], in0=rt[2].t[:], in1=rt[3].t[:], op=ALU.add),
                          [rt[2], rt[3]], [qk_])
                if CUT < 5: continue
                self.act(lambda e, vb_=vb_: e.copy(out=vb_.t[:], in_=pvz.t[:, 0:768]), [pvz], [vb_])
                self.dma("sp", self.V0.t[PAD + ti * 128:PAD + (ti + 1) * 128, :], vb_.t[:], [vb_], [self.V0], vb_)
                if CUT < 6: continue
                self.act(lambda e: e.activation(out=zg.t[:], in_=pvz.t[:, 1024:1536], func=AF.Gelu_apprx_tanh), [pvz], [zg])
                self.dve(lambda e: e.tensor_tensor(out=sqv.t[:], in0=zg.t[:, 256:512], in1=zg.t[:, 256:512], op=ALU.mult), [zg], [sqv])
                self.dve(lambda e: e.tensor_reduce(out=ss4.t[:], in_=sqv.t[:].rearrange("p (g c) -> p g c", c=64), axis=AX.X, op=ALU.add), [sqv], [ss4])
                self.dve(lambda e: e.tensor_scalar(out=r4.t[:], in0=ss4.t[:], scalar1=1.0 / 64, scalar2=EPS, op0=ALU.mult, op1=ALU.add), [ss4], [r4])
                self.act(lambda e: e.activation(out=r4.t[:], in_=r4.t[:], func=AF.Sqrt), [r4], [r4])
                self.dve(lambda e: e.reciprocal(out=r4.t[:], in_=r4.t[:]), [r4], [r4])
                self.dve(lambda e: e.tensor_tensor(out=vvn.t[:].rearrange("p (g c) -> p g c", c=64),
                                                   in0=zg.t[:, 256:512].rearrange("p (g c) -> p g c", c=64),
                                                   in1=r4.t[:].unsqueeze(2).to_broadcast([128, 4, 64]), op=ALU.mult), [zg, r4], [vvn])
                self.pool(lambda e: e.tensor_tensor(out=vvb.t[:], in0=vvn.t[:], in1=gmn.t[:], op=ALU.mult), [vvn, gmn], [vvb])
                for g in range(4):
                    self.pe(lambda e, g=g: e.matmul(pvz.t[:, 768 + g * 64:768 + (g + 1) * 64], lhsT=wsT.t[:, g, :],
                                                    rhs=vvb.t[:, g * 64:(g + 1) * 64], start=True, stop=True), [wsT, vvb], [pvz])
                self.act(lambda e: e.copy(out=sqv.t[:], in_=pvz.t[:, 768:1024]), [pvz], [sqv])
                self.dve(lambda e: e.tensor_tensor(out=mxb.t[:].rearrange("p (g c) -> p g c", c=64),
                                                   in0=sqv.t[:].rearrange("p (g c) -> p g c", c=64),
                                                   in1=bsT.t[:].unsqueeze(2).to_broadcast([128, 4, 64]), op=ALU.add), [sqv, bsT], [mxb])
                self.pool(lambda e: e.tensor_tensor(out=gout.t[:], in0=mxb.t[:], in1=zg.t[:, 0:256], op=ALU.mult), [mxb, zg], [gout])
                if CUT < 7: continue
                for half, st_ in ((0, sq_), (1, sk_)):
                    for j in range(6):
                        c0 = half * 768 + j * 128
                        self.pe(lambda e, j=j, c0=c0, qk_=qk_: e.transpose(out=T1.t[:, j * 128:(j + 1) * 128], in_=qk_.t[:, c0:c0 + 128],
                                                                         identity=ident.t[:]), [qk_, ident], [T1])
                    self.act(lambda e, st_=st_, tt=tt: e.copy(out=st_.t[:, :, tt * 128:(tt + 1) * 128],
                                                            in_=T1.t[:, 0:768].rearrange("p (j n) -> p j n", n=128)), [T1], [st_])
                for g in range(4):
                    self.pe(lambda e, g=g: e.transpose(out=T1.t[0:64, g * 128:(g + 1) * 128], in_=gout.t[:, g * 64:(g + 1) * 64],
                                                       identity=ident.t[:]), [gout, ident], [T1])
                self.dve(lambda e, sg_=sg_, tt=tt: e.tensor_copy(out=sg_.t[:, :, tt * 128:(tt + 1) * 128],
                                                               in_=T1.t[0:64, 0:512].rearrange("p (j n) -> p j n", n=128)), [T1], [sg_])
            if CUT < 8: continue
            gtok = slice(grp * 512, (grp + 1) * 512)
            self.dma("sp", self.QT0.t.rearrange("(j p) t -> p j t", p=128)[:, :, gtok], sq_.t[:], [sq_], [self.QT0], sq_)
            self.dma("sp", self.KT0.t.rearrange("(j p) t -> p j t", p=128)[:, :, gtok], sk_.t[:], [sk_], [self.KT0], sk_)
            self.dma("sp", self.catT.t[768:1024, :].rearrange("(j p) t -> p j t", p=64)[:, :, gtok], sg_.t[:], [sg_], [self.catT], sg_)
        self.phase_end(ph)

    def layer0_B(self):
        I = self.inp
        ph = self.phase_begin()
        sb = lambda n, s, d: self.sb(ph, n, s, d)
        ps = lambda n, s, d: self.ps(ph, n, s, d)
        sel = sb("sel", [65, 64], F32)
        mask = sb("mask", [128, 3, 512], BF16)
        qT = [sb(f"qT{i}", [64, S_LEN], BF16) for i in range(2)]
        kT = [sb(f"kT{i}", [64, PAD + S_LEN + PAD], BF16) for i in range(2)]
        vA = [sb(f"vA{i}", [128, 80, 65], BF16) for i in range(2)]
        acc = sb("acc", [65, S_LEN], F32)
        aTs = sb("aTs", [64, S_LEN], BF16)
        rz = [sb(f"rz{i}", [64, 512], F32) for i in range(2)]
        Eb = [sb(f"Eb{i}", [128, 512], BF16) for i in range(3)]
        Pb = [sb(f"Pb{i}", [128, 512], BF16) for i in range(3)]
        Sp = [ps(f"Sp{i}", [128, 512], F32) for i in range(3)]
        Op = [ps(f"Op{i}", [65, 512], F32) for i in range(2)]
        bc = [ps(f"bc{i}", [64, 512], F32) for i in range(2)]
        self.dma("sp", sel.t[:], I["c_sel"].t[:, :], [], [sel], sel)
        self.dma("sp", mask.t[:], I["c_mask"].t[:, :, :], [], [mask], mask)
        for i in range(2):
            self.pool(lambda e, i=i: e.memset(kT[i].t[:, 0:PAD], 0.0), [], [kT[i]])
            self.pool(lambda e, i=i: e.memset(kT[i].t[:, PAD + S_LEN:], 0.0), [], [kT[i]])
            self.pool(lambda e, i=i: e.memset(vA[i].t[:, :, 64:65], 1.0), [], [vA[i]])
        if hasattr(self, "wg_bf") and self.upto != "B0only":
            self.prologue_issue()
        it = 0
        vi = 0
        og = 0
        for h in range(12):
            q_, k_ = qT[h % 2], kT[h % 2]
            self.dma("sp", q_.t[:], self.QT0.t[h * 64:(h + 1) * 64, :], [self.QT0], [q_], q_)
            self.dma("sp", k_.t[:, PAD:PAD + S_LEN], self.KT0.t[h * 64:(h + 1) * 64, :], [self.KT0], [k_], k_)
            for pi, d in enumerate((1, 4, 16)):
                L = S_LEN // d
                nb = L // 128
                ntile = nb + 1
                v_ = vA[vi % 2]
                vi += 1
                r0 = PAD - 64 * d
                rows = ntile * 128 * d
                src = self.V0.t[r0:r0 + rows, h * 64:(h + 1) * 64].rearrange("(m a i) c -> a i m c", a=128, i=d)
                dstv = v_.t[:, 0:d * ntile, 0:64].rearrange("a (i m) c -> a i m c", i=d)
                for i in range(d):
                    for m0 in range(0, ntile, 16):
                        m1 = min(ntile, m0 + 16)
                        self.dma("sp", dstv[:, i, m0:m1, :], src[:, i, m0:m1, :], [self.V0], [v_], v_)
                for i in range(d):
                    for gq in range(nb // 4):
                        n0 = gq * 4
                        O_ = Op[og % 2]
                        og += 1
                        for pr in range(2):
                            S_ = Sp[it % 3]
                            E_ = Eb[it % 3]
                            P_ = Pb[it % 3]
                            it += 1
                            for blk in range(2):
                                n = n0 + 2 * pr + blk
                                q0 = 128 * d * n + i
                                for ab in range(2):
                                    m = n + ab
                                    k0 = PAD - 64 * d + i + 128 * d * m
                                    c0 = (blk * 2 + ab) * 128
                                    self.pe(lambda e, S_=S_, c0=c0, k0=k0, q0=q0, d=d, k_=k_, q_=q_: e.matmul(
                                        S_.t[:, c0:c0 + 128], lhsT=k_.t[:, k0:k0 + 127 * d + 1:d], rhs=q_.t[:, q0:q0 + 127 * d + 1:d],
                                        start=True, stop=True), [k_, q_], [S_])
                            self.act(lambda e, S_=S_, E_=E_: e.activation(out=E_.t[:], in_=S_.t[:], func=AF.Exp, scale=0.125), [S_], [E_])
                            first = (n0 + 2 * pr == 0)
                            last = (n0 + 2 * pr + 2 == nb)
                            assert not (first and last)
                            mi = 1 if first else (2 if last else 0)
                            self.dve(lambda e, E_=E_, P_=P_, mi=mi: e.tensor_tensor(out=P_.t[:], in0=E_.t[:], in1=mask.t[:, mi, :], op=ALU.mult),
                                     [E_, mask], [P_])
                            for blk in range(2):
                                n = n0 + 2 * pr + blk
                                oc = (2 * pr + blk) * 128
                                for ab in range(2):
                                    m = n + ab
                                    c0 = (blk * 2 + ab) * 128
                                    self.pe(lambda e, O_=O_, oc=oc, c0=c0, v_=v_, P_=P_, ti=i * ntile + m, ab=ab: e.matmul(
                                        O_.t[:, oc:oc + 128], lhsT=v_.t[:, ti, :], rhs=P_.t[:, c0:c0 + 128],
                                        start=(ab == 0), stop=(ab == 1)), [v_, P_], [O_])
                        a0 = 128 * d * n0 + i
                        av = acc.t[:, a0:a0 + 511 * d + 1:d]
                        if pi == 0:
                            self.dve(lambda e, av=av, O_=O_: e.tensor_copy(out=av, in_=O_.t[:]), [O_], [acc])
                        else:
                            self.dve(lambda e, av=av, O_=O_: e.tensor_tensor(out=av, in0=O_.t[:], in1=av, op=ALU.add), [O_, acc], [acc])
            for c in range(S_LEN // 512):
                cs_ = slice(c * 512, (c + 1) * 512)
                b_ = bc[c % 2]
                r_ = rz[c % 2]
                self.pe(lambda e, b_=b_, cs_=cs_: e.matmul(b_.t[:], lhsT=sel.t[:], rhs=acc.t[:, cs_], start=True, stop=True), [sel, acc], [b_])
                self.dve(lambda e, b_=b_, r_=r_: e.reciprocal(out=r_.t[:], in_=b_.t[:]), [b_], [r_])
                self.dve(lambda e, r_=r_, cs_=cs_: e.tensor_tensor(out=aTs.t[:, cs_], in0=acc.t[0:64, cs_], in1=r_.t[:], op=ALU.mult), [acc, r_], [aTs])
            self.dma("sp", self.catT.t[h * 64:(h + 1) * 64, :], aTs.t[:], [aTs], [self.catT], aTs)
        self.phase_end(ph)


    def phase_C(self, l, xin, wout_in):
        I = self.inp
        ph = self.phase_begin()
        sb = lambda n, s, d: self.sb(ph, n, s, d)
        ps = lambda n, s, d: self.ps(ph, n, s, d)
        ident = sb("ident", [128, 128], BF16)
        identf = sb("identf", [128, 128], F32)
        wout = sb("wout", [64, 16, D], BF16)
        gffn = sb("gffn", [128, D], F32)
        wr32 = sb("wr32", [128, 8, NEXP], F32)
        catg = [sb(f"catg{i}", [64, 16, 512], BF16) for i in range(2)]
        xt = [sb(f"xt{i}", [128, D], F32) for i in range(2)]
        x1t = [sb(f"x1t{i}", [128, D], F32) for i in range(2)]
        junk = sb("junk", [128, D], BF16)
        ssq = [sb(f"ssq{i}", [128, 1], F32) for i in range(2)]
        rstd = [sb(f"rstd{i}", [128, 1], F32) for i in range(2)]
        hnf = [sb(f"hnf{i}", [128, D], F32) for i in range(2)]
        hnb = [sb(f"hnb{i}", [128, D], BF16) for i in range(2)]
        hTs = [sb(f"hTs{i}", [128, 8, 512], BF16) for i in range(2)]
        hT32 = [sb(f"hT32{i}", [128, 8, 128], F32) for i in range(2)]
        lgs = sb("lgs", [128, NEXP], F32)
        ex = sb("ex", [128, NEXP], F32)
        mx = sb("mx", [128, 1], F32)
        se = sb("se", [128, 1], F32)
        T0 = ps("T0", [128, 1024], BF16)
        T32 = ps("T32", [128, 1024], F32)
        pm = [ps(f"pm{i}", [128, 1024], F32) for i in range(2)]
        lg = ps("lg", [128, NEXP], F32)
        self.dma("sp", ident.t[:], I["c_ident_bf"].t[:, :], [], [ident], ident)
        self.dma("sp", identf.t[:], I["c_ident_f"].t[:, :], [], [identf], identf)
        self.dma("pool", wout.t[:], wout_in.t.rearrange("(c p) n -> p c n", p=64), [], [wout], wout)
        self.dma("sp", gffn.t[:], I["norm_ffn"].t[l:l + 1, :].partition_broadcast(128), [], [gffn], gffn)
        self.dma("sp", wr32.t[:], I["moe_w_router"].t[l].rearrange("(c p) e -> p c e", p=128), [], [wr32], wr32)
        for grp in range(NT // 4):
            cg = catg[grp % 2]
            hs = hTs[grp % 2]
            gtok = slice(grp * 512, (grp + 1) * 512)
            self.dma("sp", cg.t[:], self.catT.t.rearrange("(c p) t -> p c t", p=64)[:, :, gtok], [], [cg], cg)
            for tt in range(4):
                ti = grp * 4 + tt
                b = ti % 2
                x_, x1_, ssq_, rstd_, hnf_, hnb_, h32_, pm_ = xt[b], x1t[b], ssq[b], rstd[b], hnf[b], hnb[b], hT32[b], pm[b]
                tok = slice(ti * 128, (ti + 1) * 128)
                self.dma("sp", x_.t[:], xin.t[tok, :], [], [x_], x_)
                for half in range(2):
                    for c in range(16):
                        self.pe(lambda e, half=half, c=c, cg=cg, tt=tt, pm_=pm_: e.matmul(
                            pm_.t[:, half * 512:(half + 1) * 512], lhsT=cg.t[:, c, tt * 128:(tt + 1) * 128],
                            rhs=wout.t[:, c, half * 512:(half + 1) * 512], start=(c == 0), stop=(c == 15)), [cg, wout], [pm_])
                for half in range(2):
                    hs_ = slice(half * 512, (half + 1) * 512)
                    self.dve(lambda e, hs_=hs_, x_=x_, x1_=x1_, pm_=pm_: e.tensor_tensor(out=x1_.t[:, hs_], in0=pm_.t[:, hs_], in1=x_.t[:, hs_], op=ALU.add),
                             [pm_, x_], [x1_])
                self.dma("sp", self.xa.t[tok, :], x1_.t[:], [x1_], [], x1_)
                self.rmsnorm_rstd(x1_.t[:], [x1_], junk.t[:], ssq_, rstd_, D)
                self.dve(lambda e, x1_=x1_, rstd_=rstd_, hnf_=hnf_: e.scalar_tensor_tensor(
                    out=hnf_.t[:], in0=x1_.t[:], scalar=rstd_.t[:, 0:1], in1=gffn.t[:], op0=ALU.mult, op1=ALU.mult),
                    [x1_, rstd_, gffn], [hnf_])
                self.pool(lambda e, hnf_=hnf_, hnb_=hnb_: e.tensor_copy(out=hnb_.t[:], in_=hnf_.t[:]), [hnf_], [hnb_])
                for j in range(8):
                    self.pe(lambda e, j=j, hnb_=hnb_: e.transpose(out=T0.t[:, j * 128:(j + 1) * 128], in_=hnb_.t[:, j * 128:(j + 1) * 128],
                                                                identity=ident.t[:]), [hnb_, ident], [T0])
                self.act(lambda e, hs=hs, tt=tt: e.copy(out=hs.t[:, :, tt * 128:(tt + 1) * 128],
                                                      in_=T0.t[:].rearrange("p (c n) -> p c n", n=128)), [T0], [hs])
                for j in range(8):
                    self.pe(lambda e, j=j, hnf_=hnf_: e.transpose(out=T32.t[:, j * 128:(j + 1) * 128], in_=hnf_.t[:, j * 128:(j + 1) * 128],
                                                                identity=identf.t[:]), [hnf_, identf], [T32])
                for half in range(2):
                    self.act(lambda e, half=half, h32_=h32_: e.copy(out=h32_.t[:, half * 4:(half + 1) * 4, :].rearrange("p c n -> p (c n)"),
                                                                   in_=T32.t[:, half * 512:(half + 1) * 512]), [T32], [h32_])
                for j in range(8):
                    self.pe(lambda e, j=j, h32_=h32_: e.matmul(lg.t[:, :], lhsT=h32_.t[:, j, :], rhs=wr32.t[:, j, :],
                                                             start=(j == 0), stop=(j == 7)), [h32_, wr32], [lg])
                self.act(lambda e: e.copy(out=lgs.t[:], in_=lg.t[:]), [lg], [lgs])
                self.dve(lambda e: e.reduce_max(out=mx.t[:], in_=lgs.t[:], axis=AX.X), [lgs], [mx])
                self.dve(lambda e: e.tensor_scalar_mul(out=mx.t[:], in0=mx.t[:], scalar1=-1.0), [mx], [mx])
                self.pool(lambda e: e.memset(se.t[:], 0.0), [], [se])
                self.act(lambda e: e.activation(out=ex.t[:], in_=lgs.t[:], func=AF.Exp, bias=mx.t[:, 0:1], scale=1.0, accum_out=se.t[:, 0:1]),
                         [lgs, mx], [ex, se])
                self.dve(lambda e: e.reciprocal(out=se.t[:], in_=se.t[:]), [se], [se])
                self.dve(lambda e, ti=ti: e.tensor_scalar_mul(out=self.affall.t[:, ti, :], in0=ex.t[:], scalar1=se.t[:, 0:1]), [ex, se], [self.affall])
            self.dma("sp", self.hTd.t.rearrange("(c p) t -> p c t", p=128)[:, :, gtok], hs.t[:], [hs], [], hs)
        self.phase_end(ph)

    def phase_D(self):
        ph = self.phase_begin()
        sb = lambda n, s, d: self.sb(ph, n, s, d)
        ps = lambda n, s, d: self.ps(ph, n, s, d)
        ones = sb("ones", [128, 128], BF16)
        cmp_ = sb("cmp", [128, NT * NEXP], BF16)
        cnts = sb("cnts", [128, NT * NEXP], F32)
        cnt16 = sb("cnt16", [128, NEXP], F32)
        lo = sb("lo", [128, NEXP], F32)
        mid = sb("mid", [128, NEXP], F32)
        ge = sb("ge", [128, NEXP], F32)
        msk = sb("msk", [128, NT, NEXP], F32)
        cntp = ps("cntp", [128, NT * NEXP], F32)
        aff = self.affall
        self.pool(lambda e: e.memset(ones.t[:], 1.0), [], [ones])
        self.pool(lambda e: e.memset(lo.t[:], 0.0), [], [lo])
        for k in range(1, 29):
            c = 2.0 ** -k
            self.dve(lambda e, c=c: e.tensor_scalar_add(out=mid.t[:], in0=lo.t[:], scalar1=c), [lo], [mid])
            self.dve(lambda e: e.tensor_tensor(out=cmp_.t[:].rearrange("p (t x) -> p t x", x=NEXP), in0=aff.t[:],
                                               in1=mid.t[:].unsqueeze(1).to_broadcast([128, NT, NEXP]), op=ALU.is_ge), [aff, mid], [cmp_])
            for half in range(2):
                self.pe(lambda e, half=half: e.matmul(cntp.t[:, half * 512:(half + 1) * 512], lhsT=ones.t[:],
                                                      rhs=cmp_.t[:, half * 512:(half + 1) * 512], start=True, stop=True), [ones, cmp_], [cntp])
            self.act(lambda e: e.copy(out=cnts.t[:], in_=cntp.t[:]), [cntp], [cnts])
            self.dve(lambda e: e.tensor_reduce(out=cnt16.t[:], in_=cnts.t[:].rearrange("p (t x) -> p x t", x=NEXP), axis=AX.X, op=ALU.add),
                     [cnts], [cnt16])
            self.dve(lambda e, c=c: e.tensor_scalar(out=ge.t[:], in0=cnt16.t[:], scalar1=CAP - 0.5, scalar2=c, op0=ALU.is_ge, op1=ALU.mult),
                     [cnt16], [ge])
            self.dve(lambda e: e.tensor_tensor(out=lo.t[:], in0=lo.t[:], in1=ge.t[:], op=ALU.add), [lo, ge], [lo])
        self.dve(lambda e: e.tensor_tensor(out=msk.t[:], in0=aff.t[:], in1=lo.t[:].unsqueeze(1).to_broadcast([128, NT, NEXP]), op=ALU.is_ge),
                 [aff, lo], [msk])
        self.dve(lambda e: e.tensor_tensor(out=self.gw.t[:], in0=msk.t[:], in1=aff.t[:], op=ALU.mult), [msk, aff], [self.gw])
        self.phase_end(ph)

    def phase_E(self, l, last):
        I = self.inp
        self.S.bg_done()
        ph = self.phase_begin()
        sb = lambda n, s, d: self.sb(ph, n, s, d)
        ps = lambda n, s, d: self.ps(ph, n, s, d)
        SG = 2048
        accb = sb("accb", [128, SG // 128, D], F32)
        hs = sb("hs", [128, 8, SG], BF16)
        wgt = [sb(f"wgt{i}", [128, 8, 512], BF16) for i in range(2)]
        wut = [sb(f"wut{i}", [128, 8, 512], BF16) for i in range(2)]
        wdt = [sb(f"wdt{i}", [128, 4, D], BF16) for i in range(2)]
        sgt = [sb(f"sgt{i}", [128, 512], F32) for i in range(2)]
        actT = [sb(f"actT{i}", [128, 4, 512], BF16) for i in range(2)]
        pg = [ps(f"pg{i}", [128, 512], F32) for i in range(2)]
        pu = [ps(f"pu{i}", [128, 512], F32) for i in range(2)]
        py = [ps(f"py{i}", [128, 512], F32) for i in range(4)]
        if last:
            gfin = sb("gfin", [128, D], F32)
            junk = sb("junk", [128, D], BF16)
            ssq = [sb(f"ssq{i}", [128, 1], F32) for i in range(2)]
            rstd = [sb(f"rstd{i}", [128, 1], F32) for i in range(2)]
            yo = [sb(f"yo{i}", [128, D], F32) for i in range(2)]
            self.dma("sp", gfin.t[:], I["final_norm"].t[0:1, :].partition_broadcast(128), [], [gfin], gfin)
        wi = 0
        kf = 0
        ky = 0
        ka = 0
        import os
        ECUT = int(os.environ.get('ECUT', 9))
        for sgi in range(int(os.environ.get('ESG', S_LEN // SG))):
            t0 = sgi * SG
            for q4 in range(4):
                self.dma("sp", accb.t[:, q4 * 4:(q4 + 1) * 4, :],
                         self.xa.t[t0 + q4 * 512:t0 + (q4 + 1) * 512, :].rearrange("(t p) d -> p t d", p=128), [], [accb], accb)
            self.dma("sp", hs.t[:], self.hTd.t.rearrange("(c p) t -> p c t", p=128)[:, :, t0:t0 + SG], [], [hs], hs)
            for ex_ in range(int(os.environ.get('EEXP', NEXP))):
                wg_, wu_, wd_ = wgt[wi % 2], wut[wi % 2], wdt[wi % 2]
                wi += 1
                self.dma("sp", wg_.t[:], self.wg_bf.t[l, ex_].rearrange("(c p) f -> p c f", p=128), [], [wg_], wg_)
                self.dma("sp", wu_.t[:], self.wu_bf.t[l, ex_].rearrange("(c p) f -> p c f", p=128), [], [wu_], wu_)
                self.dma("sp", wd_.t[:], self.wd_bf.t[l, ex_].rearrange("(c p) n -> p c n", p=128), [], [wd_], wd_)
                for g4 in range(SG // 512):
                    if ECUT < 2: continue
                    tk = slice(g4 * 512, (g4 + 1) * 512)
                    aT_ = actT[ka % 2]
                    ka += 1
                    for fc in range(4):
                        pg_, pu_, sg_ = pg[kf % 2], pu[kf % 2], sgt[kf % 2]
                        kf += 1
                        fs = slice(fc * 128, (fc + 1) * 128)
                        for j in range(8):
                            self.pe(lambda e, j=j, pg_=pg_, wg_=wg_, fs=fs, tk=tk: e.matmul(pg_.t[:], lhsT=wg_.t[:, j, fs], rhs=hs.t[:, j, tk],
                                                                                      start=(j == 0), stop=(j == 7)), [wg_, hs], [pg_])
                        for j in range(8):
                            self.pe(lambda e, j=j, pu_=pu_, wu_=wu_, fs=fs, tk=tk: e.matmul(pu_.t[:], lhsT=wu_.t[:, j, fs], rhs=hs.t[:, j, tk],
                                                                                      start=(j == 0), stop=(j == 7)), [wu_, hs], [pu_])
                        self.act(lambda e, pg_=pg_, sg_=sg_: e.activation(out=sg_.t[:], in_=pg_.t[:], func=AF.Silu), [pg_], [sg_])
                        self.dve(lambda e, pu_=pu_, sg_=sg_, aT_=aT_, fc=fc: e.tensor_tensor(out=aT_.t[:, fc, :], in0=pu_.t[:], in1=sg_.t[:], op=ALU.mult),
                                 [pu_, sg_], [aT_])
                    for tt in range(4):
                        if ECUT < 3: continue
                        tl = g4 * 4 + tt
                        tile = sgi * (SG // 128) + tl
                        for ch in range(2):
                            py_ = py[ky % 4]
                            ky += 1
                            for fc in range(4):
                                self.pe(lambda e, py_=py_, aT_=aT_, fc=fc, tt=tt, wd_=wd_, ch=ch: e.matmul(
                                    py_.t[:], lhsT=aT_.t[:, fc, tt * 128:(tt + 1) * 128], rhs=wd_.t[:, fc, ch * 512:(ch + 1) * 512],
                                    start=(fc == 0), stop=(fc == 3)), [aT_, wd_], [py_])
                            av = accb.t[:, tl, ch * 512:(ch + 1) * 512]
                            self.dve(lambda e, av=av, py_=py_, tile=tile, ex_=ex_: e.scalar_tensor_tensor(
                                out=av, in0=py_.t[:], scalar=self.gw.t[:, tile, ex_:ex_ + 1], in1=av, op0=ALU.mult, op1=ALU.add),
                                [py_, self.gw, accb], [accb])
            if not last:
                for q4 in range(4):
                    self.dma("sp", self.xb.t[t0 + q4 * 512:t0 + (q4 + 1) * 512, :].rearrange("(t p) d -> p t d", p=128),
                             accb.t[:, q4 * 4:(q4 + 1) * 4, :], [accb], [], accb)
            else:
                for tl in range(SG // 128):
                    b = tl % 2
                    self.rmsnorm_rstd(accb.t[:, tl, :], [accb], junk.t[:], ssq[b], rstd[b], D)
                    self.dve(lambda e, tl=tl, b=b: e.scalar_tensor_tensor(out=yo[b].t[:], in0=accb.t[:, tl, :], scalar=rstd[b].t[:, 0:1],
                                                                        in1=gfin.t[:], op0=ALU.mult, op1=ALU.mult), [accb, rstd[b], gfin], [yo[b]])
                    r0 = t0 + tl * 128
                    self.dma("sp", self.out.t[r0:r0 + 128, :], yo[b].t[:], [yo[b]], [], yo[b])
        self.phase_end(ph)


    def rope_apply(self, x1, x2, cosb, sinb, o1, o2, rt, shape, srcbufs, tabbufs, outbuf):
        v = [r.t[:, 0:shape[1], 0:shape[2]] for r in rt]
        self.dve(lambda e: e.tensor_tensor(out=v[0], in0=x1, in1=cosb, op=ALU.mult), srcbufs + tabbufs, [rt[0]])
        self.dve(lambda e: e.tensor_tensor(out=v[1], in0=x2, in1=sinb, op=ALU.mult), srcbufs + tabbufs, [rt[1]])
        self.dve(lambda e: e.tensor_tensor(out=v[2], in0=x1, in1=sinb, op=ALU.mult), srcbufs + tabbufs, [rt[2]])
        self.dve(lambda e: e.tensor_tensor(out=v[3], in0=x2, in1=cosb, op=ALU.mult), srcbufs + tabbufs, [rt[3]])
        self.pool(lambda e: e.tensor_tensor(out=o1, in0=v[0], in1=v[1], op=ALU.subtract), [rt[0], rt[1]], [outbuf])
        self.pool(lambda e: e.tensor_tensor(out=o2, in0=v[2], in1=v[3], op=ALU.add), [rt[2], rt[3]], [outbuf])

    def layer1_A(self):
        I = self.inp
        self.QcT = self.dram("QcT", [768, S_LEN], BF16)
        self.KcT = self.dram("KcT", [768, S_LEN], BF16)
        self.Vc = self.dram("Vc", [S_LEN, 512], BF16)
        self.QdT = self.dram("QdT", [512, S_LEN], BF16)
        self.KdT = self.dram("KdT", [128, S_LEN], BF16)
        self.Vd = self.dram("Vd", [S_LEN, 128], BF16)
        ph = self.phase_begin()
        sb = lambda n, s, d: self.sb(ph, n, s, d)
        ps = lambda n, s, d: self.ps(ph, n, s, d)
        ident = sb("ident", [128, 128], BF16)
        win = sb("win", [128, 8, 1184], BF16)
        wcq = sb("wcq", [128, 2, 768], BF16)
        wckv = sb("wckv", [128, 1024], BF16)
        gmix = sb("gmix", [128, D], F32)
        gcq = sb("gcq", [128, 256], F32)
        gckv = sb("gckv", [128, 128], F32)
        gdq = sb("gdq", [128, 64], F32)
        gdk = sb("gdk", [128, 64], F32)
        xt = [sb(f"xt{i}", [128, D], F32) for i in range(2)]
        junk = sb("junk", [128, D], BF16)
        ssq = [sb(f"ssq{i}", [128, 1], F32) for i in range(2)]
        rstd = [sb(f"rstd{i}", [128, 1], F32) for i in range(2)]
        ssq2 = sb("ssq2", [128, 1], F32)
        rstd2 = sb("rstd2", [128, 1], F32)
        ssq3 = sb("ssq3", [128, 1], F32)
        rstd3 = sb("rstd3", [128, 1], F32)
        hn = [sb(f"hn{i}", [128, D], BF16) for i in range(2)]
        hT = [sb(f"hT{i}", [128, 8, 128], BF16) for i in range(2)]
        tabs = [sb(f"tabs{i}", [128, 96], F32) for i in range(2)]
        pf = sb("pf", [128, 1184], F32)
        cqn = sb("cqn", [128, 256], BF16)
        cqT = sb("cqT", [128, 2, 128], BF16)
        kvn = sb("kvn", [128, 128], BF16)
        kvT = sb("kvT", [128, 128], BF16)
        qcf = sb("qcf", [128, 768], F32)
        qcb = sb("qcb", [128, 768], BF16)
        kvf = sb("kvf", [128, 1024], F32)
        kcb = sb("kcb", [128, 768], BF16)
        vcb = [sb(f"vcb{i}", [128, 512], BF16) for i in range(2)]
        krf = sb("krf", [128, 1, 32], F32)
        rt = [sb(f"rt{i}", [128, 8, 16], F32) for i in range(4)]
        sq8 = sb("sq8", [128, 640], F32)
        ss10 = sb("ss10", [128, 10], F32)
        r10 = sb("r10", [128, 10], F32)
        dn = sb("dn", [128, 640], F32)
        dqb = sb("dqb", [128, 640], BF16)
        dvb = [sb(f"dvb{i}", [128, 128], BF16) for i in range(2)]
        stqc = [sb(f"stqc{i}", [96, 8, 512], BF16) for i in range(2)]
        stkc = [sb(f"stkc{i}", [96, 8, 512], BF16) for i in range(2)]
        stqd = [sb(f"stqd{i}", [128, 4, 512], BF16) for i in range(2)]
        stkd = [sb(f"stkd{i}", [128, 512], BF16) for i in range(2)]
        T0 = ps("T0", [128, 1024], BF16)
        T1 = ps("T1", [128, 1024], BF16)
        pq = ps("pq", [128, 1536], F32)
        pc = ps("pc", [128, 1024], F32)
        self.dma("sp", ident.t[:], I["c_ident_bf"].t[:, :], [], [ident], ident)
        self.dma("pool", win.t[:], I["odd_w_in"].t.rearrange("(c p) n -> p c n", p=128), [], [win], win)
        self.dma("pool", wcq.t[:], I["odd_w_cq_up"].t.rearrange("(c p) n -> p c n", p=128), [], [wcq], wcq)
        self.dma("pool", wckv.t[:], I["odd_w_ckv_up"].t[:, :], [], [wckv], wckv)
        self.dma("sp", gmix.t[:], I["norm_mix"].t[1:2, :].partition_broadcast(128), [], [gmix], gmix)
        self.dma("sp", gcq.t[:], I["odd_cq_norm"].t[0:1, :].partition_broadcast(128), [], [gcq], gcq)
        self.dma("sp", gckv.t[:], I["odd_ckv_norm"].t[0:1, :].partition_broadcast(128), [], [gckv], gckv)
        self.dma("sp", gdq.t[:], I["odd_dq_norm"].t[0:1, :].partition_broadcast(128), [], [gdq], gdq)
        self.dma("sp", gdk.t[:], I["odd_dk_norm"].t[0:1, :].partition_broadcast(128), [], [gdk], gdk)
        import os
        for grp in range(int(os.environ.get('NGRP1', NT // 4))):
            sqc, skc, sqd, skd = stqc[grp % 2], stkc[grp % 2], stqd[grp % 2], stkd[grp % 2]
            for tt in range(4):
                ti = grp * 4 + tt
                b = ti % 2
                x_, ssq_, rstd_, hn_, hT_, tb_, vcb_, dvb_ = xt[b], ssq[b], rstd[b], hn[b], hT[b], tabs[b], vcb[b], dvb[b]
                tok = slice(ti * 128, (ti + 1) * 128)
                tsl = slice(tt * 128, (tt + 1) * 128)
                A1CUT = int(os.environ.get('A1CUT', 99))
                if A1CUT < 1: continue
                self.dma("sp", x_.t[:], self.xb.t[tok, :], [], [x_], x_)
                self.dma("sp", tb_.t[:, 0:32], I["c_ropec"].t[tok, :], [], [tb_], tb_)
                self.dma("sp", tb_.t[:, 32:64], I["c_roperow"].t[tok, :], [], [tb_], tb_)
                self.dma("sp", tb_.t[:, 64:96], I["c_ropecol"].t[tok, :], [], [tb_], tb_)
                self.rmsnorm_rstd(x_.t[:], [x_], junk.t[:], ssq_, rstd_, D)
                self.dve(lambda e, x_=x_, rstd_=rstd_, hn_=hn_: e.scalar_tensor_tensor(
                    out=hn_.t[:], in0=x_.t[:], scalar=rstd_.t[:, 0:1], in1=gmix.t[:], op0=ALU.mult, op1=ALU.mult),
                    [x_, rstd_, gmix], [hn_])
                for j in range(8):
                    self.pe(lambda e, j=j, hn_=hn_: e.transpose(out=T0.t[:, j * 128:(j + 1) * 128], in_=hn_.t[:, j * 128:(j + 1) * 128],
                                                               identity=ident.t[:]), [hn_, ident], [T0])
                self.act(lambda e, hT_=hT_: e.copy(out=hT_.t[:].rearrange("p c n -> p (c n)"), in_=T0.t[:]), [T0], [hT_])
                for (c0, n_) in ((0, 512), (512, 512), (1024, 160)):
                    for j in range(8):
                        self.pe(lambda e, j=j, c0=c0, n_=n_, hT_=hT_: e.matmul(pq.t[:, c0:c0 + n_], lhsT=hT_.t[:, j, :], rhs=win.t[:, j, c0:c0 + n_],
                                                                            start=(j == 0), stop=(j == 7)), [hT_, win], [pq])
                self.act(lambda e: e.copy(out=pf.t[:], in_=pq.t[:, 0:1184]), [pq], [pf])
                if A1CUT < 2: continue
                self.rmsnorm_rstd(pf.t[:, 0:256], [pf], junk.t[:, 0:256], ssq2, rstd2, 256)
                self.dve(lambda e: e.scalar_tensor_tensor(out=cqn.t[:], in0=pf.t[:, 0:256], scalar=rstd2.t[:, 0:1], in1=gcq.t[:],
                                                          op0=ALU.mult, op1=ALU.mult), [pf, rstd2, gcq], [cqn])
                for j in range(2):
                    self.pe(lambda e, j=j: e.transpose(out=T1.t[:, j * 128:(j + 1) * 128], in_=cqn.t[:, j * 128:(j + 1) * 128], identity=ident.t[:]),
                            [cqn, ident], [T1])
                self.act(lambda e: e.copy(out=cqT.t[:].rearrange("p c n -> p (c n)"), in_=T1.t[:, 0:256]), [T1], [cqT])
                for (c0, n_) in ((0, 512), (512, 256)):
                    for j in range(2):
                        self.pe(lambda e, j=j, c0=c0, n_=n_: e.matmul(pc.t[:, c0:c0 + n_], lhsT=cqT.t[:, j, :], rhs=wcq.t[:, j, c0:c0 + n_],
                                                                     start=(j == 0), stop=(j == 1)), [cqT, wcq], [pc])
                self.act(lambda e: e.copy(out=qcf.t[:], in_=pc.t[:, 0:768]), [pc], [qcf])
                self.pool(lambda e: e.tensor_copy(out=qcb.t[:], in_=qcf.t[:]), [qcf], [qcb])
                qv = qcf.t[:].rearrange("p (h c) -> p h c", c=96)
                qo = qcb.t[:].rearrange("p (h c) -> p h c", c=96)
                cb = tb_.t[:, 0:16].unsqueeze(1).to_broadcast([128, 8, 16])
                sn = tb_.t[:, 16:32].unsqueeze(1).to_broadcast([128, 8, 16])
                self.rope_apply(qv[:, :, 64:80], qv[:, :, 80:96], cb, sn, qo[:, :, 64:80], qo[:, :, 80:96], rt, [128, 8, 16], [qcf], [tb_], qcb)
                if A1CUT < 3: continue
                self.rmsnorm_rstd(pf.t[:, 256:384], [pf], junk.t[:, 0:128], ssq3, rstd3, 128)
                self.dve(lambda e: e.scalar_tensor_tensor(out=kvn.t[:], in0=pf.t[:, 256:384], scalar=rstd3.t[:, 0:1], in1=gckv.t[:],
                                                          op0=ALU.mult, op1=ALU.mult), [pf, rstd3, gckv], [kvn])
                self.pe(lambda e: e.transpose(out=T1.t[:, 0:128], in_=kvn.t[:], identity=ident.t[:]), [kvn, ident], [T1])
                self.act(lambda e: e.copy(out=kvT.t[:], in_=T1.t[:, 0:128]), [T1], [kvT])
                for c0 in (0, 512):
                    self.pe(lambda e, c0=c0: e.matmul(pc.t[:, c0:c0 + 512], lhsT=kvT.t[:], rhs=wckv.t[:, c0:c0 + 512], start=True, stop=True),
                            [kvT, wckv], [pc])
                self.act(lambda e: e.copy(out=kvf.t[:], in_=pc.t[:]), [pc], [kvf])
                kv3 = kvf.t[:].rearrange("p (h c) -> p h c", c=128)
                ko = kcb.t[:].rearrange("p (h c) -> p h c", c=96)
                self.pool(lambda e: e.tensor_copy(out=ko[:, :, 0:64], in_=kv3[:, :, 0:64]), [kvf], [kcb])
                self.pool(lambda e, vcb_=vcb_: e.tensor_copy(out=vcb_.t[:].rearrange("p (h c) -> p h c", c=64), in_=kv3[:, :, 64:128]), [kvf], [vcb_])
                self.dma("sp", self.Vc.t[tok, :], vcb_.t[:], [vcb_], [], vcb_)
                kr = pf.t[:, 384:416].rearrange("p (o c) -> p o c", o=1)
                cb1 = tb_.t[:, 0:16].unsqueeze(1)
                sn1 = tb_.t[:, 16:32].unsqueeze(1)
                self.rope_apply(kr[:, :, 0:16], kr[:, :, 16:32], cb1, sn1, krf.t[:, :, 0:16], krf.t[:, :, 16:32], rt, [128, 1, 16], [pf], [tb_], krf)
                self.pool(lambda e: e.tensor_copy(out=ko[:, :, 64:96], in_=krf.t[:].to_broadcast([128, 8, 32])), [krf], [kcb])
                if A1CUT < 4: continue
                self.dve(lambda e: e.tensor_tensor(out=sq8.t[:], in0=pf.t[:, 416:1056], in1=pf.t[:, 416:1056], op=ALU.mult), [pf], [sq8])
                self.dve(lambda e: e.tensor_reduce(out=ss10.t[:], in_=sq8.t[:].rearrange("p (g c) -> p g c", c=64), axis=AX.X, op=ALU.add), [sq8], [ss10])
                self.dve(lambda e: e.tensor_scalar(out=r10.t[:], in0=ss10.t[:], scalar1=1.0 / 64, scalar2=EPS, op0=ALU.mult, op1=ALU.add), [ss10], [r10])
                self.act(lambda e: e.activation(out=r10.t[:], in_=r10.t[:], func=AF.Sqrt), [r10], [r10])
                self.dve(lambda e: e.reciprocal(out=r10.t[:], in_=r10.t[:]), [r10], [r10])
                self.dve(lambda e: e.tensor_tensor(out=dn.t[:].rearrange("p (g c) -> p g c", c=64),
                                                   in0=pf.t[:, 416:1056].rearrange("p (g c) -> p g c", c=64),
                                                   in1=r10.t[:].unsqueeze(2).to_broadcast([128, 10, 64]), op=ALU.mult), [pf, r10], [dn])
                self.pool(lambda e: e.tensor_tensor(out=dn.t[:, 0:512].rearrange("p (g c) -> p g c", c=64),
                                                    in0=dn.t[:, 0:512].rearrange("p (g c) -> p g c", c=64),
                                                    in1=gdq.t[:].unsqueeze(1).to_broadcast([128, 8, 64]), op=ALU.mult), [dn, gdq], [dn])
                self.pool(lambda e: e.tensor_tensor(out=dn.t[:, 512:640].rearrange("p (g c) -> p g c", c=64),
                                                    in0=dn.t[:, 512:640].rearrange("p (g c) -> p g c", c=64),
                                                    in1=gdk.t[:].unsqueeze(1).to_broadcast([128, 2, 64]), op=ALU.mult), [dn, gdk], [dn])
                for (g0, g1) in ((0, 8), (8, 10)):
                    ng = g1 - g0
                    dv_ = dn.t[:, g0 * 64:g1 * 64].rearrange("p (g c) -> p g c", c=64)
                    do_ = dqb.t[:, g0 * 64:g1 * 64].rearrange("p (g c) -> p g c", c=64)
                    for (tb0, d0) in ((32, 0), (64, 32)):
                        cbx = tb_.t[:, tb0:tb0 + 16].unsqueeze(1).to_broadcast([128, ng, 16])
                        snx = tb_.t[:, tb0 + 16:tb0 + 32].unsqueeze(1).to_broadcast([128, ng, 16])
                        self.rope_apply(dv_[:, :, d0:d0 + 16], dv_[:, :, d0 + 16:d0 + 32], cbx, snx,
                                        do_[:, :, d0:d0 + 16], do_[:, :, d0 + 16:d0 + 32], rt, [128, ng, 16], [dn], [tb_], dqb)
                self.pool(lambda e, dvb_=dvb_: e.tensor_copy(out=dvb_.t[:], in_=pf.t[:, 1056:1184]), [pf], [dvb_])
                self.dma("sp", self.Vd.t[tok, :], dvb_.t[:], [dvb_], [], dvb_)
                if A1CUT < 5: continue
                for h in range(8):
                    self.pe(lambda e, h=h: e.transpose(out=T1.t[0:96, h * 128:(h + 1) * 128], in_=qcb.t[:, h * 96:(h + 1) * 96], identity=ident.t[:]),
                            [qcb, ident], [T1])
                self.act(lambda e, sqc=sqc, tsl=tsl: e.copy(out=sqc.t[:, :, tsl], in_=T1.t[0:96, :].rearrange("p (h n) -> p h n", n=128)), [T1], [sqc])
                for h in range(8):
                    self.pe(lambda e, h=h: e.transpose(out=T0.t[0:96, h * 128:(h + 1) * 128], in_=kcb.t[:, h * 96:(h + 1) * 96], identity=ident.t[:]),
                            [kcb, ident], [T0])
                self.act(lambda e, skc=skc, tsl=tsl: e.copy(out=skc.t[:, :, tsl], in_=T0.t[0:96, :].rearrange("p (h n) -> p h n", n=128)), [T0], [skc])
                for j in range(5):
                    self.pe(lambda e, j=j: e.transpose(out=T1.t[:, j * 128:(j + 1) * 128], in_=dqb.t[:, j * 128:(j + 1) * 128], identity=ident.t[:]),
                            [dqb, ident], [T1])
                self.act(lambda e, sqd=sqd, tsl=tsl: e.copy(out=sqd.t[:, :, tsl], in_=T1.t[:, 0:512].rearrange("p (h n) -> p h n", n=128)), [T1], [sqd])
                self.act(lambda e, skd=skd, tsl=tsl: e.copy(out=skd.t[:, tsl], in_=T1.t[:, 512:640]), [T1], [skd])
            if A1CUT < 6: continue
            gtok = slice(grp * 512, (grp + 1) * 512)
            self.dma("sp", self.QcT.t.rearrange("(h p) t -> p h t", p=96)[:, :, gtok], sqc.t[:], [sqc], [], sqc)
            self.dma("sp", self.KcT.t.rearrange("(h p) t -> p h t", p=96)[:, :, gtok], skc.t[:], [skc], [], skc)
            self.dma("sp", self.QdT.t.rearrange("(j p) t -> p j t", p=128)[:, :, gtok], sqd.t[:], [sqd], [], sqd)
            self.dma("sp", self.KdT.t[:, gtok], skd.t[:], [skd], [], skd)
        self.phase_end(ph)

    def layer1_B(self):
        I = self.inp
        ph = self.phase_begin()
        sb = lambda n, s, d: self.sb(ph, n, s, d)
        ps = lambda n, s, d: self.ps(ph, n, s, d)
        sel = sb("sel", [65, 64], F32)
        qT = [sb(f"qT{i}", [96, S_LEN], BF16) for i in range(2)]
        kT = [sb(f"kT{i}", [96, S_LEN], BF16) for i in range(2)]
        vA = [sb(f"vA{i}", [128, NT, 65], BF16) for i in range(2)]
        oTs = [sb(f"oTs{i}", [64, S_LEN], BF16) for i in range(2)]
        osb = [sb(f"osb{i}", [65, 512], F32) for i in range(2)]
        rz = [sb(f"rz{i}", [64, 512], F32) for i in range(2)]
        Eb = [sb(f"Eb{i}", [128, 512], BF16) for i in range(6)]
        Sp = [ps(f"Sp{i}", [128, 512], F32) for i in range(5)]
        Op = [ps(f"Op{i}", [65, 512], F32) for i in range(2)]
        bc = [ps(f"bc{i}", [64, 512], F32) for i in range(1)]
        self.dma("sp", sel.t[:], I["c_sel"].t[:, :], [], [sel], sel)
        for i in range(2):
            self.pool(lambda e, i=i: e.memset(vA[i].t[:, :, 64:65], 1.0), [], [vA[i]])
        import os
        NH = int(os.environ.get('NH1', 16))
        NQC = int(os.environ.get('NQC1', S_LEN // 512))
        LA = 3
        kvi = -1
        prev_kv = None
        gi = 0
        oc_i = 0
        pending = []
        for h in range(NH):
            if h < 8:
                dk, scale = 96, 96.0 ** -0.5
                qsrc = self.QcT.t[h * 96:(h + 1) * 96, :]
                ksrc = self.KcT.t[h * 96:(h + 1) * 96, :]
                vsrc = self.Vc.t[:, h * 64:(h + 1) * 64]
                kvkey = ("c", h)
            else:
                hd = h - 8
                kvh = hd // 4
                dk, scale = 64, 0.125
                qsrc = self.QdT.t[hd * 64:(hd + 1) * 64, :]
                ksrc = self.KdT.t[kvh * 64:(kvh + 1) * 64, :]
                vsrc = self.Vd.t[:, kvh * 64:(kvh + 1) * 64]
                kvkey = ("d", kvh)
            q_ = qT[h % 2]
            self.dma("sp", q_.t[0:dk, :], qsrc, [], [q_], q_)
            if kvkey != prev_kv:
                kvi += 1
                prev_kv = kvkey
                k_, v_ = kT[kvi % 2], vA[kvi % 2]
                self.dma("sp", k_.t[0:dk, :], ksrc, [], [k_], k_)
                vv = vsrc.rearrange("(m a) c -> a m c", a=128)
                for m0 in range(0, NT, 16):
                    self.dma("pool", v_.t[:, m0:m0 + 16, 0:64], vv[:, m0:m0 + 16, :], [], [v_], v_)
            o_ = oTs[h % 2]
            iters = [(qc, m) for qc in range(NQC) for m in range(NT)]
            n_it = len(iters)
            sbuf_of = {}

            def emit_qk(idx):
                nonlocal gi
                qc, m = iters[idx]
                S_ = Sp[gi % 5]
                E_ = Eb[gi % 6]
                gi += 1
                sbuf_of[idx] = (S_, E_)
                self.pe(lambda e, S_=S_, m=m, qc=qc, k_=k_, q_=q_, dk=dk: e.matmul(
                    S_.t[:], lhsT=k_.t[0:dk, m * 128:(m + 1) * 128], rhs=q_.t[0:dk, qc * 512:(qc + 1) * 512], start=True, stop=True),
                    [k_, q_], [S_])
                self.act(lambda e, S_=S_, E_=E_, scale=scale: e.activation(out=E_.t[:], in_=S_.t[:], func=AF.Exp, scale=scale), [S_], [E_])

            def flush_pending(force=False):
                keep = []
                for item in pending:
                    item[0] -= 1
                    if item[0] <= 0 or force:
                        item[1]()
                    else:
                        keep.append(item)
                pending[:] = keep

            for idx in range(min(LA, n_it)):
                emit_qk(idx)
            for idx in range(n_it):
                if idx + LA < n_it:
                    emit_qk(idx + LA)
                qc, m = iters[idx]
                if m == 0:
                    O_ = Op[oc_i % 2]
                    ob_, rz_, bc_ = osb[oc_i % 2], rz[oc_i % 2], bc[0]
                    oc_i += 1
                S_, E_ = sbuf_of.pop(idx)
                self.pe(lambda e, O_=O_, v_=v_, m=m, E_=E_: e.matmul(O_.t[:], lhsT=v_.t[:, m, :], rhs=E_.t[:], start=(m == 0), stop=(m == NT - 1)),
                        [v_, E_], [O_])
                flush_pending()
                if m == NT - 1:
                    self.dve(lambda e, O_=O_, ob_=ob_: e.tensor_copy(out=ob_.t[:], in_=O_.t[:]), [O_], [ob_])

                    def norm(ob_=ob_, rz_=rz_, bc_=bc_, qc=qc, o_=o_):
                        self.pe(lambda e: e.matmul(bc_.t[:], lhsT=sel.t[:], rhs=ob_.t[:], start=True, stop=True), [sel, ob_], [bc_])
                        self.dve(lambda e: e.reciprocal(out=rz_.t[:], in_=bc_.t[:]), [bc_], [rz_])
                        self.dve(lambda e: e.tensor_tensor(out=o_.t[:, qc * 512:(qc + 1) * 512], in0=ob_.t[0:64, :], in1=rz_.t[:], op=ALU.mult),
                                 [ob_, rz_], [o_])
                    pending.append([4, norm])
            flush_pending(force=True)
            self.dma("sp", self.catT.t[h * 64:(h + 1) * 64, :], o_.t[:], [o_], [], o_)
        self.phase_end(ph)


def prep_inputs(inputs, b):
    f = lambda a: np.ascontiguousarray(np.asarray(a, dtype=np.float32))
    m = {
        "x": f(inputs["x"][b]), "norm_mix": f(inputs["norm_mix"]), "norm_ffn": f(inputs["norm_ffn"]),
        "even_w_in": f(inputs["even_w_in"][0]), "even_gmlp_norm": f(inputs["even_gmlp_norm"][0]).reshape(1, 256),
        "even_w_spatial": f(inputs["even_w_spatial"][0]), "even_b_spatial": f(inputs["even_b_spatial"][0]),
        "even_w_out": f(inputs["even_w_out"][0]), "odd_w_in": f(inputs["odd_w_in"][0]),
        "odd_cq_norm": f(inputs["odd_cq_norm"]), "odd_w_cq_up": f(inputs["odd_w_cq_up"][0]),
        "odd_ckv_norm": f(inputs["odd_ckv_norm"]), "odd_w_ckv_up": f(inputs["odd_w_ckv_up"][0]),
        "odd_dq_norm": f(inputs["odd_dq_norm"]), "odd_dk_norm": f(inputs["odd_dk_norm"]),
        "odd_w_out": f(inputs["odd_w_out"][0]), "moe_w_router": f(inputs["moe_w_router"]),
        "moe_w_gate": f(inputs["moe_w_gate"]), "moe_w_up": f(inputs["moe_w_up"]), "moe_w_down": f(inputs["moe_w_down"]),
        "final_norm": f(inputs["final_norm"]).reshape(1, D),
    }
    return m


def kernel(**inputs):
    nb = inputs["x"].shape[0]
    nc = Builder().build()
    consts = make_consts()
    in_maps = []
    for b in range(nb):
        m = prep_inputs(inputs, b)
        m.update(consts)
        in_maps.append(m)
    res = run_bass_kernel_spmd(nc, in_maps, core_ids=list(range(nb)))
    return np.stack([np.asarray(r["y"]) for r in res.results], axis=0).astype(np.float32)
```

```python
import numpy as np
import ml_dtypes
import concourse.bass as bass
import concourse.mybir as mybir
from concourse.bass_utils import run_bass_kernel_spmd
from contextlib import ExitStack

F32 = mybir.dt.float32
BF16 = mybir.dt.bfloat16
ALU = mybir.AluOpType
AF = mybir.ActivationFunctionType
AX = mybir.AxisListType

S_LEN = 8192
D = 1024
PAD = 1024
NT = S_LEN // 128
CH = 16000
EPS = 1e-6
NEXP = 16
CAP = 2 * S_LEN // NEXP
FUSE_WAIT = True


class Res:
    __slots__ = ("name", "writers", "readers", "stream")

    def __init__(self, name):
        self.name = name
        self.writers = {}
        self.readers = {}
        self.stream = None


class Stream:
    __slots__ = ("sem", "count", "key")

    def __init__(self, sem, key):
        self.sem = sem
        self.count = 0
        self.key = key


class Buf:
    __slots__ = ("t", "r", "track")

    def __init__(self, t, name, track=True):
        self.t = t
        self.r = Res(name)
        self.track = track


class Sched:
    ENG = ("pe", "act", "dve", "pool", "sp")
    HANDLE = {"pe": "tensor", "act": "scalar", "dve": "vector", "pool": "gpsimd", "sp": "sync"}

    def __init__(self, nc, stack):
        self.nc = nc
        self.stack = stack
        self.prog = {e: [] for e in self.ENG}
        self.cnt = {e: 0 for e in self.ENG}
        self.seen = {e: {} for e in self.ENG}
        self.esems = {e: [] for e in self.ENG}
        self.streams = []
        self.free_sems = []
        self.live = []
        self.bg = set()
        self.nsem = 0
        self.total = 0

    def _new_sem(self, name):
        self.nsem += 1
        return self.stack.enter_context(self.nc.semaphore(name))

    def _esem(self, e, chunk):
        lst = self.esems[e]
        while len(lst) <= chunk:
            lst.append(self._new_sem(f"s_{e}_{len(lst)}"))
        return lst[chunk]

    def stream_of(self, res):
        if res.stream is None:
            if self.free_sems:
                self.free_sems.sort(key=lambda x: x[1])
                sem, base = self.free_sems.pop(0)
            else:
                sem, base = self._new_sem(f"d{len(self.streams)}"), 0
            res.stream = Stream(sem, ("d", len(self.streams)))
            res.stream.count = base
            self.streams.append(res.stream)
            self.live.append(res)
        return res.stream

    def mark_bg(self, res):
        self.bg.add(self.stream_of(res).key)

    def bg_done(self):
        self.bg = set()

    def release_streams(self):
        keep = []
        for res in self.live:
            st = res.stream
            if st.key in self.bg:
                keep.append(res)
                continue
            self.free_sems.append((st.sem, st.count))
            st.count = 0
            res.stream = None
        self.live = keep

    def _wait(self, e, key, val, payload):
        seen = self.seen[e]
        if seen.get(key, 0) >= val:
            return
        seen[key] = val
        self.prog[e].append(("wait", payload))

    def _wait_tok(self, e, key, val):
        if key[0] == "e":
            eng = key[1]
            if eng == e and e in ("pe", "sp"):
                return
            chunk, v = (val - 1) // CH, (val - 1) % CH + 1
            self._wait(e, key, val, (self._esem(eng, chunk), v))
        else:
            st = self.streams[key[1]]
            self._wait(e, key, val, (st.sem, val))

    def op(self, e, fn, reads=(), writes=(), stream=None):
        for r in reads:
            for k, v in r.writers.items():
                self._wait_tok(e, k, v)
        for w in writes:
            for k, v in w.writers.items():
                self._wait_tok(e, k, v)
            for k, v in w.readers.items():
                self._wait_tok(e, k, v)
        if stream is not None:
            st = self.stream_of(stream)
            st.count += 16
            key, val = st.key, st.count
            self.prog[e].append(("dma", fn, st.sem))
        else:
            self.cnt[e] += 1
            n = self.cnt[e]
            key, val = ("e", e), n
            self.prog[e].append(("ins", fn, self._esem(e, (n - 1) // CH)))
        for r in reads:
            r.readers[key] = val
        for w in writes:
            w.writers = {key: val}
            w.readers = {}

    def barrier(self, engines=None):
        for e in (engines or self.ENG):
            for st in self.streams:
                if st.count and st.key not in self.bg:
                    self._wait(e, st.key, st.count, (st.sem, st.count))
            for eng in self.ENG:
                if eng != e and self.cnt[eng]:
                    self._wait_tok(e, ("e", eng), self.cnt[eng])

    def emit(self):
        nc = self.nc
        import os
        if os.environ.get('DUMP'):
            for e in self.ENG:
                print('ENGINE', e)
                for it in self.prog[e][:int(os.environ['DUMP'])]:
                    if it[0] == 'wait':
                        print('   wait', it[1][0].name if hasattr(it[1][0], 'name') else it[1][0], it[1][1])
                    else:
                        print('  ', it[0], (it[2].name if hasattr(it[2], 'name') else it[2]), it[1].__code__.co_firstlineno)
        with nc.Block() as block:
            def mk(e):
                items = self.prog[e]

                def body(eng):
                    n = len(items)
                    for i, it in enumerate(items):
                        if it[0] == "wait":
                            if FUSE_WAIT and i + 1 < n and items[i + 1][0] != "wait":
                                continue
                            eng.wait_ge(it[1][0], it[1][1])
                        else:
                            ins = it[1](eng)
                            if FUSE_WAIT and i > 0 and items[i - 1][0] == "wait":
                                ins._wait_ge(items[i - 1][1][0], items[i - 1][1][1])
                            ins.then_inc(it[2], 16 if it[0] == "dma" else 1)
                return body

            for e in self.ENG:
                if self.prog[e]:
                    getattr(block, self.HANDLE[e])(mk(e))
        for e in self.ENG:
            self.total += len(self.prog[e])
            self.prog[e] = []


def _rope_table(pos, r, theta):
    half = r // 2
    inv = np.power(np.float32(theta), -np.arange(half, dtype=np.float32) * np.float32(2.0 / r)).astype(np.float32)
    ang = pos.astype(np.float32)[:, None] * inv[None, :]
    return np.concatenate([np.cos(ang), np.sin(ang)], axis=1).astype(np.float32)


def make_consts():
    pos = np.arange(S_LEN)
    c = {}
    c["c_ident_bf"] = np.eye(128, dtype=np.float32).astype(ml_dtypes.bfloat16)
    c["c_ident_f"] = np.eye(128, dtype=np.float32)
    sel = np.zeros((65, 64), np.float32)
    sel[64, :] = 1.0
    c["c_sel"] = sel
    c["c_rope0"] = _rope_table(pos, 16, 500000.0)
    c["c_ropec"] = _rope_table(pos, 32, 500000.0)
    c["c_roperow"] = _rope_table(pos // 64, 32, 10000.0)
    c["c_ropecol"] = _rope_table(pos % 64, 32, 10000.0)
    a = np.arange(128)[:, None]
    q = np.arange(128)[None, :]
    A = (a >= q).astype(np.float32)
    B = (a <= q).astype(np.float32)
    Ae = A * (a >= 64)
    Be = B * (a < 64)
    m = np.zeros((3, 128, 512), np.float32)
    m[0] = np.concatenate([A, B, A, B], 1)
    m[1] = np.concatenate([Ae, B, A, B], 1)
    m[2] = np.concatenate([A, B, A, Be], 1)
    c["c_mask"] = np.ascontiguousarray(m.transpose(1, 0, 2)).astype(ml_dtypes.bfloat16)
    return c


CONST_SPECS = {
    "c_ident_bf": ([128, 128], BF16), "c_ident_f": ([128, 128], F32), "c_sel": ([65, 64], F32),
    "c_rope0": ([S_LEN, 16], F32), "c_ropec": ([S_LEN, 32], F32), "c_roperow": ([S_LEN, 32], F32),
    "c_ropecol": ([S_LEN, 32], F32), "c_mask": ([128, 3, 512], BF16),
}

INPUT_SPECS = {
    "x": [S_LEN, D], "norm_mix": [2, D], "norm_ffn": [2, D], "even_w_in": [D, 2816],
    "even_gmlp_norm": [1, 256], "even_w_spatial": [4, 128, 128], "even_b_spatial": [4, 128],
    "even_w_out": [D, D], "odd_w_in": [D, 1184], "odd_cq_norm": [1, 256], "odd_w_cq_up": [256, 768],
    "odd_ckv_norm": [1, 128], "odd_w_ckv_up": [128, 1024], "odd_dq_norm": [1, 64], "odd_dk_norm": [1, 64],
    "odd_w_out": [D, D], "moe_w_router": [2, D, 16], "moe_w_gate": [2, 16, D, 512],
    "moe_w_up": [2, 16, D, 512], "moe_w_down": [2, 16, 512, D], "final_norm": [1, D],
}


class Builder:
    def __init__(self, dbg=(), upto="all"):
        self.nc = bass.Bass("TRN2", target_bir_lowering=False)
        self.dbg = set(dbg)
        self.upto = upto
        self.root = ExitStack()
        self.S = Sched(self.nc, self.root)
        self.inp = {}
        for k, shp in INPUT_SPECS.items():
            self.inp[k] = Buf(self.nc.dram_tensor(k, shp, F32, kind="ExternalInput").ap(), k, False)
        for k, (shp, dt) in CONST_SPECS.items():
            self.inp[k] = Buf(self.nc.dram_tensor(k, shp, dt, kind="ExternalInput").ap(), k, False)
        self.out = Buf(self.nc.dram_tensor("y", [S_LEN, D], F32, kind="ExternalOutput").ap(), "y", False)
        self.scr = {}
        self.phn = 0
        self.affall = self.sb(self.root, "affall", [128, NT, NEXP], F32)
        self.gw = self.sb(self.root, "gw", [128, NT, NEXP], F32)
        self.xa = self.dram("xa", [S_LEN, D], F32)
        self.xb = self.dram("xb", [S_LEN, D], F32)
        self.hTd = self.dram("hTd", [D, S_LEN], BF16)

    def dram(self, name, shape, dt):
        kind = "ExternalOutput" if name in self.dbg else "Internal"
        b = Buf(self.nc.dram_tensor(name, shape, dt, kind=kind).ap(), name, False)
        self.scr[name] = b
        return b

    def sb(self, ph, name, shape, dt):
        name = f"p{self.phn}_{name}"
        return Buf(ph.enter_context(self.nc.sbuf_tensor(name, shape, dt)), name)

    def ps(self, ph, name, shape, dt):
        name = f"p{self.phn}_{name}"
        return Buf(ph.enter_context(self.nc.psum_tensor(name, shape, dt)), name)

    def _op(self, e, fn, r, w):
        self.S.op(e, fn, reads=[b.r for b in r], writes=[b.r for b in w])

    def pe(self, fn, r, w):
        self._op("pe", fn, r, w)

    def act(self, fn, r, w):
        self._op("act", fn, r, w)

    def dve(self, fn, r, w):
        self._op("dve", fn, r, w)

    def pool(self, fn, r, w):
        self._op("pool", fn, r, w)

    def dma(self, q, out, in_, r, w, stream, **kw):
        self.S.op(q, lambda e: e.dma_start(out=out, in_=in_, **kw), reads=[b.r for b in r if b.track],
                  writes=[b.r for b in w if b.track], stream=stream.r)

    def phase_begin(self):
        self.phn += 1
        self.S.barrier()
        self.S.release_streams()
        return ExitStack()

    def phase_end(self, ph):
        self.S.emit()
        ph.close()

    def rmsnorm_rstd(self, src, srcbufs, junk, ssq, rstd, n, width=None):
        sc = float(n) ** -0.5
        self.pool(lambda e: e.memset(ssq.t[:, 0:1], 0.0), [], [ssq])
        self.act(lambda e: e.activation(out=junk, in_=src, func=AF.Square, scale=sc, accum_out=ssq.t[:, 0:1]),
                 srcbufs, [ssq])
        self.dve(lambda e: e.tensor_scalar_add(out=rstd.t[:, 0:1], in0=ssq.t[:, 0:1], scalar1=EPS), [ssq], [rstd])
        self.act(lambda e: e.activation(out=rstd.t[:, 0:1], in_=rstd.t[:, 0:1], func=AF.Sqrt), [rstd], [rstd])
        self.dve(lambda e: e.reciprocal(out=rstd.t[:, 0:1], in_=rstd.t[:, 0:1]), [rstd], [rstd])

    def build(self):
        with self.root:
            if self.upto in ("B1only", "B0only", "Eonly"):
                self.catT = self.dram("catT", [1024, S_LEN], BF16)
                if self.upto == "B1only":
                    self.QcT = self.dram("QcT", [768, S_LEN], BF16)
                    self.KcT = self.dram("KcT", [768, S_LEN], BF16)
                    self.Vc = self.dram("Vc", [S_LEN, 512], BF16)
                    self.QdT = self.dram("QdT", [512, S_LEN], BF16)
                    self.KdT = self.dram("KdT", [128, S_LEN], BF16)
                    self.Vd = self.dram("Vd", [S_LEN, 128], BF16)
                    self.layer1_B()
                elif self.upto == "B0only":
                    self.V0 = self.dram("V0", [PAD + S_LEN + PAD, 768], BF16)
                    self.QT0 = self.dram("QT0", [768, S_LEN], BF16)
                    self.KT0 = self.dram("KT0", [768, S_LEN], BF16)
                    self.layer0_B()
                else:
                    self.wg_bf = self.dram("wg_bf", [2, NEXP, D, 512], BF16)
                    self.wu_bf = self.dram("wu_bf", [2, NEXP, D, 512], BF16)
                    self.wd_bf = self.dram("wd_bf", [2, NEXP, 512, D], BF16)
                    self.phase_E(0, last=False)
                return self.finish()
            self.prologue()
            if self.upto == "pro":
                return self.finish()
            self.layer0_A()
            if self.upto == "A0":
                return self.finish()
            self.layer0_B()
            if self.upto == "B0":
                return self.finish()
            self.phase_C(0, self.inp["x"], self.inp["even_w_out"])
            if self.upto == "C0":
                return self.finish()
            self.phase_D()
            if self.upto == "D0":
                return self.finish()
            self.phase_E(0, last=False)
            if self.upto == "E0":
                return self.finish()
            self.layer1_A()
            if self.upto == "A1":
                return self.finish()
            self.layer1_B()
            if self.upto == "B1":
                return self.finish()
            self.phase_C(1, self.xb, self.inp["odd_w_out"])
            if self.upto == "C1":
                return self.finish()
            self.phase_D()
            self.phase_E(1, last=True)
            return self.finish()

    def finish(self):
        ph = self.phase_begin()
        self.phase_end(ph)
        return self.nc

    def prologue(self):
        self.wg_bf = self.dram("wg_bf", [2, NEXP, D, 512], BF16)
        self.wu_bf = self.dram("wu_bf", [2, NEXP, D, 512], BF16)
        self.wd_bf = self.dram("wd_bf", [2, NEXP, 512, D], BF16)

    def prologue_issue(self):
        for l in range(2):
            for e_ in range(NEXP):
                for src, dst in ((self.inp["moe_w_gate"], self.wg_bf), (self.inp["moe_w_up"], self.wu_bf),
                                 (self.inp["moe_w_down"], self.wd_bf)):
                    self.dma("pool", dst.t[l, e_], src.t[l, e_], [], [], dst)
        for dst in (self.wg_bf, self.wu_bf, self.wd_bf):
            self.S.mark_bg(dst.r)

    def layer0_A(self):
        nc = self.nc
        I = self.inp
        self.V0 = self.dram("V0", [PAD + S_LEN + PAD, 768], BF16)
        self.QT0 = self.dram("QT0", [768, S_LEN], BF16)
        self.KT0 = self.dram("KT0", [768, S_LEN], BF16)
        self.catT = self.dram("catT", [1024, S_LEN], BF16)
        ph = self.phase_begin()
        sb = lambda n, s, d: self.sb(ph, n, s, d)
        ps = lambda n, s, d: self.ps(ph, n, s, d)
        ident = sb("ident", [128, 128], BF16)
        win = sb("win", [128, 8, 2816], BF16)
        gmix = sb("gmix", [128, D], F32)
        gmn = sb("gmn", [128, 256], F32)
        wsf = sb("wsf", [128, 4, 128], F32)
        wsb = sb("wsb", [128, 4, 128], BF16)
        wsT = sb("wsT", [128, 4, 128], BF16)
        bsT = sb("bsT", [128, 4], F32)
        zero = sb("zero", [128, 768], BF16)
        xt = [sb(f"xt{i}", [128, D], F32) for i in range(2)]
        junk = sb("junk", [128, D], BF16)
        ssq = [sb(f"ssq{i}", [128, 1], F32) for i in range(2)]
        rstd = [sb(f"rstd{i}", [128, 1], F32) for i in range(2)]
        hn = [sb(f"hn{i}", [128, D], BF16) for i in range(2)]
        hT = [sb(f"hT{i}", [128, 8, 128], BF16) for i in range(2)]
        cs = [sb(f"cs{i}", [128, 16], F32) for i in range(2)]
        qk = [sb(f"qk{i}", [128, 1536], BF16) for i in range(2)]
        rt = [sb(f"rt{i}", [128, 24, 8], F32) for i in range(4)]
        vb = [sb(f"vb{i}", [128, 768], BF16) for i in range(2)]
        zgs = [sb(f"zg{i}", [128, 512], F32) for i in range(2)]
        qkfs = [sb(f"qkf{i}", [128, 1536], F32) for i in range(2)]
        sqv = sb("sqv", [128, 256], F32)
        ss4 = sb("ss4", [128, 4], F32)
        r4 = sb("r4", [128, 4], F32)
        vvn = sb("vvn", [128, 256], F32)
        vvb = sb("vvb", [128, 256], BF16)
        mxb = sb("mxb", [128, 256], F32)
        gout = sb("gout", [128, 256], BF16)
        stq = [sb(f"stq{i}", [128, 6, 512], BF16) for i in range(2)]
        stk = [sb(f"stk{i}", [128, 6, 512], BF16) for i in range(2)]
        stg = [sb(f"stg{i}", [64, 4, 512], BF16) for i in range(2)]
        T0 = ps("T0", [128, 1024], BF16)
        T1 = ps("T1", [128, 1024], BF16)
        pqk = ps("pqk", [128, 1536], F32)
        pvz = ps("pvz", [128, 1536], F32)

        self.dma("sp", ident.t[:], I["c_ident_bf"].t[:, :], [], [ident], ident)
        self.dma("pool", win.t[:], I["even_w_in"].t.rearrange("(c p) n -> p c n", p=128), [], [win], win)
        self.dma("sp", gmix.t[:], I["norm_mix"].t[0:1, :].partition_broadcast(128), [], [gmix], gmix)
        self.dma("sp", gmn.t[:], I["even_gmlp_norm"].t[0:1, :].partition_broadcast(128), [], [gmn], gmn)
        self.dma("sp", wsf.t[:], I["even_w_spatial"].t.rearrange("g p q -> p g q"), [], [wsf], wsf)
        self.dma("sp", bsT.t[:], I["even_b_spatial"].t.rearrange("g p -> p g"), [], [bsT], bsT,
                 allow_slow_non_contiguous=True)
        self.dve(lambda e: e.tensor_copy(out=wsb.t[:], in_=wsf.t[:]), [wsf], [wsb])
        for g in range(4):
            self.pe(lambda e, g=g: e.transpose(out=T0.t[:, g * 128:(g + 1) * 128], in_=wsb.t[:, g, :], identity=ident.t[:]),
                    [wsb, ident], [T0])
        self.act(lambda e: e.copy(out=wsT.t[:].rearrange("p g q -> p (g q)"), in_=T0.t[:, 0:512]), [T0], [wsT])
        self.pool(lambda e: e.memset(zero.t[:], 0.0), [], [zero])
        for side in range(2):
            for k in range(PAD // 128):
                r0 = (0 if side == 0 else PAD + S_LEN) + k * 128
                self.dma("sp", self.V0.t[r0:r0 + 128, :], zero.t[:], [zero], [self.V0], zero)

        import os
        NT0 = int(os.environ.get('NGRP', NT // 4)) * 4

        def stage1(ti):
            grp, tt = divmod(ti, 4)
            sq_, sk_, sg_ = stq[grp % 2], stk[grp % 2], stg[grp % 2]
            b = ti % 2
            x_, ssq_, rstd_, hn_, hT_, cs_, qk_, vb_, qkf, zg = xt[b], ssq[b], rstd[b], hn[b], hT[b], cs[b], qk[b], vb[b], qkfs[b], zgs[b]
            tok = slice(ti * 128, (ti + 1) * 128)
            self.dma("sp", x_.t[:], I["x"].t[tok, :], [I["x"]], [x_], x_)
            self.dma("sp", cs_.t[:], I["c_rope0"].t[tok, :], [], [cs_], cs_)
            self.rmsnorm_rstd(x_.t[:], [x_], junk.t[:], ssq_, rstd_, D)
            self.dve(lambda e, x_=x_, rstd_=rstd_, hn_=hn_: e.scalar_tensor_tensor(
                out=hn_.t[:], in0=x_.t[:], scalar=rstd_.t[:, 0:1], in1=gmix.t[:], op0=ALU.mult, op1=ALU.mult),
                [x_, rstd_, gmix], [hn_])
            for j in range(8):
                self.pe(lambda e, j=j, hn_=hn_: e.transpose(out=T0.t[:, j * 128:(j + 1) * 128],
                                                           in_=hn_.t[:, j * 128:(j + 1) * 128], identity=ident.t[:]),
                        [hn_, ident], [T0])
            self.act(lambda e, hT_=hT_: e.copy(out=hT_.t[:].rearrange("p c n -> p (c n)"), in_=T0.t[:]), [T0], [hT_])
            for bank in range(3):
                for j in range(8):
                    self.pe(lambda e, j=j, bank=bank, hT_=hT_: e.matmul(
                        pqk.t[:, bank * 512:(bank + 1) * 512], lhsT=hT_.t[:, j, :],
                        rhs=win.t[:, j, bank * 512:(bank + 1) * 512], start=(j == 0), stop=(j == 7)),
                        [hT_, win], [pqk])
            for (o0, n_, c0) in ((0, 512, 1536), (512, 256, 2048), (1024, 512, 2304)):
                for j in range(8):
                    self.pe(lambda e, j=j, o0=o0, n_=n_, c0=c0, hT_=hT_: e.matmul(
                        pvz.t[:, o0:o0 + n_], lhsT=hT_.t[:, j, :], rhs=win.t[:, j, c0:c0 + n_],
                        start=(j == 0), stop=(j == 7)), [hT_, win], [pvz])
            self.act(lambda e: e.copy(out=qkf.t[:], in_=pqk.t[:]), [pqk], [qkf])
            self.act(lambda e, vb_=vb_: e.copy(out=vb_.t[:], in_=pvz.t[:, 0:768]), [pvz], [vb_])
            self.dma("sp", self.V0.t[PAD + ti * 128:PAD + (ti + 1) * 128, :], vb_.t[:], [vb_], [self.V0], vb_)
            self.act(lambda e: e.activation(out=zg.t[:], in_=pvz.t[:, 1024:1536], func=AF.Gelu_apprx_tanh), [pvz], [zg])

        def stage2(ti):
            grp, tt = divmod(ti, 4)
            sq_, sk_, sg_ = stq[grp % 2], stk[grp % 2], stg[grp % 2]
            b = ti % 2
            x_, ssq_, rstd_, hn_, hT_, cs_, qk_, vb_, qkf, zg = xt[b], ssq[b], rstd[b], hn[b], hT[b], cs[b], qk[b], vb[b], qkfs[b], zgs[b]
            tok = slice(ti * 128, (ti + 1) * 128)
            self.pool(lambda e, qk_=qk_: e.tensor_copy(out=qk_.t[:], in_=qkf.t[:]), [qkf], [qk_])
            pv = qkf.t[:].rearrange("p (h c) -> p h c", c=64)
            qv = qk_.t[:].rearrange("p (h c) -> p h c", c=64)
            cosb = cs_.t[:, 0:8].unsqueeze(1).to_broadcast([128, 24, 8])
            sinb = cs_.t[:, 8:16].unsqueeze(1).to_broadcast([128, 24, 8])
            x1 = pv[:, :, 0:8]
            x2 = pv[:, :, 8:16]
            self.dve(lambda e, x1=x1, cosb=cosb: e.tensor_tensor(out=rt[0].t[:], in0=x1, in1=cosb, op=ALU.mult), [qkf, cs_], [rt[0]])
            self.dve(lambda e, x2=x2, sinb=sinb: e.tensor_tensor(out=rt[1].t[:], in0=x2, in1=sinb, op=ALU.mult), [qkf, cs_], [rt[1]])
            self.dve(lambda e, x1=x1, sinb=sinb: e.tensor_tensor(out=rt[2].t[:], in0=x1, in1=sinb, op=ALU.mult), [qkf, cs_], [rt[2]])
            self.dve(lambda e, x2=x2, cosb=cosb: e.tensor_tensor(out=rt[3].t[:], in0=x2, in1=cosb, op=ALU.mult), [qkf, cs_], [rt[3]])
            self.pool(lambda e, qv=qv: e.tensor_tensor(out=qv[:, :, 0:8], in0=rt[0].t[:], in1=rt[1].t[:], op=ALU.subtract),
                      [rt[0], rt[1]], [qk_])
            self.pool(lambda e, qv=qv: e.tensor_tensor(out=qv[:, :, 8:16], in0=rt[2].t[:], in1=rt[3].t[:], op=ALU.add),
                      [rt[2], rt[3]], [qk_])
            self.dve(lambda e: e.tensor_tensor(out=sqv.t[:], in0=zg.t[:, 256:512], in1=zg.t[:, 256:512], op=ALU.mult), [zg], [sqv])
            self.dve(lambda e: e.tensor_reduce(out=ss4.t[:], in_=sqv.t[:].rearrange("p (g c) -> p g c", c=64), axis=AX.X, op=ALU.add), [sqv], [ss4])
            self.dve(lambda e: e.tensor_scalar(out=r4.t[:], in0=ss4.t[:], scalar1=1.0 / 64, scalar2=EPS, op0=ALU.mult, op1=ALU.add), [ss4], [r4])
            self.act(lambda e: e.activation(out=r4.t[:], in_=r4.t[:], func=AF.Sqrt), [r4], [r4])
            self.dve(lambda e: e.reciprocal(out=r4.t[:], in_=r4.t[:]), [r4], [r4])
            self.dve(lambda e: e.tensor_tensor(out=vvn.t[:].rearrange("p (g c) -> p g c", c=64),
                                               in0=zg.t[:, 256:512].rearrange("p (g c) -> p g c", c=64),
                                               in1=r4.t[:].unsqueeze(2).to_broadcast([128, 4, 64]), op=ALU.mult), [zg, r4], [vvn])
            self.pool(lambda e: e.tensor_tensor(out=vvb.t[:], in0=vvn.t[:], in1=gmn.t[:], op=ALU.mult), [vvn, gmn], [vvb])
            for g in range(4):
                self.pe(lambda e, g=g: e.matmul(pvz.t[:, 768 + g * 64:768 + (g + 1) * 64], lhsT=wsT.t[:, g, :],
                                                rhs=vvb.t[:, g * 64:(g + 1) * 64], start=True, stop=True), [wsT, vvb], [pvz])
            self.act(lambda e: e.copy(out=sqv.t[:], in_=pvz.t[:, 768:1024]), [pvz], [sqv])
            self.dve(lambda e: e.tensor_tensor(out=mxb.t[:].rearrange("p (g c) -> p g c", c=64),
                                               in0=sqv.t[:].rearrange("p (g c) -> p g c", c=64),
                                               in1=bsT.t[:].unsqueeze(2).to_broadcast([128, 4, 64]), op=ALU.add), [sqv, bsT], [mxb])
            self.pool(lambda e: e.tensor_tensor(out=gout.t[:], in0=mxb.t[:], in1=zg.t[:, 0:256], op=ALU.mult), [mxb, zg], [gout])
            for half, st_ in ((0, sq_), (1, sk_)):
                for j in range(6):
                    c0 = half * 768 + j * 128
                    self.pe(lambda e, j=j, c0=c0, qk_=qk_: e.transpose(out=T1.t[:, j * 128:(j + 1) * 128], in_=qk_.t[:, c0:c0 + 128],
                                                                     identity=ident.t[:]), [qk_, ident], [T1])
                self.act(lambda e, st_=st_, tt=tt: e.copy(out=st_.t[:, :, tt * 128:(tt + 1) * 128],
                                                        in_=T1.t[:, 0:768].rearrange("p (j n) -> p j n", n=128)), [T1], [st_])
            for g in range(4):
                self.pe(lambda e, g=g: e.transpose(out=T1.t[0:64, g * 128:(g + 1) * 128], in_=gout.t[:, g * 64:(g + 1) * 64],
                                                   identity=ident.t[:]), [gout, ident], [T1])
            self.dve(lambda e, sg_=sg_, tt=tt: e.tensor_copy(out=sg_.t[:, :, tt * 128:(tt + 1) * 128],
                                                           in_=T1.t[0:64, 0:512].rearrange("p (j n) -> p j n", n=128)), [T1], [sg_])
            if tt == 3:
                gtok = slice(grp * 512, (grp + 1) * 512)
                self.dma("sp", self.QT0.t.rearrange("(j p) t -> p j t", p=128)[:, :, gtok], sq_.t[:], [sq_], [self.QT0], sq_)
                self.dma("sp", self.KT0.t.rearrange("(j p) t -> p j t", p=128)[:, :, gtok], sk_.t[:], [sk_], [self.KT0], sk_)
                self.dma("sp", self.catT.t[768:1024, :].rearrange("(j p) t -> p j t", p=64)[:, :, gtok], sg_.t[:], [sg_], [self.catT], sg_)

        for s_ in range(NT0 + 1):
            if s_ < NT0:
                stage1(s_)
            if s_ >= 1:
                stage2(s_ - 1)
        self.phase_end(ph)

    def layer0_B(self):
        I = self.inp
        ph = self.phase_begin()
        sb = lambda n, s, d: self.sb(ph, n, s, d)
        ps = lambda n, s, d: self.ps(ph, n, s, d)
        sel = sb("sel", [65, 64], F32)
        mask = sb("mask", [128, 3, 512], BF16)
        qT = [sb(f"qT{i}", [64, S_LEN], BF16) for i in range(2)]
        kT = [sb(f"kT{i}", [64, PAD + S_LEN + PAD], BF16) for i in range(2)]
        vA = [sb(f"vA{i}", [128, 80, 65], BF16) for i in range(2)]
        acc = sb("acc", [65, S_LEN], F32)
        aTs = sb("aTs", [64, S_LEN], BF16)
        rz = [sb(f"rz{i}", [64, 512], F32) for i in range(2)]
        Eb = [sb(f"Eb{i}", [128, 512], BF16) for i in range(3)]
        Pb = [sb(f"Pb{i}", [128, 512], BF16) for i in range(3)]
        Sp = [ps(f"Sp{i}", [128, 512], F32) for i in range(3)]
        Op = [ps(f"Op{i}", [65, 512], F32) for i in range(2)]
        bc = [ps(f"bc{i}", [64, 512], F32) for i in range(2)]
        self.dma("sp", sel.t[:], I["c_sel"].t[:, :], [], [sel], sel)
        self.dma("sp", mask.t[:], I["c_mask"].t[:, :, :], [], [mask], mask)
        for i in range(2):
            self.pool(lambda e, i=i: e.memset(kT[i].t[:, 0:PAD], 0.0), [], [kT[i]])
            self.pool(lambda e, i=i: e.memset(kT[i].t[:, PAD + S_LEN:], 0.0), [], [kT[i]])
            self.pool(lambda e, i=i: e.memset(vA[i].t[:, :, 64:65], 1.0), [], [vA[i]])
        if hasattr(self, "wg_bf") and self.upto != "B0only":
            self.prologue_issue()
        it = 0
        vi = 0
        og = 0
        for h in range(12):
            q_, k_ = qT[h % 2], kT[h % 2]
            self.dma("sp", q_.t[:], self.QT0.t[h * 64:(h + 1) * 64, :], [self.QT0], [q_], q_)
            self.dma("sp", k_.t[:, PAD:PAD + S_LEN], self.KT0.t[h * 64:(h + 1) * 64, :], [self.KT0], [k_], k_)
            for pi, d in enumerate((1, 4, 16)):
                L = S_LEN // d
                nb = L // 128
                ntile = nb + 1
                v_ = vA[vi % 2]
                vi += 1
                r0 = PAD - 64 * d
                rows = ntile * 128 * d
                src = self.V0.t[r0:r0 + rows, h * 64:(h + 1) * 64].rearrange("(m a i) c -> a i m c", a=128, i=d)
                dstv = v_.t[:, 0:d * ntile, 0:64].rearrange("a (i m) c -> a i m c", i=d)
                for i in range(d):
                    for m0 in range(0, ntile, 16):
                        m1 = min(ntile, m0 + 16)
                        self.dma("sp", dstv[:, i, m0:m1, :], src[:, i, m0:m1, :], [self.V0], [v_], v_)
                for i in range(d):
                    for gq in range(nb // 4):
                        n0 = gq * 4
                        O_ = Op[og % 2]
                        og += 1
                        for pr in range(2):
                            S_ = Sp[it % 3]
                            E_ = Eb[it % 3]
                            P_ = Pb[it % 3]
                            it += 1
                            for blk in range(2):
                                n = n0 + 2 * pr + blk
                                q0 = 128 * d * n + i
                                for ab in range(2):
                                    m = n + ab
                                    k0 = PAD - 64 * d + i + 128 * d * m
                                    c0 = (blk * 2 + ab) * 128
                                    self.pe(lambda e, S_=S_, c0=c0, k0=k0, q0=q0, d=d, k_=k_, q_=q_: e.matmul(
                                        S_.t[:, c0:c0 + 128], lhsT=k_.t[:, k0:k0 + 127 * d + 1:d], rhs=q_.t[:, q0:q0 + 127 * d + 1:d],
                                        start=True, stop=True), [k_, q_], [S_])
                            self.act(lambda e, S_=S_, E_=E_: e.activation(out=E_.t[:], in_=S_.t[:], func=AF.Exp, scale=0.125), [S_], [E_])
                            first = (n0 + 2 * pr == 0)
                            last = (n0 + 2 * pr + 2 == nb)
                            assert not (first and last)
                            mi = 1 if first else (2 if last else 0)
                            self.dve(lambda e, E_=E_, P_=P_, mi=mi: e.tensor_tensor(out=P_.t[:], in0=E_.t[:], in1=mask.t[:, mi, :], op=ALU.mult),
                                     [E_, mask], [P_])
                            for blk in range(2):
                                n = n0 + 2 * pr + blk
                                oc = (2 * pr + blk) * 128
                                for ab in range(2):
                                    m = n + ab
                                    c0 = (blk * 2 + ab) * 128
                                    self.pe(lambda e, O_=O_, oc=oc, c0=c0, v_=v_, P_=P_, ti=i * ntile + m, ab=ab: e.matmul(
                                        O_.t[:, oc:oc + 128], lhsT=v_.t[:, ti, :], rhs=P_.t[:, c0:c0 + 128],
                                        start=(ab == 0), stop=(ab == 1)), [v_, P_], [O_])
                        a0 = 128 * d * n0 + i
                        av = acc.t[:, a0:a0 + 511 * d + 1:d]
                        if pi == 0:
                            self.dve(lambda e, av=av, O_=O_: e.tensor_copy(out=av, in_=O_.t[:]), [O_], [acc])
                        else:
                            self.dve(lambda e, av=av, O_=O_: e.tensor_tensor(out=av, in0=O_.t[:], in1=av, op=ALU.add), [O_, acc], [acc])
            for c in range(S_LEN // 512):
                cs_ = slice(c * 512, (c + 1) * 512)
                b_ = bc[c % 2]
                r_ = rz[c % 2]
                self.pe(lambda e, b_=b_, cs_=cs_: e.matmul(b_.t[:], lhsT=sel.t[:], rhs=acc.t[:, cs_], start=True, stop=True), [sel, acc], [b_])
                self.dve(lambda e, b_=b_, r_=r_: e.reciprocal(out=r_.t[:], in_=b_.t[:]), [b_], [r_])
                self.dve(lambda e, r_=r_, cs_=cs_: e.tensor_tensor(out=aTs.t[:, cs_], in0=acc.t[0:64, cs_], in1=r_.t[:], op=ALU.mult), [acc, r_], [aTs])
            self.dma("sp", self.catT.t[h * 64:(h + 1) * 64, :], aTs.t[:], [aTs], [self.catT], aTs)
        self.phase_end(ph)


    def phase_C(self, l, xin, wout_in):
        I = self.inp
        ph = self.phase_begin()
        sb = lambda n, s, d: self.sb(ph, n, s, d)
        ps = lambda n, s, d: self.ps(ph, n, s, d)
        ident = sb("ident", [128, 128], BF16)
        identf = sb("identf", [128, 128], F32)
        wout = sb("wout", [64, 16, D], BF16)
        gffn = sb("gffn", [128, D], F32)
        wr32 = sb("wr32", [128, 8, NEXP], F32)
        catg = [sb(f"catg{i}", [64, 16, 512], BF16) for i in range(2)]
        xt = [sb(f"xt{i}", [128, D], F32) for i in range(2)]
        x1t = [sb(f"x1t{i}", [128, D], F32) for i in range(2)]
        junk = sb("junk", [128, D], BF16)
        ssq = [sb(f"ssq{i}", [128, 1], F32) for i in range(2)]
        rstd = [sb(f"rstd{i}", [128, 1], F32) for i in range(2)]
        hnf = [sb(f"hnf{i}", [128, D], F32) for i in range(2)]
        hnb = [sb(f"hnb{i}", [128, D], BF16) for i in range(2)]
        hTs = [sb(f"hTs{i}", [128, 8, 512], BF16) for i in range(2)]
        hT32 = [sb(f"hT32{i}", [128, 8, 128], F32) for i in range(2)]
        lgs = sb("lgs", [128, NEXP], F32)
        ex = sb("ex", [128, NEXP], F32)
        mx = sb("mx", [128, 1], F32)
        se = sb("se", [128, 1], F32)
        T0 = ps("T0", [128, 1024], BF16)
        T32 = ps("T32", [128, 1024], F32)
        pm = [ps(f"pm{i}", [128, 1024], F32) for i in range(2)]
        lg = ps("lg", [128, NEXP], F32)
        self.dma("sp", ident.t[:], I["c_ident_bf"].t[:, :], [], [ident], ident)
        self.dma("sp", identf.t[:], I["c_ident_f"].t[:, :], [], [identf], identf)
        self.dma("pool", wout.t[:], wout_in.t.rearrange("(c p) n -> p c n", p=64), [], [wout], wout)
        self.dma("sp", gffn.t[:], I["norm_ffn"].t[l:l + 1, :].partition_broadcast(128), [], [gffn], gffn)
        self.dma("sp", wr32.t[:], I["moe_w_router"].t[l].rearrange("(c p) e -> p c e", p=128), [], [wr32], wr32)
        def stage1(ti):
            grp, tt = divmod(ti, 4)
            cg = catg[grp % 2]
            b = ti % 2
            x_, pm_ = xt[b], pm[b]
            tok = slice(ti * 128, (ti + 1) * 128)
            if tt == 0:
                gtok = slice(grp * 512, (grp + 1) * 512)
                self.dma("sp", cg.t[:], self.catT.t.rearrange("(c p) t -> p c t", p=64)[:, :, gtok], [], [cg], cg)
            self.dma("sp", x_.t[:], xin.t[tok, :], [], [x_], x_)
            for half in range(2):
                for c in range(16):
                    self.pe(lambda e, half=half, c=c, cg=cg, tt=tt, pm_=pm_: e.matmul(
                        pm_.t[:, half * 512:(half + 1) * 512], lhsT=cg.t[:, c, tt * 128:(tt + 1) * 128],
                        rhs=wout.t[:, c, half * 512:(half + 1) * 512], start=(c == 0), stop=(c == 15)), [cg, wout], [pm_])

        def stage2(ti):
            b = ti % 2
            x_, x1_, ssq_, rstd_, hnf_, hnb_, pm_ = xt[b], x1t[b], ssq[b], rstd[b], hnf[b], hnb[b], pm[b]
            tok = slice(ti * 128, (ti + 1) * 128)
            for half in range(2):
                hs_ = slice(half * 512, (half + 1) * 512)
                self.dve(lambda e, hs_=hs_, x_=x_, x1_=x1_, pm_=pm_: e.tensor_tensor(out=x1_.t[:, hs_], in0=pm_.t[:, hs_], in1=x_.t[:, hs_], op=ALU.add),
                         [pm_, x_], [x1_])
            self.dma("sp", self.xa.t[tok, :], x1_.t[:], [x1_], [], x1_)
            self.rmsnorm_rstd(x1_.t[:], [x1_], junk.t[:], ssq_, rstd_, D)
            self.dve(lambda e, x1_=x1_, rstd_=rstd_, hnf_=hnf_: e.scalar_tensor_tensor(
                out=hnf_.t[:], in0=x1_.t[:], scalar=rstd_.t[:, 0:1], in1=gffn.t[:], op0=ALU.mult, op1=ALU.mult),
                [x1_, rstd_, gffn], [hnf_])
            self.pool(lambda e, hnf_=hnf_, hnb_=hnb_: e.tensor_copy(out=hnb_.t[:], in_=hnf_.t[:]), [hnf_], [hnb_])

        def stage3(ti):
            grp, tt = divmod(ti, 4)
            hs = hTs[grp % 2]
            b = ti % 2
            hnf_, hnb_, h32_ = hnf[b], hnb[b], hT32[b]
            for j in range(8):
                self.pe(lambda e, j=j, hnb_=hnb_: e.transpose(out=T0.t[:, j * 128:(j + 1) * 128], in_=hnb_.t[:, j * 128:(j + 1) * 128],
                                                            identity=ident.t[:]), [hnb_, ident], [T0])
            self.act(lambda e, hs=hs, tt=tt: e.copy(out=hs.t[:, :, tt * 128:(tt + 1) * 128],
                                                  in_=T0.t[:].rearrange("p (c n) -> p c n", n=128)), [T0], [hs])
            for j in range(8):
                self.pe(lambda e, j=j, hnf_=hnf_: e.transpose(out=T32.t[:, j * 128:(j + 1) * 128], in_=hnf_.t[:, j * 128:(j + 1) * 128],
                                                            identity=identf.t[:]), [hnf_, identf], [T32])
            for half in range(2):
                self.act(lambda e, half=half, h32_=h32_: e.copy(out=h32_.t[:, half * 4:(half + 1) * 4, :].rearrange("p c n -> p (c n)"),
                                                               in_=T32.t[:, half * 512:(half + 1) * 512]), [T32], [h32_])
            for j in range(8):
                self.pe(lambda e, j=j, h32_=h32_: e.matmul(lg.t[:, :], lhsT=h32_.t[:, j, :], rhs=wr32.t[:, j, :],
                                                         start=(j == 0), stop=(j == 7)), [h32_, wr32], [lg])
            self.act(lambda e: e.copy(out=lgs.t[:], in_=lg.t[:]), [lg], [lgs])
            self.dve(lambda e: e.reduce_max(out=mx.t[:], in_=lgs.t[:], axis=AX.X), [lgs], [mx])
            self.dve(lambda e: e.tensor_scalar_mul(out=mx.t[:], in0=mx.t[:], scalar1=-1.0), [mx], [mx])
            self.pool(lambda e: e.memset(se.t[:], 0.0), [], [se])
            self.act(lambda e: e.activation(out=ex.t[:], in_=lgs.t[:], func=AF.Exp, bias=mx.t[:, 0:1], scale=1.0, accum_out=se.t[:, 0:1]),
                     [lgs, mx], [ex, se])
            self.dve(lambda e: e.reciprocal(out=se.t[:], in_=se.t[:]), [se], [se])
            self.dve(lambda e, ti=ti: e.tensor_scalar_mul(out=self.affall.t[:, ti, :], in0=ex.t[:], scalar1=se.t[:, 0:1]), [ex, se], [self.affall])
            if tt == 3:
                gtok = slice(grp * 512, (grp + 1) * 512)
                self.dma("sp", self.hTd.t.rearrange("(c p) t -> p c t", p=128)[:, :, gtok], hs.t[:], [hs], [], hs)

        for s_ in range(NT + 2):
            if s_ < NT:
                stage1(s_)
            if 0 <= s_ - 1 < NT:
                stage2(s_ - 1)
            if 0 <= s_ - 2 < NT:
                stage3(s_ - 2)
        self.phase_end(ph)

    def phase_D(self):
        ph = self.phase_begin()
        sb = lambda n, s, d: self.sb(ph, n, s, d)
        ps = lambda n, s, d: self.ps(ph, n, s, d)
        ones = sb("ones", [128, 128], BF16)
        cmp_ = sb("cmp", [128, NT * NEXP], BF16)
        cnts = sb("cnts", [128, NT * NEXP], F32)
        cnt16 = sb("cnt16", [128, NEXP], F32)
        lo = sb("lo", [128, NEXP], F32)
        mid = sb("mid", [128, NEXP], F32)
        ge = sb("ge", [128, NEXP], F32)
        msk = sb("msk", [128, NT, NEXP], F32)
        cntp = ps("cntp", [128, NT * NEXP], F32)
        aff = self.affall
        self.pool(lambda e: e.memset(ones.t[:], 1.0), [], [ones])
        self.pool(lambda e: e.memset(lo.t[:], 0.0), [], [lo])
        for k in range(1, 29):
            c = 2.0 ** -k
            self.dve(lambda e, c=c: e.tensor_scalar_add(out=mid.t[:], in0=lo.t[:], scalar1=c), [lo], [mid])
            self.dve(lambda e: e.tensor_tensor(out=cmp_.t[:].rearrange("p (t x) -> p t x", x=NEXP), in0=aff.t[:],
                                               in1=mid.t[:].unsqueeze(1).to_broadcast([128, NT, NEXP]), op=ALU.is_ge), [aff, mid], [cmp_])
            for half in range(2):
                self.pe(lambda e, half=half: e.matmul(cntp.t[:, half * 512:(half + 1) * 512], lhsT=ones.t[:],
                                                      rhs=cmp_.t[:, half * 512:(half + 1) * 512], start=True, stop=True), [ones, cmp_], [cntp])
            self.act(lambda e: e.copy(out=cnts.t[:], in_=cntp.t[:]), [cntp], [cnts])
            self.dve(lambda e: e.tensor_reduce(out=cnt16.t[:], in_=cnts.t[:].rearrange("p (t x) -> p x t", x=NEXP), axis=AX.X, op=ALU.add),
                     [cnts], [cnt16])
            self.dve(lambda e, c=c: e.tensor_scalar(out=ge.t[:], in0=cnt16.t[:], scalar1=CAP - 0.5, scalar2=c, op0=ALU.is_ge, op1=ALU.mult),
                     [cnt16], [ge])
            self.dve(lambda e: e.tensor_tensor(out=lo.t[:], in0=lo.t[:], in1=ge.t[:], op=ALU.add), [lo, ge], [lo])
        self.dve(lambda e: e.tensor_tensor(out=msk.t[:], in0=aff.t[:], in1=lo.t[:].unsqueeze(1).to_broadcast([128, NT, NEXP]), op=ALU.is_ge),
                 [aff, lo], [msk])
        self.dve(lambda e: e.tensor_tensor(out=self.gw.t[:], in0=msk.t[:], in1=aff.t[:], op=ALU.mult), [msk, aff], [self.gw])
        self.phase_end(ph)

    def phase_E(self, l, last):
        I = self.inp
        self.S.bg_done()
        ph = self.phase_begin()
        sb = lambda n, s, d: self.sb(ph, n, s, d)
        ps = lambda n, s, d: self.ps(ph, n, s, d)
        SG = 2048
        accb = sb("accb", [128, SG // 128, D], F32)
        hs = sb("hs", [128, 8, SG], BF16)
        wgt = [sb(f"wgt{i}", [128, 8, 512], BF16) for i in range(2)]
        wut = [sb(f"wut{i}", [128, 8, 512], BF16) for i in range(2)]
        wdt = [sb(f"wdt{i}", [128, 4, D], BF16) for i in range(2)]
        sgt = [sb(f"sgt{i}", [128, 512], F32) for i in range(2)]
        actT = [sb(f"actT{i}", [128, 4, 512], BF16) for i in range(2)]
        pg = [ps(f"pg{i}", [128, 512], F32) for i in range(2)]
        pu = [ps(f"pu{i}", [128, 512], F32) for i in range(2)]
        py = [ps(f"py{i}", [128, 512], F32) for i in range(4)]
        if last:
            gfin = sb("gfin", [128, D], F32)
            junk = sb("junk", [128, D], BF16)
            ssq = [sb(f"ssq{i}", [128, 1], F32) for i in range(2)]
            rstd = [sb(f"rstd{i}", [128, 1], F32) for i in range(2)]
            yo = [sb(f"yo{i}", [128, D], F32) for i in range(2)]
            self.dma("sp", gfin.t[:], I["final_norm"].t[0:1, :].partition_broadcast(128), [], [gfin], gfin)
        wi = 0
        kf = 0
        ky = 0
        ka = 0
        import os
        ECUT = int(os.environ.get('ECUT', 9))
        for sgi in range(int(os.environ.get('ESG', S_LEN // SG))):
            t0 = sgi * SG
            for q4 in range(4):
                self.dma("sp", accb.t[:, q4 * 4:(q4 + 1) * 4, :],
                         self.xa.t[t0 + q4 * 512:t0 + (q4 + 1) * 512, :].rearrange("(t p) d -> p t d", p=128), [], [accb], accb)
            self.dma("sp", hs.t[:], self.hTd.t.rearrange("(c p) t -> p c t", p=128)[:, :, t0:t0 + SG], [], [hs], hs)
            for ex_ in range(int(os.environ.get('EEXP', NEXP))):
                wg_, wu_, wd_ = wgt[wi % 2], wut[wi % 2], wdt[wi % 2]
                wi += 1
                self.dma("sp", wg_.t[:], self.wg_bf.t[l, ex_].rearrange("(c p) f -> p c f", p=128), [], [wg_], wg_)
                self.dma("sp", wu_.t[:], self.wu_bf.t[l, ex_].rearrange("(c p) f -> p c f", p=128), [], [wu_], wu_)
                self.dma("sp", wd_.t[:], self.wd_bf.t[l, ex_].rearrange("(c p) n -> p c n", p=128), [], [wd_], wd_)
                for g4 in range(SG // 512):
                    if ECUT < 2: continue
                    tk = slice(g4 * 512, (g4 + 1) * 512)
                    aT_ = actT[ka % 2]
                    ka += 1
                    for fc in range(4):
                        pg_, pu_, sg_ = pg[kf % 2], pu[kf % 2], sgt[kf % 2]
                        kf += 1
                        fs = slice(fc * 128, (fc + 1) * 128)
                        for j in range(8):
                            self.pe(lambda e, j=j, pg_=pg_, wg_=wg_, fs=fs, tk=tk: e.matmul(pg_.t[:], lhsT=wg_.t[:, j, fs], rhs=hs.t[:, j, tk],
                                                                                      start=(j == 0), stop=(j == 7)), [wg_, hs], [pg_])
                        for j in range(8):
                            self.pe(lambda e, j=j, pu_=pu_, wu_=wu_, fs=fs, tk=tk: e.matmul(pu_.t[:], lhsT=wu_.t[:, j, fs], rhs=hs.t[:, j, tk],
                                                                                      start=(j == 0), stop=(j == 7)), [wu_, hs], [pu_])
                        self.act(lambda e, pg_=pg_, sg_=sg_: e.activation(out=sg_.t[:], in_=pg_.t[:], func=AF.Silu), [pg_], [sg_])
                        self.dve(lambda e, pu_=pu_, sg_=sg_, aT_=aT_, fc=fc: e.tensor_tensor(out=aT_.t[:, fc, :], in0=pu_.t[:], in1=sg_.t[:], op=ALU.mult),
                                 [pu_, sg_], [aT_])
                    for tt in range(4):
                        if ECUT < 3: continue
                        tl = g4 * 4 + tt
                        tile = sgi * (SG // 128) + tl
                        for ch in range(2):
                            py_ = py[ky % 4]
                            ky += 1
                            for fc in range(4):
                                self.pe(lambda e, py_=py_, aT_=aT_, fc=fc, tt=tt, wd_=wd_, ch=ch: e.matmul(
                                    py_.t[:], lhsT=aT_.t[:, fc, tt * 128:(tt + 1) * 128], rhs=wd_.t[:, fc, ch * 512:(ch + 1) * 512],
                                    start=(fc == 0), stop=(fc == 3)), [aT_, wd_], [py_])
                            av = accb.t[:, tl, ch * 512:(ch + 1) * 512]
                            self.dve(lambda e, av=av, py_=py_, tile=tile, ex_=ex_: e.scalar_tensor_tensor(
                                out=av, in0=py_.t[:], scalar=self.gw.t[:, tile, ex_:ex_ + 1], in1=av, op0=ALU.mult, op1=ALU.add),
                                [py_, self.gw, accb], [accb])
            if not last:
                for q4 in range(4):
                    self.dma("sp", self.xb.t[t0 + q4 * 512:t0 + (q4 + 1) * 512, :].rearrange("(t p) d -> p t d", p=128),
                             accb.t[:, q4 * 4:(q4 + 1) * 4, :], [accb], [], accb)
            else:
                for tl in range(SG // 128):
                    b = tl % 2
                    self.rmsnorm_rstd(accb.t[:, tl, :], [accb], junk.t[:], ssq[b], rstd[b], D)
                    self.dve(lambda e, tl=tl, b=b: e.scalar_tensor_tensor(out=yo[b].t[:], in0=accb.t[:, tl, :], scalar=rstd[b].t[:, 0:1],
                                                                        in1=gfin.t[:], op0=ALU.mult, op1=ALU.mult), [accb, rstd[b], gfin], [yo[b]])
                    r0 = t0 + tl * 128
                    self.dma("sp", self.out.t[r0:r0 + 128, :], yo[b].t[:], [yo[b]], [], yo[b])
        self.phase_end(ph)


    def rope_apply(self, x1, x2, cosb, sinb, o1, o2, rt, shape, srcbufs, tabbufs, outbuf):
        v = [r.t[:, 0:shape[1], 0:shape[2]] for r in rt]
        self.dve(lambda e: e.tensor_tensor(out=v[0], in0=x1, in1=cosb, op=ALU.mult), srcbufs + tabbufs, [rt[0]])
        self.dve(lambda e: e.tensor_tensor(out=v[1], in0=x2, in1=sinb, op=ALU.mult), srcbufs + tabbufs, [rt[1]])
        self.dve(lambda e: e.tensor_tensor(out=v[2], in0=x1, in1=sinb, op=ALU.mult), srcbufs + tabbufs, [rt[2]])
        self.dve(lambda e: e.tensor_tensor(out=v[3], in0=x2, in1=cosb, op=ALU.mult), srcbufs + tabbufs, [rt[3]])
        self.pool(lambda e: e.tensor_tensor(out=o1, in0=v[0], in1=v[1], op=ALU.subtract), [rt[0], rt[1]], [outbuf])
        self.pool(lambda e: e.tensor_tensor(out=o2, in0=v[2], in1=v[3], op=ALU.add), [rt[2], rt[3]], [outbuf])

    def layer1_A(self):
        I = self.inp
        self.QcT = self.dram("QcT", [768, S_LEN], BF16)
        self.KcT = self.dram("KcT", [768, S_LEN], BF16)
        self.Vc = self.dram("Vc", [S_LEN, 512], BF16)
        self.QdT = self.dram("QdT", [512, S_LEN], BF16)
        self.KdT = self.dram("KdT", [128, S_LEN], BF16)
        self.Vd = self.dram("Vd", [S_LEN, 128], BF16)
        ph = self.phase_begin()
        sb = lambda n, s, d: self.sb(ph, n, s, d)
        ps = lambda n, s, d: self.ps(ph, n, s, d)
        ident = sb("ident", [128, 128], BF16)
        win = sb("win", [128, 8, 1184], BF16)
        wcq = sb("wcq", [128, 2, 768], BF16)
        wckv = sb("wckv", [128, 1024], BF16)
        gmix = sb("gmix", [128, D], F32)
        gcq = sb("gcq", [128, 256], F32)
        gckv = sb("gckv", [128, 128], F32)
        gdq = sb("gdq", [128, 64], F32)
        gdk = sb("gdk", [128, 64], F32)
        xt = [sb(f"xt{i}", [128, D], F32) for i in range(2)]
        junk = sb("junk", [128, D], BF16)
        ssq = [sb(f"ssq{i}", [128, 1], F32) for i in range(2)]
        rstd = [sb(f"rstd{i}", [128, 1], F32) for i in range(2)]
        ssq2 = sb("ssq2", [128, 1], F32)
        rstd2 = sb("rstd2", [128, 1], F32)
        ssq3 = sb("ssq3", [128, 1], F32)
        rstd3 = sb("rstd3", [128, 1], F32)
        hn = [sb(f"hn{i}", [128, D], BF16) for i in range(2)]
        hT = [sb(f"hT{i}", [128, 8, 128], BF16) for i in range(2)]
        tabs = [sb(f"tabs{i}", [128, 96], F32) for i in range(2)]
        pfs = [sb(f"pf{i}", [128, 1184], F32) for i in range(2)]
        cqn = sb("cqn", [128, 256], BF16)
        cqT = sb("cqT", [128, 2, 128], BF16)
        kvn = sb("kvn", [128, 128], BF16)
        kvT = sb("kvT", [128, 128], BF16)
        qcf = sb("qcf", [128, 768], F32)
        qcb = sb("qcb", [128, 768], BF16)
        kvf = sb("kvf", [128, 1024], F32)
        kcb = sb("kcb", [128, 768], BF16)
        vcb = [sb(f"vcb{i}", [128, 512], BF16) for i in range(2)]
        krf = sb("krf", [128, 1, 32], F32)
        rt = [sb(f"rt{i}", [128, 8, 16], F32) for i in range(4)]
        sq8 = sb("sq8", [128, 640], F32)
        ss10 = sb("ss10", [128, 10], F32)
        r10 = sb("r10", [128, 10], F32)
        dn = sb("dn", [128, 640], F32)
        dqb = sb("dqb", [128, 640], BF16)
        dvb = [sb(f"dvb{i}", [128, 128], BF16) for i in range(2)]
        stqc = [sb(f"stqc{i}", [96, 8, 512], BF16) for i in range(2)]
        stkc = [sb(f"stkc{i}", [96, 8, 512], BF16) for i in range(2)]
        stqd = [sb(f"stqd{i}", [128, 4, 512], BF16) for i in range(2)]
        stkd = [sb(f"stkd{i}", [128, 512], BF16) for i in range(2)]
        T0 = ps("T0", [128, 1024], BF16)
        T1 = ps("T1", [128, 1024], BF16)
        pq = ps("pq", [128, 1536], F32)
        pc = ps("pc", [128, 1024], F32)
        self.dma("sp", ident.t[:], I["c_ident_bf"].t[:, :], [], [ident], ident)
        self.dma("pool", win.t[:], I["odd_w_in"].t.rearrange("(c p) n -> p c n", p=128), [], [win], win)
        self.dma("pool", wcq.t[:], I["odd_w_cq_up"].t.rearrange("(c p) n -> p c n", p=128), [], [wcq], wcq)
        self.dma("pool", wckv.t[:], I["odd_w_ckv_up"].t[:, :], [], [wckv], wckv)
        self.dma("sp", gmix.t[:], I["norm_mix"].t[1:2, :].partition_broadcast(128), [], [gmix], gmix)
        self.dma("sp", gcq.t[:], I["odd_cq_norm"].t[0:1, :].partition_broadcast(128), [], [gcq], gcq)
        self.dma("sp", gckv.t[:], I["odd_ckv_norm"].t[0:1, :].partition_broadcast(128), [], [gckv], gckv)
        self.dma("sp", gdq.t[:], I["odd_dq_norm"].t[0:1, :].partition_broadcast(128), [], [gdq], gdq)
        self.dma("sp", gdk.t[:], I["odd_dk_norm"].t[0:1, :].partition_broadcast(128), [], [gdk], gdk)
        import os
        NT1 = int(os.environ.get('NGRP1', NT // 4)) * 4

        def stage1(ti):
            grp, tt = divmod(ti, 4)
            sqc, skc, sqd, skd = stqc[grp % 2], stkc[grp % 2], stqd[grp % 2], stkd[grp % 2]
            b = ti % 2
            x_, ssq_, rstd_, hn_, hT_, tb_, vcb_, dvb_, pf = xt[b], ssq[b], rstd[b], hn[b], hT[b], tabs[b], vcb[b], dvb[b], pfs[b]
            tok = slice(ti * 128, (ti + 1) * 128)
            tsl = slice(tt * 128, (tt + 1) * 128)
            self.dma("sp", x_.t[:], self.xb.t[tok, :], [], [x_], x_)
            self.dma("sp", tb_.t[:, 0:32], I["c_ropec"].t[tok, :], [], [tb_], tb_)
            self.dma("sp", tb_.t[:, 32:64], I["c_roperow"].t[tok, :], [], [tb_], tb_)
            self.dma("sp", tb_.t[:, 64:96], I["c_ropecol"].t[tok, :], [], [tb_], tb_)
            self.rmsnorm_rstd(x_.t[:], [x_], junk.t[:], ssq_, rstd_, D)
            self.dve(lambda e, x_=x_, rstd_=rstd_, hn_=hn_: e.scalar_tensor_tensor(
                out=hn_.t[:], in0=x_.t[:], scalar=rstd_.t[:, 0:1], in1=gmix.t[:], op0=ALU.mult, op1=ALU.mult),
                [x_, rstd_, gmix], [hn_])
            for j in range(8):
                self.pe(lambda e, j=j, hn_=hn_: e.transpose(out=T0.t[:, j * 128:(j + 1) * 128], in_=hn_.t[:, j * 128:(j + 1) * 128],
                                                           identity=ident.t[:]), [hn_, ident], [T0])
            self.act(lambda e, hT_=hT_: e.copy(out=hT_.t[:].rearrange("p c n -> p (c n)"), in_=T0.t[:]), [T0], [hT_])
            for (c0, n_) in ((0, 512), (512, 512), (1024, 160)):
                for j in range(8):
                    self.pe(lambda e, j=j, c0=c0, n_=n_, hT_=hT_: e.matmul(pq.t[:, c0:c0 + n_], lhsT=hT_.t[:, j, :], rhs=win.t[:, j, c0:c0 + n_],
                                                                        start=(j == 0), stop=(j == 7)), [hT_, win], [pq])
            self.act(lambda e: e.copy(out=pf.t[:], in_=pq.t[:, 0:1184]), [pq], [pf])

        def stage2(ti):
            grp, tt = divmod(ti, 4)
            sqc, skc, sqd, skd = stqc[grp % 2], stkc[grp % 2], stqd[grp % 2], stkd[grp % 2]
            b = ti % 2
            x_, ssq_, rstd_, hn_, hT_, tb_, vcb_, dvb_, pf = xt[b], ssq[b], rstd[b], hn[b], hT[b], tabs[b], vcb[b], dvb[b], pfs[b]
            tok = slice(ti * 128, (ti + 1) * 128)
            tsl = slice(tt * 128, (tt + 1) * 128)
            self.rmsnorm_rstd(pf.t[:, 0:256], [pf], junk.t[:, 0:256], ssq2, rstd2, 256)
            self.dve(lambda e: e.scalar_tensor_tensor(out=cqn.t[:], in0=pf.t[:, 0:256], scalar=rstd2.t[:, 0:1], in1=gcq.t[:],
                                                      op0=ALU.mult, op1=ALU.mult), [pf, rstd2, gcq], [cqn])
            for j in range(2):
                self.pe(lambda e, j=j: e.transpose(out=T1.t[:, j * 128:(j + 1) * 128], in_=cqn.t[:, j * 128:(j + 1) * 128], identity=ident.t[:]),
                        [cqn, ident], [T1])
            self.act(lambda e: e.copy(out=cqT.t[:].rearrange("p c n -> p (c n)"), in_=T1.t[:, 0:256]), [T1], [cqT])
            for (c0, n_) in ((0, 512), (512, 256)):
                for j in range(2):
                    self.pe(lambda e, j=j, c0=c0, n_=n_: e.matmul(pc.t[:, c0:c0 + n_], lhsT=cqT.t[:, j, :], rhs=wcq.t[:, j, c0:c0 + n_],
                                                                 start=(j == 0), stop=(j == 1)), [cqT, wcq], [pc])
            self.act(lambda e: e.copy(out=qcf.t[:], in_=pc.t[:, 0:768]), [pc], [qcf])
            self.pool(lambda e: e.tensor_copy(out=qcb.t[:], in_=qcf.t[:]), [qcf], [qcb])
            qv = qcf.t[:].rearrange("p (h c) -> p h c", c=96)
            qo = qcb.t[:].rearrange("p (h c) -> p h c", c=96)
            cb = tb_.t[:, 0:16].unsqueeze(1).to_broadcast([128, 8, 16])
            sn = tb_.t[:, 16:32].unsqueeze(1).to_broadcast([128, 8, 16])
            self.rope_apply(qv[:, :, 64:80], qv[:, :, 80:96], cb, sn, qo[:, :, 64:80], qo[:, :, 80:96], rt, [128, 8, 16], [qcf], [tb_], qcb)
            self.rmsnorm_rstd(pf.t[:, 256:384], [pf], junk.t[:, 0:128], ssq3, rstd3, 128)
            self.dve(lambda e: e.scalar_tensor_tensor(out=kvn.t[:], in0=pf.t[:, 256:384], scalar=rstd3.t[:, 0:1], in1=gckv.t[:],
                                                      op0=ALU.mult, op1=ALU.mult), [pf, rstd3, gckv], [kvn])
            self.pe(lambda e: e.transpose(out=T1.t[:, 0:128], in_=kvn.t[:], identity=ident.t[:]), [kvn, ident], [T1])
            self.act(lambda e: e.copy(out=kvT.t[:], in_=T1.t[:, 0:128]), [T1], [kvT])
            for c0 in (0, 512):
                self.pe(lambda e, c0=c0: e.matmul(pc.t[:, c0:c0 + 512], lhsT=kvT.t[:], rhs=wckv.t[:, c0:c0 + 512], start=True, stop=True),
                        [kvT, wckv], [pc])
            self.act(lambda e: e.copy(out=kvf.t[:], in_=pc.t[:]), [pc], [kvf])
            kv3 = kvf.t[:].rearrange("p (h c) -> p h c", c=128)
            ko = kcb.t[:].rearrange("p (h c) -> p h c", c=96)
            self.pool(lambda e: e.tensor_copy(out=ko[:, :, 0:64], in_=kv3[:, :, 0:64]), [kvf], [kcb])
            self.pool(lambda e, vcb_=vcb_: e.tensor_copy(out=vcb_.t[:].rearrange("p (h c) -> p h c", c=64), in_=kv3[:, :, 64:128]), [kvf], [vcb_])
            self.dma("sp", self.Vc.t[tok, :], vcb_.t[:], [vcb_], [], vcb_)
            kr = pf.t[:, 384:416].rearrange("p (o c) -> p o c", o=1)
            cb1 = tb_.t[:, 0:16].unsqueeze(1)
            sn1 = tb_.t[:, 16:32].unsqueeze(1)
            self.rope_apply(kr[:, :, 0:16], kr[:, :, 16:32], cb1, sn1, krf.t[:, :, 0:16], krf.t[:, :, 16:32], rt, [128, 1, 16], [pf], [tb_], krf)
            self.pool(lambda e: e.tensor_copy(out=ko[:, :, 64:96], in_=krf.t[:].to_broadcast([128, 8, 32])), [krf], [kcb])
            self.dve(lambda e: e.tensor_tensor(out=sq8.t[:], in0=pf.t[:, 416:1056], in1=pf.t[:, 416:1056], op=ALU.mult), [pf], [sq8])
            self.dve(lambda e: e.tensor_reduce(out=ss10.t[:], in_=sq8.t[:].rearrange("p (g c) -> p g c", c=64), axis=AX.X, op=ALU.add), [sq8], [ss10])
            self.dve(lambda e: e.tensor_scalar(out=r10.t[:], in0=ss10.t[:], scalar1=1.0 / 64, scalar2=EPS, op0=ALU.mult, op1=ALU.add), [ss10], [r10])
            self.act(lambda e: e.activation(out=r10.t[:], in_=r10.t[:], func=AF.Sqrt), [r10], [r10])
            self.dve(lambda e: e.reciprocal(out=r10.t[:], in_=r10.t[:]), [r10], [r10])
            self.dve(lambda e: e.tensor_tensor(out=dn.t[:].rearrange("p (g c) -> p g c", c=64),
                                               in0=pf.t[:, 416:1056].rearrange("p (g c) -> p g c", c=64),
                                               in1=r10.t[:].unsqueeze(2).to_broadcast([128, 10, 64]), op=ALU.mult), [pf, r10], [dn])
            self.pool(lambda e: e.tensor_tensor(out=dn.t[:, 0:512].rearrange("p (g c) -> p g c", c=64),
                                                in0=dn.t[:, 0:512].rearrange("p (g c) -> p g c", c=64),
                                                in1=gdq.t[:].unsqueeze(1).to_broadcast([128, 8, 64]), op=ALU.mult), [dn, gdq], [dn])
            self.pool(lambda e: e.tensor_tensor(out=dn.t[:, 512:640].rearrange("p (g c) -> p g c", c=64),
                                                in0=dn.t[:, 512:640].rearrange("p (g c) -> p g c", c=64),
                                                in1=gdk.t[:].unsqueeze(1).to_broadcast([128, 2, 64]), op=ALU.mult), [dn, gdk], [dn])
            for (g0, g1) in ((0, 8), (8, 10)):
                ng = g1 - g0
                dv_ = dn.t[:, g0 * 64:g1 * 64].rearrange("p (g c) -> p g c", c=64)
                do_ = dqb.t[:, g0 * 64:g1 * 64].rearrange("p (g c) -> p g c", c=64)
                for (tb0, d0) in ((32, 0), (64, 32)):
                    cbx = tb_.t[:, tb0:tb0 + 16].unsqueeze(1).to_broadcast([128, ng, 16])
                    snx = tb_.t[:, tb0 + 16:tb0 + 32].unsqueeze(1).to_broadcast([128, ng, 16])
                    self.rope_apply(dv_[:, :, d0:d0 + 16], dv_[:, :, d0 + 16:d0 + 32], cbx, snx,
                                    do_[:, :, d0:d0 + 16], do_[:, :, d0 + 16:d0 + 32], rt, [128, ng, 16], [dn], [tb_], dqb)
            self.pool(lambda e, dvb_=dvb_: e.tensor_copy(out=dvb_.t[:], in_=pf.t[:, 1056:1184]), [pf], [dvb_])
            self.dma("sp", self.Vd.t[tok, :], dvb_.t[:], [dvb_], [], dvb_)
            for h in range(8):
                self.pe(lambda e, h=h: e.transpose(out=T1.t[0:96, h * 128:(h + 1) * 128], in_=qcb.t[:, h * 96:(h + 1) * 96], identity=ident.t[:]),
                        [qcb, ident], [T1])
            self.act(lambda e, sqc=sqc, tsl=tsl: e.copy(out=sqc.t[:, :, tsl], in_=T1.t[0:96, :].rearrange("p (h n) -> p h n", n=128)), [T1], [sqc])
            for h in range(8):
                self.pe(lambda e, h=h: e.transpose(out=T0.t[0:96, h * 128:(h + 1) * 128], in_=kcb.t[:, h * 96:(h + 1) * 96], identity=ident.t[:]),
                        [kcb, ident], [T0])
            self.act(lambda e, skc=skc, tsl=tsl: e.copy(out=skc.t[:, :, tsl], in_=T0.t[0:96, :].rearrange("p (h n) -> p h n", n=128)), [T0], [skc])
            for j in range(5):
                self.pe(lambda e, j=j: e.transpose(out=T1.t[:, j * 128:(j + 1) * 128], in_=dqb.t[:, j * 128:(j + 1) * 128], identity=ident.t[:]),
                        [dqb, ident], [T1])
            self.act(lambda e, sqd=sqd, tsl=tsl: e.copy(out=sqd.t[:, :, tsl], in_=T1.t[:, 0:512].rearrange("p (h n) -> p h n", n=128)), [T1], [sqd])
            self.act(lambda e, skd=skd, tsl=tsl: e.copy(out=skd.t[:, tsl], in_=T1.t[:, 512:640]), [T1], [skd])
            if tt == 3:
                gtok = slice(grp * 512, (grp + 1) * 512)
                self.dma("sp", self.QcT.t.rearrange("(h p) t -> p h t", p=96)[:, :, gtok], sqc.t[:], [sqc], [], sqc)
                self.dma("sp", self.KcT.t.rearrange("(h p) t -> p h t", p=96)[:, :, gtok], skc.t[:], [skc], [], skc)
                self.dma("sp", self.QdT.t.rearrange("(j p) t -> p j t", p=128)[:, :, gtok], sqd.t[:], [sqd], [], sqd)
                self.dma("sp", self.KdT.t[:, gtok], skd.t[:], [skd], [], skd)

        for s_ in range(NT1 + 1):
            if s_ < NT1:
                stage1(s_)
            if s_ >= 1:
                stage2(s_ - 1)
        self.phase_end(ph)

    def layer1_B(self):
        I = self.inp
        ph = self.phase_begin()
        sb = lambda n, s, d: self.sb(ph, n, s, d)
        ps = lambda n, s, d: self.ps(ph, n, s, d)
        sel = sb("sel", [65, 64], F32)
        qT = [sb(f"qT{i}", [96, S_LEN], BF16) for i in range(2)]
        kT = [sb(f"kT{i}", [96, S_LEN], BF16) for i in range(2)]
        vA = [sb(f"vA{i}", [128, NT, 65], BF16) for i in range(2)]
        oTs = [sb(f"oTs{i}", [64, S_LEN], BF16) for i in range(2)]
        osb = [sb(f"osb{i}", [65, 512], F32) for i in range(2)]
        rz = [sb(f"rz{i}", [64, 512], F32) for i in range(2)]
        Eb = [sb(f"Eb{i}", [128, 512], BF16) for i in range(6)]
        Sp = [ps(f"Sp{i}", [128, 512], F32) for i in range(5)]
        Op = [ps(f"Op{i}", [65, 512], F32) for i in range(2)]
        bc = [ps(f"bc{i}", [64, 512], F32) for i in range(1)]
        self.dma("sp", sel.t[:], I["c_sel"].t[:, :], [], [sel], sel)
        for i in range(2):
            self.pool(lambda e, i=i: e.memset(vA[i].t[:, :, 64:65], 1.0), [], [vA[i]])
        import os
        NH = int(os.environ.get('NH1', 16))
        NQC = int(os.environ.get('NQC1', S_LEN // 512))
        LA = 3
        kvi = -1
        prev_kv = None
        gi = 0
        oc_i = 0
        pending = []
        for h in range(NH):
            if h < 8:
                dk, scale = 96, 96.0 ** -0.5
                qsrc = self.QcT.t[h * 96:(h + 1) * 96, :]
                ksrc = self.KcT.t[h * 96:(h + 1) * 96, :]
                vsrc = self.Vc.t[:, h * 64:(h + 1) * 64]
                kvkey = ("c", h)
            else:
                hd = h - 8
                kvh = hd // 4
                dk, scale = 64, 0.125
                qsrc = self.QdT.t[hd * 64:(hd + 1) * 64, :]
                ksrc = self.KdT.t[kvh * 64:(kvh + 1) * 64, :]
                vsrc = self.Vd.t[:, kvh * 64:(kvh + 1) * 64]
                kvkey = ("d", kvh)
            q_ = qT[h % 2]
            self.dma("sp", q_.t[0:dk, :], qsrc, [], [q_], q_)
            if kvkey != prev_kv:
                kvi += 1
                prev_kv = kvkey
                k_, v_ = kT[kvi % 2], vA[kvi % 2]
                self.dma("sp", k_.t[0:dk, :], ksrc, [], [k_], k_)
                vv = vsrc.rearrange("(m a) c -> a m c", a=128)
                for m0 in range(0, NT, 16):
                    self.dma("pool", v_.t[:, m0:m0 + 16, 0:64], vv[:, m0:m0 + 16, :], [], [v_], v_)
            o_ = oTs[h % 2]
            iters = [(qc, m) for qc in range(NQC) for m in range(NT)]
            n_it = len(iters)
            sbuf_of = {}

            def emit_qk(idx):
                nonlocal gi
                qc, m = iters[idx]
                S_ = Sp[gi % 5]
                E_ = Eb[gi % 6]
                gi += 1
                sbuf_of[idx] = (S_, E_)
                self.pe(lambda e, S_=S_, m=m, qc=qc, k_=k_, q_=q_, dk=dk: e.matmul(
                    S_.t[:], lhsT=k_.t[0:dk, m * 128:(m + 1) * 128], rhs=q_.t[0:dk, qc * 512:(qc + 1) * 512], start=True, stop=True),
                    [k_, q_], [S_])
                self.act(lambda e, S_=S_, E_=E_, scale=scale: e.activation(out=E_.t[:], in_=S_.t[:], func=AF.Exp, scale=scale), [S_], [E_])

            def flush_pending(force=False):
                keep = []
                for item in pending:
                    item[0] -= 1
                    if item[0] <= 0 or force:
                        item[1]()
                    else:
                        keep.append(item)
                pending[:] = keep

            for idx in range(min(LA, n_it)):
                emit_qk(idx)
            for idx in range(n_it):
                if idx + LA < n_it:
                    emit_qk(idx + LA)
                qc, m = iters[idx]
                if m == 0:
                    O_ = Op[oc_i % 2]
                    ob_, rz_, bc_ = osb[oc_i % 2], rz[oc_i % 2], bc[0]
                    oc_i += 1
                S_, E_ = sbuf_of.pop(idx)
                self.pe(lambda e, O_=O_, v_=v_, m=m, E_=E_: e.matmul(O_.t[:], lhsT=v_.t[:, m, :], rhs=E_.t[:], start=(m == 0), stop=(m == NT - 1)),
                        [v_, E_], [O_])
                flush_pending()
                if m == NT - 1:
                    self.dve(lambda e, O_=O_, ob_=ob_: e.tensor_copy(out=ob_.t[:], in_=O_.t[:]), [O_], [ob_])

                    def norm(ob_=ob_, rz_=rz_, bc_=bc_, qc=qc, o_=o_):
                        self.pe(lambda e: e.matmul(bc_.t[:], lhsT=sel.t[:], rhs=ob_.t[:], start=True, stop=True), [sel, ob_], [bc_])
                        self.dve(lambda e: e.reciprocal(out=rz_.t[:], in_=bc_.t[:]), [bc_], [rz_])
                        self.dve(lambda e: e.tensor_tensor(out=o_.t[:, qc * 512:(qc + 1) * 512], in0=ob_.t[0:64, :], in1=rz_.t[:], op=ALU.mult),
                                 [ob_, rz_], [o_])
                    pending.append([4, norm])
            flush_pending(force=True)
            self.dma("sp", self.catT.t[h * 64:(h + 1) * 64, :], o_.t[:], [o_], [], o_)
        self.phase_end(ph)


def prep_inputs(inputs, b):
    f = lambda a: np.ascontiguousarray(np.asarray(a, dtype=np.float32))
    m = {
        "x": f(inputs["x"][b]), "norm_mix": f(inputs["norm_mix"]), "norm_ffn": f(inputs["norm_ffn"]),
        "even_w_in": f(inputs["even_w_in"][0]), "even_gmlp_norm": f(inputs["even_gmlp_norm"][0]).reshape(1, 256),
        "even_w_spatial": f(inputs["even_w_spatial"][0]), "even_b_spatial": f(inputs["even_b_spatial"][0]),
        "even_w_out": f(inputs["even_w_out"][0]), "odd_w_in": f(inputs["odd_w_in"][0]),
        "odd_cq_norm": f(inputs["odd_cq_norm"]), "odd_w_cq_up": f(inputs["odd_w_cq_up"][0]),
        "odd_ckv_norm": f(inputs["odd_ckv_norm"]), "odd_w_ckv_up": f(inputs["odd_w_ckv_up"][0]),
        "odd_dq_norm": f(inputs["odd_dq_norm"]), "odd_dk_norm": f(inputs["odd_dk_norm"]),
        "odd_w_out": f(inputs["odd_w_out"][0]), "moe_w_router": f(inputs["moe_w_router"]),
        "moe_w_gate": f(inputs["moe_w_gate"]), "moe_w_up": f(inputs["moe_w_up"]), "moe_w_down": f(inputs["moe_w_down"]),
        "final_norm": f(inputs["final_norm"]).reshape(1, D),
    }
    return m


def kernel(**inputs):
    nb = inputs["x"].shape[0]
    nc = Builder().build()
    consts = make_consts()
    in_maps = []
    for b in range(nb):
        m = prep_inputs(inputs, b)
        m.update(consts)
        in_maps.append(m)
    res = run_bass_kernel_spmd(nc, in_maps, core_ids=list(range(nb)))
    return np.stack([np.asarray(r["y"]) for r in res.results], axis=0).astype(np.float32)
```

```python
import numpy as np
import ml_dtypes
import concourse.bass as bass
import concourse.mybir as mybir
from concourse.bass_utils import run_bass_kernel_spmd
from contextlib import ExitStack

F32 = mybir.dt.float32
BF16 = mybir.dt.bfloat16
ALU = mybir.AluOpType
AF = mybir.ActivationFunctionType
AX = mybir.AxisListType

S_LEN = 8192
D = 1024
PAD = 1024
NT = S_LEN // 128
CH = 16000
EPS = 1e-6
NEXP = 16
CAP = 2 * S_LEN // NEXP
FUSE_WAIT = True


class Res:
    __slots__ = ("name", "writers", "readers", "stream")

    def __init__(self, name):
        self.name = name
        self.writers = {}
        self.readers = {}
        self.stream = None


class Stream:
    __slots__ = ("sem", "count", "key", "kind")

    def __init__(self, sem, key):
        self.sem = sem
        self.count = 0
        self.key = key


class Buf:
    __slots__ = ("t", "r", "track")

    def __init__(self, t, name, track=True):
        self.t = t
        self.r = Res(name)
        self.track = track


class Sched:
    ENG = ("pe", "act", "dve", "pool", "sp")
    HANDLE = {"pe": "tensor", "act": "scalar", "dve": "vector", "pool": "gpsimd", "sp": "sync"}

    def __init__(self, nc, stack):
        self.nc = nc
        self.stack = stack
        self.prog = {e: [] for e in self.ENG}
        self.cnt = {e: 0 for e in self.ENG}
        self.seen = {e: {} for e in self.ENG}
        self.esems = {e: [] for e in self.ENG}
        self.streams = []
        self.free_sems = {"sw": [], "hw": []}
        self.live = []
        self.bg = set()
        self.nsem = 0
        self.total = 0

    def _new_sem(self, name):
        self.nsem += 1
        return self.stack.enter_context(self.nc.semaphore(name))

    def _esem(self, e, chunk):
        lst = self.esems[e]
        while len(lst) <= chunk:
            lst.append(self._new_sem(f"s_{e}_{len(lst)}"))
        return lst[chunk]

    def stream_of(self, res, q="sp"):
        kind = "sw" if q == "pool" else "hw"
        if res.stream is None:
            pool_ = self.free_sems[kind]
            if pool_:
                pool_.sort(key=lambda x: x[1])
                sem, base = pool_.pop(0)
            else:
                sem, base = self._new_sem(f"d{len(self.streams)}"), 0
            res.stream = Stream(sem, ("d", len(self.streams)))
            res.stream.count = base
            res.stream.kind = kind
            self.streams.append(res.stream)
            self.live.append(res)
        assert res.stream.kind == kind, f"stream of {res.name} used from both DGE kinds"
        return res.stream

    def mark_bg(self, res):
        self.bg.add(res.stream.key)

    def bg_done(self):
        self.bg = set()

    def release_streams(self):
        keep = []
        for res in self.live:
            st = res.stream
            if st.key in self.bg:
                keep.append(res)
                continue
            self.free_sems[st.kind].append((st.sem, st.count))
            st.count = 0
            res.stream = None
        self.live = keep

    def _wait(self, e, key, val, payload):
        seen = self.seen[e]
        if seen.get(key, 0) >= val:
            return
        seen[key] = val
        self.prog[e].append(("wait", payload))

    def _wait_tok(self, e, key, val):
        if key[0] == "e":
            eng = key[1]
            if eng == e and e in ("pe", "sp"):
                return
            chunk, v = (val - 1) // CH, (val - 1) % CH + 1
            self._wait(e, key, val, (self._esem(eng, chunk), v))
        else:
            st = self.streams[key[1]]
            self._wait(e, key, val, (st.sem, val))

    def op(self, e, fn, reads=(), writes=(), stream=None):
        for r in reads:
            for k, v in r.writers.items():
                self._wait_tok(e, k, v)
        for w in writes:
            for k, v in w.writers.items():
                self._wait_tok(e, k, v)
            for k, v in w.readers.items():
                self._wait_tok(e, k, v)
        if stream is not None:
            st = self.stream_of(stream, e)
            st.count += 16
            key, val = st.key, st.count
            self.prog[e].append(("dma", fn, st.sem))
        else:
            self.cnt[e] += 1
            n = self.cnt[e]
            key, val = ("e", e), n
            self.prog[e].append(("ins", fn, self._esem(e, (n - 1) // CH)))
        for r in reads:
            r.readers[key] = val
        for w in writes:
            w.writers = {key: val}
            w.readers = {}

    def barrier(self, engines=None):
        for e in (engines or self.ENG):
            for st in self.streams:
                if st.count and st.key not in self.bg:
                    self._wait(e, st.key, st.count, (st.sem, st.count))
            for eng in self.ENG:
                if eng != e and self.cnt[eng]:
                    self._wait_tok(e, ("e", eng), self.cnt[eng])

    def emit(self):
        nc = self.nc
        import os
        if os.environ.get('DUMP'):
            for e in self.ENG:
                print('ENGINE', e)
                for it in self.prog[e][:int(os.environ['DUMP'])]:
                    if it[0] == 'wait':
                        print('   wait', it[1][0].name if hasattr(it[1][0], 'name') else it[1][0], it[1][1])
                    else:
                        print('  ', it[0], (it[2].name if hasattr(it[2], 'name') else it[2]), it[1].__code__.co_firstlineno)
        with nc.Block() as block:
            def mk(e):
                items = self.prog[e]

                def body(eng):
                    n = len(items)
                    for i, it in enumerate(items):
                        if it[0] == "wait":
                            if FUSE_WAIT and i + 1 < n and items[i + 1][0] != "wait":
                                continue
                            eng.wait_ge(it[1][0], it[1][1])
                        else:
                            ins = it[1](eng)
                            if FUSE_WAIT and i > 0 and items[i - 1][0] == "wait":
                                ins._wait_ge(items[i - 1][1][0], items[i - 1][1][1])
                            ins.then_inc(it[2], 16 if it[0] == "dma" else 1)
                return body

            for e in self.ENG:
                if self.prog[e]:
                    getattr(block, self.HANDLE[e])(mk(e))
        for e in self.ENG:
            self.total += len(self.prog[e])
            self.prog[e] = []


def _rope_table(pos, r, theta):
    half = r // 2
    inv = np.power(np.float32(theta), -np.arange(half, dtype=np.float32) * np.float32(2.0 / r)).astype(np.float32)
    ang = pos.astype(np.float32)[:, None] * inv[None, :]
    return np.concatenate([np.cos(ang), np.sin(ang)], axis=1).astype(np.float32)


def make_consts():
    pos = np.arange(S_LEN)
    c = {}
    c["c_ident_bf"] = np.eye(128, dtype=np.float32).astype(ml_dtypes.bfloat16)
    c["c_ident_f"] = np.eye(128, dtype=np.float32)
    sel = np.zeros((65, 64), np.float32)
    sel[64, :] = 1.0
    c["c_sel"] = sel
    c["c_rope0"] = _rope_table(pos, 16, 500000.0)
    c["c_ropec"] = _rope_table(pos, 32, 500000.0)
    c["c_roperow"] = _rope_table(pos // 64, 32, 10000.0)
    c["c_ropecol"] = _rope_table(pos % 64, 32, 10000.0)
    a = np.arange(128)[:, None]
    q = np.arange(128)[None, :]
    A = (a >= q).astype(np.float32)
    B = (a <= q).astype(np.float32)
    Ae = A * (a >= 64)
    Be = B * (a < 64)
    m = np.zeros((3, 128, 512), np.float32)
    m[0] = np.concatenate([A, B, A, B], 1)
    m[1] = np.concatenate([Ae, B, A, B], 1)
    m[2] = np.concatenate([A, B, A, Be], 1)
    c["c_mask"] = np.ascontiguousarray(m.transpose(1, 0, 2)).astype(ml_dtypes.bfloat16)
    return c


CONST_SPECS = {
    "c_ident_bf": ([128, 128], BF16), "c_ident_f": ([128, 128], F32), "c_sel": ([65, 64], F32),
    "c_rope0": ([S_LEN, 16], F32), "c_ropec": ([S_LEN, 32], F32), "c_roperow": ([S_LEN, 32], F32),
    "c_ropecol": ([S_LEN, 32], F32), "c_mask": ([128, 3, 512], BF16),
}

INPUT_SPECS = {
    "x": [S_LEN, D], "norm_mix": [2, D], "norm_ffn": [2, D], "even_w_in": [D, 2816],
    "even_gmlp_norm": [1, 256], "even_w_spatial": [4, 128, 128], "even_b_spatial": [4, 128],
    "even_w_out": [D, D], "odd_w_in": [D, 1184], "odd_cq_norm": [1, 256], "odd_w_cq_up": [256, 768],
    "odd_ckv_norm": [1, 128], "odd_w_ckv_up": [128, 1024], "odd_dq_norm": [1, 64], "odd_dk_norm": [1, 64],
    "odd_w_out": [D, D], "moe_w_router": [2, D, 16], "moe_w_gate": [2, 16, D, 512],
    "moe_w_up": [2, 16, D, 512], "moe_w_down": [2, 16, 512, D], "final_norm": [1, D],
}


class Builder:
    def __init__(self, dbg=(), upto="all"):
        self.nc = bass.Bass("TRN2", target_bir_lowering=False)
        self.dbg = set(dbg)
        self.upto = upto
        self.root = ExitStack()
        self.S = Sched(self.nc, self.root)
        self.inp = {}
        for k, shp in INPUT_SPECS.items():
            self.inp[k] = Buf(self.nc.dram_tensor(k, shp, F32, kind="ExternalInput").ap(), k, False)
        for k, (shp, dt) in CONST_SPECS.items():
            self.inp[k] = Buf(self.nc.dram_tensor(k, shp, dt, kind="ExternalInput").ap(), k, False)
        self.out = Buf(self.nc.dram_tensor("y", [S_LEN, D], F32, kind="ExternalOutput").ap(), "y", False)
        self.scr = {}
        self.phn = 0
        self.affall = self.sb(self.root, "affall", [128, NT, NEXP], F32)
        self.gw = self.sb(self.root, "gw", [128, NT, NEXP], F32)
        self.xa = self.dram("xa", [S_LEN, D], F32)
        self.xb = self.dram("xb", [S_LEN, D], F32)
        self.hTd = self.dram("hTd", [D, S_LEN], BF16)

    def dram(self, name, shape, dt):
        kind = "ExternalOutput" if name in self.dbg else "Internal"
        b = Buf(self.nc.dram_tensor(name, shape, dt, kind=kind).ap(), name, False)
        self.scr[name] = b
        return b

    def sb(self, ph, name, shape, dt):
        name = f"p{self.phn}_{name}"
        return Buf(ph.enter_context(self.nc.sbuf_tensor(name, shape, dt)), name)

    def ps(self, ph, name, shape, dt):
        name = f"p{self.phn}_{name}"
        return Buf(ph.enter_context(self.nc.psum_tensor(name, shape, dt)), name)

    def _op(self, e, fn, r, w):
        self.S.op(e, fn, reads=[b.r for b in r], writes=[b.r for b in w])

    def pe(self, fn, r, w):
        self._op("pe", fn, r, w)

    def act(self, fn, r, w):
        self._op("act", fn, r, w)

    def dve(self, fn, r, w):
        self._op("dve", fn, r, w)

    def pool(self, fn, r, w):
        self._op("pool", fn, r, w)

    def dma(self, q, out, in_, r, w, stream, **kw):
        self.S.op(q, lambda e: e.dma_start(out=out, in_=in_, **kw), reads=[b.r for b in r if b.track],
                  writes=[b.r for b in w if b.track], stream=stream.r)

    def phase_begin(self):
        self.phn += 1
        self.S.barrier()
        self.S.release_streams()
        return ExitStack()

    def phase_end(self, ph):
        self.S.emit()
        ph.close()

    def rmsnorm_rstd(self, src, srcbufs, junk, ssq, rstd, n, width=None):
        sc = float(n) ** -0.5
        self.pool(lambda e: e.memset(ssq.t[:, 0:1], 0.0), [], [ssq])
        self.act(lambda e: e.activation(out=junk, in_=src, func=AF.Square, scale=sc, accum_out=ssq.t[:, 0:1]),
                 srcbufs, [ssq])
        self.dve(lambda e: e.tensor_scalar_add(out=rstd.t[:, 0:1], in0=ssq.t[:, 0:1], scalar1=EPS), [ssq], [rstd])
        self.act(lambda e: e.activation(out=rstd.t[:, 0:1], in_=rstd.t[:, 0:1], func=AF.Sqrt), [rstd], [rstd])
        self.dve(lambda e: e.reciprocal(out=rstd.t[:, 0:1], in_=rstd.t[:, 0:1]), [rstd], [rstd])

    def build(self):
        with self.root:
            if self.upto in ("B1only", "B0only", "Eonly"):
                self.catT = self.dram("catT", [1024, S_LEN], BF16)
                if self.upto == "B1only":
                    self.QcT = self.dram("QcT", [768, S_LEN], BF16)
                    self.KcT = self.dram("KcT", [768, S_LEN], BF16)
                    self.Vc = self.dram("Vc", [S_LEN, 512], BF16)
                    self.QdT = self.dram("QdT", [512, S_LEN], BF16)
                    self.KdT = self.dram("KdT", [128, S_LEN], BF16)
                    self.Vd = self.dram("Vd", [S_LEN, 128], BF16)
                    self.layer1_B()
                elif self.upto == "B0only":
                    self.V0 = self.dram("V0", [PAD + S_LEN + PAD, 768], BF16)
                    self.QT0 = self.dram("QT0", [768, S_LEN], BF16)
                    self.KT0 = self.dram("KT0", [768, S_LEN], BF16)
                    self.layer0_B()
                else:
                    self.wg_bf = self.dram("wg_bf", [2, NEXP, D, 512], BF16)
                    self.wu_bf = self.dram("wu_bf", [2, NEXP, D, 512], BF16)
                    self.wd_bf = self.dram("wd_bf", [2, NEXP, 512, D], BF16)
                    self.phase_E(0, last=False)
                return self.finish()
            self.prologue()
            if self.upto == "pro":
                return self.finish()
            self.layer0_A()
            if self.upto == "A0":
                return self.finish()
            self.layer0_B()
            if self.upto == "B0":
                return self.finish()
            self.phase_C(0, self.inp["x"], self.inp["even_w_out"])
            if self.upto == "C0":
                return self.finish()
            self.phase_D()
            if self.upto == "D0":
                return self.finish()
            self.phase_E(0, last=False)
            if self.upto == "E0":
                return self.finish()
            self.layer1_A()
            if self.upto == "A1":
                return self.finish()
            self.layer1_B()
            if self.upto == "B1":
                return self.finish()
            self.phase_C(1, self.xb, self.inp["odd_w_out"])
            if self.upto == "C1":
                return self.finish()
            self.phase_D()
            self.phase_E(1, last=True)
            return self.finish()

    def finish(self):
        ph = self.phase_begin()
        self.phase_end(ph)
        return self.nc

    def prologue(self):
        self.wg_bf = self.dram("wg_bf", [2, NEXP, D, 512], BF16)
        self.wu_bf = self.dram("wu_bf", [2, NEXP, D, 512], BF16)
        self.wd_bf = self.dram("wd_bf", [2, NEXP, 512, D], BF16)

    def prologue_issue(self):
        for l in range(2):
            for e_ in range(NEXP):
                for src, dst in ((self.inp["moe_w_gate"], self.wg_bf), (self.inp["moe_w_up"], self.wu_bf),
                                 (self.inp["moe_w_down"], self.wd_bf)):
                    self.dma("pool", dst.t[l, e_], src.t[l, e_], [], [], dst)
        for dst in (self.wg_bf, self.wu_bf, self.wd_bf):
            self.S.mark_bg(dst.r)

    def layer0_A(self):
        nc = self.nc
        I = self.inp
        self.V0 = self.dram("V0", [PAD + S_LEN + PAD, 768], BF16)
        self.QT0 = self.dram("QT0", [768, S_LEN], BF16)
        self.KT0 = self.dram("KT0", [768, S_LEN], BF16)
        self.catT = self.dram("catT", [1024, S_LEN], BF16)
        ph = self.phase_begin()
        sb = lambda n, s, d: self.sb(ph, n, s, d)
        ps = lambda n, s, d: self.ps(ph, n, s, d)
        ident = sb("ident", [128, 128], BF16)
        win = sb("win", [128, 8, 2816], BF16)
        gmix = sb("gmix", [128, D], F32)
        gmn = sb("gmn", [128, 256], F32)
        wsf = sb("wsf", [128, 4, 128], F32)
        wsb = sb("wsb", [128, 4, 128], BF16)
        wsT = sb("wsT", [128, 4, 128], BF16)
        bsT = sb("bsT", [128, 4], F32)
        zero = sb("zero", [128, 768], BF16)
        xt = [sb(f"xt{i}", [128, D], F32) for i in range(2)]
        junk = sb("junk", [128, D], BF16)
        ssq = [sb(f"ssq{i}", [128, 1], F32) for i in range(2)]
        rstd = [sb(f"rstd{i}", [128, 1], F32) for i in range(2)]
        hn = [sb(f"hn{i}", [128, D], BF16) for i in range(2)]
        hT = [sb(f"hT{i}", [128, 8, 128], BF16) for i in range(2)]
        cs = [sb(f"cs{i}", [128, 16], F32) for i in range(2)]
        qk = [sb(f"qk{i}", [128, 1536], BF16) for i in range(2)]
        rt = [sb(f"rt{i}", [128, 24, 8], F32) for i in range(4)]
        vb = [sb(f"vb{i}", [128, 768], BF16) for i in range(2)]
        zgs = [sb(f"zg{i}", [128, 512], F32) for i in range(2)]
        qkfs = [sb(f"qkf{i}", [128, 1536], F32) for i in range(2)]
        sqv = sb("sqv", [128, 256], F32)
        ss4 = sb("ss4", [128, 4], F32)
        r4 = sb("r4", [128, 4], F32)
        vvn = sb("vvn", [128, 256], F32)
        vvb = sb("vvb", [128, 256], BF16)
        mxb = sb("mxb", [128, 256], F32)
        gout = sb("gout", [128, 256], BF16)
        stq = [sb(f"stq{i}", [128, 6, 512], BF16) for i in range(2)]
        stk = [sb(f"stk{i}", [128, 6, 512], BF16) for i in range(2)]
        stg = [sb(f"stg{i}", [64, 4, 512], BF16) for i in range(2)]
        T0 = ps("T0", [128, 1024], BF16)
        T1 = ps("T1", [128, 1024], BF16)
        pqk = ps("pqk", [128, 1536], F32)
        pvz = ps("pvz", [128, 1536], F32)

        self.dma("sp", ident.t[:], I["c_ident_bf"].t[:, :], [], [ident], ident)
        self.dma("pool", win.t[:], I["even_w_in"].t.rearrange("(c p) n -> p c n", p=128), [], [win], win)
        self.dma("sp", gmix.t[:], I["norm_mix"].t[0:1, :].partition_broadcast(128), [], [gmix], gmix)
        self.dma("sp", gmn.t[:], I["even_gmlp_norm"].t[0:1, :].partition_broadcast(128), [], [gmn], gmn)
        self.dma("sp", wsf.t[:], I["even_w_spatial"].t.rearrange("g p q -> p g q"), [], [wsf], wsf)
        self.dma("sp", bsT.t[:], I["even_b_spatial"].t.rearrange("g p -> p g"), [], [bsT], bsT,
                 allow_slow_non_contiguous=True)
        self.dve(lambda e: e.tensor_copy(out=wsb.t[:], in_=wsf.t[:]), [wsf], [wsb])
        for g in range(4):
            self.pe(lambda e, g=g: e.transpose(out=T0.t[:, g * 128:(g + 1) * 128], in_=wsb.t[:, g, :], identity=ident.t[:]),
                    [wsb, ident], [T0])
        self.act(lambda e: e.copy(out=wsT.t[:].rearrange("p g q -> p (g q)"), in_=T0.t[:, 0:512]), [T0], [wsT])
        self.pool(lambda e: e.memset(zero.t[:], 0.0), [], [zero])
        for side in range(2):
            for k in range(PAD // 128):
                r0 = (0 if side == 0 else PAD + S_LEN) + k * 128
                self.dma("sp", self.V0.t[r0:r0 + 128, :], zero.t[:], [zero], [self.V0], zero)

        import os
        NT0 = int(os.environ.get('NGRP', NT // 4)) * 4

        def stage1(ti):
            grp, tt = divmod(ti, 4)
            sq_, sk_, sg_ = stq[grp % 2], stk[grp % 2], stg[grp % 2]
            b = ti % 2
            x_, ssq_, rstd_, hn_, hT_, cs_, qk_, vb_, qkf, zg = xt[b], ssq[b], rstd[b], hn[b], hT[b], cs[b], qk[b], vb[b], qkfs[b], zgs[b]
            tok = slice(ti * 128, (ti + 1) * 128)
            self.dma("sp", x_.t[:], I["x"].t[tok, :], [I["x"]], [x_], x_)
            self.dma("sp", cs_.t[:], I["c_rope0"].t[tok, :], [], [cs_], cs_)
            self.rmsnorm_rstd(x_.t[:], [x_], junk.t[:], ssq_, rstd_, D)
            self.dve(lambda e, x_=x_, rstd_=rstd_, hn_=hn_: e.scalar_tensor_tensor(
                out=hn_.t[:], in0=x_.t[:], scalar=rstd_.t[:, 0:1], in1=gmix.t[:], op0=ALU.mult, op1=ALU.mult),
                [x_, rstd_, gmix], [hn_])
            for j in range(8):
                self.pe(lambda e, j=j, hn_=hn_: e.transpose(out=T0.t[:, j * 128:(j + 1) * 128],
                                                           in_=hn_.t[:, j * 128:(j + 1) * 128], identity=ident.t[:]),
                        [hn_, ident], [T0])
            self.act(lambda e, hT_=hT_: e.copy(out=hT_.t[:].rearrange("p c n -> p (c n)"), in_=T0.t[:]), [T0], [hT_])
            for bank in range(3):
                for j in range(8):
                    self.pe(lambda e, j=j, bank=bank, hT_=hT_: e.matmul(
                        pqk.t[:, bank * 512:(bank + 1) * 512], lhsT=hT_.t[:, j, :],
                        rhs=win.t[:, j, bank * 512:(bank + 1) * 512], start=(j == 0), stop=(j == 7)),
                        [hT_, win], [pqk])
            for (o0, n_, c0) in ((0, 512, 1536), (512, 256, 2048), (1024, 512, 2304)):
                for j in range(8):
                    self.pe(lambda e, j=j, o0=o0, n_=n_, c0=c0, hT_=hT_: e.matmul(
                        pvz.t[:, o0:o0 + n_], lhsT=hT_.t[:, j, :], rhs=win.t[:, j, c0:c0 + n_],
                        start=(j == 0), stop=(j == 7)), [hT_, win], [pvz])
            self.act(lambda e: e.copy(out=qkf.t[:], in_=pqk.t[:]), [pqk], [qkf])
            self.act(lambda e, vb_=vb_: e.copy(out=vb_.t[:], in_=pvz.t[:, 0:768]), [pvz], [vb_])
            self.dma("sp", self.V0.t[PAD + ti * 128:PAD + (ti + 1) * 128, :], vb_.t[:], [vb_], [self.V0], vb_)
            self.act(lambda e: e.activation(out=zg.t[:], in_=pvz.t[:, 1024:1536], func=AF.Gelu_apprx_tanh), [pvz], [zg])

        def stage2(ti):
            grp, tt = divmod(ti, 4)
            sq_, sk_, sg_ = stq[grp % 2], stk[grp % 2], stg[grp % 2]
            b = ti % 2
            x_, ssq_, rstd_, hn_, hT_, cs_, qk_, vb_, qkf, zg = xt[b], ssq[b], rstd[b], hn[b], hT[b], cs[b], qk[b], vb[b], qkfs[b], zgs[b]
            tok = slice(ti * 128, (ti + 1) * 128)
            self.pool(lambda e, qk_=qk_: e.tensor_copy(out=qk_.t[:], in_=qkf.t[:]), [qkf], [qk_])
            pv = qkf.t[:].rearrange("p (h c) -> p h c", c=64)
            qv = qk_.t[:].rearrange("p (h c) -> p h c", c=64)
            cosb = cs_.t[:, 0:8].unsqueeze(1).to_broadcast([128, 24, 8])
            sinb = cs_.t[:, 8:16].unsqueeze(1).to_broadcast([128, 24, 8])
            x1 = pv[:, :, 0:8]
            x2 = pv[:, :, 8:16]
            self.dve(lambda e, x1=x1, cosb=cosb: e.tensor_tensor(out=rt[0].t[:], in0=x1, in1=cosb, op=ALU.mult), [qkf, cs_], [rt[0]])
            self.dve(lambda e, x2=x2, sinb=sinb: e.tensor_tensor(out=rt[1].t[:], in0=x2, in1=sinb, op=ALU.mult), [qkf, cs_], [rt[1]])
            self.dve(lambda e, x1=x1, sinb=sinb: e.tensor_tensor(out=rt[2].t[:], in0=x1, in1=sinb, op=ALU.mult), [qkf, cs_], [rt[2]])
            self.dve(lambda e, x2=x2, cosb=cosb: e.tensor_tensor(out=rt[3].t[:], in0=x2, in1=cosb, op=ALU.mult), [qkf, cs_], [rt[3]])
            self.pool(lambda e, qv=qv: e.tensor_tensor(out=qv[:, :, 0:8], in0=rt[0].t[:], in1=rt[1].t[:], op=ALU.subtract),
                      [rt[0], rt[1]], [qk_])
            self.pool(lambda e, qv=qv: e.tensor_tensor(out=qv[:, :, 8:16], in0=rt[2].t[:], in1=rt[3].t[:], op=ALU.add),
                      [rt[2], rt[3]], [qk_])
            self.dve(lambda e: e.tensor_tensor(out=sqv.t[:], in0=zg.t[:, 256:512], in1=zg.t[:, 256:512], op=ALU.mult), [zg], [sqv])
            self.dve(lambda e: e.tensor_reduce(out=ss4.t[:], in_=sqv.t[:].rearrange("p (g c) -> p g c", c=64), axis=AX.X, op=ALU.add), [sqv], [ss4])
            self.dve(lambda e: e.tensor_scalar(out=r4.t[:], in0=ss4.t[:], scalar1=1.0 / 64, scalar2=EPS, op0=ALU.mult, op1=ALU.add), [ss4], [r4])
            self.act(lambda e: e.activation(out=r4.t[:], in_=r4.t[:], func=AF.Sqrt), [r4], [r4])
            self.dve(lambda e: e.reciprocal(out=r4.t[:], in_=r4.t[:]), [r4], [r4])
            self.dve(lambda e: e.tensor_tensor(out=vvn.t[:].rearrange("p (g c) -> p g c", c=64),
                                               in0=zg.t[:, 256:512].rearrange("p (g c) -> p g c", c=64),
                                               in1=r4.t[:].unsqueeze(2).to_broadcast([128, 4, 64]), op=ALU.mult), [zg, r4], [vvn])
            self.pool(lambda e: e.tensor_tensor(out=vvb.t[:], in0=vvn.t[:], in1=gmn.t[:], op=ALU.mult), [vvn, gmn], [vvb])
            for g in range(4):
                self.pe(lambda e, g=g: e.matmul(pvz.t[:, 768 + g * 64:768 + (g + 1) * 64], lhsT=wsT.t[:, g, :],
                                                rhs=vvb.t[:, g * 64:(g + 1) * 64], start=True, stop=True), [wsT, vvb], [pvz])
            self.act(lambda e: e.copy(out=sqv.t[:], in_=pvz.t[:, 768:1024]), [pvz], [sqv])
            self.dve(lambda e: e.tensor_tensor(out=mxb.t[:].rearrange("p (g c) -> p g c", c=64),
                                               in0=sqv.t[:].rearrange("p (g c) -> p g c", c=64),
                                               in1=bsT.t[:].unsqueeze(2).to_broadcast([128, 4, 64]), op=ALU.add), [sqv, bsT], [mxb])
            self.pool(lambda e: e.tensor_tensor(out=gout.t[:], in0=mxb.t[:], in1=zg.t[:, 0:256], op=ALU.mult), [mxb, zg], [gout])
            for half, st_ in ((0, sq_), (1, sk_)):
                for j in range(6):
                    c0 = half * 768 + j * 128
                    self.pe(lambda e, j=j, c0=c0, qk_=qk_: e.transpose(out=T1.t[:, j * 128:(j + 1) * 128], in_=qk_.t[:, c0:c0 + 128],
                                                                     identity=ident.t[:]), [qk_, ident], [T1])
                self.act(lambda e, st_=st_, tt=tt: e.copy(out=st_.t[:, :, tt * 128:(tt + 1) * 128],
                                                        in_=T1.t[:, 0:768].rearrange("p (j n) -> p j n", n=128)), [T1], [st_])
            for g in range(4):
                self.pe(lambda e, g=g: e.transpose(out=T1.t[0:64, g * 128:(g + 1) * 128], in_=gout.t[:, g * 64:(g + 1) * 64],
                                                   identity=ident.t[:]), [gout, ident], [T1])
            self.dve(lambda e, sg_=sg_, tt=tt: e.tensor_copy(out=sg_.t[:, :, tt * 128:(tt + 1) * 128],
                                                           in_=T1.t[0:64, 0:512].rearrange("p (j n) -> p j n", n=128)), [T1], [sg_])
            if tt == 3:
                gtok = slice(grp * 512, (grp + 1) * 512)
                self.dma("sp", self.QT0.t.rearrange("(j p) t -> p j t", p=128)[:, :, gtok], sq_.t[:], [sq_], [self.QT0], sq_)
                self.dma("sp", self.KT0.t.rearrange("(j p) t -> p j t", p=128)[:, :, gtok], sk_.t[:], [sk_], [self.KT0], sk_)
                self.dma("sp", self.catT.t[768:1024, :].rearrange("(j p) t -> p j t", p=64)[:, :, gtok], sg_.t[:], [sg_], [self.catT], sg_)

        for s_ in range(NT0 + 1):
            if s_ < NT0:
                stage1(s_)
            if s_ >= 1:
                stage2(s_ - 1)
        self.phase_end(ph)

    def layer0_B(self):
        I = self.inp
        ph = self.phase_begin()
        sb = lambda n, s, d: self.sb(ph, n, s, d)
        ps = lambda n, s, d: self.ps(ph, n, s, d)
        sel = sb("sel", [65, 64], F32)
        mask = sb("mask", [128, 3, 512], BF16)
        qT = [sb(f"qT{i}", [64, S_LEN], BF16) for i in range(2)]
        kT = [sb(f"kT{i}", [64, PAD + S_LEN + PAD], BF16) for i in range(2)]
        vA = [sb(f"vA{i}", [128, 80, 65], BF16) for i in range(2)]
        acc = sb("acc", [65, S_LEN], F32)
        aTs = sb("aTs", [64, S_LEN], BF16)
        rz = [sb(f"rz{i}", [64, 512], F32) for i in range(2)]
        Eb = [sb(f"Eb{i}", [128, 512], BF16) for i in range(3)]
        Pb = [sb(f"Pb{i}", [128, 512], BF16) for i in range(3)]
        Sp = [ps(f"Sp{i}", [128, 512], F32) for i in range(3)]
        Op = [ps(f"Op{i}", [65, 512], F32) for i in range(2)]
        bc = [ps(f"bc{i}", [64, 512], F32) for i in range(2)]
        self.dma("sp", sel.t[:], I["c_sel"].t[:, :], [], [sel], sel)
        self.dma("sp", mask.t[:], I["c_mask"].t[:, :, :], [], [mask], mask)
        for i in range(2):
            self.pool(lambda e, i=i: e.memset(kT[i].t[:, 0:PAD], 0.0), [], [kT[i]])
            self.pool(lambda e, i=i: e.memset(kT[i].t[:, PAD + S_LEN:], 0.0), [], [kT[i]])
            self.pool(lambda e, i=i: e.memset(vA[i].t[:, :, 64:65], 1.0), [], [vA[i]])
        if hasattr(self, "wg_bf") and self.upto != "B0only":
            self.prologue_issue()
        import os
        it = 0
        vi = 0
        og = 0
        for h in range(12):
            q_, k_ = qT[h % 2], kT[h % 2]
            self.dma("sp", q_.t[:], self.QT0.t[h * 64:(h + 1) * 64, :], [self.QT0], [q_], q_)
            self.dma("sp", k_.t[:, PAD:PAD + S_LEN], self.KT0.t[h * 64:(h + 1) * 64, :], [self.KT0], [k_], k_)
            for pi, d in enumerate((1, 4, 16)):
                L = S_LEN // d
                nb = L // 128
                ntile = nb + 1
                v_ = vA[vi % 2]
                vi += 1
                r0 = PAD - 64 * d
                rows = ntile * 128 * d
                src = self.V0.t[r0:r0 + rows, h * 64:(h + 1) * 64].rearrange("(m a i) c -> a i m c", a=128, i=d)
                dstv = v_.t[:, 0:d * ntile, 0:64].rearrange("a (i m) c -> a i m c", i=d)
                for i in range(d):
                    for m0 in range(0, ntile, 16):
                        m1 = min(ntile, m0 + 16)
                        if os.environ.get('NOV') and not (h == 0 and pi == 0):
                            continue
                        self.dma("sp", dstv[:, i, m0:m1, :], src[:, i, m0:m1, :], [self.V0], [v_], v_)
                for i in range(d):
                    for gq in range(nb // 4):
                        n0 = gq * 4
                        O_ = Op[og % 2]
                        og += 1
                        for pr in range(2):
                            S_ = Sp[it % 3]
                            E_ = Eb[it % 3]
                            P_ = Pb[it % 3]
                            it += 1
                            nA = n0 + 2 * pr
                            for (m, qb, nq, c0) in ((nA, nA, 128, 0), (nA + 1, nA, 256, 128), (nA + 2, nA + 1, 128, 384)):
                                k0 = PAD - 64 * d + i + 128 * d * m
                                q0 = 128 * d * qb + i
                                self.pe(lambda e, S_=S_, c0=c0, k0=k0, q0=q0, nq=nq, d=d, k_=k_, q_=q_: e.matmul(
                                    S_.t[:, c0:c0 + nq], lhsT=k_.t[:, k0:k0 + 127 * d + 1:d], rhs=q_.t[:, q0:q0 + (nq - 1) * d + 1:d],
                                    start=True, stop=True), [k_, q_], [S_])
                            self.act(lambda e, S_=S_, E_=E_: e.activation(out=E_.t[:], in_=S_.t[:], func=AF.Exp, scale=0.125), [S_], [E_])
                            first = (n0 + 2 * pr == 0)
                            last = (n0 + 2 * pr + 2 == nb)
                            assert not (first and last)
                            mi = 1 if first else (2 if last else 0)
                            self.dve(lambda e, E_=E_, P_=P_, mi=mi: e.tensor_tensor(out=P_.t[:], in0=E_.t[:], in1=mask.t[:, mi, :], op=ALU.mult),
                                     [E_, mask], [P_])
                            for blk in range(2):
                                n = n0 + 2 * pr + blk
                                oc = (2 * pr + blk) * 128
                                for ab in range(2):
                                    m = n + ab
                                    c0 = (blk * 2 + ab) * 128
                                    self.pe(lambda e, O_=O_, oc=oc, c0=c0, v_=v_, P_=P_, ti=i * ntile + m, ab=ab: e.matmul(
                                        O_.t[:, oc:oc + 128], lhsT=v_.t[:, ti, :], rhs=P_.t[:, c0:c0 + 128],
                                        start=(ab == 0), stop=(ab == 1)), [v_, P_], [O_])
                        a0 = 128 * d * n0 + i
                        av = acc.t[:, a0:a0 + 511 * d + 1:d]
                        if pi == 0:
                            self.dve(lambda e, av=av, O_=O_: e.tensor_copy(out=av, in_=O_.t[:]), [O_], [acc])
                        else:
                            self.dve(lambda e, av=av, O_=O_: e.tensor_tensor(out=av, in0=O_.t[:], in1=av, op=ALU.add), [O_, acc], [acc])
            for c in range(S_LEN // 512):
                cs_ = slice(c * 512, (c + 1) * 512)
                b_ = bc[c % 2]
                r_ = rz[c % 2]
                self.pe(lambda e, b_=b_, cs_=cs_: e.matmul(b_.t[:], lhsT=sel.t[:], rhs=acc.t[:, cs_], start=True, stop=True), [sel, acc], [b_])
                self.dve(lambda e, b_=b_, r_=r_: e.reciprocal(out=r_.t[:], in_=b_.t[:]), [b_], [r_])
                self.dve(lambda e, r_=r_, cs_=cs_: e.tensor_tensor(out=aTs.t[:, cs_], in0=acc.t[0:64, cs_], in1=r_.t[:], op=ALU.mult), [acc, r_], [aTs])
            self.dma("sp", self.catT.t[h * 64:(h + 1) * 64, :], aTs.t[:], [aTs], [self.catT], aTs)
        self.phase_end(ph)


    def phase_C(self, l, xin, wout_in):
        I = self.inp
        ph = self.phase_begin()
        sb = lambda n, s, d: self.sb(ph, n, s, d)
        ps = lambda n, s, d: self.ps(ph, n, s, d)
        ident = sb("ident", [128, 128], BF16)
        identf = sb("identf", [128, 128], F32)
        wout = sb("wout", [64, 16, D], BF16)
        gffn = sb("gffn", [128, D], F32)
        wr32 = sb("wr32", [128, 8, NEXP], F32)
        catg = [sb(f"catg{i}", [64, 16, 512], BF16) for i in range(2)]
        xt = [sb(f"xt{i}", [128, D], F32) for i in range(2)]
        x1t = [sb(f"x1t{i}", [128, D], F32) for i in range(2)]
        junk = sb("junk", [128, D], BF16)
        ssq = [sb(f"ssq{i}", [128, 1], F32) for i in range(2)]
        rstd = [sb(f"rstd{i}", [128, 1], F32) for i in range(2)]
        hnf = [sb(f"hnf{i}", [128, D], F32) for i in range(2)]
        hnb = [sb(f"hnb{i}", [128, D], BF16) for i in range(2)]
        hTs = [sb(f"hTs{i}", [128, 8, 512], BF16) for i in range(2)]
        hT32 = [sb(f"hT32{i}", [128, 8, 128], F32) for i in range(2)]
        lgs = sb("lgs", [128, NEXP], F32)
        ex = sb("ex", [128, NEXP], F32)
        mx = sb("mx", [128, 1], F32)
        se = sb("se", [128, 1], F32)
        T0 = ps("T0", [128, 1024], BF16)
        T32 = ps("T32", [128, 1024], F32)
        pm = [ps(f"pm{i}", [128, 1024], F32) for i in range(2)]
        lg = ps("lg", [128, NEXP], F32)
        self.dma("sp", ident.t[:], I["c_ident_bf"].t[:, :], [], [ident], ident)
        self.dma("sp", identf.t[:], I["c_ident_f"].t[:, :], [], [identf], identf)
        self.dma("pool", wout.t[:], wout_in.t.rearrange("(c p) n -> p c n", p=64), [], [wout], wout)
        self.dma("sp", gffn.t[:], I["norm_ffn"].t[l:l + 1, :].partition_broadcast(128), [], [gffn], gffn)
        self.dma("sp", wr32.t[:], I["moe_w_router"].t[l].rearrange("(c p) e -> p c e", p=128), [], [wr32], wr32)
        def stage1(ti):
            grp, tt = divmod(ti, 4)
            cg = catg[grp % 2]
            b = ti % 2
            x_, pm_ = xt[b], pm[b]
            tok = slice(ti * 128, (ti + 1) * 128)
            if tt == 0:
                gtok = slice(grp * 512, (grp + 1) * 512)
                self.dma("sp", cg.t[:], self.catT.t.rearrange("(c p) t -> p c t", p=64)[:, :, gtok], [], [cg], cg)
            self.dma("sp", x_.t[:], xin.t[tok, :], [], [x_], x_)
            for half in range(2):
                for c in range(16):
                    self.pe(lambda e, half=half, c=c, cg=cg, tt=tt, pm_=pm_: e.matmul(
                        pm_.t[:, half * 512:(half + 1) * 512], lhsT=cg.t[:, c, tt * 128:(tt + 1) * 128],
                        rhs=wout.t[:, c, half * 512:(half + 1) * 512], start=(c == 0), stop=(c == 15)), [cg, wout], [pm_])

        def stage2(ti):
            b = ti % 2
            x_, x1_, ssq_, rstd_, hnf_, hnb_, pm_ = xt[b], x1t[b], ssq[b], rstd[b], hnf[b], hnb[b], pm[b]
            tok = slice(ti * 128, (ti + 1) * 128)
            for half in range(2):
                hs_ = slice(half * 512, (half + 1) * 512)
                self.dve(lambda e, hs_=hs_, x_=x_, x1_=x1_, pm_=pm_: e.tensor_tensor(out=x1_.t[:, hs_], in0=pm_.t[:, hs_], in1=x_.t[:, hs_], op=ALU.add),
                         [pm_, x_], [x1_])
            self.dma("sp", self.xa.t[tok, :], x1_.t[:], [x1_], [], x1_)
            self.rmsnorm_rstd(x1_.t[:], [x1_], junk.t[:], ssq_, rstd_, D)
            self.dve(lambda e, x1_=x1_, rstd_=rstd_, hnf_=hnf_: e.scalar_tensor_tensor(
                out=hnf_.t[:], in0=x1_.t[:], scalar=rstd_.t[:, 0:1], in1=gffn.t[:], op0=ALU.mult, op1=ALU.mult),
                [x1_, rstd_, gffn], [hnf_])
            self.pool(lambda e, hnf_=hnf_, hnb_=hnb_: e.tensor_copy(out=hnb_.t[:], in_=hnf_.t[:]), [hnf_], [hnb_])

        def stage3(ti):
            grp, tt = divmod(ti, 4)
            hs = hTs[grp % 2]
            b = ti % 2
            hnf_, hnb_, h32_ = hnf[b], hnb[b], hT32[b]
            for j in range(8):
                self.pe(lambda e, j=j, hnb_=hnb_: e.transpose(out=T0.t[:, j * 128:(j + 1) * 128], in_=hnb_.t[:, j * 128:(j + 1) * 128],
                                                            identity=ident.t[:]), [hnb_, ident], [T0])
            self.act(lambda e, hs=hs, tt=tt: e.copy(out=hs.t[:, :, tt * 128:(tt + 1) * 128],
                                                  in_=T0.t[:].rearrange("p (c n) -> p c n", n=128)), [T0], [hs])
            for j in range(8):
                self.pe(lambda e, j=j, hnf_=hnf_: e.transpose(out=T32.t[:, j * 128:(j + 1) * 128], in_=hnf_.t[:, j * 128:(j + 1) * 128],
                                                            identity=identf.t[:]), [hnf_, identf], [T32])
            for half in range(2):
                self.act(lambda e, half=half, h32_=h32_: e.copy(out=h32_.t[:, half * 4:(half + 1) * 4, :].rearrange("p c n -> p (c n)"),
                                                               in_=T32.t[:, half * 512:(half + 1) * 512]), [T32], [h32_])
            for j in range(8):
                self.pe(lambda e, j=j, h32_=h32_: e.matmul(lg.t[:, :], lhsT=h32_.t[:, j, :], rhs=wr32.t[:, j, :],
                                                         start=(j == 0), stop=(j == 7)), [h32_, wr32], [lg])
            self.act(lambda e: e.copy(out=lgs.t[:], in_=lg.t[:]), [lg], [lgs])
            self.dve(lambda e: e.reduce_max(out=mx.t[:], in_=lgs.t[:], axis=AX.X), [lgs], [mx])
            self.dve(lambda e: e.tensor_scalar_mul(out=mx.t[:], in0=mx.t[:], scalar1=-1.0), [mx], [mx])
            self.pool(lambda e: e.memset(se.t[:], 0.0), [], [se])
            self.act(lambda e: e.activation(out=ex.t[:], in_=lgs.t[:], func=AF.Exp, bias=mx.t[:, 0:1], scale=1.0, accum_out=se.t[:, 0:1]),
                     [lgs, mx], [ex, se])
            self.dve(lambda e: e.reciprocal(out=se.t[:], in_=se.t[:]), [se], [se])
            self.dve(lambda e, ti=ti: e.tensor_scalar_mul(out=self.affall.t[:, ti, :], in0=ex.t[:], scalar1=se.t[:, 0:1]), [ex, se], [self.affall])
            if tt == 3:
                gtok = slice(grp * 512, (grp + 1) * 512)
                self.dma("sp", self.hTd.t.rearrange("(c p) t -> p c t", p=128)[:, :, gtok], hs.t[:], [hs], [], hs)

        for s_ in range(NT + 2):
            if s_ < NT:
                stage1(s_)
            if 0 <= s_ - 1 < NT:
                stage2(s_ - 1)
            if 0 <= s_ - 2 < NT:
                stage3(s_ - 2)
        self.phase_end(ph)

    def phase_D(self):
        ph = self.phase_begin()
        sb = lambda n, s, d: self.sb(ph, n, s, d)
        ps = lambda n, s, d: self.ps(ph, n, s, d)
        ones = sb("ones", [128, 128], BF16)
        cmp_ = sb("cmp", [128, NT * NEXP], BF16)
        cnts = sb("cnts", [128, NT * NEXP], F32)
        cnt16 = sb("cnt16", [128, NEXP], F32)
        lo = sb("lo", [128, NEXP], F32)
        mid = sb("mid", [128, NEXP], F32)
        ge = sb("ge", [128, NEXP], F32)
        msk = sb("msk", [128, NT, NEXP], F32)
        cntp = ps("cntp", [128, NT * NEXP], F32)
        aff = self.affall
        self.pool(lambda e: e.memset(ones.t[:], 1.0), [], [ones])
        self.pool(lambda e: e.memset(lo.t[:], 0.0), [], [lo])
        for k in range(1, 29):
            c = 2.0 ** -k
            self.dve(lambda e, c=c: e.tensor_scalar_add(out=mid.t[:], in0=lo.t[:], scalar1=c), [lo], [mid])
            self.dve(lambda e: e.tensor_tensor(out=cmp_.t[:].rearrange("p (t x) -> p t x", x=NEXP), in0=aff.t[:],
                                               in1=mid.t[:].unsqueeze(1).to_broadcast([128, NT, NEXP]), op=ALU.is_ge), [aff, mid], [cmp_])
            for half in range(2):
                self.pe(lambda e, half=half: e.matmul(cntp.t[:, half * 512:(half + 1) * 512], lhsT=ones.t[:],
                                                      rhs=cmp_.t[:, half * 512:(half + 1) * 512], start=True, stop=True), [ones, cmp_], [cntp])
            self.act(lambda e: e.copy(out=cnts.t[:], in_=cntp.t[:]), [cntp], [cnts])
            self.dve(lambda e: e.tensor_reduce(out=cnt16.t[:], in_=cnts.t[:].rearrange("p (t x) -> p x t", x=NEXP), axis=AX.X, op=ALU.add),
                     [cnts], [cnt16])
            self.dve(lambda e, c=c: e.tensor_scalar(out=ge.t[:], in0=cnt16.t[:], scalar1=CAP - 0.5, scalar2=c, op0=ALU.is_ge, op1=ALU.mult),
                     [cnt16], [ge])
            self.dve(lambda e: e.tensor_tensor(out=lo.t[:], in0=lo.t[:], in1=ge.t[:], op=ALU.add), [lo, ge], [lo])
        self.dve(lambda e: e.tensor_tensor(out=msk.t[:], in0=aff.t[:], in1=lo.t[:].unsqueeze(1).to_broadcast([128, NT, NEXP]), op=ALU.is_ge),
                 [aff, lo], [msk])
        self.dve(lambda e: e.tensor_tensor(out=self.gw.t[:], in0=msk.t[:], in1=aff.t[:], op=ALU.mult), [msk, aff], [self.gw])
        self.phase_end(ph)

    def phase_E(self, l, last):
        I = self.inp
        self.S.bg_done()
        ph = self.phase_begin()
        sb = lambda n, s, d: self.sb(ph, n, s, d)
        ps = lambda n, s, d: self.ps(ph, n, s, d)
        SG = 2048
        accb = sb("accb", [128, SG // 128, D], F32)
        hs = sb("hs", [128, 8, SG], BF16)
        wgt = [sb(f"wgt{i}", [128, 8, 512], BF16) for i in range(2)]
        wut = [sb(f"wut{i}", [128, 8, 512], BF16) for i in range(2)]
        wdt = [sb(f"wdt{i}", [128, 4, D], BF16) for i in range(2)]
        sgt = [sb(f"sgt{i}", [128, 512], F32) for i in range(2)]
        actT = [sb(f"actT{i}", [128, 4, 512], BF16) for i in range(2)]
        pg = [ps(f"pg{i}", [128, 512], F32) for i in range(2)]
        pu = [ps(f"pu{i}", [128, 512], F32) for i in range(2)]
        py = [ps(f"py{i}", [128, 512], F32) for i in range(4)]
        if last:
            gfin = sb("gfin", [128, D], F32)
            junk = sb("junk", [128, D], BF16)
            ssq = [sb(f"ssq{i}", [128, 1], F32) for i in range(2)]
            rstd = [sb(f"rstd{i}", [128, 1], F32) for i in range(2)]
            yo = [sb(f"yo{i}", [128, D], F32) for i in range(2)]
            self.dma("sp", gfin.t[:], I["final_norm"].t[0:1, :].partition_broadcast(128), [], [gfin], gfin)
        wi = 0
        kf = 0
        ky = 0
        ka = 0
        import os
        ECUT = int(os.environ.get('ECUT', 9))
        for sgi in range(int(os.environ.get('ESG', S_LEN // SG))):
            t0 = sgi * SG
            for q4 in range(4):
                self.dma("sp", accb.t[:, q4 * 4:(q4 + 1) * 4, :],
                         self.xa.t[t0 + q4 * 512:t0 + (q4 + 1) * 512, :].rearrange("(t p) d -> p t d", p=128), [], [accb], accb)
            self.dma("sp", hs.t[:], self.hTd.t.rearrange("(c p) t -> p c t", p=128)[:, :, t0:t0 + SG], [], [hs], hs)
            for ex_ in range(int(os.environ.get('EEXP', NEXP))):
                wg_, wu_, wd_ = wgt[wi % 2], wut[wi % 2], wdt[wi % 2]
                wi += 1
                self.dma("sp", wg_.t[:], self.wg_bf.t[l, ex_].rearrange("(c p) f -> p c f", p=128), [], [wg_], wg_)
                self.dma("sp", wu_.t[:], self.wu_bf.t[l, ex_].rearrange("(c p) f -> p c f", p=128), [], [wu_], wu_)
                self.dma("sp", wd_.t[:], self.wd_bf.t[l, ex_].rearrange("(c p) n -> p c n", p=128), [], [wd_], wd_)
                for g4 in range(SG // 512):
                    if ECUT < 2: continue
                    tk = slice(g4 * 512, (g4 + 1) * 512)
                    aT_ = actT[ka % 2]
                    ka += 1
                    for fc in range(4):
                        pg_, pu_, sg_ = pg[kf % 2], pu[kf % 2], sgt[kf % 2]
                        kf += 1
                        fs = slice(fc * 128, (fc + 1) * 128)
                        for j in range(8):
                            self.pe(lambda e, j=j, pg_=pg_, wg_=wg_, fs=fs, tk=tk: e.matmul(pg_.t[:], lhsT=wg_.t[:, j, fs], rhs=hs.t[:, j, tk],
                                                                                      start=(j == 0), stop=(j == 7)), [wg_, hs], [pg_])
                        for j in range(8):
                            self.pe(lambda e, j=j, pu_=pu_, wu_=wu_, fs=fs, tk=tk: e.matmul(pu_.t[:], lhsT=wu_.t[:, j, fs], rhs=hs.t[:, j, tk],
                                                                                      start=(j == 0), stop=(j == 7)), [wu_, hs], [pu_])
                        self.act(lambda e, pg_=pg_, sg_=sg_: e.activation(out=sg_.t[:], in_=pg_.t[:], func=AF.Silu), [pg_], [sg_])
                        self.dve(lambda e, pu_=pu_, sg_=sg_, aT_=aT_, fc=fc: e.tensor_tensor(out=aT_.t[:, fc, :], in0=pu_.t[:], in1=sg_.t[:], op=ALU.mult),
                                 [pu_, sg_], [aT_])
                    for tt in range(4):
                        if ECUT < 3: continue
                        tl = g4 * 4 + tt
                        tile = sgi * (SG // 128) + tl
                        for ch in range(2):
                            py_ = py[ky % 4]
                            ky += 1
                            for fc in range(4):
                                self.pe(lambda e, py_=py_, aT_=aT_, fc=fc, tt=tt, wd_=wd_, ch=ch: e.matmul(
                                    py_.t[:], lhsT=aT_.t[:, fc, tt * 128:(tt + 1) * 128], rhs=wd_.t[:, fc, ch * 512:(ch + 1) * 512],
                                    start=(fc == 0), stop=(fc == 3)), [aT_, wd_], [py_])
                            av = accb.t[:, tl, ch * 512:(ch + 1) * 512]
                            self.dve(lambda e, av=av, py_=py_, tile=tile, ex_=ex_: e.scalar_tensor_tensor(
                                out=av, in0=py_.t[:], scalar=self.gw.t[:, tile, ex_:ex_ + 1], in1=av, op0=ALU.mult, op1=ALU.add),
                                [py_, self.gw, accb], [accb])
            if not last:
                for q4 in range(4):
                    self.dma("sp", self.xb.t[t0 + q4 * 512:t0 + (q4 + 1) * 512, :].rearrange("(t p) d -> p t d", p=128),
                             accb.t[:, q4 * 4:(q4 + 1) * 4, :], [accb], [], accb)
            else:
                for tl in range(SG // 128):
                    b = tl % 2
                    self.rmsnorm_rstd(accb.t[:, tl, :], [accb], junk.t[:], ssq[b], rstd[b], D)
                    self.dve(lambda e, tl=tl, b=b: e.scalar_tensor_tensor(out=yo[b].t[:], in0=accb.t[:, tl, :], scalar=rstd[b].t[:, 0:1],
                                                                        in1=gfin.t[:], op0=ALU.mult, op1=ALU.mult), [accb, rstd[b], gfin], [yo[b]])
                    r0 = t0 + tl * 128
                    self.dma("sp", self.out.t[r0:r0 + 128, :], yo[b].t[:], [yo[b]], [], yo[b])
        self.phase_end(ph)


    def rope_apply(self, x1, x2, cosb, sinb, o1, o2, rt, shape, srcbufs, tabbufs, outbuf):
        v = [r.t[:, 0:shape[1], 0:shape[2]] for r in rt]
        self.dve(lambda e: e.tensor_tensor(out=v[0], in0=x1, in1=cosb, op=ALU.mult), srcbufs + tabbufs, [rt[0]])
        self.dve(lambda e: e.tensor_tensor(out=v[1], in0=x2, in1=sinb, op=ALU.mult), srcbufs + tabbufs, [rt[1]])
        self.dve(lambda e: e.tensor_tensor(out=v[2], in0=x1, in1=sinb, op=ALU.mult), srcbufs + tabbufs, [rt[2]])
        self.dve(lambda e: e.tensor_tensor(out=v[3], in0=x2, in1=cosb, op=ALU.mult), srcbufs + tabbufs, [rt[3]])
        self.pool(lambda e: e.tensor_tensor(out=o1, in0=v[0], in1=v[1], op=ALU.subtract), [rt[0], rt[1]], [outbuf])
        self.pool(lambda e: e.tensor_tensor(out=o2, in0=v[2], in1=v[3], op=ALU.add), [rt[2], rt[3]], [outbuf])

    def layer1_A(self):
        I = self.inp
        self.QcT = self.dram("QcT", [768, S_LEN], BF16)
        self.KcT = self.dram("KcT", [768, S_LEN], BF16)
        self.Vc = self.dram("Vc", [S_LEN, 512], BF16)
        self.QdT = self.dram("QdT", [512, S_LEN], BF16)
        self.KdT = self.dram("KdT", [128, S_LEN], BF16)
        self.Vd = self.dram("Vd", [S_LEN, 128], BF16)
        ph = self.phase_begin()
        sb = lambda n, s, d: self.sb(ph, n, s, d)
        ps = lambda n, s, d: self.ps(ph, n, s, d)
        ident = sb("ident", [128, 128], BF16)
        win = sb("win", [128, 8, 1184], BF16)
        wcq = sb("wcq", [128, 2, 768], BF16)
        wckv = sb("wckv", [128, 1024], BF16)
        gmix = sb("gmix", [128, D], F32)
        gcq = sb("gcq", [128, 256], F32)
        gckv = sb("gckv", [128, 128], F32)
        gdq = sb("gdq", [128, 64], F32)
        gdk = sb("gdk", [128, 64], F32)
        xt = [sb(f"xt{i}", [128, D], F32) for i in range(2)]
        junk = sb("junk", [128, D], BF16)
        ssq = [sb(f"ssq{i}", [128, 1], F32) for i in range(2)]
        rstd = [sb(f"rstd{i}", [128, 1], F32) for i in range(2)]
        ssq2 = sb("ssq2", [128, 1], F32)
        rstd2 = sb("rstd2", [128, 1], F32)
        ssq3 = sb("ssq3", [128, 1], F32)
        rstd3 = sb("rstd3", [128, 1], F32)
        hn = [sb(f"hn{i}", [128, D], BF16) for i in range(2)]
        hT = [sb(f"hT{i}", [128, 8, 128], BF16) for i in range(2)]
        tabs = [sb(f"tabs{i}", [128, 96], F32) for i in range(2)]
        pfs = [sb(f"pf{i}", [128, 1184], F32) for i in range(2)]
        cqn = sb("cqn", [128, 256], BF16)
        cqT = sb("cqT", [128, 2, 128], BF16)
        kvn = sb("kvn", [128, 128], BF16)
        kvT = sb("kvT", [128, 128], BF16)
        qcf = sb("qcf", [128, 768], F32)
        qcb = sb("qcb", [128, 768], BF16)
        kvf = sb("kvf", [128, 1024], F32)
        kcb = sb("kcb", [128, 768], BF16)
        vcb = [sb(f"vcb{i}", [128, 512], BF16) for i in range(2)]
        krf = sb("krf", [128, 1, 32], F32)
        rt = [sb(f"rt{i}", [128, 8, 16], F32) for i in range(4)]
        sq8 = sb("sq8", [128, 640], F32)
        ss10 = sb("ss10", [128, 10], F32)
        r10 = sb("r10", [128, 10], F32)
        dn = sb("dn", [128, 640], F32)
        dqb = sb("dqb", [128, 640], BF16)
        dvb = [sb(f"dvb{i}", [128, 128], BF16) for i in range(2)]
        stqc = [sb(f"stqc{i}", [96, 8, 512], BF16) for i in range(2)]
        stkc = [sb(f"stkc{i}", [96, 8, 512], BF16) for i in range(2)]
        stqd = [sb(f"stqd{i}", [128, 4, 512], BF16) for i in range(2)]
        stkd = [sb(f"stkd{i}", [128, 512], BF16) for i in range(2)]
        T0 = ps("T0", [128, 1024], BF16)
        T1 = ps("T1", [128, 1024], BF16)
        pq = ps("pq", [128, 1536], F32)
        pc = ps("pc", [128, 1024], F32)
        self.dma("sp", ident.t[:], I["c_ident_bf"].t[:, :], [], [ident], ident)
        self.dma("pool", win.t[:], I["odd_w_in"].t.rearrange("(c p) n -> p c n", p=128), [], [win], win)
        self.dma("pool", wcq.t[:], I["odd_w_cq_up"].t.rearrange("(c p) n -> p c n", p=128), [], [wcq], wcq)
        self.dma("pool", wckv.t[:], I["odd_w_ckv_up"].t[:, :], [], [wckv], wckv)
        self.dma("sp", gmix.t[:], I["norm_mix"].t[1:2, :].partition_broadcast(128), [], [gmix], gmix)
        self.dma("sp", gcq.t[:], I["odd_cq_norm"].t[0:1, :].partition_broadcast(128), [], [gcq], gcq)
        self.dma("sp", gckv.t[:], I["odd_ckv_norm"].t[0:1, :].partition_broadcast(128), [], [gckv], gckv)
        self.dma("sp", gdq.t[:], I["odd_dq_norm"].t[0:1, :].partition_broadcast(128), [], [gdq], gdq)
        self.dma("sp", gdk.t[:], I["odd_dk_norm"].t[0:1, :].partition_broadcast(128), [], [gdk], gdk)
        import os
        NT1 = int(os.environ.get('NGRP1', NT // 4)) * 4

        def stage1(ti):
            grp, tt = divmod(ti, 4)
            sqc, skc, sqd, skd = stqc[grp % 2], stkc[grp % 2], stqd[grp % 2], stkd[grp % 2]
            b = ti % 2
            x_, ssq_, rstd_, hn_, hT_, tb_, vcb_, dvb_, pf = xt[b], ssq[b], rstd[b], hn[b], hT[b], tabs[b], vcb[b], dvb[b], pfs[b]
            tok = slice(ti * 128, (ti + 1) * 128)
            tsl = slice(tt * 128, (tt + 1) * 128)
            self.dma("sp", x_.t[:], self.xb.t[tok, :], [], [x_], x_)
            self.dma("sp", tb_.t[:, 0:32], I["c_ropec"].t[tok, :], [], [tb_], tb_)
            self.dma("sp", tb_.t[:, 32:64], I["c_roperow"].t[tok, :], [], [tb_], tb_)
            self.dma("sp", tb_.t[:, 64:96], I["c_ropecol"].t[tok, :], [], [tb_], tb_)
            self.rmsnorm_rstd(x_.t[:], [x_], junk.t[:], ssq_, rstd_, D)
            self.dve(lambda e, x_=x_, rstd_=rstd_, hn_=hn_: e.scalar_tensor_tensor(
                out=hn_.t[:], in0=x_.t[:], scalar=rstd_.t[:, 0:1], in1=gmix.t[:], op0=ALU.mult, op1=ALU.mult),
                [x_, rstd_, gmix], [hn_])
            for j in range(8):
                self.pe(lambda e, j=j, hn_=hn_: e.transpose(out=T0.t[:, j * 128:(j + 1) * 128], in_=hn_.t[:, j * 128:(j + 1) * 128],
                                                           identity=ident.t[:]), [hn_, ident], [T0])
            self.act(lambda e, hT_=hT_: e.copy(out=hT_.t[:].rearrange("p c n -> p (c n)"), in_=T0.t[:]), [T0], [hT_])
            for (c0, n_) in ((0, 512), (512, 512), (1024, 160)):
                for j in range(8):
                    self.pe(lambda e, j=j, c0=c0, n_=n_, hT_=hT_: e.matmul(pq.t[:, c0:c0 + n_], lhsT=hT_.t[:, j, :], rhs=win.t[:, j, c0:c0 + n_],
                                                                        start=(j == 0), stop=(j == 7)), [hT_, win], [pq])
            self.act(lambda e: e.copy(out=pf.t[:], in_=pq.t[:, 0:1184]), [pq], [pf])

        def stage2(ti):
            grp, tt = divmod(ti, 4)
            sqc, skc, sqd, skd = stqc[grp % 2], stkc[grp % 2], stqd[grp % 2], stkd[grp % 2]
            b = ti % 2
            x_, ssq_, rstd_, hn_, hT_, tb_, vcb_, dvb_, pf = xt[b], ssq[b], rstd[b], hn[b], hT[b], tabs[b], vcb[b], dvb[b], pfs[b]
            tok = slice(ti * 128, (ti + 1) * 128)
            tsl = slice(tt * 128, (tt + 1) * 128)
            self.rmsnorm_rstd(pf.t[:, 0:256], [pf], junk.t[:, 0:256], ssq2, rstd2, 256)
            self.dve(lambda e: e.scalar_tensor_tensor(out=cqn.t[:], in0=pf.t[:, 0:256], scalar=rstd2.t[:, 0:1], in1=gcq.t[:],
                                                      op0=ALU.mult, op1=ALU.mult), [pf, rstd2, gcq], [cqn])
            for j in range(2):
                self.pe(lambda e, j=j: e.transpose(out=T1.t[:, j * 128:(j + 1) * 128], in_=cqn.t[:, j * 128:(j + 1) * 128], identity=ident.t[:]),
                        [cqn, ident], [T1])
            self.act(lambda e: e.copy(out=cqT.t[:].rearrange("p c n -> p (c n)"), in_=T1.t[:, 0:256]), [T1], [cqT])
            for (c0, n_) in ((0, 512), (512, 256)):
                for j in range(2):
                    self.pe(lambda e, j=j, c0=c0, n_=n_: e.matmul(pc.t[:, c0:c0 + n_], lhsT=cqT.t[:, j, :], rhs=wcq.t[:, j, c0:c0 + n_],
                                                                 start=(j == 0), stop=(j == 1)), [cqT, wcq], [pc])
            self.act(lambda e: e.copy(out=qcf.t[:], in_=pc.t[:, 0:768]), [pc], [qcf])
            self.pool(lambda e: e.tensor_copy(out=qcb.t[:], in_=qcf.t[:]), [qcf], [qcb])
            qv = qcf.t[:].rearrange("p (h c) -> p h c", c=96)
            qo = qcb.t[:].rearrange("p (h c) -> p h c", c=96)
            cb = tb_.t[:, 0:16].unsqueeze(1).to_broadcast([128, 8, 16])
            sn = tb_.t[:, 16:32].unsqueeze(1).to_broadcast([128, 8, 16])
            self.rope_apply(qv[:, :, 64:80], qv[:, :, 80:96], cb, sn, qo[:, :, 64:80], qo[:, :, 80:96], rt, [128, 8, 16], [qcf], [tb_], qcb)
            self.rmsnorm_rstd(pf.t[:, 256:384], [pf], junk.t[:, 0:128], ssq3, rstd3, 128)
            self.dve(lambda e: e.scalar_tensor_tensor(out=kvn.t[:], in0=pf.t[:, 256:384], scalar=rstd3.t[:, 0:1], in1=gckv.t[:],
                                                      op0=ALU.mult, op1=ALU.mult), [pf, rstd3, gckv], [kvn])
            self.pe(lambda e: e.transpose(out=T1.t[:, 0:128], in_=kvn.t[:], identity=ident.t[:]), [kvn, ident], [T1])
            self.act(lambda e: e.copy(out=kvT.t[:], in_=T1.t[:, 0:128]), [T1], [kvT])
            for c0 in (0, 512):
                self.pe(lambda e, c0=c0: e.matmul(pc.t[:, c0:c0 + 512], lhsT=kvT.t[:], rhs=wckv.t[:, c0:c0 + 512], start=True, stop=True),
                        [kvT, wckv], [pc])
            self.act(lambda e: e.copy(out=kvf.t[:], in_=pc.t[:]), [pc], [kvf])
            kv3 = kvf.t[:].rearrange("p (h c) -> p h c", c=128)
            ko = kcb.t[:].rearrange("p (h c) -> p h c", c=96)
            self.pool(lambda e: e.tensor_copy(out=ko[:, :, 0:64], in_=kv3[:, :, 0:64]), [kvf], [kcb])
            self.pool(lambda e, vcb_=vcb_: e.tensor_copy(out=vcb_.t[:].rearrange("p (h c) -> p h c", c=64), in_=kv3[:, :, 64:128]), [kvf], [vcb_])
            self.dma("sp", self.Vc.t[tok, :], vcb_.t[:], [vcb_], [], vcb_)
            kr = pf.t[:, 384:416].rearrange("p (o c) -> p o c", o=1)
            cb1 = tb_.t[:, 0:16].unsqueeze(1)
            sn1 = tb_.t[:, 16:32].unsqueeze(1)
            self.rope_apply(kr[:, :, 0:16], kr[:, :, 16:32], cb1, sn1, krf.t[:, :, 0:16], krf.t[:, :, 16:32], rt, [128, 1, 16], [pf], [tb_], krf)
            self.pool(lambda e: e.tensor_copy(out=ko[:, :, 64:96], in_=krf.t[:].to_broadcast([128, 8, 32])), [krf], [kcb])
            self.dve(lambda e: e.tensor_tensor(out=sq8.t[:], in0=pf.t[:, 416:1056], in1=pf.t[:, 416:1056], op=ALU.mult), [pf], [sq8])
            self.dve(lambda e: e.tensor_reduce(out=ss10.t[:], in_=sq8.t[:].rearrange("p (g c) -> p g c", c=64), axis=AX.X, op=ALU.add), [sq8], [ss10])
            self.dve(lambda e: e.tensor_scalar(out=r10.t[:], in0=ss10.t[:], scalar1=1.0 / 64, scalar2=EPS, op0=ALU.mult, op1=ALU.add), [ss10], [r10])
            self.act(lambda e: e.activation(out=r10.t[:], in_=r10.t[:], func=AF.Sqrt), [r10], [r10])
            self.dve(lambda e: e.reciprocal(out=r10.t[:], in_=r10.t[:]), [r10], [r10])
            self.dve(lambda e: e.tensor_tensor(out=dn.t[:].rearrange("p (g c) -> p g c", c=64),
                                               in0=pf.t[:, 416:1056].rearrange("p (g c) -> p g c", c=64),
                                               in1=r10.t[:].unsqueeze(2).to_broadcast([128, 10, 64]), op=ALU.mult), [pf, r10], [dn])
            self.pool(lambda e: e.tensor_tensor(out=dn.t[:, 0:512].rearrange("p (g c) -> p g c", c=64),
                                                in0=dn.t[:, 0:512].rearrange("p (g c) -> p g c", c=64),
                                                in1=gdq.t[:].unsqueeze(1).to_broadcast([128, 8, 64]), op=ALU.mult), [dn, gdq], [dn])
            self.pool(lambda e: e.tensor_tensor(out=dn.t[:, 512:640].rearrange("p (g c) -> p g c", c=64),
                                                in0=dn.t[:, 512:640].rearrange("p (g c) -> p g c", c=64),
                                                in1=gdk.t[:].unsqueeze(1).to_broadcast([128, 2, 64]), op=ALU.mult), [dn, gdk], [dn])
            for (g0, g1) in ((0, 8), (8, 10)):
                ng = g1 - g0
                dv_ = dn.t[:, g0 * 64:g1 * 64].rearrange("p (g c) -> p g c", c=64)
                do_ = dqb.t[:, g0 * 64:g1 * 64].rearrange("p (g c) -> p g c", c=64)
                for (tb0, d0) in ((32, 0), (64, 32)):
                    cbx = tb_.t[:, tb0:tb0 + 16].unsqueeze(1).to_broadcast([128, ng, 16])
                    snx = tb_.t[:, tb0 + 16:tb0 + 32].unsqueeze(1).to_broadcast([128, ng, 16])
                    self.rope_apply(dv_[:, :, d0:d0 + 16], dv_[:, :, d0 + 16:d0 + 32], cbx, snx,
                                    do_[:, :, d0:d0 + 16], do_[:, :, d0 + 16:d0 + 32], rt, [128, ng, 16], [dn], [tb_], dqb)
            self.pool(lambda e, dvb_=dvb_: e.tensor_copy(out=dvb_.t[:], in_=pf.t[:, 1056:1184]), [pf], [dvb_])
            self.dma("sp", self.Vd.t[tok, :], dvb_.t[:], [dvb_], [], dvb_)
            for h in range(8):
                self.pe(lambda e, h=h: e.transpose(out=T1.t[0:96, h * 128:(h + 1) * 128], in_=qcb.t[:, h * 96:(h + 1) * 96], identity=ident.t[:]),
                        [qcb, ident], [T1])
            self.act(lambda e, sqc=sqc, tsl=tsl: e.copy(out=sqc.t[:, :, tsl], in_=T1.t[0:96, :].rearrange("p (h n) -> p h n", n=128)), [T1], [sqc])
            for h in range(8):
                self.pe(lambda e, h=h: e.transpose(out=T0.t[0:96, h * 128:(h + 1) * 128], in_=kcb.t[:, h * 96:(h + 1) * 96], identity=ident.t[:]),
                        [kcb, ident], [T0])
            self.act(lambda e, skc=skc, tsl=tsl: e.copy(out=skc.t[:, :, tsl], in_=T0.t[0:96, :].rearrange("p (h n) -> p h n", n=128)), [T0], [skc])
            for j in range(5):
                self.pe(lambda e, j=j: e.transpose(out=T1.t[:, j * 128:(j + 1) * 128], in_=dqb.t[:, j * 128:(j + 1) * 128], identity=ident.t[:]),
                        [dqb, ident], [T1])
            self.act(lambda e, sqd=sqd, tsl=tsl: e.copy(out=sqd.t[:, :, tsl], in_=T1.t[:, 0:512].rearrange("p (h n) -> p h n", n=128)), [T1], [sqd])
            self.act(lambda e, skd=skd, tsl=tsl: e.copy(out=skd.t[:, tsl], in_=T1.t[:, 512:640]), [T1], [skd])
            if tt == 3:
                gtok = slice(grp * 512, (grp + 1) * 512)
                self.dma("sp", self.QcT.t.rearrange("(h p) t -> p h t", p=96)[:, :, gtok], sqc.t[:], [sqc], [], sqc)
                self.dma("sp", self.KcT.t.rearrange("(h p) t -> p h t", p=96)[:, :, gtok], skc.t[:], [skc], [], skc)
                self.dma("sp", self.QdT.t.rearrange("(j p) t -> p j t", p=128)[:, :, gtok], sqd.t[:], [sqd], [], sqd)
                self.dma("sp", self.KdT.t[:, gtok], skd.t[:], [skd], [], skd)

        for s_ in range(NT1 + 1):
            if s_ < NT1:
                stage1(s_)
            if s_ >= 1:
                stage2(s_ - 1)
        self.phase_end(ph)

    def layer1_B(self):
        I = self.inp
        ph = self.phase_begin()
        sb = lambda n, s, d: self.sb(ph, n, s, d)
        ps = lambda n, s, d: self.ps(ph, n, s, d)
        sel = sb("sel", [65, 64], F32)
        qT = [sb(f"qT{i}", [96, S_LEN], BF16) for i in range(2)]
        kT = [sb(f"kT{i}", [96, S_LEN], BF16) for i in range(2)]
        vA = [sb(f"vA{i}", [128, NT, 65], BF16) for i in range(2)]
        oTs = [sb(f"oTs{i}", [64, S_LEN], BF16) for i in range(2)]
        osb = [sb(f"osb{i}", [65, 512], F32) for i in range(2)]
        rz = [sb(f"rz{i}", [64, 512], F32) for i in range(2)]
        Eb = [sb(f"Eb{i}", [128, 512], BF16) for i in range(6)]
        Sp = [ps(f"Sp{i}", [128, 512], F32) for i in range(5)]
        Op = [ps(f"Op{i}", [65, 512], F32) for i in range(2)]
        bc = [ps(f"bc{i}", [64, 512], F32) for i in range(1)]
        self.dma("sp", sel.t[:], I["c_sel"].t[:, :], [], [sel], sel)
        for i in range(2):
            self.pool(lambda e, i=i: e.memset(vA[i].t[:, :, 64:65], 1.0), [], [vA[i]])
        import os
        NH = int(os.environ.get('NH1', 16))
        NQC = int(os.environ.get('NQC1', S_LEN // 512))
        LA = 3
        kvi = -1
        prev_kv = None
        gi = 0
        oc_i = 0
        pending = []
        for h in range(NH):
            if h < 8:
                dk, scale = 96, 96.0 ** -0.5
                qsrc = self.QcT.t[h * 96:(h + 1) * 96, :]
                ksrc = self.KcT.t[h * 96:(h + 1) * 96, :]
                vsrc = self.Vc.t[:, h * 64:(h + 1) * 64]
                kvkey = ("c", h)
            else:
                hd = h - 8
                kvh = hd // 4
                dk, scale = 64, 0.125
                qsrc = self.QdT.t[hd * 64:(hd + 1) * 64, :]
                ksrc = self.KdT.t[kvh * 64:(kvh + 1) * 64, :]
                vsrc = self.Vd.t[:, kvh * 64:(kvh + 1) * 64]
                kvkey = ("d", kvh)
            q_ = qT[h % 2]
            self.dma("sp", q_.t[0:dk, :], qsrc, [], [q_], q_)
            if kvkey != prev_kv:
                kvi += 1
                prev_kv = kvkey
                k_, v_ = kT[kvi % 2], vA[kvi % 2]
                self.dma("sp", k_.t[0:dk, :], ksrc, [], [k_], k_)
                vv = vsrc.rearrange("(m a) c -> a m c", a=128)
                for m0 in range(0, NT, 16):
                    self.dma("pool", v_.t[:, m0:m0 + 16, 0:64], vv[:, m0:m0 + 16, :], [], [v_], v_)
            o_ = oTs[h % 2]
            iters = [(qc, m) for qc in range(NQC) for m in range(NT)]
            n_it = len(iters)
            sbuf_of = {}

            def emit_qk(idx):
                nonlocal gi
                qc, m = iters[idx]
                S_ = Sp[gi % 5]
                E_ = Eb[gi % 6]
                gi += 1
                sbuf_of[idx] = (S_, E_)
                self.pe(lambda e, S_=S_, m=m, qc=qc, k_=k_, q_=q_, dk=dk: e.matmul(
                    S_.t[:], lhsT=k_.t[0:dk, m * 128:(m + 1) * 128], rhs=q_.t[0:dk, qc * 512:(qc + 1) * 512], start=True, stop=True),
                    [k_, q_], [S_])
                self.act(lambda e, S_=S_, E_=E_, scale=scale: e.activation(out=E_.t[:], in_=S_.t[:], func=AF.Exp, scale=scale), [S_], [E_])

            def flush_pending(force=False):
                keep = []
                for item in pending:
                    item[0] -= 1
                    if item[0] <= 0 or force:
                        item[1]()
                    else:
                        keep.append(item)
                pending[:] = keep

            for idx in range(min(LA, n_it)):
                emit_qk(idx)
            for idx in range(n_it):
                if idx + LA < n_it:
                    emit_qk(idx + LA)
                qc, m = iters[idx]
                if m == 0:
                    O_ = Op[oc_i % 2]
                    ob_, rz_, bc_ = osb[oc_i % 2], rz[oc_i % 2], bc[0]
                    oc_i += 1
                S_, E_ = sbuf_of.pop(idx)
                self.pe(lambda e, O_=O_, v_=v_, m=m, E_=E_: e.matmul(O_.t[:], lhsT=v_.t[:, m, :], rhs=E_.t[:], start=(m == 0), stop=(m == NT - 1)),
                        [v_, E_], [O_])
                flush_pending()
                if m == NT - 1:
                    self.dve(lambda e, O_=O_, ob_=ob_: e.tensor_copy(out=ob_.t[:], in_=O_.t[:]), [O_], [ob_])

                    def norm(ob_=ob_, rz_=rz_, bc_=bc_, qc=qc, o_=o_):
                        self.pe(lambda e: e.matmul(bc_.t[:], lhsT=sel.t[:], rhs=ob_.t[:], start=True, stop=True), [sel, ob_], [bc_])
                        self.dve(lambda e: e.reciprocal(out=rz_.t[:], in_=bc_.t[:]), [bc_], [rz_])
                        self.dve(lambda e: e.tensor_tensor(out=o_.t[:, qc * 512:(qc + 1) * 512], in0=ob_.t[0:64, :], in1=rz_.t[:], op=ALU.mult),
                                 [ob_, rz_], [o_])
                    pending.append([4, norm])
            flush_pending(force=True)
            self.dma("sp", self.catT.t[h * 64:(h + 1) * 64, :], o_.t[:], [o_], [], o_)
        self.phase_end(ph)


def prep_inputs(inputs, b):
    f = lambda a: np.ascontiguousarray(np.asarray(a, dtype=np.float32))
    m = {
        "x": f(inputs["x"][b]), "norm_mix": f(inputs["norm_mix"]), "norm_ffn": f(inputs["norm_ffn"]),
        "even_w_in": f(inputs["even_w_in"][0]), "even_gmlp_norm": f(inputs["even_gmlp_norm"][0]).reshape(1, 256),
        "even_w_spatial": f(inputs["even_w_spatial"][0]), "even_b_spatial": f(inputs["even_b_spatial"][0]),
        "even_w_out": f(inputs["even_w_out"][0]), "odd_w_in": f(inputs["odd_w_in"][0]),
        "odd_cq_norm": f(inputs["odd_cq_norm"]), "odd_w_cq_up": f(inputs["odd_w_cq_up"][0]),
        "odd_ckv_norm": f(inputs["odd_ckv_norm"]), "odd_w_ckv_up": f(inputs["odd_w_ckv_up"][0]),
        "odd_dq_norm": f(inputs["odd_dq_norm"]), "odd_dk_norm": f(inputs["odd_dk_norm"]),
        "odd_w_out": f(inputs["odd_w_out"][0]), "moe_w_router": f(inputs["moe_w_router"]),
        "moe_w_gate": f(inputs["moe_w_gate"]), "moe_w_up": f(inputs["moe_w_up"]), "moe_w_down": f(inputs["moe_w_down"]),
        "final_norm": f(inputs["final_norm"]).reshape(1, D),
    }
    return m


def kernel(**inputs):
    nb = inputs["x"].shape[0]
    nc = Builder().build()
    consts = make_consts()
    in_maps = []
    for b in range(nb):
        m = prep_inputs(inputs, b)
        m.update(consts)
        in_maps.append(m)
    res = run_bass_kernel_spmd(nc, in_maps, core_ids=list(range(nb)))
    return np.stack([np.asarray(r["y"]) for r in res.results], axis=0).astype(np.float32)
```

```python
import numpy as np
import ml_dtypes
import concourse.bass as bass
import concourse.mybir as mybir
from concourse.bass_utils import run_bass_kernel_spmd
from contextlib import ExitStack

F32 = mybir.dt.float32
BF16 = mybir.dt.bfloat16
ALU = mybir.AluOpType
AF = mybir.ActivationFunctionType
AX = mybir.AxisListType

S_LEN = 8192
D = 1024
PAD = 1024
NT = S_LEN // 128
CH = 16000
EPS = 1e-6
NEXP = 16
CAP = 2 * S_LEN // NEXP
FUSE_WAIT = True


class Res:
    __slots__ = ("name", "writers", "readers", "stream")

    def __init__(self, name):
        self.name = name
        self.writers = {}
        self.readers = {}
        self.stream = None


class Stream:
    __slots__ = ("sem", "count", "key", "kind")

    def __init__(self, sem, key):
        self.sem = sem
        self.count = 0
        self.key = key


class Buf:
    __slots__ = ("t", "r", "track")

    def __init__(self, t, name, track=True):
        self.t = t
        self.r = Res(name)
        self.track = track


class Sched:
    ENG = ("pe", "act", "dve", "pool", "sp")
    HANDLE = {"pe": "tensor", "act": "scalar", "dve": "vector", "pool": "gpsimd", "sp": "sync"}

    def __init__(self, nc, stack):
        self.nc = nc
        self.stack = stack
        self.prog = {e: [] for e in self.ENG}
        self.cnt = {e: 0 for e in self.ENG}
        self.seen = {e: {} for e in self.ENG}
        self.esems = {e: [] for e in self.ENG}
        self.streams = []
        self.free_sems = {"sw": [], "hw": []}
        self.live = []
        self.bg = set()
        self.nsem = 0
        self.total = 0

    def _new_sem(self, name):
        self.nsem += 1
        return self.stack.enter_context(self.nc.semaphore(name))

    def _esem(self, e, chunk):
        lst = self.esems[e]
        while len(lst) <= chunk:
            lst.append(self._new_sem(f"s_{e}_{len(lst)}"))
        return lst[chunk]

    def stream_of(self, res, q="sp"):
        kind = "sw" if q == "pool" else "hw"
        if res.stream is None:
            pool_ = self.free_sems[kind]
            if pool_:
                pool_.sort(key=lambda x: x[1])
                sem, base = pool_.pop(0)
            else:
                sem, base = self._new_sem(f"d{len(self.streams)}"), 0
            res.stream = Stream(sem, ("d", len(self.streams)))
            res.stream.count = base
            res.stream.kind = kind
            self.streams.append(res.stream)
            self.live.append(res)
        assert res.stream.kind == kind, f"stream of {res.name} used from both DGE kinds"
        return res.stream

    def mark_bg(self, res):
        self.bg.add(res.stream.key)

    def bg_done(self):
        self.bg = set()

    def release_streams(self):
        keep = []
        for res in self.live:
            st = res.stream
            if st.key in self.bg:
                keep.append(res)
                continue
            self.free_sems[st.kind].append((st.sem, st.count))
            st.count = 0
            res.stream = None
        self.live = keep

    def _wait(self, e, key, val, payload):
        seen = self.seen[e]
        if seen.get(key, 0) >= val:
            return
        seen[key] = val
        self.prog[e].append(("wait", payload))

    def _wait_tok(self, e, key, val):
        if key[0] == "e":
            eng = key[1]
            if eng == e and e in ("pe", "sp"):
                return
            chunk, v = (val - 1) // CH, (val - 1) % CH + 1
            self._wait(e, key, val, (self._esem(eng, chunk), v))
        else:
            st = self.streams[key[1]]
            self._wait(e, key, val, (st.sem, val))

    def op(self, e, fn, reads=(), writes=(), stream=None):
        for r in reads:
            for k, v in r.writers.items():
                self._wait_tok(e, k, v)
        for w in writes:
            for k, v in w.writers.items():
                self._wait_tok(e, k, v)
            for k, v in w.readers.items():
                self._wait_tok(e, k, v)
        if stream is not None:
            st = self.stream_of(stream, e)
            st.count += 16
            key, val = st.key, st.count
            self.prog[e].append(("dma", fn, st.sem))
        else:
            self.cnt[e] += 1
            n = self.cnt[e]
            key, val = ("e", e), n
            self.prog[e].append(("ins", fn, self._esem(e, (n - 1) // CH)))
        for r in reads:
            r.readers[key] = val
        for w in writes:
            w.writers = {key: val}
            w.readers = {}

    def barrier(self, engines=None):
        for e in (engines or self.ENG):
            for st in self.streams:
                if st.count and st.key not in self.bg:
                    self._wait(e, st.key, st.count, (st.sem, st.count))
            for eng in self.ENG:
                if eng != e and self.cnt[eng]:
                    self._wait_tok(e, ("e", eng), self.cnt[eng])

    def emit(self):
        nc = self.nc
        import os
        if os.environ.get('DUMP'):
            for e in self.ENG:
                print('ENGINE', e)
                for it in self.prog[e][:int(os.environ['DUMP'])]:
                    if it[0] == 'wait':
                        print('   wait', it[1][0].name if hasattr(it[1][0], 'name') else it[1][0], it[1][1])
                    else:
                        print('  ', it[0], (it[2].name if hasattr(it[2], 'name') else it[2]), it[1].__code__.co_firstlineno)
        with nc.Block() as block:
            def mk(e):
                items = self.prog[e]

                def body(eng):
                    n = len(items)
                    for i, it in enumerate(items):
                        if it[0] == "wait":
                            if FUSE_WAIT and i + 1 < n and items[i + 1][0] != "wait":
                                continue
                            eng.wait_ge(it[1][0], it[1][1])
                        else:
                            ins = it[1](eng)
                            if FUSE_WAIT and i > 0 and items[i - 1][0] == "wait":
                                ins._wait_ge(items[i - 1][1][0], items[i - 1][1][1])
                            ins.then_inc(it[2], 16 if it[0] == "dma" else 1)
                return body

            for e in self.ENG:
                if self.prog[e]:
                    getattr(block, self.HANDLE[e])(mk(e))
        for e in self.ENG:
            self.total += len(self.prog[e])
            self.prog[e] = []


def _rope_table(pos, r, theta):
    half = r // 2
    inv = np.power(np.float32(theta), -np.arange(half, dtype=np.float32) * np.float32(2.0 / r)).astype(np.float32)
    ang = pos.astype(np.float32)[:, None] * inv[None, :]
    return np.concatenate([np.cos(ang), np.sin(ang)], axis=1).astype(np.float32)


def make_consts():
    pos = np.arange(S_LEN)
    c = {}
    c["c_ident_bf"] = np.eye(128, dtype=np.float32).astype(ml_dtypes.bfloat16)
    c["c_ident_f"] = np.eye(128, dtype=np.float32)
    sel = np.zeros((65, 64), np.float32)
    sel[64, :] = 1.0
    c["c_sel"] = sel
    c["c_rope0"] = _rope_table(pos, 16, 500000.0)
    c["c_ropec"] = _rope_table(pos, 32, 500000.0)
    c["c_roperow"] = _rope_table(pos // 64, 32, 10000.0)
    c["c_ropecol"] = _rope_table(pos % 64, 32, 10000.0)
    a = np.arange(128)[:, None]
    q = np.arange(128)[None, :]
    A = (a >= q).astype(np.float32)
    B = (a <= q).astype(np.float32)
    Ae = A * (a >= 64)
    Be = B * (a < 64)
    m = np.zeros((3, 128, 512), np.float32)
    m[0] = np.concatenate([A, B, A, B], 1)
    m[1] = np.concatenate([Ae, B, A, B], 1)
    m[2] = np.concatenate([A, B, A, Be], 1)
    c["c_mask"] = np.ascontiguousarray(m.transpose(1, 0, 2)).astype(ml_dtypes.bfloat16)
    return c


CONST_SPECS = {
    "c_ident_bf": ([128, 128], BF16), "c_ident_f": ([128, 128], F32), "c_sel": ([65, 64], F32),
    "c_rope0": ([S_LEN, 16], F32), "c_ropec": ([S_LEN, 32], F32), "c_roperow": ([S_LEN, 32], F32),
    "c_ropecol": ([S_LEN, 32], F32), "c_mask": ([128, 3, 512], BF16),
}

INPUT_SPECS = {
    "x": [S_LEN, D], "norm_mix": [2, D], "norm_ffn": [2, D], "even_w_in": [D, 2816],
    "even_gmlp_norm": [1, 256], "even_w_spatial": [4, 128, 128], "even_b_spatial": [4, 128],
    "even_w_out": [D, D], "odd_w_in": [D, 1184], "odd_cq_norm": [1, 256], "odd_w_cq_up": [256, 768],
    "odd_ckv_norm": [1, 128], "odd_w_ckv_up": [128, 1024], "odd_dq_norm": [1, 64], "odd_dk_norm": [1, 64],
    "odd_w_out": [D, D], "moe_w_router": [2, D, 16], "moe_w_gate": [2, 16, D, 512],
    "moe_w_up": [2, 16, D, 512], "moe_w_down": [2, 16, 512, D], "final_norm": [1, D],
}


class Builder:
    def __init__(self, dbg=(), upto="all"):
        self.nc = bass.Bass("TRN2", target_bir_lowering=False)
        self.dbg = set(dbg)
        self.upto = upto
        self.root = ExitStack()
        self.S = Sched(self.nc, self.root)
        self.inp = {}
        for k, shp in INPUT_SPECS.items():
            self.inp[k] = Buf(self.nc.dram_tensor(k, shp, F32, kind="ExternalInput").ap(), k, False)
        for k, (shp, dt) in CONST_SPECS.items():
            self.inp[k] = Buf(self.nc.dram_tensor(k, shp, dt, kind="ExternalInput").ap(), k, False)
        self.out = Buf(self.nc.dram_tensor("y", [S_LEN, D], F32, kind="ExternalOutput").ap(), "y", False)
        self.scr = {}
        self.phn = 0
        self.affall = self.sb(self.root, "affall", [128, NT, NEXP], F32)
        self.gw = self.sb(self.root, "gw", [128, NT, NEXP], F32)
        self.xa = self.dram("xa", [S_LEN, D], F32)
        self.xb = self.dram("xb", [S_LEN, D], F32)
        self.hTd = self.dram("hTd", [D, S_LEN], BF16)

    def dram(self, name, shape, dt):
        kind = "ExternalOutput" if name in self.dbg else "Internal"
        b = Buf(self.nc.dram_tensor(name, shape, dt, kind=kind).ap(), name, False)
        self.scr[name] = b
        return b

    def sb(self, ph, name, shape, dt):
        name = f"p{self.phn}_{name}"
        return Buf(ph.enter_context(self.nc.sbuf_tensor(name, shape, dt)), name)

    def ps(self, ph, name, shape, dt):
        name = f"p{self.phn}_{name}"
        return Buf(ph.enter_context(self.nc.psum_tensor(name, shape, dt)), name)

    def _op(self, e, fn, r, w):
        self.S.op(e, fn, reads=[b.r for b in r], writes=[b.r for b in w])

    def pe(self, fn, r, w):
        self._op("pe", fn, r, w)

    def act(self, fn, r, w):
        self._op("act", fn, r, w)

    def dve(self, fn, r, w):
        self._op("dve", fn, r, w)

    def pool(self, fn, r, w):
        self._op("pool", fn, r, w)

    def dma(self, q, out, in_, r, w, stream, **kw):
        self.S.op(q, lambda e: e.dma_start(out=out, in_=in_, **kw), reads=[b.r for b in r if b.track],
                  writes=[b.r for b in w if b.track], stream=stream.r)

    def phase_begin(self):
        self.phn += 1
        self.S.barrier()
        self.S.release_streams()
        return ExitStack()

    def phase_end(self, ph):
        self.S.emit()
        ph.close()

    def rmsnorm_rstd(self, src, srcbufs, junk, ssq, rstd, n, width=None):
        sc = float(n) ** -0.5
        self.pool(lambda e: e.memset(ssq.t[:, 0:1], 0.0), [], [ssq])
        self.act(lambda e: e.activation(out=junk, in_=src, func=AF.Square, scale=sc, accum_out=ssq.t[:, 0:1]),
                 srcbufs, [ssq])
        self.dve(lambda e: e.tensor_scalar_add(out=rstd.t[:, 0:1], in0=ssq.t[:, 0:1], scalar1=EPS), [ssq], [rstd])
        self.act(lambda e: e.activation(out=rstd.t[:, 0:1], in_=rstd.t[:, 0:1], func=AF.Sqrt), [rstd], [rstd])
        self.dve(lambda e: e.reciprocal(out=rstd.t[:, 0:1], in_=rstd.t[:, 0:1]), [rstd], [rstd])

    def build(self):
        with self.root:
            if self.upto in ("B1only", "B0only", "Eonly"):
                self.catT = self.dram("catT", [1024, S_LEN], BF16)
                if self.upto == "B1only":
                    self.QcT = self.dram("QcT", [768, S_LEN], BF16)
                    self.KcT = self.dram("KcT", [768, S_LEN], BF16)
                    self.Vc = self.dram("Vc", [S_LEN, 512], BF16)
                    self.QdT = self.dram("QdT", [512, S_LEN], BF16)
                    self.KdT = self.dram("KdT", [128, S_LEN], BF16)
                    self.Vd = self.dram("Vd", [S_LEN, 128], BF16)
                    self.layer1_B()
                elif self.upto == "B0only":
                    self.V0 = self.dram("V0", [PAD + S_LEN + PAD, 768], BF16)
                    self.QT0 = self.dram("QT0", [768, S_LEN], BF16)
                    self.KT0 = self.dram("KT0", [768, S_LEN], BF16)
                    self.layer0_B()
                else:
                    self.wg_bf = self.dram("wg_bf", [2, NEXP, D, 512], BF16)
                    self.wu_bf = self.dram("wu_bf", [2, NEXP, D, 512], BF16)
                    self.wd_bf = self.dram("wd_bf", [2, NEXP, 512, D], BF16)
                    self.phase_E(0, last=False)
                return self.finish()
            self.prologue()
            if self.upto == "pro":
                return self.finish()
            self.layer0_A()
            if self.upto == "A0":
                return self.finish()
            self.layer0_B()
            if self.upto == "B0":
                return self.finish()
            self.phase_C(0, self.inp["x"], self.inp["even_w_out"])
            if self.upto == "C0":
                return self.finish()
            self.phase_D()
            if self.upto == "D0":
                return self.finish()
            self.phase_E(0, last=False)
            if self.upto == "E0":
                return self.finish()
            self.layer1_A()
            if self.upto == "A1":
                return self.finish()
            self.layer1_B()
            if self.upto == "B1":
                return self.finish()
            self.phase_C(1, self.xb, self.inp["odd_w_out"])
            if self.upto == "C1":
                return self.finish()
            self.phase_D()
            self.phase_E(1, last=True)
            return self.finish()

    def finish(self):
        ph = self.phase_begin()
        self.phase_end(ph)
        return self.nc

    def prologue(self):
        self.wg_bf = self.dram("wg_bf", [2, NEXP, D, 512], BF16)
        self.wu_bf = self.dram("wu_bf", [2, NEXP, D, 512], BF16)
        self.wd_bf = self.dram("wd_bf", [2, NEXP, 512, D], BF16)

    def prologue_issue(self):
        for l in range(2):
            for e_ in range(NEXP):
                for src, dst in ((self.inp["moe_w_gate"], self.wg_bf), (self.inp["moe_w_up"], self.wu_bf),
                                 (self.inp["moe_w_down"], self.wd_bf)):
                    self.dma("pool", dst.t[l, e_], src.t[l, e_], [], [], dst)
        for dst in (self.wg_bf, self.wu_bf, self.wd_bf):
            self.S.mark_bg(dst.r)

    def layer0_A(self):
        nc = self.nc
        I = self.inp
        self.V0 = self.dram("V0", [PAD + S_LEN + PAD, 768], BF16)
        self.QT0 = self.dram("QT0", [768, S_LEN], BF16)
        self.KT0 = self.dram("KT0", [768, S_LEN], BF16)
        self.catT = self.dram("catT", [1024, S_LEN], BF16)
        ph = self.phase_begin()
        sb = lambda n, s, d: self.sb(ph, n, s, d)
        ps = lambda n, s, d: self.ps(ph, n, s, d)
        ident = sb("ident", [128, 128], BF16)
        win = sb("win", [128, 8, 2816], BF16)
        gmix = sb("gmix", [128, D], F32)
        gmn = sb("gmn", [128, 256], F32)
        wsf = sb("wsf", [128, 4, 128], F32)
        wsb = sb("wsb", [128, 4, 128], BF16)
        wsT = sb("wsT", [128, 4, 128], BF16)
        bsT = sb("bsT", [128, 4], F32)
        zero = sb("zero", [128, 768], BF16)
        xt = [sb(f"xt{i}", [128, D], F32) for i in range(2)]
        junk = sb("junk", [128, D], BF16)
        ssq = [sb(f"ssq{i}", [128, 1], F32) for i in range(2)]
        rstd = [sb(f"rstd{i}", [128, 1], F32) for i in range(2)]
        hn = [sb(f"hn{i}", [128, D], BF16) for i in range(2)]
        hT = [sb(f"hT{i}", [128, 8, 128], BF16) for i in range(2)]
        cs = [sb(f"cs{i}", [128, 16], F32) for i in range(2)]
        qk = [sb(f"qk{i}", [128, 1536], BF16) for i in range(2)]
        rt = [sb(f"rt{i}", [128, 24, 8], F32) for i in range(4)]
        vb = [sb(f"vb{i}", [128, 768], BF16) for i in range(2)]
        zgs = [sb(f"zg{i}", [128, 512], F32) for i in range(2)]
        qkfs = [sb(f"qkf{i}", [128, 1536], F32) for i in range(2)]
        sqv = sb("sqv", [128, 256], F32)
        ss4 = sb("ss4", [128, 4], F32)
        r4 = sb("r4", [128, 4], F32)
        vvn = sb("vvn", [128, 256], F32)
        vvb = sb("vvb", [128, 256], BF16)
        mxb = sb("mxb", [128, 256], F32)
        gout = sb("gout", [128, 256], BF16)
        stq = [sb(f"stq{i}", [128, 6, 512], BF16) for i in range(2)]
        stk = [sb(f"stk{i}", [128, 6, 512], BF16) for i in range(2)]
        stg = [sb(f"stg{i}", [64, 4, 512], BF16) for i in range(2)]
        T0 = ps("T0", [128, 1024], BF16)
        T1 = ps("T1", [128, 1024], BF16)
        pqk = ps("pqk", [128, 1536], F32)
        pvz = ps("pvz", [128, 1536], F32)

        self.dma("sp", ident.t[:], I["c_ident_bf"].t[:, :], [], [ident], ident)
        self.dma("pool", win.t[:], I["even_w_in"].t.rearrange("(c p) n -> p c n", p=128), [], [win], win)
        self.dma("sp", gmix.t[:], I["norm_mix"].t[0:1, :].partition_broadcast(128), [], [gmix], gmix)
        self.dma("sp", gmn.t[:], I["even_gmlp_norm"].t[0:1, :].partition_broadcast(128), [], [gmn], gmn)
        self.dma("sp", wsf.t[:], I["even_w_spatial"].t.rearrange("g p q -> p g q"), [], [wsf], wsf)
        self.dma("sp", bsT.t[:], I["even_b_spatial"].t.rearrange("g p -> p g"), [], [bsT], bsT,
                 allow_slow_non_contiguous=True)
        self.dve(lambda e: e.tensor_copy(out=wsb.t[:], in_=wsf.t[:]), [wsf], [wsb])
        for g in range(4):
            self.pe(lambda e, g=g: e.transpose(out=T0.t[:, g * 128:(g + 1) * 128], in_=wsb.t[:, g, :], identity=ident.t[:]),
                    [wsb, ident], [T0])
        self.act(lambda e: e.copy(out=wsT.t[:].rearrange("p g q -> p (g q)"), in_=T0.t[:, 0:512]), [T0], [wsT])
        self.pool(lambda e: e.memset(zero.t[:], 0.0), [], [zero])
        for side in range(2):
            for k in range(PAD // 128):
                r0 = (0 if side == 0 else PAD + S_LEN) + k * 128
                self.dma("sp", self.V0.t[r0:r0 + 128, :], zero.t[:], [zero], [self.V0], zero)

        import os
        NT0 = int(os.environ.get('NGRP', NT // 4)) * 4

        def stage1(ti):
            grp, tt = divmod(ti, 4)
            sq_, sk_, sg_ = stq[grp % 2], stk[grp % 2], stg[grp % 2]
            b = ti % 2
            x_, ssq_, rstd_, hn_, hT_, cs_, qk_, vb_, qkf, zg = xt[b], ssq[b], rstd[b], hn[b], hT[b], cs[b], qk[b], vb[b], qkfs[b], zgs[b]
            tok = slice(ti * 128, (ti + 1) * 128)
            self.dma("sp", x_.t[:], I["x"].t[tok, :], [I["x"]], [x_], x_)
            self.dma("sp", cs_.t[:], I["c_rope0"].t[tok, :], [], [cs_], cs_)
            self.rmsnorm_rstd(x_.t[:], [x_], junk.t[:], ssq_, rstd_, D)
            self.dve(lambda e, x_=x_, rstd_=rstd_, hn_=hn_: e.scalar_tensor_tensor(
                out=hn_.t[:], in0=x_.t[:], scalar=rstd_.t[:, 0:1], in1=gmix.t[:], op0=ALU.mult, op1=ALU.mult),
                [x_, rstd_, gmix], [hn_])
            for j in range(8):
                self.pe(lambda e, j=j, hn_=hn_: e.transpose(out=T0.t[:, j * 128:(j + 1) * 128],
                                                           in_=hn_.t[:, j * 128:(j + 1) * 128], identity=ident.t[:]),
                        [hn_, ident], [T0])
            self.act(lambda e, hT_=hT_: e.copy(out=hT_.t[:].rearrange("p c n -> p (c n)"), in_=T0.t[:]), [T0], [hT_])
            for bank in range(3):
                for j in range(8):
                    self.pe(lambda e, j=j, bank=bank, hT_=hT_: e.matmul(
                        pqk.t[:, bank * 512:(bank + 1) * 512], lhsT=hT_.t[:, j, :],
                        rhs=win.t[:, j, bank * 512:(bank + 1) * 512], start=(j == 0), stop=(j == 7)),
                        [hT_, win], [pqk])
            for (o0, n_, c0) in ((0, 512, 1536), (512, 256, 2048), (1024, 512, 2304)):
                for j in range(8):
                    self.pe(lambda e, j=j, o0=o0, n_=n_, c0=c0, hT_=hT_: e.matmul(
                        pvz.t[:, o0:o0 + n_], lhsT=hT_.t[:, j, :], rhs=win.t[:, j, c0:c0 + n_],
                        start=(j == 0), stop=(j == 7)), [hT_, win], [pvz])
            self.act(lambda e: e.copy(out=qkf.t[:], in_=pqk.t[:]), [pqk], [qkf])
            self.act(lambda e, vb_=vb_: e.copy(out=vb_.t[:], in_=pvz.t[:, 0:768]), [pvz], [vb_])
            self.dma("sp", self.V0.t[PAD + ti * 128:PAD + (ti + 1) * 128, :], vb_.t[:], [vb_], [self.V0], vb_)
            self.act(lambda e: e.activation(out=zg.t[:], in_=pvz.t[:, 1024:1536], func=AF.Gelu_apprx_tanh), [pvz], [zg])

        def stage2(ti):
            grp, tt = divmod(ti, 4)
            sq_, sk_, sg_ = stq[grp % 2], stk[grp % 2], stg[grp % 2]
            b = ti % 2
            x_, ssq_, rstd_, hn_, hT_, cs_, qk_, vb_, qkf, zg = xt[b], ssq[b], rstd[b], hn[b], hT[b], cs[b], qk[b], vb[b], qkfs[b], zgs[b]
            tok = slice(ti * 128, (ti + 1) * 128)
            self.pool(lambda e, qk_=qk_: e.tensor_copy(out=qk_.t[:], in_=qkf.t[:]), [qkf], [qk_])
            pv = qkf.t[:].rearrange("p (h c) -> p h c", c=64)
            qv = qk_.t[:].rearrange("p (h c) -> p h c", c=64)
            cosb = cs_.t[:, 0:8].unsqueeze(1).to_broadcast([128, 24, 8])
            sinb = cs_.t[:, 8:16].unsqueeze(1).to_broadcast([128, 24, 8])
            x1 = pv[:, :, 0:8]
            x2 = pv[:, :, 8:16]
            self.dve(lambda e, x1=x1, cosb=cosb: e.tensor_tensor(out=rt[0].t[:], in0=x1, in1=cosb, op=ALU.mult), [qkf, cs_], [rt[0]])
            self.dve(lambda e, x2=x2, sinb=sinb: e.tensor_tensor(out=rt[1].t[:], in0=x2, in1=sinb, op=ALU.mult), [qkf, cs_], [rt[1]])
            self.dve(lambda e, x1=x1, sinb=sinb: e.tensor_tensor(out=rt[2].t[:], in0=x1, in1=sinb, op=ALU.mult), [qkf, cs_], [rt[2]])
            self.dve(lambda e, x2=x2, cosb=cosb: e.tensor_tensor(out=rt[3].t[:], in0=x2, in1=cosb, op=ALU.mult), [qkf, cs_], [rt[3]])
            self.pool(lambda e, qv=qv: e.tensor_tensor(out=qv[:, :, 0:8], in0=rt[0].t[:], in1=rt[1].t[:], op=ALU.subtract),
                      [rt[0], rt[1]], [qk_])
            self.pool(lambda e, qv=qv: e.tensor_tensor(out=qv[:, :, 8:16], in0=rt[2].t[:], in1=rt[3].t[:], op=ALU.add),
                      [rt[2], rt[3]], [qk_])
            self.dve(lambda e: e.tensor_tensor(out=sqv.t[:], in0=zg.t[:, 256:512], in1=zg.t[:, 256:512], op=ALU.mult), [zg], [sqv])
            self.dve(lambda e: e.tensor_reduce(out=ss4.t[:], in_=sqv.t[:].rearrange("p (g c) -> p g c", c=64), axis=AX.X, op=ALU.add), [sqv], [ss4])
            self.dve(lambda e: e.tensor_scalar(out=r4.t[:], in0=ss4.t[:], scalar1=1.0 / 64, scalar2=EPS, op0=ALU.mult, op1=ALU.add), [ss4], [r4])
            self.act(lambda e: e.activation(out=r4.t[:], in_=r4.t[:], func=AF.Sqrt), [r4], [r4])
            self.dve(lambda e: e.reciprocal(out=r4.t[:], in_=r4.t[:]), [r4], [r4])
            self.dve(lambda e: e.tensor_tensor(out=vvn.t[:].rearrange("p (g c) -> p g c", c=64),
                                               in0=zg.t[:, 256:512].rearrange("p (g c) -> p g c", c=64),
                                               in1=r4.t[:].unsqueeze(2).to_broadcast([128, 4, 64]), op=ALU.mult), [zg, r4], [vvn])
            self.pool(lambda e: e.tensor_tensor(out=vvb.t[:], in0=vvn.t[:], in1=gmn.t[:], op=ALU.mult), [vvn, gmn], [vvb])
            for g in range(4):
                self.pe(lambda e, g=g: e.matmul(pvz.t[:, 768 + g * 64:768 + (g + 1) * 64], lhsT=wsT.t[:, g, :],
                                                rhs=vvb.t[:, g * 64:(g + 1) * 64], start=True, stop=True), [wsT, vvb], [pvz])
            self.act(lambda e: e.copy(out=sqv.t[:], in_=pvz.t[:, 768:1024]), [pvz], [sqv])
            self.dve(lambda e: e.tensor_tensor(out=mxb.t[:].rearrange("p (g c) -> p g c", c=64),
                                               in0=sqv.t[:].rearrange("p (g c) -> p g c", c=64),
                                               in1=bsT.t[:].unsqueeze(2).to_broadcast([128, 4, 64]), op=ALU.add), [sqv, bsT], [mxb])
            self.pool(lambda e: e.tensor_tensor(out=gout.t[:], in0=mxb.t[:], in1=zg.t[:, 0:256], op=ALU.mult), [mxb, zg], [gout])
            for half, st_ in ((0, sq_), (1, sk_)):
                for j in range(6):
                    c0 = half * 768 + j * 128
                    self.pe(lambda e, j=j, c0=c0, qk_=qk_: e.transpose(out=T1.t[:, j * 128:(j + 1) * 128], in_=qk_.t[:, c0:c0 + 128],
                                                                     identity=ident.t[:]), [qk_, ident], [T1])
                self.act(lambda e, st_=st_, tt=tt: e.copy(out=st_.t[:, :, tt * 128:(tt + 1) * 128],
                                                        in_=T1.t[:, 0:768].rearrange("p (j n) -> p j n", n=128)), [T1], [st_])
            for g in range(4):
                self.pe(lambda e, g=g: e.transpose(out=T1.t[0:64, g * 128:(g + 1) * 128], in_=gout.t[:, g * 64:(g + 1) * 64],
                                                   identity=ident.t[:]), [gout, ident], [T1])
            self.dve(lambda e, sg_=sg_, tt=tt: e.tensor_copy(out=sg_.t[:, :, tt * 128:(tt + 1) * 128],
                                                           in_=T1.t[0:64, 0:512].rearrange("p (j n) -> p j n", n=128)), [T1], [sg_])
            if tt == 3:
                gtok = slice(grp * 512, (grp + 1) * 512)
                self.dma("sp", self.QT0.t.rearrange("(j p) t -> p j t", p=128)[:, :, gtok], sq_.t[:], [sq_], [self.QT0], sq_)
                self.dma("sp", self.KT0.t.rearrange("(j p) t -> p j t", p=128)[:, :, gtok], sk_.t[:], [sk_], [self.KT0], sk_)
                self.dma("sp", self.catT.t[768:1024, :].rearrange("(j p) t -> p j t", p=64)[:, :, gtok], sg_.t[:], [sg_], [self.catT], sg_)

        for s_ in range(NT0 + 1):
            if s_ < NT0:
                stage1(s_)
            if s_ >= 1:
                stage2(s_ - 1)
        self.phase_end(ph)

    def layer0_B(self):
        I = self.inp
        ph = self.phase_begin()
        sb = lambda n, s, d: self.sb(ph, n, s, d)
        ps = lambda n, s, d: self.ps(ph, n, s, d)
        sel = sb("sel", [65, 64], F32)
        mask = sb("mask", [128, 3, 512], BF16)
        qT = [sb(f"qT{i}", [64, S_LEN], BF16) for i in range(2)]
        kT = [sb(f"kT{i}", [64, PAD + S_LEN + PAD], BF16) for i in range(2)]
        vA = [sb(f"vA{i}", [128, 80, 65], BF16) for i in range(2)]
        acc = sb("acc", [65, S_LEN], F32)
        aTs = sb("aTs", [64, S_LEN], BF16)
        rz = [sb(f"rz{i}", [64, 512], F32) for i in range(2)]
        Eb = [sb(f"Eb{i}", [128, 512], BF16) for i in range(3)]
        Pb = [sb(f"Pb{i}", [128, 512], BF16) for i in range(3)]
        Sp = [ps(f"Sp{i}", [128, 512], F32) for i in range(3)]
        Op = [ps(f"Op{i}", [65, 512], F32) for i in range(2)]
        bc = [ps(f"bc{i}", [64, 512], F32) for i in range(2)]
        self.dma("sp", sel.t[:], I["c_sel"].t[:, :], [], [sel], sel)
        self.dma("sp", mask.t[:], I["c_mask"].t[:, :, :], [], [mask], mask)
        for i in range(2):
            self.pool(lambda e, i=i: e.memset(kT[i].t[:, 0:PAD], 0.0), [], [kT[i]])
            self.pool(lambda e, i=i: e.memset(kT[i].t[:, PAD + S_LEN:], 0.0), [], [kT[i]])
            self.pool(lambda e, i=i: e.memset(vA[i].t[:, :, 64:65], 1.0), [], [vA[i]])
        if hasattr(self, "wg_bf") and self.upto != "B0only":
            self.prologue_issue()
        import os
        it = 0
        vi = 0
        og = 0
        for h in range(12):
            q_, k_ = qT[h % 2], kT[h % 2]
            self.dma("sp", q_.t[:], self.QT0.t[h * 64:(h + 1) * 64, :], [self.QT0], [q_], q_)
            self.dma("sp", k_.t[:, PAD:PAD + S_LEN], self.KT0.t[h * 64:(h + 1) * 64, :], [self.KT0], [k_], k_)
            for pi, d in enumerate((1, 4, 16)):
                L = S_LEN // d
                nb = L // 128
                ntile = nb + 1
                v_ = vA[vi % 2]
                vi += 1
                r0 = PAD - 64 * d
                rows = ntile * 128 * d
                src = self.V0.t[r0:r0 + rows, h * 64:(h + 1) * 64].rearrange("(m a i) c -> a i m c", a=128, i=d)
                dstv = v_.t[:, 0:d * ntile, 0:64].rearrange("a (i m) c -> a i m c", i=d)
                for i in range(d):
                    for m0 in range(0, ntile, 16):
                        m1 = min(ntile, m0 + 16)
                        if os.environ.get('NOV') and not (h == 0 and pi == 0):
                            continue
                        self.dma("sp", dstv[:, i, m0:m1, :], src[:, i, m0:m1, :], [self.V0], [v_], v_)
                for i in range(d):
                    for gq in range(nb // 4):
                        n0 = gq * 4
                        O_ = Op[og % 2]
                        og += 1
                        for pr in range(2):
                            S_ = Sp[it % 3]
                            E_ = Eb[it % 3]
                            P_ = Pb[it % 3]
                            it += 1
                            nA = n0 + 2 * pr
                            for (m, qb, nq, c0) in ((nA, nA, 128, 0), (nA + 1, nA, 256, 128), (nA + 2, nA + 1, 128, 384)):
                                k0 = PAD - 64 * d + i + 128 * d * m
                                q0 = 128 * d * qb + i
                                self.pe(lambda e, S_=S_, c0=c0, k0=k0, q0=q0, nq=nq, d=d, k_=k_, q_=q_: e.matmul(
                                    S_.t[:, c0:c0 + nq], lhsT=k_.t[:, k0:k0 + 127 * d + 1:d], rhs=q_.t[:, q0:q0 + (nq - 1) * d + 1:d],
                                    start=True, stop=True), [k_, q_], [S_])
                            self.act(lambda e, S_=S_, E_=E_: e.activation(out=E_.t[:], in_=S_.t[:], func=AF.Exp, scale=0.125), [S_], [E_])
                            first = (n0 + 2 * pr == 0)
                            last = (n0 + 2 * pr + 2 == nb)
                            assert not (first and last)
                            mi = 1 if first else (2 if last else 0)
                            self.dve(lambda e, E_=E_, P_=P_, mi=mi: e.tensor_tensor(out=P_.t[:], in0=E_.t[:], in1=mask.t[:, mi, :], op=ALU.mult),
                                     [E_, mask], [P_])
                            for blk in range(2):
                                n = n0 + 2 * pr + blk
                                oc = (2 * pr + blk) * 128
                                for ab in range(2):
                                    m = n + ab
                                    c0 = (blk * 2 + ab) * 128
                                    self.pe(lambda e, O_=O_, oc=oc, c0=c0, v_=v_, P_=P_, ti=i * ntile + m, ab=ab: e.matmul(
                                        O_.t[:, oc:oc + 128], lhsT=v_.t[:, ti, :], rhs=P_.t[:, c0:c0 + 128],
                                        start=(ab == 0), stop=(ab == 1)), [v_, P_], [O_])
                        a0 = 128 * d * n0 + i
                        av = acc.t[:, a0:a0 + 511 * d + 1:d]
                        if pi == 0:
                            self.dve(lambda e, av=av, O_=O_: e.tensor_copy(out=av, in_=O_.t[:]), [O_], [acc])
                        else:
                            self.dve(lambda e, av=av, O_=O_: e.tensor_tensor(out=av, in0=O_.t[:], in1=av, op=ALU.add), [O_, acc], [acc])
            for c in range(S_LEN // 512):
                cs_ = slice(c * 512, (c + 1) * 512)
                b_ = bc[c % 2]
                r_ = rz[c % 2]
                self.pe(lambda e, b_=b_, cs_=cs_: e.matmul(b_.t[:], lhsT=sel.t[:], rhs=acc.t[:, cs_], start=True, stop=True), [sel, acc], [b_])
                self.dve(lambda e, b_=b_, r_=r_: e.reciprocal(out=r_.t[:], in_=b_.t[:]), [b_], [r_])
                self.dve(lambda e, r_=r_, cs_=cs_: e.tensor_tensor(out=aTs.t[:, cs_], in0=acc.t[0:64, cs_], in1=r_.t[:], op=ALU.mult), [acc, r_], [aTs])
            self.dma("sp", self.catT.t[h * 64:(h + 1) * 64, :], aTs.t[:], [aTs], [self.catT], aTs)
        self.phase_end(ph)


    def phase_C(self, l, xin, wout_in):
        I = self.inp
        ph = self.phase_begin()
        sb = lambda n, s, d: self.sb(ph, n, s, d)
        ps = lambda n, s, d: self.ps(ph, n, s, d)
        ident = sb("ident", [128, 128], BF16)
        identf = sb("identf", [128, 128], F32)
        wout = sb("wout", [64, 16, D], BF16)
        gffn = sb("gffn", [128, D], F32)
        wr32 = sb("wr32", [128, 8, NEXP], F32)
        catg = [sb(f"catg{i}", [64, 16, 512], BF16) for i in range(2)]
        xt = [sb(f"xt{i}", [128, D], F32) for i in range(2)]
        x1t = [sb(f"x1t{i}", [128, D], F32) for i in range(2)]
        junk = sb("junk", [128, D], BF16)
        ssq = [sb(f"ssq{i}", [128, 1], F32) for i in range(2)]
        rstd = [sb(f"rstd{i}", [128, 1], F32) for i in range(2)]
        hnf = [sb(f"hnf{i}", [128, D], F32) for i in range(2)]
        hnb = [sb(f"hnb{i}", [128, D], BF16) for i in range(2)]
        hTs = [sb(f"hTs{i}", [128, 8, 512], BF16) for i in range(2)]
        hT32 = [sb(f"hT32{i}", [128, 8, 128], F32) for i in range(2)]
        lgs = sb("lgs", [128, NEXP], F32)
        ex = sb("ex", [128, NEXP], F32)
        mx = sb("mx", [128, 1], F32)
        se = sb("se", [128, 1], F32)
        T0 = ps("T0", [128, 1024], BF16)
        T32 = ps("T32", [128, 1024], F32)
        pm = [ps(f"pm{i}", [128, 1024], F32) for i in range(2)]
        lg = ps("lg", [128, NEXP], F32)
        self.dma("sp", ident.t[:], I["c_ident_bf"].t[:, :], [], [ident], ident)
        self.dma("sp", identf.t[:], I["c_ident_f"].t[:, :], [], [identf], identf)
        self.dma("pool", wout.t[:], wout_in.t.rearrange("(c p) n -> p c n", p=64), [], [wout], wout)
        self.dma("sp", gffn.t[:], I["norm_ffn"].t[l:l + 1, :].partition_broadcast(128), [], [gffn], gffn)
        self.dma("sp", wr32.t[:], I["moe_w_router"].t[l].rearrange("(c p) e -> p c e", p=128), [], [wr32], wr32)
        def stage1(ti):
            grp, tt = divmod(ti, 4)
            cg = catg[grp % 2]
            b = ti % 2
            x_, pm_ = xt[b], pm[b]
            tok = slice(ti * 128, (ti + 1) * 128)
            if tt == 0:
                gtok = slice(grp * 512, (grp + 1) * 512)
                self.dma("sp", cg.t[:], self.catT.t.rearrange("(c p) t -> p c t", p=64)[:, :, gtok], [], [cg], cg)
            self.dma("sp", x_.t[:], xin.t[tok, :], [], [x_], x_)
            for half in range(2):
                for c in range(16):
                    self.pe(lambda e, half=half, c=c, cg=cg, tt=tt, pm_=pm_: e.matmul(
                        pm_.t[:, half * 512:(half + 1) * 512], lhsT=cg.t[:, c, tt * 128:(tt + 1) * 128],
                        rhs=wout.t[:, c, half * 512:(half + 1) * 512], start=(c == 0), stop=(c == 15)), [cg, wout], [pm_])

        def stage2(ti):
            b = ti % 2
            x_, x1_, ssq_, rstd_, hnf_, hnb_, pm_ = xt[b], x1t[b], ssq[b], rstd[b], hnf[b], hnb[b], pm[b]
            tok = slice(ti * 128, (ti + 1) * 128)
            for half in range(2):
                hs_ = slice(half * 512, (half + 1) * 512)
                self.dve(lambda e, hs_=hs_, x_=x_, x1_=x1_, pm_=pm_: e.tensor_tensor(out=x1_.t[:, hs_], in0=pm_.t[:, hs_], in1=x_.t[:, hs_], op=ALU.add),
                         [pm_, x_], [x1_])
            self.dma("sp", self.xa.t[tok, :], x1_.t[:], [x1_], [], x1_)
            self.rmsnorm_rstd(x1_.t[:], [x1_], junk.t[:], ssq_, rstd_, D)
            self.dve(lambda e, x1_=x1_, rstd_=rstd_, hnf_=hnf_: e.scalar_tensor_tensor(
                out=hnf_.t[:], in0=x1_.t[:], scalar=rstd_.t[:, 0:1], in1=gffn.t[:], op0=ALU.mult, op1=ALU.mult),
                [x1_, rstd_, gffn], [hnf_])
            self.pool(lambda e, hnf_=hnf_, hnb_=hnb_: e.tensor_copy(out=hnb_.t[:], in_=hnf_.t[:]), [hnf_], [hnb_])

        def stage3(ti):
            grp, tt = divmod(ti, 4)
            hs = hTs[grp % 2]
            b = ti % 2
            hnf_, hnb_, h32_ = hnf[b], hnb[b], hT32[b]
            for j in range(8):
                self.pe(lambda e, j=j, hnb_=hnb_: e.transpose(out=T0.t[:, j * 128:(j + 1) * 128], in_=hnb_.t[:, j * 128:(j + 1) * 128],
                                                            identity=ident.t[:]), [hnb_, ident], [T0])
            self.act(lambda e, hs=hs, tt=tt: e.copy(out=hs.t[:, :, tt * 128:(tt + 1) * 128],
                                                  in_=T0.t[:].rearrange("p (c n) -> p c n", n=128)), [T0], [hs])
            for j in range(8):
                self.pe(lambda e, j=j, hnf_=hnf_: e.transpose(out=T32.t[:, j * 128:(j + 1) * 128], in_=hnf_.t[:, j * 128:(j + 1) * 128],
                                                            identity=identf.t[:]), [hnf_, identf], [T32])
            for half in range(2):
                self.act(lambda e, half=half, h32_=h32_: e.copy(out=h32_.t[:, half * 4:(half + 1) * 4, :].rearrange("p c n -> p (c n)"),
                                                               in_=T32.t[:, half * 512:(half + 1) * 512]), [T32], [h32_])
            for j in range(8):
                self.pe(lambda e, j=j, h32_=h32_: e.matmul(lg.t[:, :], lhsT=h32_.t[:, j, :], rhs=wr32.t[:, j, :],
                                                         start=(j == 0), stop=(j == 7)), [h32_, wr32], [lg])
            self.act(lambda e: e.copy(out=lgs.t[:], in_=lg.t[:]), [lg], [lgs])
            self.dve(lambda e: e.reduce_max(out=mx.t[:], in_=lgs.t[:], axis=AX.X), [lgs], [mx])
            self.dve(lambda e: e.tensor_scalar_mul(out=mx.t[:], in0=mx.t[:], scalar1=-1.0), [mx], [mx])
            self.pool(lambda e: e.memset(se.t[:], 0.0), [], [se])
            self.act(lambda e: e.activation(out=ex.t[:], in_=lgs.t[:], func=AF.Exp, bias=mx.t[:, 0:1], scale=1.0, accum_out=se.t[:, 0:1]),
                     [lgs, mx], [ex, se])
            self.dve(lambda e: e.reciprocal(out=se.t[:], in_=se.t[:]), [se], [se])
            self.dve(lambda e, ti=ti: e.tensor_scalar_mul(out=self.affall.t[:, ti, :], in0=ex.t[:], scalar1=se.t[:, 0:1]), [ex, se], [self.affall])
            if tt == 3:
                gtok = slice(grp * 512, (grp + 1) * 512)
                self.dma("sp", self.hTd.t.rearrange("(c p) t -> p c t", p=128)[:, :, gtok], hs.t[:], [hs], [], hs)

        for s_ in range(NT + 2):
            if s_ < NT:
                stage1(s_)
            if 0 <= s_ - 1 < NT:
                stage2(s_ - 1)
            if 0 <= s_ - 2 < NT:
                stage3(s_ - 2)
        self.phase_end(ph)

    def phase_D(self):
        ph = self.phase_begin()
        sb = lambda n, s, d: self.sb(ph, n, s, d)
        ps = lambda n, s, d: self.ps(ph, n, s, d)
        ones = sb("ones", [128, 128], BF16)
        cmp_ = sb("cmp", [128, NT * NEXP], BF16)
        cnts = sb("cnts", [128, NT * NEXP], F32)
        cnt16 = sb("cnt16", [128, NEXP], F32)
        lo = sb("lo", [128, NEXP], F32)
        mid = sb("mid", [128, NEXP], F32)
        ge = sb("ge", [128, NEXP], F32)
        msk = sb("msk", [128, NT, NEXP], F32)
        cntp = ps("cntp", [128, NT * NEXP], F32)
        aff = self.affall
        self.pool(lambda e: e.memset(ones.t[:], 1.0), [], [ones])
        self.pool(lambda e: e.memset(lo.t[:], 0.0), [], [lo])
        for k in range(1, 29):
            c = 2.0 ** -k
            self.dve(lambda e, c=c: e.tensor_scalar_add(out=mid.t[:], in0=lo.t[:], scalar1=c), [lo], [mid])
            self.dve(lambda e: e.tensor_tensor(out=cmp_.t[:].rearrange("p (t x) -> p t x", x=NEXP), in0=aff.t[:],
                                               in1=mid.t[:].unsqueeze(1).to_broadcast([128, NT, NEXP]), op=ALU.is_ge), [aff, mid], [cmp_])
            for half in range(2):
                self.pe(lambda e, half=half: e.matmul(cntp.t[:, half * 512:(half + 1) * 512], lhsT=ones.t[:],
                                                      rhs=cmp_.t[:, half * 512:(half + 1) * 512], start=True, stop=True), [ones, cmp_], [cntp])
            self.act(lambda e: e.copy(out=cnts.t[:], in_=cntp.t[:]), [cntp], [cnts])
            self.dve(lambda e: e.tensor_reduce(out=cnt16.t[:], in_=cnts.t[:].rearrange("p (t x) -> p x t", x=NEXP), axis=AX.X, op=ALU.add),
                     [cnts], [cnt16])
            self.dve(lambda e, c=c: e.tensor_scalar(out=ge.t[:], in0=cnt16.t[:], scalar1=CAP - 0.5, scalar2=c, op0=ALU.is_ge, op1=ALU.mult),
                     [cnt16], [ge])
            self.dve(lambda e: e.tensor_tensor(out=lo.t[:], in0=lo.t[:], in1=ge.t[:], op=ALU.add), [lo, ge], [lo])
        self.dve(lambda e: e.tensor_tensor(out=msk.t[:], in0=aff.t[:], in1=lo.t[:].unsqueeze(1).to_broadcast([128, NT, NEXP]), op=ALU.is_ge),
                 [aff, lo], [msk])
        self.dve(lambda e: e.tensor_tensor(out=self.gw.t[:], in0=msk.t[:], in1=aff.t[:], op=ALU.mult), [msk, aff], [self.gw])
        self.phase_end(ph)

    def phase_E(self, l, last):
        I = self.inp
        self.S.bg_done()
        ph = self.phase_begin()
        sb = lambda n, s, d: self.sb(ph, n, s, d)
        ps = lambda n, s, d: self.ps(ph, n, s, d)
        SG = 1024
        NQ = SG // 512
        accbs = [sb(f"accb{i}", [128, SG // 128, D], F32) for i in range(2)]
        hss = [sb(f"hs{i}", [128, 8, SG], BF16) for i in range(2)]
        wgt = [sb(f"wgt{i}", [128, 8, 512], BF16) for i in range(2)]
        wut = [sb(f"wut{i}", [128, 8, 512], BF16) for i in range(2)]
        wdt = [sb(f"wdt{i}", [128, 4, D], BF16) for i in range(2)]
        sgt = [sb(f"sgt{i}", [128, 512], F32) for i in range(2)]
        actT = [sb(f"actT{i}", [128, 4, 512], BF16) for i in range(2)]
        pg = [ps(f"pg{i}", [128, 512], F32) for i in range(2)]
        pu = [ps(f"pu{i}", [128, 512], F32) for i in range(2)]
        py = [ps(f"py{i}", [128, 512], F32) for i in range(4)]
        if last:
            gfin = sb("gfin", [128, D], F32)
            junk = sb("junk", [128, D], BF16)
            ssq = [sb(f"ssq{i}", [128, 1], F32) for i in range(2)]
            rstd = [sb(f"rstd{i}", [128, 1], F32) for i in range(2)]
            yo = [sb(f"yo{i}", [128, D], F32) for i in range(2)]
            self.dma("sp", gfin.t[:], I["final_norm"].t[0:1, :].partition_broadcast(128), [], [gfin], gfin)
        wi = 0
        kf = 0
        ky = 0
        ka = 0
        import os
        ECUT = int(os.environ.get('ECUT', 9))
        NSG = int(os.environ.get('ESG', S_LEN // SG))

        def sg_loads(sgi):
            t0 = sgi * SG
            a_, h_ = accbs[sgi % 2], hss[sgi % 2]
            for q4 in range(NQ):
                self.dma("pool", a_.t[:, q4 * 4:(q4 + 1) * 4, :],
                         self.xa.t[t0 + q4 * 512:t0 + (q4 + 1) * 512, :].rearrange("(t p) d -> p t d", p=128), [], [a_], a_)
            self.dma("pool", h_.t[:], self.hTd.t.rearrange("(c p) t -> p c t", p=128)[:, :, t0:t0 + SG], [], [h_], h_)

        sg_loads(0)
        for sgi in range(NSG):
            t0 = sgi * SG
            accb, hs = accbs[sgi % 2], hss[sgi % 2]
            if sgi + 1 < NSG:
                sg_loads(sgi + 1)
            for ex_ in range(int(os.environ.get('EEXP', NEXP))):
                wg_, wu_, wd_ = wgt[wi % 2], wut[wi % 2], wdt[wi % 2]
                wi += 1
                self.dma("sp", wg_.t[:], self.wg_bf.t[l, ex_].rearrange("(c p) f -> p c f", p=128), [], [wg_], wg_)
                self.dma("sp", wu_.t[:], self.wu_bf.t[l, ex_].rearrange("(c p) f -> p c f", p=128), [], [wu_], wu_)
                self.dma("sp", wd_.t[:], self.wd_bf.t[l, ex_].rearrange("(c p) n -> p c n", p=128), [], [wd_], wd_)
                for g4 in range(SG // 512):
                    if ECUT < 2: continue
                    tk = slice(g4 * 512, (g4 + 1) * 512)
                    aT_ = actT[ka % 2]
                    ka += 1
                    for fc in range(4):
                        pg_, pu_, sg_ = pg[kf % 2], pu[kf % 2], sgt[kf % 2]
                        kf += 1
                        fs = slice(fc * 128, (fc + 1) * 128)
                        for j in range(8):
                            self.pe(lambda e, j=j, pg_=pg_, wg_=wg_, fs=fs, tk=tk, hs=hs: e.matmul(pg_.t[:], lhsT=wg_.t[:, j, fs], rhs=hs.t[:, j, tk],
                                                                                      start=(j == 0), stop=(j == 7)), [wg_, hs], [pg_])
                        for j in range(8):
                            self.pe(lambda e, j=j, pu_=pu_, wu_=wu_, fs=fs, tk=tk, hs=hs: e.matmul(pu_.t[:], lhsT=wu_.t[:, j, fs], rhs=hs.t[:, j, tk],
                                                                                      start=(j == 0), stop=(j == 7)), [wu_, hs], [pu_])
                        self.act(lambda e, pg_=pg_, sg_=sg_: e.activation(out=sg_.t[:], in_=pg_.t[:], func=AF.Silu), [pg_], [sg_])
                        self.dve(lambda e, pu_=pu_, sg_=sg_, aT_=aT_, fc=fc: e.tensor_tensor(out=aT_.t[:, fc, :], in0=pu_.t[:], in1=sg_.t[:], op=ALU.mult),
                                 [pu_, sg_], [aT_])
                    for tt in range(4):
                        if ECUT < 3: continue
                        tl = g4 * 4 + tt
                        tile = sgi * (SG // 128) + tl
                        for ch in range(2):
                            py_ = py[ky % 4]
                            ky += 1
                            for fc in range(4):
                                self.pe(lambda e, py_=py_, aT_=aT_, fc=fc, tt=tt, wd_=wd_, ch=ch: e.matmul(
                                    py_.t[:], lhsT=aT_.t[:, fc, tt * 128:(tt + 1) * 128], rhs=wd_.t[:, fc, ch * 512:(ch + 1) * 512],
                                    start=(fc == 0), stop=(fc == 3)), [aT_, wd_], [py_])
                            av = accb.t[:, tl, ch * 512:(ch + 1) * 512]
                            self.dve(lambda e, av=av, py_=py_, tile=tile, ex_=ex_: e.scalar_tensor_tensor(
                                out=av, in0=py_.t[:], scalar=self.gw.t[:, tile, ex_:ex_ + 1], in1=av, op0=ALU.mult, op1=ALU.add),
                                [py_, self.gw, accb], [accb])
            if not last:
                for q4 in range(NQ):
                    self.dma("pool", self.xb.t[t0 + q4 * 512:t0 + (q4 + 1) * 512, :].rearrange("(t p) d -> p t d", p=128),
                             accb.t[:, q4 * 4:(q4 + 1) * 4, :], [accb], [], accb)
            else:
                for tl in range(SG // 128):
                    b = tl % 2
                    self.rmsnorm_rstd(accb.t[:, tl, :], [accb], junk.t[:], ssq[b], rstd[b], D)
                    self.dve(lambda e, tl=tl, b=b, accb=accb: e.scalar_tensor_tensor(out=yo[b].t[:], in0=accb.t[:, tl, :], scalar=rstd[b].t[:, 0:1],
                                                                        in1=gfin.t[:], op0=ALU.mult, op1=ALU.mult), [accb, rstd[b], gfin], [yo[b]])
                    r0 = t0 + tl * 128
                    self.dma("sp", self.out.t[r0:r0 + 128, :], yo[b].t[:], [yo[b]], [], yo[b])
        self.phase_end(ph)


    def rope_apply(self, x1, x2, cosb, sinb, o1, o2, rt, shape, srcbufs, tabbufs, outbuf):
        v = [r.t[:, 0:shape[1], 0:shape[2]] for r in rt]
        self.dve(lambda e: e.tensor_tensor(out=v[0], in0=x1, in1=cosb, op=ALU.mult), srcbufs + tabbufs, [rt[0]])
        self.dve(lambda e: e.tensor_tensor(out=v[1], in0=x2, in1=sinb, op=ALU.mult), srcbufs + tabbufs, [rt[1]])
        self.dve(lambda e: e.tensor_tensor(out=v[2], in0=x1, in1=sinb, op=ALU.mult), srcbufs + tabbufs, [rt[2]])
        self.dve(lambda e: e.tensor_tensor(out=v[3], in0=x2, in1=cosb, op=ALU.mult), srcbufs + tabbufs, [rt[3]])
        self.pool(lambda e: e.tensor_tensor(out=o1, in0=v[0], in1=v[1], op=ALU.subtract), [rt[0], rt[1]], [outbuf])
        self.pool(lambda e: e.tensor_tensor(out=o2, in0=v[2], in1=v[3], op=ALU.add), [rt[2], rt[3]], [outbuf])

    def layer1_A(self):
        I = self.inp
        self.QcT = self.dram("QcT", [768, S_LEN], BF16)
        self.KcT = self.dram("KcT", [768, S_LEN], BF16)
        self.Vc = self.dram("Vc", [S_LEN, 512], BF16)
        self.QdT = self.dram("QdT", [512, S_LEN], BF16)
        self.KdT = self.dram("KdT", [128, S_LEN], BF16)
        self.Vd = self.dram("Vd", [S_LEN, 128], BF16)
        ph = self.phase_begin()
        sb = lambda n, s, d: self.sb(ph, n, s, d)
        ps = lambda n, s, d: self.ps(ph, n, s, d)
        ident = sb("ident", [128, 128], BF16)
        win = sb("win", [128, 8, 1184], BF16)
        wcq = sb("wcq", [128, 2, 768], BF16)
        wckv = sb("wckv", [128, 1024], BF16)
        gmix = sb("gmix", [128, D], F32)
        gcq = sb("gcq", [128, 256], F32)
        gckv = sb("gckv", [128, 128], F32)
        gdq = sb("gdq", [128, 64], F32)
        gdk = sb("gdk", [128, 64], F32)
        xt = [sb(f"xt{i}", [128, D], F32) for i in range(2)]
        junk = sb("junk", [128, D], BF16)
        ssq = [sb(f"ssq{i}", [128, 1], F32) for i in range(2)]
        rstd = [sb(f"rstd{i}", [128, 1], F32) for i in range(2)]
        ssq2 = sb("ssq2", [128, 1], F32)
        rstd2 = sb("rstd2", [128, 1], F32)
        ssq3 = sb("ssq3", [128, 1], F32)
        rstd3 = sb("rstd3", [128, 1], F32)
        hn = [sb(f"hn{i}", [128, D], BF16) for i in range(2)]
        hT = [sb(f"hT{i}", [128, 8, 128], BF16) for i in range(2)]
        tabs = [sb(f"tabs{i}", [128, 96], F32) for i in range(2)]
        pfs = [sb(f"pf{i}", [128, 1184], F32) for i in range(2)]
        cqn = sb("cqn", [128, 256], BF16)
        cqT = sb("cqT", [128, 2, 128], BF16)
        kvn = sb("kvn", [128, 128], BF16)
        kvT = sb("kvT", [128, 128], BF16)
        qcf = sb("qcf", [128, 768], F32)
        qcb = sb("qcb", [128, 768], BF16)
        kvf = sb("kvf", [128, 1024], F32)
        kcb = sb("kcb", [128, 768], BF16)
        vcb = [sb(f"vcb{i}", [128, 512], BF16) for i in range(2)]
        krf = sb("krf", [128, 1, 32], F32)
        rt = [sb(f"rt{i}", [128, 8, 16], F32) for i in range(4)]
        sq8 = sb("sq8", [128, 640], F32)
        ss10 = sb("ss10", [128, 10], F32)
        r10 = sb("r10", [128, 10], F32)
        dn = sb("dn", [128, 640], F32)
        dqb = sb("dqb", [128, 640], BF16)
        dvb = [sb(f"dvb{i}", [128, 128], BF16) for i in range(2)]
        stqc = [sb(f"stqc{i}", [96, 8, 512], BF16) for i in range(2)]
        stkc = [sb(f"stkc{i}", [96, 8, 512], BF16) for i in range(2)]
        stqd = [sb(f"stqd{i}", [128, 4, 512], BF16) for i in range(2)]
        stkd = [sb(f"stkd{i}", [128, 512], BF16) for i in range(2)]
        T0 = ps("T0", [128, 1024], BF16)
        T1 = ps("T1", [128, 1024], BF16)
        pq = ps("pq", [128, 1536], F32)
        pc = ps("pc", [128, 1024], F32)
        self.dma("sp", ident.t[:], I["c_ident_bf"].t[:, :], [], [ident], ident)
        self.dma("pool", win.t[:], I["odd_w_in"].t.rearrange("(c p) n -> p c n", p=128), [], [win], win)
        self.dma("pool", wcq.t[:], I["odd_w_cq_up"].t.rearrange("(c p) n -> p c n", p=128), [], [wcq], wcq)
        self.dma("pool", wckv.t[:], I["odd_w_ckv_up"].t[:, :], [], [wckv], wckv)
        self.dma("sp", gmix.t[:], I["norm_mix"].t[1:2, :].partition_broadcast(128), [], [gmix], gmix)
        self.dma("sp", gcq.t[:], I["odd_cq_norm"].t[0:1, :].partition_broadcast(128), [], [gcq], gcq)
        self.dma("sp", gckv.t[:], I["odd_ckv_norm"].t[0:1, :].partition_broadcast(128), [], [gckv], gckv)
        self.dma("sp", gdq.t[:], I["odd_dq_norm"].t[0:1, :].partition_broadcast(128), [], [gdq], gdq)
        self.dma("sp", gdk.t[:], I["odd_dk_norm"].t[0:1, :].partition_broadcast(128), [], [gdk], gdk)
        import os
        NT1 = int(os.environ.get('NGRP1', NT // 4)) * 4

        def stage1(ti):
            grp, tt = divmod(ti, 4)
            sqc, skc, sqd, skd = stqc[grp % 2], stkc[grp % 2], stqd[grp % 2], stkd[grp % 2]
            b = ti % 2
            x_, ssq_, rstd_, hn_, hT_, tb_, vcb_, dvb_, pf = xt[b], ssq[b], rstd[b], hn[b], hT[b], tabs[b], vcb[b], dvb[b], pfs[b]
            tok = slice(ti * 128, (ti + 1) * 128)
            tsl = slice(tt * 128, (tt + 1) * 128)
            self.dma("sp", x_.t[:], self.xb.t[tok, :], [], [x_], x_)
            self.dma("sp", tb_.t[:, 0:32], I["c_ropec"].t[tok, :], [], [tb_], tb_)
            self.dma("sp", tb_.t[:, 32:64], I["c_roperow"].t[tok, :], [], [tb_], tb_)
            self.dma("sp", tb_.t[:, 64:96], I["c_ropecol"].t[tok, :], [], [tb_], tb_)
            self.rmsnorm_rstd(x_.t[:], [x_], junk.t[:], ssq_, rstd_, D)
            self.dve(lambda e, x_=x_, rstd_=rstd_, hn_=hn_: e.scalar_tensor_tensor(
                out=hn_.t[:], in0=x_.t[:], scalar=rstd_.t[:, 0:1], in1=gmix.t[:], op0=ALU.mult, op1=ALU.mult),
                [x_, rstd_, gmix], [hn_])
            for j in range(8):
                self.pe(lambda e, j=j, hn_=hn_: e.transpose(out=T0.t[:, j * 128:(j + 1) * 128], in_=hn_.t[:, j * 128:(j + 1) * 128],
                                                           identity=ident.t[:]), [hn_, ident], [T0])
            self.act(lambda e, hT_=hT_: e.copy(out=hT_.t[:].rearrange("p c n -> p (c n)"), in_=T0.t[:]), [T0], [hT_])
            for (c0, n_) in ((0, 512), (512, 512), (1024, 160)):
                for j in range(8):
                    self.pe(lambda e, j=j, c0=c0, n_=n_, hT_=hT_: e.matmul(pq.t[:, c0:c0 + n_], lhsT=hT_.t[:, j, :], rhs=win.t[:, j, c0:c0 + n_],
                                                                        start=(j == 0), stop=(j == 7)), [hT_, win], [pq])
            self.act(lambda e: e.copy(out=pf.t[:], in_=pq.t[:, 0:1184]), [pq], [pf])

        def stage2(ti):
            grp, tt = divmod(ti, 4)
            sqc, skc, sqd, skd = stqc[grp % 2], stkc[grp % 2], stqd[grp % 2], stkd[grp % 2]
            b = ti % 2
            x_, ssq_, rstd_, hn_, hT_, tb_, vcb_, dvb_, pf = xt[b], ssq[b], rstd[b], hn[b], hT[b], tabs[b], vcb[b], dvb[b], pfs[b]
            tok = slice(ti * 128, (ti + 1) * 128)
            tsl = slice(tt * 128, (tt + 1) * 128)
            self.rmsnorm_rstd(pf.t[:, 0:256], [pf], junk.t[:, 0:256], ssq2, rstd2, 256)
            self.dve(lambda e: e.scalar_tensor_tensor(out=cqn.t[:], in0=pf.t[:, 0:256], scalar=rstd2.t[:, 0:1], in1=gcq.t[:],
                                                      op0=ALU.mult, op1=ALU.mult), [pf, rstd2, gcq], [cqn])
            for j in range(2):
                self.pe(lambda e, j=j: e.transpose(out=T1.t[:, j * 128:(j + 1) * 128], in_=cqn.t[:, j * 128:(j + 1) * 128], identity=ident.t[:]),
                        [cqn, ident], [T1])
            self.act(lambda e: e.copy(out=cqT.t[:].rearrange("p c n -> p (c n)"), in_=T1.t[:, 0:256]), [T1], [cqT])
            for (c0, n_) in ((0, 512), (512, 256)):
                for j in range(2):
                    self.pe(lambda e, j=j, c0=c0, n_=n_: e.matmul(pc.t[:, c0:c0 + n_], lhsT=cqT.t[:, j, :], rhs=wcq.t[:, j, c0:c0 + n_],
                                                                 start=(j == 0), stop=(j == 1)), [cqT, wcq], [pc])
            self.act(lambda e: e.copy(out=qcf.t[:], in_=pc.t[:, 0:768]), [pc], [qcf])
            self.pool(lambda e: e.tensor_copy(out=qcb.t[:], in_=qcf.t[:]), [qcf], [qcb])
            qv = qcf.t[:].rearrange("p (h c) -> p h c", c=96)
            qo = qcb.t[:].rearrange("p (h c) -> p h c", c=96)
            cb = tb_.t[:, 0:16].unsqueeze(1).to_broadcast([128, 8, 16])
            sn = tb_.t[:, 16:32].unsqueeze(1).to_broadcast([128, 8, 16])
            self.rope_apply(qv[:, :, 64:80], qv[:, :, 80:96], cb, sn, qo[:, :, 64:80], qo[:, :, 80:96], rt, [128, 8, 16], [qcf], [tb_], qcb)
            self.rmsnorm_rstd(pf.t[:, 256:384], [pf], junk.t[:, 0:128], ssq3, rstd3, 128)
            self.dve(lambda e: e.scalar_tensor_tensor(out=kvn.t[:], in0=pf.t[:, 256:384], scalar=rstd3.t[:, 0:1], in1=gckv.t[:],
                                                      op0=ALU.mult, op1=ALU.mult), [pf, rstd3, gckv], [kvn])
            self.pe(lambda e: e.transpose(out=T1.t[:, 0:128], in_=kvn.t[:], identity=ident.t[:]), [kvn, ident], [T1])
            self.act(lambda e: e.copy(out=kvT.t[:], in_=T1.t[:, 0:128]), [T1], [kvT])
            for c0 in (0, 512):
                self.pe(lambda e, c0=c0: e.matmul(pc.t[:, c0:c0 + 512], lhsT=kvT.t[:], rhs=wckv.t[:, c0:c0 + 512], start=True, stop=True),
                        [kvT, wckv], [pc])
            self.act(lambda e: e.copy(out=kvf.t[:], in_=pc.t[:]), [pc], [kvf])
            kv3 = kvf.t[:].rearrange("p (h c) -> p h c", c=128)
            ko = kcb.t[:].rearrange("p (h c) -> p h c", c=96)
            self.pool(lambda e: e.tensor_copy(out=ko[:, :, 0:64], in_=kv3[:, :, 0:64]), [kvf], [kcb])
            self.pool(lambda e, vcb_=vcb_: e.tensor_copy(out=vcb_.t[:].rearrange("p (h c) -> p h c", c=64), in_=kv3[:, :, 64:128]), [kvf], [vcb_])
            self.dma("sp", self.Vc.t[tok, :], vcb_.t[:], [vcb_], [], vcb_)
            kr = pf.t[:, 384:416].rearrange("p (o c) -> p o c", o=1)
            cb1 = tb_.t[:, 0:16].unsqueeze(1)
            sn1 = tb_.t[:, 16:32].unsqueeze(1)
            self.rope_apply(kr[:, :, 0:16], kr[:, :, 16:32], cb1, sn1, krf.t[:, :, 0:16], krf.t[:, :, 16:32], rt, [128, 1, 16], [pf], [tb_], krf)
            self.pool(lambda e: e.tensor_copy(out=ko[:, :, 64:96], in_=krf.t[:].to_broadcast([128, 8, 32])), [krf], [kcb])
            self.dve(lambda e: e.tensor_tensor(out=sq8.t[:], in0=pf.t[:, 416:1056], in1=pf.t[:, 416:1056], op=ALU.mult), [pf], [sq8])
            self.dve(lambda e: e.tensor_reduce(out=ss10.t[:], in_=sq8.t[:].rearrange("p (g c) -> p g c", c=64), axis=AX.X, op=ALU.add), [sq8], [ss10])
            self.dve(lambda e: e.tensor_scalar(out=r10.t[:], in0=ss10.t[:], scalar1=1.0 / 64, scalar2=EPS, op0=ALU.mult, op1=ALU.add), [ss10], [r10])
            self.act(lambda e: e.activation(out=r10.t[:], in_=r10.t[:], func=AF.Sqrt), [r10], [r10])
            self.dve(lambda e: e.reciprocal(out=r10.t[:], in_=r10.t[:]), [r10], [r10])
            self.dve(lambda e: e.tensor_tensor(out=dn.t[:].rearrange("p (g c) -> p g c", c=64),
                                               in0=pf.t[:, 416:1056].rearrange("p (g c) -> p g c", c=64),
                                               in1=r10.t[:].unsqueeze(2).to_broadcast([128, 10, 64]), op=ALU.mult), [pf, r10], [dn])
            self.pool(lambda e: e.tensor_tensor(out=dn.t[:, 0:512].rearrange("p (g c) -> p g c", c=64),
                                                in0=dn.t[:, 0:512].rearrange("p (g c) -> p g c", c=64),
                                                in1=gdq.t[:].unsqueeze(1).to_broadcast([128, 8, 64]), op=ALU.mult), [dn, gdq], [dn])
            self.pool(lambda e: e.tensor_tensor(out=dn.t[:, 512:640].rearrange("p (g c) -> p g c", c=64),
                                                in0=dn.t[:, 512:640].rearrange("p (g c) -> p g c", c=64),
                                                in1=gdk.t[:].unsqueeze(1).to_broadcast([128, 2, 64]), op=ALU.mult), [dn, gdk], [dn])
            for (g0, g1) in ((0, 8), (8, 10)):
                ng = g1 - g0
                dv_ = dn.t[:, g0 * 64:g1 * 64].rearrange("p (g c) -> p g c", c=64)
                do_ = dqb.t[:, g0 * 64:g1 * 64].rearrange("p (g c) -> p g c", c=64)
                for (tb0, d0) in ((32, 0), (64, 32)):
                    cbx = tb_.t[:, tb0:tb0 + 16].unsqueeze(1).to_broadcast([128, ng, 16])
                    snx = tb_.t[:, tb0 + 16:tb0 + 32].unsqueeze(1).to_broadcast([128, ng, 16])
                    self.rope_apply(dv_[:, :, d0:d0 + 16], dv_[:, :, d0 + 16:d0 + 32], cbx, snx,
                                    do_[:, :, d0:d0 + 16], do_[:, :, d0 + 16:d0 + 32], rt, [128, ng, 16], [dn], [tb_], dqb)
            self.pool(lambda e, dvb_=dvb_: e.tensor_copy(out=dvb_.t[:], in_=pf.t[:, 1056:1184]), [pf], [dvb_])
            self.dma("sp", self.Vd.t[tok, :], dvb_.t[:], [dvb_], [], dvb_)
            for h in range(8):
                self.pe(lambda e, h=h: e.transpose(out=T1.t[0:96, h * 128:(h + 1) * 128], in_=qcb.t[:, h * 96:(h + 1) * 96], identity=ident.t[:]),
                        [qcb, ident], [T1])
            self.act(lambda e, sqc=sqc, tsl=tsl: e.copy(out=sqc.t[:, :, tsl], in_=T1.t[0:96, :].rearrange("p (h n) -> p h n", n=128)), [T1], [sqc])
            for h in range(8):
                self.pe(lambda e, h=h: e.transpose(out=T0.t[0:96, h * 128:(h + 1) * 128], in_=kcb.t[:, h * 96:(h + 1) * 96], identity=ident.t[:]),
                        [kcb, ident], [T0])
            self.act(lambda e, skc=skc, tsl=tsl: e.copy(out=skc.t[:, :, tsl], in_=T0.t[0:96, :].rearrange("p (h n) -> p h n", n=128)), [T0], [skc])
            for j in range(5):
                self.pe(lambda e, j=j: e.transpose(out=T1.t[:, j * 128:(j + 1) * 128], in_=dqb.t[:, j * 128:(j + 1) * 128], identity=ident.t[:]),
                        [dqb, ident], [T1])
            self.act(lambda e, sqd=sqd, tsl=tsl: e.copy(out=sqd.t[:, :, tsl], in_=T1.t[:, 0:512].rearrange("p (h n) -> p h n", n=128)), [T1], [sqd])
            self.act(lambda e, skd=skd, tsl=tsl: e.copy(out=skd.t[:, tsl], in_=T1.t[:, 512:640]), [T1], [skd])
            if tt == 3:
                gtok = slice(grp * 512, (grp + 1) * 512)
                self.dma("sp", self.QcT.t.rearrange("(h p) t -> p h t", p=96)[:, :, gtok], sqc.t[:], [sqc], [], sqc)
                self.dma("sp", self.KcT.t.rearrange("(h p) t -> p h t", p=96)[:, :, gtok], skc.t[:], [skc], [], skc)
                self.dma("sp", self.QdT.t.rearrange("(j p) t -> p j t", p=128)[:, :, gtok], sqd.t[:], [sqd], [], sqd)
                self.dma("sp", self.KdT.t[:, gtok], skd.t[:], [skd], [], skd)

        for s_ in range(NT1 + 1):
            if s_ < NT1:
                stage1(s_)
            if s_ >= 1:
                stage2(s_ - 1)
        self.phase_end(ph)

    def layer1_B(self):
        I = self.inp
        ph = self.phase_begin()
        sb = lambda n, s, d: self.sb(ph, n, s, d)
        ps = lambda n, s, d: self.ps(ph, n, s, d)
        sel = sb("sel", [65, 64], F32)
        qT = [sb(f"qT{i}", [96, S_LEN], BF16) for i in range(2)]
        kT = [sb(f"kT{i}", [96, S_LEN], BF16) for i in range(2)]
        vA = [sb(f"vA{i}", [128, NT, 65], BF16) for i in range(2)]
        oTs = [sb(f"oTs{i}", [64, S_LEN], BF16) for i in range(2)]
        osb = [sb(f"osb{i}", [65, 512], F32) for i in range(2)]
        rz = [sb(f"rz{i}", [64, 512], F32) for i in range(2)]
        Eb = [sb(f"Eb{i}", [128, 512], BF16) for i in range(6)]
        Sp = [ps(f"Sp{i}", [128, 512], F32) for i in range(5)]
        Op = [ps(f"Op{i}", [65, 512], F32) for i in range(2)]
        bc = [ps(f"bc{i}", [64, 512], F32) for i in range(1)]
        self.dma("sp", sel.t[:], I["c_sel"].t[:, :], [], [sel], sel)
        for i in range(2):
            self.pool(lambda e, i=i: e.memset(vA[i].t[:, :, 64:65], 1.0), [], [vA[i]])
        import os
        NH = int(os.environ.get('NH1', 16))
        NQC = int(os.environ.get('NQC1', S_LEN // 512))
        LA = 3
        kvi = -1
        prev_kv = None
        gi = 0
        oc_i = 0
        pending = []
        for h in range(NH):
            if h < 8:
                dk, scale = 96, 96.0 ** -0.5
                qsrc = self.QcT.t[h * 96:(h + 1) * 96, :]
                ksrc = self.KcT.t[h * 96:(h + 1) * 96, :]
                vsrc = self.Vc.t[:, h * 64:(h + 1) * 64]
                kvkey = ("c", h)
            else:
                hd = h - 8
                kvh = hd // 4
                dk, scale = 64, 0.125
                qsrc = self.QdT.t[hd * 64:(hd + 1) * 64, :]
                ksrc = self.KdT.t[kvh * 64:(kvh + 1) * 64, :]
                vsrc = self.Vd.t[:, kvh * 64:(kvh + 1) * 64]
                kvkey = ("d", kvh)
            q_ = qT[h % 2]
            self.dma("sp", q_.t[0:dk, :], qsrc, [], [q_], q_)
            if kvkey != prev_kv:
                kvi += 1
                prev_kv = kvkey
                k_, v_ = kT[kvi % 2], vA[kvi % 2]
                self.dma("sp", k_.t[0:dk, :], ksrc, [], [k_], k_)
                vv = vsrc.rearrange("(m a) c -> a m c", a=128)
                for m0 in range(0, NT, 16):
                    self.dma("pool", v_.t[:, m0:m0 + 16, 0:64], vv[:, m0:m0 + 16, :], [], [v_], v_)
            o_ = oTs[h % 2]
            iters = [(qc, m) for qc in range(NQC) for m in range(NT)]
            n_it = len(iters)
            sbuf_of = {}

            def emit_qk(idx):
                nonlocal gi
                qc, m = iters[idx]
                S_ = Sp[gi % 5]
                E_ = Eb[gi % 6]
                gi += 1
                sbuf_of[idx] = (S_, E_)
                self.pe(lambda e, S_=S_, m=m, qc=qc, k_=k_, q_=q_, dk=dk: e.matmul(
                    S_.t[:], lhsT=k_.t[0:dk, m * 128:(m + 1) * 128], rhs=q_.t[0:dk, qc * 512:(qc + 1) * 512], start=True, stop=True),
                    [k_, q_], [S_])
                self.act(lambda e, S_=S_, E_=E_, scale=scale: e.activation(out=E_.t[:], in_=S_.t[:], func=AF.Exp, scale=scale), [S_], [E_])

            def flush_pending(force=False):
                keep = []
                for item in pending:
                    item[0] -= 1
                    if item[0] <= 0 or force:
                        item[1]()
                    else:
                        keep.append(item)
                pending[:] = keep

            for idx in range(min(LA, n_it)):
                emit_qk(idx)
            for idx in range(n_it):
                if idx + LA < n_it:
                    emit_qk(idx + LA)
                qc, m = iters[idx]
                if m == 0:
                    O_ = Op[oc_i % 2]
                    ob_, rz_, bc_ = osb[oc_i % 2], rz[oc_i % 2], bc[0]
                    oc_i += 1
                S_, E_ = sbuf_of.pop(idx)
                self.pe(lambda e, O_=O_, v_=v_, m=m, E_=E_: e.matmul(O_.t[:], lhsT=v_.t[:, m, :], rhs=E_.t[:], start=(m == 0), stop=(m == NT - 1)),
                        [v_, E_], [O_])
                flush_pending()
                if m == NT - 1:
                    self.dve(lambda e, O_=O_, ob_=ob_: e.tensor_copy(out=ob_.t[:], in_=O_.t[:]), [O_], [ob_])

                    def norm(ob_=ob_, rz_=rz_, bc_=bc_, qc=qc, o_=o_):
                        self.pe(lambda e: e.matmul(bc_.t[:], lhsT=sel.t[:], rhs=ob_.t[:], start=True, stop=True), [sel, ob_], [bc_])
                        self.dve(lambda e: e.reciprocal(out=rz_.t[:], in_=bc_.t[:]), [bc_], [rz_])
                        self.dve(lambda e: e.tensor_tensor(out=o_.t[:, qc * 512:(qc + 1) * 512], in0=ob_.t[0:64, :], in1=rz_.t[:], op=ALU.mult),
                                 [ob_, rz_], [o_])
                    pending.append([4, norm])
            flush_pending(force=True)
            self.dma("sp", self.catT.t[h * 64:(h + 1) * 64, :], o_.t[:], [o_], [], o_)
        self.phase_end(ph)


def prep_inputs(inputs, b):
    f = lambda a: np.ascontiguousarray(np.asarray(a, dtype=np.float32))
    m = {
        "x": f(inputs["x"][b]), "norm_mix": f(inputs["norm_mix"]), "norm_ffn": f(inputs["norm_ffn"]),
        "even_w_in": f(inputs["even_w_in"][0]), "even_gmlp_norm": f(inputs["even_gmlp_norm"][0]).reshape(1, 256),
        "even_w_spatial": f(inputs["even_w_spatial"][0]), "even_b_spatial": f(inputs["even_b_spatial"][0]),
        "even_w_out": f(inputs["even_w_out"][0]), "odd_w_in": f(inputs["odd_w_in"][0]),
        "odd_cq_norm": f(inputs["odd_cq_norm"]), "odd_w_cq_up": f(inputs["odd_w_cq_up"][0]),
        "odd_ckv_norm": f(inputs["odd_ckv_norm"]), "odd_w_ckv_up": f(inputs["odd_w_ckv_up"][0]),
        "odd_dq_norm": f(inputs["odd_dq_norm"]), "odd_dk_norm": f(inputs["odd_dk_norm"]),
        "odd_w_out": f(inputs["odd_w_out"][0]), "moe_w_router": f(inputs["moe_w_router"]),
        "moe_w_gate": f(inputs["moe_w_gate"]), "moe_w_up": f(inputs["moe_w_up"]), "moe_w_down": f(inputs["moe_w_down"]),
        "final_norm": f(inputs["final_norm"]).reshape(1, D),
    }
    return m


def kernel(**inputs):
    nb = inputs["x"].shape[0]
    nc = Builder().build()
    consts = make_consts()
    in_maps = []
    for b in range(nb):
        m = prep_inputs(inputs, b)
        m.update(consts)
        in_maps.append(m)
    res = run_bass_kernel_spmd(nc, in_maps, core_ids=list(range(nb)))
    return np.stack([np.asarray(r["y"]) for r in res.results], axis=0).astype(np.float32)
```

```python
import numpy as np
import ml_dtypes
import concourse.bass as bass
import concourse.mybir as mybir
from concourse.bass_utils import run_bass_kernel_spmd
from contextlib import ExitStack

F32 = mybir.dt.float32
BF16 = mybir.dt.bfloat16
ALU = mybir.AluOpType
AF = mybir.ActivationFunctionType
AX = mybir.AxisListType

S_LEN = 8192
D = 1024
PAD = 1024
NT = S_LEN // 128
CH = 16000
EPS = 1e-6
NEXP = 16
CAP = 2 * S_LEN // NEXP
FUSE_WAIT = True


class Res:
    __slots__ = ("name", "writers", "readers", "stream")

    def __init__(self, name):
        self.name = name
        self.writers = {}
        self.readers = {}
        self.stream = None


class Stream:
    __slots__ = ("sem", "count", "key", "kind")

    def __init__(self, sem, key):
        self.sem = sem
        self.count = 0
        self.key = key


class Buf:
    __slots__ = ("t", "r", "track")

    def __init__(self, t, name, track=True):
        self.t = t
        self.r = Res(name)
        self.track = track


class Sched:
    ENG = ("pe", "act", "dve", "pool", "sp")
    HANDLE = {"pe": "tensor", "act": "scalar", "dve": "vector", "pool": "gpsimd", "sp": "sync"}

    def __init__(self, nc, stack):
        self.nc = nc
        self.stack = stack
        self.prog = {e: [] for e in self.ENG}
        self.cnt = {e: 0 for e in self.ENG}
        self.seen = {e: {} for e in self.ENG}
        self.esems = {e: [] for e in self.ENG}
        self.streams = []
        self.free_sems = {"sw": [], "hw": []}
        self.live = []
        self.bg = set()
        self.nsem = 0
        self.total = 0

    def _new_sem(self, name):
        self.nsem += 1
        return self.stack.enter_context(self.nc.semaphore(name))

    def _esem(self, e, chunk):
        lst = self.esems[e]
        while len(lst) <= chunk:
            lst.append(self._new_sem(f"s_{e}_{len(lst)}"))
        return lst[chunk]

    def stream_of(self, res, q="sp"):
        kind = "sw" if q == "pool" else "hw"
        if res.stream is None:
            pool_ = self.free_sems[kind]
            if pool_:
                pool_.sort(key=lambda x: x[1])
                sem, base = pool_.pop(0)
            else:
                sem, base = self._new_sem(f"d{len(self.streams)}"), 0
            res.stream = Stream(sem, ("d", len(self.streams)))
            res.stream.count = base
            res.stream.kind = kind
            self.streams.append(res.stream)
            self.live.append(res)
        assert res.stream.kind == kind, f"stream of {res.name} used from both DGE kinds"
        return res.stream

    def mark_bg(self, res):
        self.bg.add(res.stream.key)

    def bg_done(self):
        self.bg = set()

    def release_streams(self):
        keep = []
        for res in self.live:
            st = res.stream
            if st.key in self.bg:
                keep.append(res)
                continue
            self.free_sems[st.kind].append((st.sem, st.count))
            st.count = 0
            res.stream = None
        self.live = keep

    def _wait(self, e, key, val, payload):
        seen = self.seen[e]
        if seen.get(key, 0) >= val:
            return
        seen[key] = val
        self.prog[e].append(("wait", payload))

    def _wait_tok(self, e, key, val):
        if key[0] == "e":
            eng = key[1]
            if eng == e and e in ("pe", "sp"):
                return
            chunk, v = (val - 1) // CH, (val - 1) % CH + 1
            self._wait(e, key, val, (self._esem(eng, chunk), v))
        else:
            st = self.streams[key[1]]
            self._wait(e, key, val, (st.sem, val))

    def op(self, e, fn, reads=(), writes=(), stream=None):
        for r in reads:
            for k, v in r.writers.items():
                self._wait_tok(e, k, v)
        for w in writes:
            for k, v in w.writers.items():
                self._wait_tok(e, k, v)
            for k, v in w.readers.items():
                self._wait_tok(e, k, v)
        if stream is not None:
            st = self.stream_of(stream, e)
            st.count += 16
            key, val = st.key, st.count
            self.prog[e].append(("dma", fn, st.sem))
        else:
            self.cnt[e] += 1
            n = self.cnt[e]
            key, val = ("e", e), n
            self.prog[e].append(("ins", fn, self._esem(e, (n - 1) // CH)))
        for r in reads:
            r.readers[key] = val
        for w in writes:
            w.writers = {key: val}
            w.readers = {}

    def barrier(self, engines=None):
        for e in (engines or self.ENG):
            for st in self.streams:
                if st.count and st.key not in self.bg:
                    self._wait(e, st.key, st.count, (st.sem, st.count))
            for eng in self.ENG:
                if eng != e and self.cnt[eng]:
                    self._wait_tok(e, ("e", eng), self.cnt[eng])

    def emit(self):
        nc = self.nc
        import os
        if os.environ.get('DUMP'):
            for e in self.ENG:
                print('ENGINE', e)
                for it in self.prog[e][:int(os.environ['DUMP'])]:
                    if it[0] == 'wait':
                        print('   wait', it[1][0].name if hasattr(it[1][0], 'name') else it[1][0], it[1][1])
                    else:
                        print('  ', it[0], (it[2].name if hasattr(it[2], 'name') else it[2]), it[1].__code__.co_firstlineno)
        with nc.Block() as block:
            def mk(e):
                items = self.prog[e]

                def body(eng):
                    n = len(items)
                    for i, it in enumerate(items):
                        if it[0] == "wait":
                            if FUSE_WAIT and i + 1 < n and items[i + 1][0] != "wait":
                                continue
                            eng.wait_ge(it[1][0], it[1][1])
                        else:
                            ins = it[1](eng)
                            if FUSE_WAIT and i > 0 and items[i - 1][0] == "wait":
                                ins._wait_ge(items[i - 1][1][0], items[i - 1][1][1])
                            ins.then_inc(it[2], 16 if it[0] == "dma" else 1)
                return body

            for e in self.ENG:
                if self.prog[e]:
                    getattr(block, self.HANDLE[e])(mk(e))
        for e in self.ENG:
            self.total += len(self.prog[e])
            self.prog[e] = []


def _rope_table(pos, r, theta):
    half = r // 2
    inv = np.power(np.float32(theta), -np.arange(half, dtype=np.float32) * np.float32(2.0 / r)).astype(np.float32)
    ang = pos.astype(np.float32)[:, None] * inv[None, :]
    return np.concatenate([np.cos(ang), np.sin(ang)], axis=1).astype(np.float32)


def make_consts():
    pos = np.arange(S_LEN)
    c = {}
    c["c_ident_bf"] = np.eye(128, dtype=np.float32).astype(ml_dtypes.bfloat16)
    c["c_ident_f"] = np.eye(128, dtype=np.float32)
    sel = np.zeros((65, 64), np.float32)
    sel[64, :] = 1.0
    c["c_sel"] = sel
    c["c_rope0"] = _rope_table(pos, 16, 500000.0)
    c["c_ropec"] = _rope_table(pos, 32, 500000.0)
    c["c_roperow"] = _rope_table(pos // 64, 32, 10000.0)
    c["c_ropecol"] = _rope_table(pos % 64, 32, 10000.0)
    a = np.arange(128)[:, None]
    q = np.arange(128)[None, :]
    A = (a >= q).astype(np.float32)
    B = (a <= q).astype(np.float32)
    Ae = A * (a >= 64)
    Be = B * (a < 64)
    m = np.zeros((3, 128, 512), np.float32)
    m[0] = np.concatenate([A, B, A, B], 1)
    m[1] = np.concatenate([Ae, B, A, B], 1)
    m[2] = np.concatenate([A, B, A, Be], 1)
    c["c_mask"] = np.ascontiguousarray(m.transpose(1, 0, 2)).astype(ml_dtypes.bfloat16)
    return c


CONST_SPECS = {
    "c_ident_bf": ([128, 128], BF16), "c_ident_f": ([128, 128], F32), "c_sel": ([65, 64], F32),
    "c_rope0": ([S_LEN, 16], F32), "c_ropec": ([S_LEN, 32], F32), "c_roperow": ([S_LEN, 32], F32),
    "c_ropecol": ([S_LEN, 32], F32), "c_mask": ([128, 3, 512], BF16),
}

INPUT_SPECS = {
    "x": [S_LEN, D], "norm_mix": [2, D], "norm_ffn": [2, D], "even_w_in": [D, 2816],
    "even_gmlp_norm": [1, 256], "even_w_spatial": [4, 128, 128], "even_b_spatial": [4, 128],
    "even_w_out": [D, D], "odd_w_in": [D, 1184], "odd_cq_norm": [1, 256], "odd_w_cq_up": [256, 768],
    "odd_ckv_norm": [1, 128], "odd_w_ckv_up": [128, 1024], "odd_dq_norm": [1, 64], "odd_dk_norm": [1, 64],
    "odd_w_out": [D, D], "moe_w_router": [2, D, 16], "moe_w_gate": [2, 16, D, 512],
    "moe_w_up": [2, 16, D, 512], "moe_w_down": [2, 16, 512, D], "final_norm": [1, D],
}


class Builder:
    def __init__(self, dbg=(), upto="all"):
        self.nc = bass.Bass("TRN2", target_bir_lowering=False)
        self.dbg = set(dbg)
        self.upto = upto
        self.root = ExitStack()
        self.S = Sched(self.nc, self.root)
        self.inp = {}
        for k, shp in INPUT_SPECS.items():
            self.inp[k] = Buf(self.nc.dram_tensor(k, shp, F32, kind="ExternalInput").ap(), k, False)
        for k, (shp, dt) in CONST_SPECS.items():
            self.inp[k] = Buf(self.nc.dram_tensor(k, shp, dt, kind="ExternalInput").ap(), k, False)
        self.out = Buf(self.nc.dram_tensor("y", [S_LEN, D], F32, kind="ExternalOutput").ap(), "y", False)
        self.scr = {}
        self.phn = 0
        self.affall = self.sb(self.root, "affall", [128, NT, NEXP], F32)
        self.gw = self.sb(self.root, "gw", [128, NT, NEXP], F32)
        self.xa = self.dram("xa", [S_LEN, D], F32)
        self.xb = self.dram("xb", [S_LEN, D], F32)
        self.hTd = self.dram("hTd", [D, S_LEN], BF16)

    def dram(self, name, shape, dt):
        kind = "ExternalOutput" if name in self.dbg else "Internal"
        b = Buf(self.nc.dram_tensor(name, shape, dt, kind=kind).ap(), name, False)
        self.scr[name] = b
        return b

    def sb(self, ph, name, shape, dt):
        name = f"p{self.phn}_{name}"
        return Buf(ph.enter_context(self.nc.sbuf_tensor(name, shape, dt)), name)

    def ps(self, ph, name, shape, dt):
        name = f"p{self.phn}_{name}"
        return Buf(ph.enter_context(self.nc.psum_tensor(name, shape, dt)), name)

    def _op(self, e, fn, r, w):
        self.S.op(e, fn, reads=[b.r for b in r], writes=[b.r for b in w])

    def pe(self, fn, r, w):
        self._op("pe", fn, r, w)

    def act(self, fn, r, w):
        self._op("act", fn, r, w)

    def dve(self, fn, r, w):
        self._op("dve", fn, r, w)

    def pool(self, fn, r, w):
        self._op("pool", fn, r, w)

    def dma(self, q, out, in_, r, w, stream, **kw):
        self.S.op(q, lambda e: e.dma_start(out=out, in_=in_, **kw), reads=[b.r for b in r if b.track],
                  writes=[b.r for b in w if b.track], stream=stream.r)

    def phase_begin(self):
        self.phn += 1
        self.S.barrier()
        self.S.release_streams()
        return ExitStack()

    def phase_end(self, ph):
        self.S.emit()
        ph.close()

    def rmsnorm_rstd(self, src, srcbufs, junk, ssq, rstd, n, width=None):
        sc = float(n) ** -0.5
        self.pool(lambda e: e.memset(ssq.t[:, 0:1], 0.0), [], [ssq])
        self.act(lambda e: e.activation(out=junk, in_=src, func=AF.Square, scale=sc, accum_out=ssq.t[:, 0:1]),
                 srcbufs, [ssq])
        self.dve(lambda e: e.tensor_scalar_add(out=rstd.t[:, 0:1], in0=ssq.t[:, 0:1], scalar1=EPS), [ssq], [rstd])
        self.act(lambda e: e.activation(out=rstd.t[:, 0:1], in_=rstd.t[:, 0:1], func=AF.Sqrt), [rstd], [rstd])
        self.dve(lambda e: e.reciprocal(out=rstd.t[:, 0:1], in_=rstd.t[:, 0:1]), [rstd], [rstd])

    def build(self):
        with self.root:
            if self.upto in ("B1only", "B0only", "Eonly"):
                self.catT = self.dram("catT", [1024, S_LEN], BF16)
                if self.upto == "B1only":
                    self.QcT = self.dram("QcT", [768, S_LEN], BF16)
                    self.KcT = self.dram("KcT", [768, S_LEN], BF16)
                    self.Vc = self.dram("Vc", [S_LEN, 512], BF16)
                    self.QdT = self.dram("QdT", [512, S_LEN], BF16)
                    self.KdT = self.dram("KdT", [128, S_LEN], BF16)
                    self.Vd = self.dram("Vd", [S_LEN, 128], BF16)
                    self.layer1_B()
                elif self.upto == "B0only":
                    self.V0 = self.dram("V0", [PAD + S_LEN + PAD, 768], BF16)
                    self.QT0 = self.dram("QT0", [768, S_LEN], BF16)
                    self.KT0 = self.dram("KT0", [768, S_LEN], BF16)
                    self.layer0_B()
                else:
                    self.wg_bf = self.dram("wg_bf", [2, NEXP, D, 512], BF16)
                    self.wu_bf = self.dram("wu_bf", [2, NEXP, D, 512], BF16)
                    self.wd_bf = self.dram("wd_bf", [2, NEXP, 512, D], BF16)
                    self.phase_E(0, last=False)
                return self.finish()
            self.prologue()
            if self.upto == "pro":
                return self.finish()
            self.layer0_A()
            if self.upto == "A0":
                return self.finish()
            self.layer0_B()
            if self.upto == "B0":
                return self.finish()
            self.phase_C(0, self.inp["x"], self.inp["even_w_out"])
            if self.upto == "C0":
                return self.finish()
            self.phase_D()
            if self.upto == "D0":
                return self.finish()
            self.phase_E(0, last=False)
            if self.upto == "E0":
                return self.finish()
            self.layer1_A()
            if self.upto == "A1":
                return self.finish()
            self.layer1_B()
            if self.upto == "B1":
                return self.finish()
            self.phase_C(1, self.xb, self.inp["odd_w_out"])
            if self.upto == "C1":
                return self.finish()
            self.phase_D()
            self.phase_E(1, last=True)
            return self.finish()

    def finish(self):
        ph = self.phase_begin()
        self.phase_end(ph)
        return self.nc

    def prologue(self):
        self.wg_bf = self.dram("wg_bf", [2, NEXP, D, 512], BF16)
        self.wu_bf = self.dram("wu_bf", [2, NEXP, D, 512], BF16)
        self.wd_bf = self.dram("wd_bf", [2, NEXP, 512, D], BF16)

    def prologue_issue(self):
        for l in range(2):
            for e_ in range(NEXP):
                for src, dst in ((self.inp["moe_w_gate"], self.wg_bf), (self.inp["moe_w_up"], self.wu_bf),
                                 (self.inp["moe_w_down"], self.wd_bf)):
                    self.dma("pool", dst.t[l, e_], src.t[l, e_], [], [], dst)
        for dst in (self.wg_bf, self.wu_bf, self.wd_bf):
            self.S.mark_bg(dst.r)

    def layer0_A(self):
        nc = self.nc
        I = self.inp
        self.V0 = self.dram("V0", [PAD + S_LEN + PAD, 768], BF16)
        self.QT0 = self.dram("QT0", [768, S_LEN], BF16)
        self.KT0 = self.dram("KT0", [768, S_LEN], BF16)
        self.catT = self.dram("catT", [1024, S_LEN], BF16)
        ph = self.phase_begin()
        sb = lambda n, s, d: self.sb(ph, n, s, d)
        ps = lambda n, s, d: self.ps(ph, n, s, d)
        ident = sb("ident", [128, 128], BF16)
        win = sb("win", [128, 8, 2816], BF16)
        gmix = sb("gmix", [128, D], F32)
        gmn = sb("gmn", [128, 256], F32)
        wsf = sb("wsf", [128, 4, 128], F32)
        wsb = sb("wsb", [128, 4, 128], BF16)
        wsT = sb("wsT", [128, 4, 128], BF16)
        bsT = sb("bsT", [128, 4], F32)
        zero = sb("zero", [128, 768], BF16)
        xt = [sb(f"xt{i}", [128, D], F32) for i in range(2)]
        junk = sb("junk", [128, D], BF16)
        ssq = [sb(f"ssq{i}", [128, 1], F32) for i in range(2)]
        rstd = [sb(f"rstd{i}", [128, 1], F32) for i in range(2)]
        hn = [sb(f"hn{i}", [128, D], BF16) for i in range(2)]
        hT = [sb(f"hT{i}", [128, 8, 128], BF16) for i in range(2)]
        cs = [sb(f"cs{i}", [128, 16], F32) for i in range(2)]
        qk = [sb(f"qk{i}", [128, 1536], BF16) for i in range(2)]
        rt = [sb(f"rt{i}", [128, 24, 8], F32) for i in range(4)]
        vb = [sb(f"vb{i}", [128, 768], BF16) for i in range(2)]
        zgs = [sb(f"zg{i}", [128, 512], F32) for i in range(2)]
        qkfs = [sb(f"qkf{i}", [128, 1536], F32) for i in range(2)]
        sqv = sb("sqv", [128, 256], F32)
        ss4 = sb("ss4", [128, 4], F32)
        r4 = sb("r4", [128, 4], F32)
        vvn = sb("vvn", [128, 256], F32)
        vvb = sb("vvb", [128, 256], BF16)
        mxb = sb("mxb", [128, 256], F32)
        gout = sb("gout", [128, 256], BF16)
        stq = [sb(f"stq{i}", [128, 6, 512], BF16) for i in range(2)]
        stk = [sb(f"stk{i}", [128, 6, 512], BF16) for i in range(2)]
        stg = [sb(f"stg{i}", [64, 4, 512], BF16) for i in range(2)]
        T0 = ps("T0", [128, 1024], BF16)
        T1 = ps("T1", [128, 1024], BF16)
        pqk = ps("pqk", [128, 1536], F32)
        pvz = ps("pvz", [128, 1536], F32)

        self.dma("sp", ident.t[:], I["c_ident_bf"].t[:, :], [], [ident], ident)
        self.dma("pool", win.t[:], I["even_w_in"].t.rearrange("(c p) n -> p c n", p=128), [], [win], win)
        self.dma("sp", gmix.t[:], I["norm_mix"].t[0:1, :].partition_broadcast(128), [], [gmix], gmix)
        self.dma("sp", gmn.t[:], I["even_gmlp_norm"].t[0:1, :].partition_broadcast(128), [], [gmn], gmn)
        self.dma("sp", wsf.t[:], I["even_w_spatial"].t.rearrange("g p q -> p g q"), [], [wsf], wsf)
        self.dma("sp", bsT.t[:], I["even_b_spatial"].t.rearrange("g p -> p g"), [], [bsT], bsT,
                 allow_slow_non_contiguous=True)
        self.dve(lambda e: e.tensor_copy(out=wsb.t[:], in_=wsf.t[:]), [wsf], [wsb])
        for g in range(4):
            self.pe(lambda e, g=g: e.transpose(out=T0.t[:, g * 128:(g + 1) * 128], in_=wsb.t[:, g, :], identity=ident.t[:]),
                    [wsb, ident], [T0])
        self.act(lambda e: e.copy(out=wsT.t[:].rearrange("p g q -> p (g q)"), in_=T0.t[:, 0:512]), [T0], [wsT])
        self.pool(lambda e: e.memset(zero.t[:], 0.0), [], [zero])
        for side in range(2):
            for k in range(PAD // 128):
                r0 = (0 if side == 0 else PAD + S_LEN) + k * 128
                self.dma("sp", self.V0.t[r0:r0 + 128, :], zero.t[:], [zero], [self.V0], zero)

        import os
        NT0 = int(os.environ.get('NGRP', NT // 4)) * 4

        def stage1(ti):
            grp, tt = divmod(ti, 4)
            sq_, sk_, sg_ = stq[grp % 2], stk[grp % 2], stg[grp % 2]
            b = ti % 2
            x_, ssq_, rstd_, hn_, hT_, cs_, qk_, vb_, qkf, zg = xt[b], ssq[b], rstd[b], hn[b], hT[b], cs[b], qk[b], vb[b], qkfs[b], zgs[b]
            tok = slice(ti * 128, (ti + 1) * 128)
            self.dma("sp", x_.t[:], I["x"].t[tok, :], [I["x"]], [x_], x_)
            self.dma("sp", cs_.t[:], I["c_rope0"].t[tok, :], [], [cs_], cs_)
            self.rmsnorm_rstd(x_.t[:], [x_], junk.t[:], ssq_, rstd_, D)
            self.dve(lambda e, x_=x_, rstd_=rstd_, hn_=hn_: e.scalar_tensor_tensor(
                out=hn_.t[:], in0=x_.t[:], scalar=rstd_.t[:, 0:1], in1=gmix.t[:], op0=ALU.mult, op1=ALU.mult),
                [x_, rstd_, gmix], [hn_])
            for j in range(8):
                self.pe(lambda e, j=j, hn_=hn_: e.transpose(out=T0.t[:, j * 128:(j + 1) * 128],
                                                           in_=hn_.t[:, j * 128:(j + 1) * 128], identity=ident.t[:]),
                        [hn_, ident], [T0])
            self.act(lambda e, hT_=hT_: e.copy(out=hT_.t[:].rearrange("p c n -> p (c n)"), in_=T0.t[:]), [T0], [hT_])
            for bank in range(3):
                for j in range(8):
                    self.pe(lambda e, j=j, bank=bank, hT_=hT_: e.matmul(
                        pqk.t[:, bank * 512:(bank + 1) * 512], lhsT=hT_.t[:, j, :],
                        rhs=win.t[:, j, bank * 512:(bank + 1) * 512], start=(j == 0), stop=(j == 7)),
                        [hT_, win], [pqk])
            for (o0, n_, c0) in ((0, 512, 1536), (512, 256, 2048), (1024, 512, 2304)):
                for j in range(8):
                    self.pe(lambda e, j=j, o0=o0, n_=n_, c0=c0, hT_=hT_: e.matmul(
                        pvz.t[:, o0:o0 + n_], lhsT=hT_.t[:, j, :], rhs=win.t[:, j, c0:c0 + n_],
                        start=(j == 0), stop=(j == 7)), [hT_, win], [pvz])
            self.act(lambda e: e.copy(out=qkf.t[:], in_=pqk.t[:]), [pqk], [qkf])
            self.act(lambda e, vb_=vb_: e.copy(out=vb_.t[:], in_=pvz.t[:, 0:768]), [pvz], [vb_])
            self.dma("sp", self.V0.t[PAD + ti * 128:PAD + (ti + 1) * 128, :], vb_.t[:], [vb_], [self.V0], vb_)
            self.act(lambda e: e.activation(out=zg.t[:], in_=pvz.t[:, 1024:1536], func=AF.Gelu_apprx_tanh), [pvz], [zg])

        def stage2(ti):
            grp, tt = divmod(ti, 4)
            sq_, sk_, sg_ = stq[grp % 2], stk[grp % 2], stg[grp % 2]
            b = ti % 2
            x_, ssq_, rstd_, hn_, hT_, cs_, qk_, vb_, qkf, zg = xt[b], ssq[b], rstd[b], hn[b], hT[b], cs[b], qk[b], vb[b], qkfs[b], zgs[b]
            tok = slice(ti * 128, (ti + 1) * 128)
            self.pool(lambda e, qk_=qk_: e.tensor_copy(out=qk_.t[:], in_=qkf.t[:]), [qkf], [qk_])
            pv = qkf.t[:].rearrange("p (h c) -> p h c", c=64)
            qv = qk_.t[:].rearrange("p (h c) -> p h c", c=64)
            cosb = cs_.t[:, 0:8].unsqueeze(1).to_broadcast([128, 24, 8])
            sinb = cs_.t[:, 8:16].unsqueeze(1).to_broadcast([128, 24, 8])
            x1 = pv[:, :, 0:8]
            x2 = pv[:, :, 8:16]
            self.dve(lambda e, x1=x1, cosb=cosb: e.tensor_tensor(out=rt[0].t[:], in0=x1, in1=cosb, op=ALU.mult), [qkf, cs_], [rt[0]])
            self.dve(lambda e, x2=x2, sinb=sinb: e.tensor_tensor(out=rt[1].t[:], in0=x2, in1=sinb, op=ALU.mult), [qkf, cs_], [rt[1]])
            self.dve(lambda e, x1=x1, sinb=sinb: e.tensor_tensor(out=rt[2].t[:], in0=x1, in1=sinb, op=ALU.mult), [qkf, cs_], [rt[2]])
            self.dve(lambda e, x2=x2, cosb=cosb: e.tensor_tensor(out=rt[3].t[:], in0=x2, in1=cosb, op=ALU.mult), [qkf, cs_], [rt[3]])
            self.pool(lambda e, qv=qv: e.tensor_tensor(out=qv[:, :, 0:8], in0=rt[0].t[:], in1=rt[1].t[:], op=ALU.subtract),
                      [rt[0], rt[1]], [qk_])
            self.pool(lambda e, qv=qv: e.tensor_tensor(out=qv[:, :, 8:16], in0=rt[2].t[:], in1=rt[3].t[:], op=ALU.add),
                      [rt[2], rt[3]], [qk_])
            self.dve(lambda e: e.tensor_tensor(out=sqv.t[:], in0=zg.t[:, 256:512], in1=zg.t[:, 256:512], op=ALU.mult), [zg], [sqv])
            self.dve(lambda e: e.tensor_reduce(out=ss4.t[:], in_=sqv.t[:].rearrange("p (g c) -> p g c", c=64), axis=AX.X, op=ALU.add), [sqv], [ss4])
            self.dve(lambda e: e.tensor_scalar(out=r4.t[:], in0=ss4.t[:], scalar1=1.0 / 64, scalar2=EPS, op0=ALU.mult, op1=ALU.add), [ss4], [r4])
            self.act(lambda e: e.activation(out=r4.t[:], in_=r4.t[:], func=AF.Sqrt), [r4], [r4])
            self.dve(lambda e: e.reciprocal(out=r4.t[:], in_=r4.t[:]), [r4], [r4])
            self.dve(lambda e: e.tensor_tensor(out=vvn.t[:].rearrange("p (g c) -> p g c", c=64),
                                               in0=zg.t[:, 256:512].rearrange("p (g c) -> p g c", c=64),
                                               in1=r4.t[:].unsqueeze(2).to_broadcast([128, 4, 64]), op=ALU.mult), [zg, r4], [vvn])
            self.pool(lambda e: e.tensor_tensor(out=vvb.t[:], in0=vvn.t[:], in1=gmn.t[:], op=ALU.mult), [vvn, gmn], [vvb])
            for g in range(4):
                self.pe(lambda e, g=g: e.matmul(pvz.t[:, 768 + g * 64:768 + (g + 1) * 64], lhsT=wsT.t[:, g, :],
                                                rhs=vvb.t[:, g * 64:(g + 1) * 64], start=True, stop=True), [wsT, vvb], [pvz])
            self.act(lambda e: e.copy(out=sqv.t[:], in_=pvz.t[:, 768:1024]), [pvz], [sqv])
            self.dve(lambda e: e.tensor_tensor(out=mxb.t[:].rearrange("p (g c) -> p g c", c=64),
                                               in0=sqv.t[:].rearrange("p (g c) -> p g c", c=64),
                                               in1=bsT.t[:].unsqueeze(2).to_broadcast([128, 4, 64]), op=ALU.add), [sqv, bsT], [mxb])
            self.pool(lambda e: e.tensor_tensor(out=gout.t[:], in0=mxb.t[:], in1=zg.t[:, 0:256], op=ALU.mult), [mxb, zg], [gout])
            for half, st_ in ((0, sq_), (1, sk_)):
                for j in range(6):
                    c0 = half * 768 + j * 128
                    self.pe(lambda e, j=j, c0=c0, qk_=qk_: e.transpose(out=T1.t[:, j * 128:(j + 1) * 128], in_=qk_.t[:, c0:c0 + 128],
                                                                     identity=ident.t[:]), [qk_, ident], [T1])
                self.act(lambda e, st_=st_, tt=tt: e.copy(out=st_.t[:, :, tt * 128:(tt + 1) * 128],
                                                        in_=T1.t[:, 0:768].rearrange("p (j n) -> p j n", n=128)), [T1], [st_])
            for g in range(4):
                self.pe(lambda e, g=g: e.transpose(out=T1.t[0:64, g * 128:(g + 1) * 128], in_=gout.t[:, g * 64:(g + 1) * 64],
                                                   identity=ident.t[:]), [gout, ident], [T1])
            self.dve(lambda e, sg_=sg_, tt=tt: e.tensor_copy(out=sg_.t[:, :, tt * 128:(tt + 1) * 128],
                                                           in_=T1.t[0:64, 0:512].rearrange("p (j n) -> p j n", n=128)), [T1], [sg_])
            if tt == 3:
                gtok = slice(grp * 512, (grp + 1) * 512)
                self.dma("sp", self.QT0.t.rearrange("(j p) t -> p j t", p=128)[:, :, gtok], sq_.t[:], [sq_], [self.QT0], sq_)
                self.dma("sp", self.KT0.t.rearrange("(j p) t -> p j t", p=128)[:, :, gtok], sk_.t[:], [sk_], [self.KT0], sk_)
                self.dma("sp", self.catT.t[768:1024, :].rearrange("(j p) t -> p j t", p=64)[:, :, gtok], sg_.t[:], [sg_], [self.catT], sg_)

        for s_ in range(NT0 + 1):
            if s_ < NT0:
                stage1(s_)
            if s_ >= 1:
                stage2(s_ - 1)
        self.phase_end(ph)

    def layer0_B(self):
        I = self.inp
        ph = self.phase_begin()
        sb = lambda n, s, d: self.sb(ph, n, s, d)
        ps = lambda n, s, d: self.ps(ph, n, s, d)
        sel = sb("sel", [65, 64], F32)
        mask = sb("mask", [128, 3, 512], BF16)
        qT = [sb(f"qT{i}", [64, S_LEN], BF16) for i in range(2)]
        kT = [sb(f"kT{i}", [64, PAD + S_LEN + PAD], BF16) for i in range(2)]
        vA = [sb(f"vA{i}", [128, 80, 65], BF16) for i in range(2)]
        acc = sb("acc", [65, S_LEN], F32)
        aTs = sb("aTs", [64, S_LEN], BF16)
        rz = [sb(f"rz{i}", [64, 512], F32) for i in range(2)]
        Eb = [sb(f"Eb{i}", [128, 512], BF16) for i in range(4)]
        Pb = [sb(f"Pb{i}", [128, 512], BF16) for i in range(4)]
        Sp = [ps(f"Sp{i}", [128, 512], F32) for i in range(4)]
        Op = [ps(f"Op{i}", [65, 512], F32) for i in range(2)]
        bc = [ps(f"bc{i}", [64, 512], F32) for i in range(2)]
        self.dma("sp", sel.t[:], I["c_sel"].t[:, :], [], [sel], sel)
        self.dma("sp", mask.t[:], I["c_mask"].t[:, :, :], [], [mask], mask)
        for i in range(2):
            self.pool(lambda e, i=i: e.memset(kT[i].t[:, 0:PAD], 0.0), [], [kT[i]])
            self.pool(lambda e, i=i: e.memset(kT[i].t[:, PAD + S_LEN:], 0.0), [], [kT[i]])
            self.pool(lambda e, i=i: e.memset(vA[i].t[:, :, 64:65], 1.0), [], [vA[i]])
        if hasattr(self, "wg_bf") and self.upto != "B0only":
            self.prologue_issue()
        import os
        hp = [(h, pi, d) for h in range(12) for pi, d in enumerate((1, 4, 16))]

        def load_qk(h):
            q_, k_ = qT[h % 2], kT[h % 2]
            self.dma("sp", q_.t[:], self.QT0.t[h * 64:(h + 1) * 64, :], [], [q_], q_)
            self.dma("sp", k_.t[:, PAD:PAD + S_LEN], self.KT0.t[h * 64:(h + 1) * 64, :], [], [k_], k_)

        def load_v(k):
            h, pi, d = hp[k]
            ntile = S_LEN // d // 128 + 1
            v_ = vA[k % 2]
            r0 = PAD - 64 * d
            rows = ntile * 128 * d
            src = self.V0.t[r0:r0 + rows, h * 64:(h + 1) * 64].rearrange("(m a i) c -> a i m c", a=128, i=d)
            dstv = v_.t[:, 0:d * ntile, 0:64].rearrange("a (i m) c -> a i m c", i=d)
            for i in range(d):
                for m0 in range(0, ntile, 16):
                    m1 = min(ntile, m0 + 16)
                    self.dma("sp", dstv[:, i, m0:m1, :], src[:, i, m0:m1, :], [], [v_], v_)

        load_qk(0)
        load_v(0)
        cnt = {"it": 0, "og": 0}
        LA = 2
        for k, (h, pi, d) in enumerate(hp):
            q_, k_, v_ = qT[h % 2], kT[h % 2], vA[k % 2]
            if pi == 0 and h + 1 < 12:
                load_qk(h + 1)
            if k + 1 < len(hp):
                load_v(k + 1)
            nb = S_LEN // d // 128
            ntile = nb + 1
            pairs = [(i, gq, pr) for i in range(d) for gq in range(nb // 4) for pr in range(2)]
            bufs = {}
            obuf = {}

            def emit_qk(idx):
                i, gq, pr = pairs[idx]
                n0 = gq * 4
                b4 = cnt["it"] % 4
                cnt["it"] += 1
                S_, E_, P_ = Sp[b4], Eb[b4], Pb[b4]
                bufs[idx] = P_
                nA = n0 + 2 * pr
                for (m, qb, nq, c0) in ((nA, nA, 128, 0), (nA + 1, nA, 256, 128), (nA + 2, nA + 1, 128, 384)):
                    k0 = PAD - 64 * d + i + 128 * d * m
                    q0 = 128 * d * qb + i
                    self.pe(lambda e, S_=S_, c0=c0, k0=k0, q0=q0, nq=nq, d=d, k_=k_, q_=q_: e.matmul(
                        S_.t[:, c0:c0 + nq], lhsT=k_.t[:, k0:k0 + 127 * d + 1:d], rhs=q_.t[:, q0:q0 + (nq - 1) * d + 1:d],
                        start=True, stop=True), [k_, q_], [S_])
                self.act(lambda e, S_=S_, E_=E_: e.activation(out=E_.t[:], in_=S_.t[:], func=AF.Exp, scale=0.125), [S_], [E_])
                first = (nA == 0)
                last = (nA + 2 == nb)
                assert not (first and last)
                mi = 1 if first else (2 if last else 0)
                self.dve(lambda e, E_=E_, P_=P_, mi=mi: e.tensor_tensor(out=P_.t[:], in0=E_.t[:], in1=mask.t[:, mi, :], op=ALU.mult),
                         [E_, mask], [P_])

            def emit_pv(idx):
                i, gq, pr = pairs[idx]
                n0 = gq * 4
                P_ = bufs.pop(idx)
                if pr == 0:
                    obuf[(i, gq)] = Op[cnt["og"] % 2]
                    cnt["og"] += 1
                O_ = obuf[(i, gq)]
                for blk in range(2):
                    n = n0 + 2 * pr + blk
                    oc = (2 * pr + blk) * 128
                    for ab in range(2):
                        m = n + ab
                        c0 = (blk * 2 + ab) * 128
                        self.pe(lambda e, O_=O_, oc=oc, c0=c0, P_=P_, ti=i * ntile + m, ab=ab, v_=v_: e.matmul(
                            O_.t[:, oc:oc + 128], lhsT=v_.t[:, ti, :], rhs=P_.t[:, c0:c0 + 128],
                            start=(ab == 0), stop=(ab == 1)), [v_, P_], [O_])
                if pr == 1:
                    del obuf[(i, gq)]
                    a0 = 128 * d * n0 + i
                    av = acc.t[:, a0:a0 + 511 * d + 1:d]
                    if pi == 0:
                        self.dve(lambda e, av=av, O_=O_: e.tensor_copy(out=av, in_=O_.t[:]), [O_], [acc])
                    else:
                        self.dve(lambda e, av=av, O_=O_: e.tensor_tensor(out=av, in0=O_.t[:], in1=av, op=ALU.add), [O_, acc], [acc])

            npair = len(pairs)
            for idx in range(min(LA, npair)):
                emit_qk(idx)
            for idx in range(npair):
                if idx + LA < npair:
                    emit_qk(idx + LA)
                emit_pv(idx)
            if pi == 2:
                for c in range(S_LEN // 512):
                    cs_ = slice(c * 512, (c + 1) * 512)
                    b_ = bc[c % 2]
                    r_ = rz[c % 2]
                    self.pe(lambda e, b_=b_, cs_=cs_: e.matmul(b_.t[:], lhsT=sel.t[:], rhs=acc.t[:, cs_], start=True, stop=True), [sel, acc], [b_])
                    self.dve(lambda e, b_=b_, r_=r_: e.reciprocal(out=r_.t[:], in_=b_.t[:]), [b_], [r_])
                    self.dve(lambda e, r_=r_, cs_=cs_: e.tensor_tensor(out=aTs.t[:, cs_], in0=acc.t[0:64, cs_], in1=r_.t[:], op=ALU.mult), [acc, r_], [aTs])
                self.dma("sp", self.catT.t[h * 64:(h + 1) * 64, :], aTs.t[:], [aTs], [], aTs)
        self.phase_end(ph)


    def phase_C(self, l, xin, wout_in):
        I = self.inp
        ph = self.phase_begin()
        sb = lambda n, s, d: self.sb(ph, n, s, d)
        ps = lambda n, s, d: self.ps(ph, n, s, d)
        ident = sb("ident", [128, 128], BF16)
        identf = sb("identf", [128, 128], F32)
        wout = sb("wout", [64, 16, D], BF16)
        gffn = sb("gffn", [128, D], F32)
        wr32 = sb("wr32", [128, 8, NEXP], F32)
        catg = [sb(f"catg{i}", [64, 16, 512], BF16) for i in range(2)]
        xt = [sb(f"xt{i}", [128, D], F32) for i in range(2)]
        x1t = [sb(f"x1t{i}", [128, D], F32) for i in range(2)]
        junk = sb("junk", [128, D], BF16)
        ssq = [sb(f"ssq{i}", [128, 1], F32) for i in range(2)]
        rstd = [sb(f"rstd{i}", [128, 1], F32) for i in range(2)]
        hnf = [sb(f"hnf{i}", [128, D], F32) for i in range(2)]
        hnb = [sb(f"hnb{i}", [128, D], BF16) for i in range(2)]
        hTs = [sb(f"hTs{i}", [128, 8, 512], BF16) for i in range(2)]
        hT32 = [sb(f"hT32{i}", [128, 8, 128], F32) for i in range(2)]
        lgs = sb("lgs", [128, NEXP], F32)
        ex = sb("ex", [128, NEXP], F32)
        mx = sb("mx", [128, 1], F32)
        se = sb("se", [128, 1], F32)
        T0 = ps("T0", [128, 1024], BF16)
        T32 = ps("T32", [128, 1024], F32)
        pm = [ps(f"pm{i}", [128, 1024], F32) for i in range(2)]
        lg = ps("lg", [128, NEXP], F32)
        self.dma("sp", ident.t[:], I["c_ident_bf"].t[:, :], [], [ident], ident)
        self.dma("sp", identf.t[:], I["c_ident_f"].t[:, :], [], [identf], identf)
        self.dma("pool", wout.t[:], wout_in.t.rearrange("(c p) n -> p c n", p=64), [], [wout], wout)
        self.dma("sp", gffn.t[:], I["norm_ffn"].t[l:l + 1, :].partition_broadcast(128), [], [gffn], gffn)
        self.dma("sp", wr32.t[:], I["moe_w_router"].t[l].rearrange("(c p) e -> p c e", p=128), [], [wr32], wr32)
        def stage1(ti):
            grp, tt = divmod(ti, 4)
            cg = catg[grp % 2]
            b = ti % 2
            x_, pm_ = xt[b], pm[b]
            tok = slice(ti * 128, (ti + 1) * 128)
            if tt == 0:
                gtok = slice(grp * 512, (grp + 1) * 512)
                self.dma("sp", cg.t[:], self.catT.t.rearrange("(c p) t -> p c t", p=64)[:, :, gtok], [], [cg], cg)
            self.dma("sp", x_.t[:], xin.t[tok, :], [], [x_], x_)
            for half in range(2):
                for c in range(16):
                    self.pe(lambda e, half=half, c=c, cg=cg, tt=tt, pm_=pm_: e.matmul(
                        pm_.t[:, half * 512:(half + 1) * 512], lhsT=cg.t[:, c, tt * 128:(tt + 1) * 128],
                        rhs=wout.t[:, c, half * 512:(half + 1) * 512], start=(c == 0), stop=(c == 15)), [cg, wout], [pm_])

        def stage2(ti):
            b = ti % 2
            x_, x1_, ssq_, rstd_, hnf_, hnb_, pm_ = xt[b], x1t[b], ssq[b], rstd[b], hnf[b], hnb[b], pm[b]
            tok = slice(ti * 128, (ti + 1) * 128)
            for half in range(2):
                hs_ = slice(half * 512, (half + 1) * 512)
                self.dve(lambda e, hs_=hs_, x_=x_, x1_=x1_, pm_=pm_: e.tensor_tensor(out=x1_.t[:, hs_], in0=pm_.t[:, hs_], in1=x_.t[:, hs_], op=ALU.add),
                         [pm_, x_], [x1_])
            self.dma("sp", self.xa.t[tok, :], x1_.t[:], [x1_], [], x1_)
            self.rmsnorm_rstd(x1_.t[:], [x1_], junk.t[:], ssq_, rstd_, D)
            self.dve(lambda e, x1_=x1_, rstd_=rstd_, hnf_=hnf_: e.scalar_tensor_tensor(
                out=hnf_.t[:], in0=x1_.t[:], scalar=rstd_.t[:, 0:1], in1=gffn.t[:], op0=ALU.mult, op1=ALU.mult),
                [x1_, rstd_, gffn], [hnf_])
            self.pool(lambda e, hnf_=hnf_, hnb_=hnb_: e.tensor_copy(out=hnb_.t[:], in_=hnf_.t[:]), [hnf_], [hnb_])

        def stage3(ti):
            grp, tt = divmod(ti, 4)
            hs = hTs[grp % 2]
            b = ti % 2
            hnf_, hnb_, h32_ = hnf[b], hnb[b], hT32[b]
            for j in range(8):
                self.pe(lambda e, j=j, hnb_=hnb_: e.transpose(out=T0.t[:, j * 128:(j + 1) * 128], in_=hnb_.t[:, j * 128:(j + 1) * 128],
                                                            identity=ident.t[:]), [hnb_, ident], [T0])
            self.act(lambda e, hs=hs, tt=tt: e.copy(out=hs.t[:, :, tt * 128:(tt + 1) * 128],
                                                  in_=T0.t[:].rearrange("p (c n) -> p c n", n=128)), [T0], [hs])
            for j in range(8):
                self.pe(lambda e, j=j, hnf_=hnf_: e.transpose(out=T32.t[:, j * 128:(j + 1) * 128], in_=hnf_.t[:, j * 128:(j + 1) * 128],
                                                            identity=identf.t[:]), [hnf_, identf], [T32])
            for half in range(2):
                self.act(lambda e, half=half, h32_=h32_: e.copy(out=h32_.t[:, half * 4:(half + 1) * 4, :].rearrange("p c n -> p (c n)"),
                                                               in_=T32.t[:, half * 512:(half + 1) * 512]), [T32], [h32_])
            for j in range(8):
                self.pe(lambda e, j=j, h32_=h32_: e.matmul(lg.t[:, :], lhsT=h32_.t[:, j, :], rhs=wr32.t[:, j, :],
                                                         start=(j == 0), stop=(j == 7)), [h32_, wr32], [lg])
            self.act(lambda e: e.copy(out=lgs.t[:], in_=lg.t[:]), [lg], [lgs])
            self.dve(lambda e: e.reduce_max(out=mx.t[:], in_=lgs.t[:], axis=AX.X), [lgs], [mx])
            self.dve(lambda e: e.tensor_scalar_mul(out=mx.t[:], in0=mx.t[:], scalar1=-1.0), [mx], [mx])
            self.pool(lambda e: e.memset(se.t[:], 0.0), [], [se])
            self.act(lambda e: e.activation(out=ex.t[:], in_=lgs.t[:], func=AF.Exp, bias=mx.t[:, 0:1], scale=1.0, accum_out=se.t[:, 0:1]),
                     [lgs, mx], [ex, se])
            self.dve(lambda e: e.reciprocal(out=se.t[:], in_=se.t[:]), [se], [se])
            self.dve(lambda e, ti=ti: e.tensor_scalar_mul(out=self.affall.t[:, ti, :], in0=ex.t[:], scalar1=se.t[:, 0:1]), [ex, se], [self.affall])
            if tt == 3:
                gtok = slice(grp * 512, (grp + 1) * 512)
                self.dma("sp", self.hTd.t.rearrange("(c p) t -> p c t", p=128)[:, :, gtok], hs.t[:], [hs], [], hs)

        for s_ in range(NT + 2):
            if s_ < NT:
                stage1(s_)
            if 0 <= s_ - 1 < NT:
                stage2(s_ - 1)
            if 0 <= s_ - 2 < NT:
                stage3(s_ - 2)
        self.phase_end(ph)

    def phase_D(self):
        ph = self.phase_begin()
        sb = lambda n, s, d: self.sb(ph, n, s, d)
        ps = lambda n, s, d: self.ps(ph, n, s, d)
        ones = sb("ones", [128, 128], BF16)
        cmp_ = sb("cmp", [128, NT * NEXP], BF16)
        cnts = sb("cnts", [128, NT * NEXP], F32)
        cnt16 = sb("cnt16", [128, NEXP], F32)
        lo = sb("lo", [128, NEXP], F32)
        mid = sb("mid", [128, NEXP], F32)
        ge = sb("ge", [128, NEXP], F32)
        msk = sb("msk", [128, NT, NEXP], F32)
        cntp = ps("cntp", [128, NT * NEXP], F32)
        aff = self.affall
        self.pool(lambda e: e.memset(ones.t[:], 1.0), [], [ones])
        self.pool(lambda e: e.memset(lo.t[:], 0.0), [], [lo])
        for k in range(1, 29):
            c = 2.0 ** -k
            self.dve(lambda e, c=c: e.tensor_scalar_add(out=mid.t[:], in0=lo.t[:], scalar1=c), [lo], [mid])
            self.dve(lambda e: e.tensor_tensor(out=cmp_.t[:].rearrange("p (t x) -> p t x", x=NEXP), in0=aff.t[:],
                                               in1=mid.t[:].unsqueeze(1).to_broadcast([128, NT, NEXP]), op=ALU.is_ge), [aff, mid], [cmp_])
            for half in range(2):
                self.pe(lambda e, half=half: e.matmul(cntp.t[:, half * 512:(half + 1) * 512], lhsT=ones.t[:],
                                                      rhs=cmp_.t[:, half * 512:(half + 1) * 512], start=True, stop=True), [ones, cmp_], [cntp])
            self.act(lambda e: e.copy(out=cnts.t[:], in_=cntp.t[:]), [cntp], [cnts])
            self.dve(lambda e: e.tensor_reduce(out=cnt16.t[:], in_=cnts.t[:].rearrange("p (t x) -> p x t", x=NEXP), axis=AX.X, op=ALU.add),
                     [cnts], [cnt16])
            self.dve(lambda e, c=c: e.tensor_scalar(out=ge.t[:], in0=cnt16.t[:], scalar1=CAP - 0.5, scalar2=c, op0=ALU.is_ge, op1=ALU.mult),
                     [cnt16], [ge])
            self.dve(lambda e: e.tensor_tensor(out=lo.t[:], in0=lo.t[:], in1=ge.t[:], op=ALU.add), [lo, ge], [lo])
        self.dve(lambda e: e.tensor_tensor(out=msk.t[:], in0=aff.t[:], in1=lo.t[:].unsqueeze(1).to_broadcast([128, NT, NEXP]), op=ALU.is_ge),
                 [aff, lo], [msk])
        self.dve(lambda e: e.tensor_tensor(out=self.gw.t[:], in0=msk.t[:], in1=aff.t[:], op=ALU.mult), [msk, aff], [self.gw])
        self.phase_end(ph)

    def phase_E(self, l, last):
        I = self.inp
        self.S.bg_done()
        ph = self.phase_begin()
        sb = lambda n, s, d: self.sb(ph, n, s, d)
        ps = lambda n, s, d: self.ps(ph, n, s, d)
        SG = 1024
        NQ = SG // 512
        accbs = [sb(f"accb{i}", [128, SG // 128, D], F32) for i in range(2)]
        hss = [sb(f"hs{i}", [128, 8, SG], BF16) for i in range(2)]
        wgt = [sb(f"wgt{i}", [128, 8, 512], BF16) for i in range(2)]
        wut = [sb(f"wut{i}", [128, 8, 512], BF16) for i in range(2)]
        wdt = [sb(f"wdt{i}", [128, 4, D], BF16) for i in range(2)]
        sgt = [sb(f"sgt{i}", [128, 512], F32) for i in range(2)]
        actT = [sb(f"actT{i}", [128, 4, 512], BF16) for i in range(2)]
        pg = [ps(f"pg{i}", [128, 512], F32) for i in range(2)]
        pu = [ps(f"pu{i}", [128, 512], F32) for i in range(2)]
        py = [ps(f"py{i}", [128, 512], F32) for i in range(4)]
        if last:
            gfin = sb("gfin", [128, D], F32)
            junk = sb("junk", [128, D], BF16)
            ssq = [sb(f"ssq{i}", [128, 1], F32) for i in range(2)]
            rstd = [sb(f"rstd{i}", [128, 1], F32) for i in range(2)]
            yo = [sb(f"yo{i}", [128, D], F32) for i in range(2)]
            self.dma("sp", gfin.t[:], I["final_norm"].t[0:1, :].partition_broadcast(128), [], [gfin], gfin)
        wi = 0
        kf = 0
        ky = 0
        ka = 0
        import os
        ECUT = int(os.environ.get('ECUT', 9))
        NSG = int(os.environ.get('ESG', S_LEN // SG))

        def sg_loads(sgi):
            t0 = sgi * SG
            a_, h_ = accbs[sgi % 2], hss[sgi % 2]
            for q4 in range(NQ):
                self.dma("pool", a_.t[:, q4 * 4:(q4 + 1) * 4, :],
                         self.xa.t[t0 + q4 * 512:t0 + (q4 + 1) * 512, :].rearrange("(t p) d -> p t d", p=128), [], [a_], a_)
            self.dma("pool", h_.t[:], self.hTd.t.rearrange("(c p) t -> p c t", p=128)[:, :, t0:t0 + SG], [], [h_], h_)

        sg_loads(0)
        for sgi in range(NSG):
            t0 = sgi * SG
            accb, hs = accbs[sgi % 2], hss[sgi % 2]
            if sgi + 1 < NSG:
                sg_loads(sgi + 1)
            for ex_ in range(int(os.environ.get('EEXP', NEXP))):
                wg_, wu_, wd_ = wgt[wi % 2], wut[wi % 2], wdt[wi % 2]
                wi += 1
                self.dma("sp", wg_.t[:], self.wg_bf.t[l, ex_].rearrange("(c p) f -> p c f", p=128), [], [wg_], wg_)
                self.dma("sp", wu_.t[:], self.wu_bf.t[l, ex_].rearrange("(c p) f -> p c f", p=128), [], [wu_], wu_)
                self.dma("sp", wd_.t[:], self.wd_bf.t[l, ex_].rearrange("(c p) n -> p c n", p=128), [], [wd_], wd_)
                for g4 in range(SG // 512):
                    if ECUT < 2: continue
                    tk = slice(g4 * 512, (g4 + 1) * 512)
                    aT_ = actT[ka % 2]
                    ka += 1
                    for fc in range(4):
                        pg_, pu_, sg_ = pg[kf % 2], pu[kf % 2], sgt[kf % 2]
                        kf += 1
                        fs = slice(fc * 128, (fc + 1) * 128)
                        for j in range(8):
                            self.pe(lambda e, j=j, pg_=pg_, wg_=wg_, fs=fs, tk=tk, hs=hs: e.matmul(pg_.t[:], lhsT=wg_.t[:, j, fs], rhs=hs.t[:, j, tk],
                                                                                      start=(j == 0), stop=(j == 7)), [wg_, hs], [pg_])
                        for j in range(8):
                            self.pe(lambda e, j=j, pu_=pu_, wu_=wu_, fs=fs, tk=tk, hs=hs: e.matmul(pu_.t[:], lhsT=wu_.t[:, j, fs], rhs=hs.t[:, j, tk],
                                                                                      start=(j == 0), stop=(j == 7)), [wu_, hs], [pu_])
                        self.act(lambda e, pg_=pg_, sg_=sg_: e.activation(out=sg_.t[:], in_=pg_.t[:], func=AF.Silu), [pg_], [sg_])
                        self.dve(lambda e, pu_=pu_, sg_=sg_, aT_=aT_, fc=fc: e.tensor_tensor(out=aT_.t[:, fc, :], in0=pu_.t[:], in1=sg_.t[:], op=ALU.mult),
                                 [pu_, sg_], [aT_])
                    for tt in range(4):
                        if ECUT < 3: continue
                        tl = g4 * 4 + tt
                        tile = sgi * (SG // 128) + tl
                        for ch in range(2):
                            py_ = py[ky % 4]
                            ky += 1
                            for fc in range(4):
                                self.pe(lambda e, py_=py_, aT_=aT_, fc=fc, tt=tt, wd_=wd_, ch=ch: e.matmul(
                                    py_.t[:], lhsT=aT_.t[:, fc, tt * 128:(tt + 1) * 128], rhs=wd_.t[:, fc, ch * 512:(ch + 1) * 512],
                                    start=(fc == 0), stop=(fc == 3)), [aT_, wd_], [py_])
                            av = accb.t[:, tl, ch * 512:(ch + 1) * 512]
                            self.dve(lambda e, av=av, py_=py_, tile=tile, ex_=ex_: e.scalar_tensor_tensor(
                                out=av, in0=py_.t[:], scalar=self.gw.t[:, tile, ex_:ex_ + 1], in1=av, op0=ALU.mult, op1=ALU.add),
                                [py_, self.gw, accb], [accb])
            if not last:
                for q4 in range(NQ):
                    self.dma("pool", self.xb.t[t0 + q4 * 512:t0 + (q4 + 1) * 512, :].rearrange("(t p) d -> p t d", p=128),
                             accb.t[:, q4 * 4:(q4 + 1) * 4, :], [accb], [], accb)
            else:
                for tl in range(SG // 128):
                    b = tl % 2
                    self.rmsnorm_rstd(accb.t[:, tl, :], [accb], junk.t[:], ssq[b], rstd[b], D)
                    self.dve(lambda e, tl=tl, b=b, accb=accb: e.scalar_tensor_tensor(out=yo[b].t[:], in0=accb.t[:, tl, :], scalar=rstd[b].t[:, 0:1],
                                                                        in1=gfin.t[:], op0=ALU.mult, op1=ALU.mult), [accb, rstd[b], gfin], [yo[b]])
                    r0 = t0 + tl * 128
                    self.dma("sp", self.out.t[r0:r0 + 128, :], yo[b].t[:], [yo[b]], [], yo[b])
        self.phase_end(ph)


    def rope_apply(self, x1, x2, cosb, sinb, o1, o2, rt, shape, srcbufs, tabbufs, outbuf):
        v = [r.t[:, 0:shape[1], 0:shape[2]] for r in rt]
        self.dve(lambda e: e.tensor_tensor(out=v[0], in0=x1, in1=cosb, op=ALU.mult), srcbufs + tabbufs, [rt[0]])
        self.dve(lambda e: e.tensor_tensor(out=v[1], in0=x2, in1=sinb, op=ALU.mult), srcbufs + tabbufs, [rt[1]])
        self.dve(lambda e: e.tensor_tensor(out=v[2], in0=x1, in1=sinb, op=ALU.mult), srcbufs + tabbufs, [rt[2]])
        self.dve(lambda e: e.tensor_tensor(out=v[3], in0=x2, in1=cosb, op=ALU.mult), srcbufs + tabbufs, [rt[3]])
        self.pool(lambda e: e.tensor_tensor(out=o1, in0=v[0], in1=v[1], op=ALU.subtract), [rt[0], rt[1]], [outbuf])
        self.pool(lambda e: e.tensor_tensor(out=o2, in0=v[2], in1=v[3], op=ALU.add), [rt[2], rt[3]], [outbuf])

    def layer1_A(self):
        I = self.inp
        self.QcT = self.dram("QcT", [768, S_LEN], BF16)
        self.KcT = self.dram("KcT", [768, S_LEN], BF16)
        self.Vc = self.dram("Vc", [S_LEN, 512], BF16)
        self.QdT = self.dram("QdT", [512, S_LEN], BF16)
        self.KdT = self.dram("KdT", [128, S_LEN], BF16)
        self.Vd = self.dram("Vd", [S_LEN, 128], BF16)
        ph = self.phase_begin()
        sb = lambda n, s, d: self.sb(ph, n, s, d)
        ps = lambda n, s, d: self.ps(ph, n, s, d)
        ident = sb("ident", [128, 128], BF16)
        win = sb("win", [128, 8, 1184], BF16)
        wcq = sb("wcq", [128, 2, 768], BF16)
        wckv = sb("wckv", [128, 1024], BF16)
        gmix = sb("gmix", [128, D], F32)
        gcq = sb("gcq", [128, 256], F32)
        gckv = sb("gckv", [128, 128], F32)
        gdq = sb("gdq", [128, 64], F32)
        gdk = sb("gdk", [128, 64], F32)
        xt = [sb(f"xt{i}", [128, D], F32) for i in range(2)]
        junk = sb("junk", [128, D], BF16)
        ssq = [sb(f"ssq{i}", [128, 1], F32) for i in range(2)]
        rstd = [sb(f"rstd{i}", [128, 1], F32) for i in range(2)]
        ssq2 = sb("ssq2", [128, 1], F32)
        rstd2 = sb("rstd2", [128, 1], F32)
        ssq3 = sb("ssq3", [128, 1], F32)
        rstd3 = sb("rstd3", [128, 1], F32)
        hn = [sb(f"hn{i}", [128, D], BF16) for i in range(2)]
        hT = [sb(f"hT{i}", [128, 8, 128], BF16) for i in range(2)]
        tabs = [sb(f"tabs{i}", [128, 96], F32) for i in range(2)]
        pfs = [sb(f"pf{i}", [128, 1184], F32) for i in range(2)]
        cqn = sb("cqn", [128, 256], BF16)
        cqT = sb("cqT", [128, 2, 128], BF16)
        kvn = sb("kvn", [128, 128], BF16)
        kvT = sb("kvT", [128, 128], BF16)
        qcf = sb("qcf", [128, 768], F32)
        qcb = sb("qcb", [128, 768], BF16)
        kvf = sb("kvf", [128, 1024], F32)
        kcb = sb("kcb", [128, 768], BF16)
        vcb = [sb(f"vcb{i}", [128, 512], BF16) for i in range(2)]
        krf = sb("krf", [128, 1, 32], F32)
        rt = [sb(f"rt{i}", [128, 8, 16], F32) for i in range(4)]
        sq8 = sb("sq8", [128, 640], F32)
        ss10 = sb("ss10", [128, 10], F32)
        r10 = sb("r10", [128, 10], F32)
        dn = sb("dn", [128, 640], F32)
        dqb = sb("dqb", [128, 640], BF16)
        dvb = [sb(f"dvb{i}", [128, 128], BF16) for i in range(2)]
        stqc = [sb(f"stqc{i}", [96, 8, 512], BF16) for i in range(2)]
        stkc = [sb(f"stkc{i}", [96, 8, 512], BF16) for i in range(2)]
        stqd = [sb(f"stqd{i}", [128, 4, 512], BF16) for i in range(2)]
        stkd = [sb(f"stkd{i}", [128, 512], BF16) for i in range(2)]
        T0 = ps("T0", [128, 1024], BF16)
        T1 = ps("T1", [128, 1024], BF16)
        pq = ps("pq", [128, 1536], F32)
        pc = ps("pc", [128, 1024], F32)
        self.dma("sp", ident.t[:], I["c_ident_bf"].t[:, :], [], [ident], ident)
        self.dma("pool", win.t[:], I["odd_w_in"].t.rearrange("(c p) n -> p c n", p=128), [], [win], win)
        self.dma("pool", wcq.t[:], I["odd_w_cq_up"].t.rearrange("(c p) n -> p c n", p=128), [], [wcq], wcq)
        self.dma("pool", wckv.t[:], I["odd_w_ckv_up"].t[:, :], [], [wckv], wckv)
        self.dma("sp", gmix.t[:], I["norm_mix"].t[1:2, :].partition_broadcast(128), [], [gmix], gmix)
        self.dma("sp", gcq.t[:], I["odd_cq_norm"].t[0:1, :].partition_broadcast(128), [], [gcq], gcq)
        self.dma("sp", gckv.t[:], I["odd_ckv_norm"].t[0:1, :].partition_broadcast(128), [], [gckv], gckv)
        self.dma("sp", gdq.t[:], I["odd_dq_norm"].t[0:1, :].partition_broadcast(128), [], [gdq], gdq)
        self.dma("sp", gdk.t[:], I["odd_dk_norm"].t[0:1, :].partition_broadcast(128), [], [gdk], gdk)
        import os
        NT1 = int(os.environ.get('NGRP1', NT // 4)) * 4

        def stage1(ti):
            grp, tt = divmod(ti, 4)
            sqc, skc, sqd, skd = stqc[grp % 2], stkc[grp % 2], stqd[grp % 2], stkd[grp % 2]
            b = ti % 2
            x_, ssq_, rstd_, hn_, hT_, tb_, vcb_, dvb_, pf = xt[b], ssq[b], rstd[b], hn[b], hT[b], tabs[b], vcb[b], dvb[b], pfs[b]
            tok = slice(ti * 128, (ti + 1) * 128)
            tsl = slice(tt * 128, (tt + 1) * 128)
            self.dma("sp", x_.t[:], self.xb.t[tok, :], [], [x_], x_)
            self.dma("sp", tb_.t[:, 0:32], I["c_ropec"].t[tok, :], [], [tb_], tb_)
            self.dma("sp", tb_.t[:, 32:64], I["c_roperow"].t[tok, :], [], [tb_], tb_)
            self.dma("sp", tb_.t[:, 64:96], I["c_ropecol"].t[tok, :], [], [tb_], tb_)
            self.rmsnorm_rstd(x_.t[:], [x_], junk.t[:], ssq_, rstd_, D)
            self.dve(lambda e, x_=x_, rstd_=rstd_, hn_=hn_: e.scalar_tensor_tensor(
                out=hn_.t[:], in0=x_.t[:], scalar=rstd_.t[:, 0:1], in1=gmix.t[:], op0=ALU.mult, op1=ALU.mult),
                [x_, rstd_, gmix], [hn_])
            for j in range(8):
                self.pe(lambda e, j=j, hn_=hn_: e.transpose(out=T0.t[:, j * 128:(j + 1) * 128], in_=hn_.t[:, j * 128:(j + 1) * 128],
                                                           identity=ident.t[:]), [hn_, ident], [T0])
            self.act(lambda e, hT_=hT_: e.copy(out=hT_.t[:].rearrange("p c n -> p (c n)"), in_=T0.t[:]), [T0], [hT_])
            for (c0, n_) in ((0, 512), (512, 512), (1024, 160)):
                for j in range(8):
                    self.pe(lambda e, j=j, c0=c0, n_=n_, hT_=hT_: e.matmul(pq.t[:, c0:c0 + n_], lhsT=hT_.t[:, j, :], rhs=win.t[:, j, c0:c0 + n_],
                                                                        start=(j == 0), stop=(j == 7)), [hT_, win], [pq])
            self.act(lambda e: e.copy(out=pf.t[:], in_=pq.t[:, 0:1184]), [pq], [pf])

        def stage2(ti):
            grp, tt = divmod(ti, 4)
            sqc, skc, sqd, skd = stqc[grp % 2], stkc[grp % 2], stqd[grp % 2], stkd[grp % 2]
            b = ti % 2
            x_, ssq_, rstd_, hn_, hT_, tb_, vcb_, dvb_, pf = xt[b], ssq[b], rstd[b], hn[b], hT[b], tabs[b], vcb[b], dvb[b], pfs[b]
            tok = slice(ti * 128, (ti + 1) * 128)
            tsl = slice(tt * 128, (tt + 1) * 128)
            self.rmsnorm_rstd(pf.t[:, 0:256], [pf], junk.t[:, 0:256], ssq2, rstd2, 256)
            self.dve(lambda e: e.scalar_tensor_tensor(out=cqn.t[:], in0=pf.t[:, 0:256], scalar=rstd2.t[:, 0:1], in1=gcq.t[:],
                                                      op0=ALU.mult, op1=ALU.mult), [pf, rstd2, gcq], [cqn])
            for j in range(2):
                self.pe(lambda e, j=j: e.transpose(out=T1.t[:, j * 128:(j + 1) * 128], in_=cqn.t[:, j * 128:(j + 1) * 128], identity=ident.t[:]),
                        [cqn, ident], [T1])
            self.act(lambda e: e.copy(out=cqT.t[:].rearrange("p c n -> p (c n)"), in_=T1.t[:, 0:256]), [T1], [cqT])
            for (c0, n_) in ((0, 512), (512, 256)):
                for j in range(2):
                    self.pe(lambda e, j=j, c0=c0, n_=n_: e.matmul(pc.t[:, c0:c0 + n_], lhsT=cqT.t[:, j, :], rhs=wcq.t[:, j, c0:c0 + n_],
                                                                 start=(j == 0), stop=(j == 1)), [cqT, wcq], [pc])
            self.act(lambda e: e.copy(out=qcf.t[:], in_=pc.t[:, 0:768]), [pc], [qcf])
            self.pool(lambda e: e.tensor_copy(out=qcb.t[:], in_=qcf.t[:]), [qcf], [qcb])
            qv = qcf.t[:].rearrange("p (h c) -> p h c", c=96)
            qo = qcb.t[:].rearrange("p (h c) -> p h c", c=96)
            cb = tb_.t[:, 0:16].unsqueeze(1).to_broadcast([128, 8, 16])
            sn = tb_.t[:, 16:32].unsqueeze(1).to_broadcast([128, 8, 16])
            self.rope_apply(qv[:, :, 64:80], qv[:, :, 80:96], cb, sn, qo[:, :, 64:80], qo[:, :, 80:96], rt, [128, 8, 16], [qcf], [tb_], qcb)
            self.rmsnorm_rstd(pf.t[:, 256:384], [pf], junk.t[:, 0:128], ssq3, rstd3, 128)
            self.dve(lambda e: e.scalar_tensor_tensor(out=kvn.t[:], in0=pf.t[:, 256:384], scalar=rstd3.t[:, 0:1], in1=gckv.t[:],
                                                      op0=ALU.mult, op1=ALU.mult), [pf, rstd3, gckv], [kvn])
            self.pe(lambda e: e.transpose(out=T1.t[:, 0:128], in_=kvn.t[:], identity=ident.t[:]), [kvn, ident], [T1])
            self.act(lambda e: e.copy(out=kvT.t[:], in_=T1.t[:, 0:128]), [T1], [kvT])
            for c0 in (0, 512):
                self.pe(lambda e, c0=c0: e.matmul(pc.t[:, c0:c0 + 512], lhsT=kvT.t[:], rhs=wckv.t[:, c0:c0 + 512], start=True, stop=True),
                        [kvT, wckv], [pc])
            self.act(lambda e: e.copy(out=kvf.t[:], in_=pc.t[:]), [pc], [kvf])
            kv3 = kvf.t[:].rearrange("p (h c) -> p h c", c=128)
            ko = kcb.t[:].rearrange("p (h c) -> p h c", c=96)
            self.pool(lambda e: e.tensor_copy(out=ko[:, :, 0:64], in_=kv3[:, :, 0:64]), [kvf], [kcb])
            self.pool(lambda e, vcb_=vcb_: e.tensor_copy(out=vcb_.t[:].rearrange("p (h c) -> p h c", c=64), in_=kv3[:, :, 64:128]), [kvf], [vcb_])
            self.dma("sp", self.Vc.t[tok, :], vcb_.t[:], [vcb_], [], vcb_)
            kr = pf.t[:, 384:416].rearrange("p (o c) -> p o c", o=1)
            cb1 = tb_.t[:, 0:16].unsqueeze(1)
            sn1 = tb_.t[:, 16:32].unsqueeze(1)
            self.rope_apply(kr[:, :, 0:16], kr[:, :, 16:32], cb1, sn1, krf.t[:, :, 0:16], krf.t[:, :, 16:32], rt, [128, 1, 16], [pf], [tb_], krf)
            self.pool(lambda e: e.tensor_copy(out=ko[:, :, 64:96], in_=krf.t[:].to_broadcast([128, 8, 32])), [krf], [kcb])
            self.dve(lambda e: e.tensor_tensor(out=sq8.t[:], in0=pf.t[:, 416:1056], in1=pf.t[:, 416:1056], op=ALU.mult), [pf], [sq8])
            self.dve(lambda e: e.tensor_reduce(out=ss10.t[:], in_=sq8.t[:].rearrange("p (g c) -> p g c", c=64), axis=AX.X, op=ALU.add), [sq8], [ss10])
            self.dve(lambda e: e.tensor_scalar(out=r10.t[:], in0=ss10.t[:], scalar1=1.0 / 64, scalar2=EPS, op0=ALU.mult, op1=ALU.add), [ss10], [r10])
            self.act(lambda e: e.activation(out=r10.t[:], in_=r10.t[:], func=AF.Sqrt), [r10], [r10])
            self.dve(lambda e: e.reciprocal(out=r10.t[:], in_=r10.t[:]), [r10], [r10])
            self.dve(lambda e: e.tensor_tensor(out=dn.t[:].rearrange("p (g c) -> p g c", c=64),
                                               in0=pf.t[:, 416:1056].rearrange("p (g c) -> p g c", c=64),
                                               in1=r10.t[:].unsqueeze(2).to_broadcast([128, 10, 64]), op=ALU.mult), [pf, r10], [dn])
            self.pool(lambda e: e.tensor_tensor(out=dn.t[:, 0:512].rearrange("p (g c) -> p g c", c=64),
                                                in0=dn.t[:, 0:512].rearrange("p (g c) -> p g c", c=64),
                                                in1=gdq.t[:].unsqueeze(1).to_broadcast([128, 8, 64]), op=ALU.mult), [dn, gdq], [dn])
            self.pool(lambda e: e.tensor_tensor(out=dn.t[:, 512:640].rearrange("p (g c) -> p g c", c=64),
                                                in0=dn.t[:, 512:640].rearrange("p (g c) -> p g c", c=64),
                                                in1=gdk.t[:].unsqueeze(1).to_broadcast([128, 2, 64]), op=ALU.mult), [dn, gdk], [dn])
            for (g0, g1) in ((0, 8), (8, 10)):
                ng = g1 - g0
                dv_ = dn.t[:, g0 * 64:g1 * 64].rearrange("p (g c) -> p g c", c=64)
                do_ = dqb.t[:, g0 * 64:g1 * 64].rearrange("p (g c) -> p g c", c=64)
                for (tb0, d0) in ((32, 0), (64, 32)):
                    cbx = tb_.t[:, tb0:tb0 + 16].unsqueeze(1).to_broadcast([128, ng, 16])
                    snx = tb_.t[:, tb0 + 16:tb0 + 32].unsqueeze(1).to_broadcast([128, ng, 16])
                    self.rope_apply(dv_[:, :, d0:d0 + 16], dv_[:, :, d0 + 16:d0 + 32], cbx, snx,
                                    do_[:, :, d0:d0 + 16], do_[:, :, d0 + 16:d0 + 32], rt, [128, ng, 16], [dn], [tb_], dqb)
            self.pool(lambda e, dvb_=dvb_: e.tensor_copy(out=dvb_.t[:], in_=pf.t[:, 1056:1184]), [pf], [dvb_])
            self.dma("sp", self.Vd.t[tok, :], dvb_.t[:], [dvb_], [], dvb_)
            for h in range(8):
                self.pe(lambda e, h=h: e.transpose(out=T1.t[0:96, h * 128:(h + 1) * 128], in_=qcb.t[:, h * 96:(h + 1) * 96], identity=ident.t[:]),
                        [qcb, ident], [T1])
            self.act(lambda e, sqc=sqc, tsl=tsl: e.copy(out=sqc.t[:, :, tsl], in_=T1.t[0:96, :].rearrange("p (h n) -> p h n", n=128)), [T1], [sqc])
            for h in range(8):
                self.pe(lambda e, h=h: e.transpose(out=T0.t[0:96, h * 128:(h + 1) * 128], in_=kcb.t[:, h * 96:(h + 1) * 96], identity=ident.t[:]),
                        [kcb, ident], [T0])
            self.act(lambda e, skc=skc, tsl=tsl: e.copy(out=skc.t[:, :, tsl], in_=T0.t[0:96, :].rearrange("p (h n) -> p h n", n=128)), [T0], [skc])
            for j in range(5):
                self.pe(lambda e, j=j: e.transpose(out=T1.t[:, j * 128:(j + 1) * 128], in_=dqb.t[:, j * 128:(j + 1) * 128], identity=ident.t[:]),
                        [dqb, ident], [T1])
            self.act(lambda e, sqd=sqd, tsl=tsl: e.copy(out=sqd.t[:, :, tsl], in_=T1.t[:, 0:512].rearrange("p (h n) -> p h n", n=128)), [T1], [sqd])
            self.act(lambda e, skd=skd, tsl=tsl: e.copy(out=skd.t[:, tsl], in_=T1.t[:, 512:640]), [T1], [skd])
            if tt == 3:
                gtok = slice(grp * 512, (grp + 1) * 512)
                self.dma("sp", self.QcT.t.rearrange("(h p) t -> p h t", p=96)[:, :, gtok], sqc.t[:], [sqc], [], sqc)
                self.dma("sp", self.KcT.t.rearrange("(h p) t -> p h t", p=96)[:, :, gtok], skc.t[:], [skc], [], skc)
                self.dma("sp", self.QdT.t.rearrange("(j p) t -> p j t", p=128)[:, :, gtok], sqd.t[:], [sqd], [], sqd)
                self.dma("sp", self.KdT.t[:, gtok], skd.t[:], [skd], [], skd)

        for s_ in range(NT1 + 1):
            if s_ < NT1:
                stage1(s_)
            if s_ >= 1:
                stage2(s_ - 1)
        self.phase_end(ph)

    def layer1_B(self):
        I = self.inp
        ph = self.phase_begin()
        sb = lambda n, s, d: self.sb(ph, n, s, d)
        ps = lambda n, s, d: self.ps(ph, n, s, d)
        sel = sb("sel", [65, 64], F32)
        qT = [sb(f"qT{i}", [96, S_LEN], BF16) for i in range(2)]
        kT = [sb(f"kT{i}", [96, S_LEN], BF16) for i in range(2)]
        vA = [sb(f"vA{i}", [128, NT, 65], BF16) for i in range(2)]
        oTs = [sb(f"oTs{i}", [64, S_LEN], BF16) for i in range(2)]
        osb = [sb(f"osb{i}", [65, 512], F32) for i in range(2)]
        rz = [sb(f"rz{i}", [64, 512], F32) for i in range(2)]
        Eb = [sb(f"Eb{i}", [128, 512], BF16) for i in range(6)]
        Sp = [ps(f"Sp{i}", [128, 512], F32) for i in range(5)]
        Op = [ps(f"Op{i}", [65, 512], F32) for i in range(2)]
        bc = [ps(f"bc{i}", [64, 512], F32) for i in range(1)]
        self.dma("sp", sel.t[:], I["c_sel"].t[:, :], [], [sel], sel)
        for i in range(2):
            self.pool(lambda e, i=i: e.memset(vA[i].t[:, :, 64:65], 1.0), [], [vA[i]])
        import os
        NH = int(os.environ.get('NH1', 16))
        NQC = int(os.environ.get('NQC1', S_LEN // 512))
        LA = 3
        kvi = -1
        prev_kv = None
        gi = 0
        oc_i = 0
        pending = []
        for h in range(NH):
            if h < 8:
                dk, scale = 96, 96.0 ** -0.5
                qsrc = self.QcT.t[h * 96:(h + 1) * 96, :]
                ksrc = self.KcT.t[h * 96:(h + 1) * 96, :]
                vsrc = self.Vc.t[:, h * 64:(h + 1) * 64]
                kvkey = ("c", h)
            else:
                hd = h - 8
                kvh = hd // 4
                dk, scale = 64, 0.125
                qsrc = self.QdT.t[hd * 64:(hd + 1) * 64, :]
                ksrc = self.KdT.t[kvh * 64:(kvh + 1) * 64, :]
                vsrc = self.Vd.t[:, kvh * 64:(kvh + 1) * 64]
                kvkey = ("d", kvh)
            q_ = qT[h % 2]
            self.dma("sp", q_.t[0:dk, :], qsrc, [], [q_], q_)
            if kvkey != prev_kv:
                kvi += 1
                prev_kv = kvkey
                k_, v_ = kT[kvi % 2], vA[kvi % 2]
                self.dma("sp", k_.t[0:dk, :], ksrc, [], [k_], k_)
                vv = vsrc.rearrange("(m a) c -> a m c", a=128)
                for m0 in range(0, NT, 16):
                    self.dma("pool", v_.t[:, m0:m0 + 16, 0:64], vv[:, m0:m0 + 16, :], [], [v_], v_)
            o_ = oTs[h % 2]
            iters = [(qc, m) for qc in range(NQC) for m in range(NT)]
            n_it = len(iters)
            sbuf_of = {}

            def emit_qk(idx):
                nonlocal gi
                qc, m = iters[idx]
                S_ = Sp[gi % 5]
                E_ = Eb[gi % 6]
                gi += 1
                sbuf_of[idx] = (S_, E_)
                self.pe(lambda e, S_=S_, m=m, qc=qc, k_=k_, q_=q_, dk=dk: e.matmul(
                    S_.t[:], lhsT=k_.t[0:dk, m * 128:(m + 1) * 128], rhs=q_.t[0:dk, qc * 512:(qc + 1) * 512], start=True, stop=True),
                    [k_, q_], [S_])
                self.act(lambda e, S_=S_, E_=E_, scale=scale: e.activation(out=E_.t[:], in_=S_.t[:], func=AF.Exp, scale=scale), [S_], [E_])

            def flush_pending(force=False):
                keep = []
                for item in pending:
                    item[0] -= 1
                    if item[0] <= 0 or force:
                        item[1]()
                    else:
                        keep.append(item)
                pending[:] = keep

            for idx in range(min(LA, n_it)):
                emit_qk(idx)
            for idx in range(n_it):
                if idx + LA < n_it:
                    emit_qk(idx + LA)
                qc, m = iters[idx]
                if m == 0:
                    O_ = Op[oc_i % 2]
                    ob_, rz_, bc_ = osb[oc_i % 2], rz[oc_i % 2], bc[0]
                    oc_i += 1
                S_, E_ = sbuf_of.pop(idx)
                self.pe(lambda e, O_=O_, v_=v_, m=m, E_=E_: e.matmul(O_.t[:], lhsT=v_.t[:, m, :], rhs=E_.t[:], start=(m == 0), stop=(m == NT - 1)),
                        [v_, E_], [O_])
                flush_pending()
                if m == NT - 1:
                    self.dve(lambda e, O_=O_, ob_=ob_: e.tensor_copy(out=ob_.t[:], in_=O_.t[:]), [O_], [ob_])

                    def norm(ob_=ob_, rz_=rz_, bc_=bc_, qc=qc, o_=o_):
                        self.pe(lambda e: e.matmul(bc_.t[:], lhsT=sel.t[:], rhs=ob_.t[:], start=True, stop=True), [sel, ob_], [bc_])
                        self.dve(lambda e: e.reciprocal(out=rz_.t[:], in_=bc_.t[:]), [bc_], [rz_])
                        self.dve(lambda e: e.tensor_tensor(out=o_.t[:, qc * 512:(qc + 1) * 512], in0=ob_.t[0:64, :], in1=rz_.t[:], op=ALU.mult),
                                 [ob_, rz_], [o_])
                    pending.append([4, norm])
            flush_pending(force=True)
            self.dma("sp", self.catT.t[h * 64:(h + 1) * 64, :], o_.t[:], [o_], [], o_)
        self.phase_end(ph)


def prep_inputs(inputs, b):
    f = lambda a: np.ascontiguousarray(np.asarray(a, dtype=np.float32))
    m = {
        "x": f(inputs["x"][b]), "norm_mix": f(inputs["norm_mix"]), "norm_ffn": f(inputs["norm_ffn"]),
        "even_w_in": f(inputs["even_w_in"][0]), "even_gmlp_norm": f(inputs["even_gmlp_norm"][0]).reshape(1, 256),
        "even_w_spatial": f(inputs["even_w_spatial"][0]), "even_b_spatial": f(inputs["even_b_spatial"][0]),
        "even_w_out": f(inputs["even_w_out"][0]), "odd_w_in": f(inputs["odd_w_in"][0]),
        "odd_cq_norm": f(inputs["odd_cq_norm"]), "odd_w_cq_up": f(inputs["odd_w_cq_up"][0]),
        "odd_ckv_norm": f(inputs["odd_ckv_norm"]), "odd_w_ckv_up": f(inputs["odd_w_ckv_up"][0]),
        "odd_dq_norm": f(inputs["odd_dq_norm"]), "odd_dk_norm": f(inputs["odd_dk_norm"]),
        "odd_w_out": f(inputs["odd_w_out"][0]), "moe_w_router": f(inputs["moe_w_router"]),
        "moe_w_gate": f(inputs["moe_w_gate"]), "moe_w_up": f(inputs["moe_w_up"]), "moe_w_down": f(inputs["moe_w_down"]),
        "final_norm": f(inputs["final_norm"]).reshape(1, D),
    }
    return m


def kernel(**inputs):
    nb = inputs["x"].shape[0]
    nc = Builder().build()
    consts = make_consts()
    in_maps = []
    for b in range(nb):
        m = prep_inputs(inputs, b)
        m.update(consts)
        in_maps.append(m)
    res = run_bass_kernel_spmd(nc, in_maps, core_ids=list(range(nb)))
    return np.stack([np.asarray(r["y"]) for r in res.results], axis=0).astype(np.float32)
```

```python
import numpy as np
import ml_dtypes
import concourse.bass as bass
import concourse.mybir as mybir
from concourse.bass_utils import run_bass_kernel_spmd
from contextlib import ExitStack

F32 = mybir.dt.float32
BF16 = mybir.dt.bfloat16
ALU = mybir.AluOpType
AF = mybir.ActivationFunctionType
AX = mybir.AxisListType

S_LEN = 8192
D = 1024
PAD = 1024
NT = S_LEN // 128
CH = 16000
EPS = 1e-6
NEXP = 16
CAP = 2 * S_LEN // NEXP
FUSE_WAIT = True


class Res:
    __slots__ = ("name", "writers", "readers", "stream")

    def __init__(self, name):
        self.name = name
        self.writers = {}
        self.readers = {}
        self.stream = None


class Stream:
    __slots__ = ("sem", "count", "key", "kind")

    def __init__(self, sem, key):
        self.sem = sem
        self.count = 0
        self.key = key


class Buf:
    __slots__ = ("t", "r", "track")

    def __init__(self, t, name, track=True):
        self.t = t
        self.r = Res(name)
        self.track = track


class Sched:
    ENG = ("pe", "act", "dve", "pool", "sp")
    HANDLE = {"pe": "tensor", "act": "scalar", "dve": "vector", "pool": "gpsimd", "sp": "sync"}

    def __init__(self, nc, stack):
        self.nc = nc
        self.stack = stack
        self.prog = {e: [] for e in self.ENG}
        self.cnt = {e: 0 for e in self.ENG}
        self.seen = {e: {} for e in self.ENG}
        self.esems = {e: [] for e in self.ENG}
        self.streams = []
        self.free_sems = {"sw": [], "hw": []}
        self.live = []
        self.bg = set()
        self.nsem = 0
        self.total = 0

    def _new_sem(self, name):
        self.nsem += 1
        return self.stack.enter_context(self.nc.semaphore(name))

    def _esem(self, e, chunk):
        lst = self.esems[e]
        while len(lst) <= chunk:
            lst.append(self._new_sem(f"s_{e}_{len(lst)}"))
        return lst[chunk]

    def stream_of(self, res, q="sp"):
        kind = "sw" if q == "pool" else "hw"
        if res.stream is None:
            pool_ = self.free_sems[kind]
            if pool_:
                pool_.sort(key=lambda x: x[1])
                sem, base = pool_.pop(0)
            else:
                sem, base = self._new_sem(f"d{len(self.streams)}"), 0
            res.stream = Stream(sem, ("d", len(self.streams)))
            res.stream.count = base
            res.stream.kind = kind
            self.streams.append(res.stream)
            self.live.append(res)
        assert res.stream.kind == kind, f"stream of {res.name} used from both DGE kinds"
        return res.stream

    def mark_bg(self, res):
        self.bg.add(res.stream.key)

    def bg_done(self):
        self.bg = set()

    def release_streams(self):
        keep = []
        for res in self.live:
            st = res.stream
            if st.key in self.bg:
                keep.append(res)
                continue
            self.free_sems[st.kind].append((st.sem, st.count))
            st.count = 0
            res.stream = None
        self.live = keep

    def _wait(self, e, key, val, payload):
        seen = self.seen[e]
        if seen.get(key, 0) >= val:
            return
        seen[key] = val
        self.prog[e].append(("wait", payload))

    def _wait_tok(self, e, key, val):
        if key[0] == "e":
            eng = key[1]
            if eng == e and e in ("pe", "sp"):
                return
            chunk, v = (val - 1) // CH, (val - 1) % CH + 1
            self._wait(e, key, val, (self._esem(eng, chunk), v))
        else:
            st = self.streams[key[1]]
            self._wait(e, key, val, (st.sem, val))

    def op(self, e, fn, reads=(), writes=(), stream=None):
        for r in reads:
            for k, v in r.writers.items():
                self._wait_tok(e, k, v)
        for w in writes:
            for k, v in w.writers.items():
                self._wait_tok(e, k, v)
            for k, v in w.readers.items():
                self._wait_tok(e, k, v)
        if stream is not None:
            st = self.stream_of(stream, e)
            st.count += 16
            key, val = st.key, st.count
            self.prog[e].append(("dma", fn, st.sem))
        else:
            self.cnt[e] += 1
            n = self.cnt[e]
            key, val = ("e", e), n
            self.prog[e].append(("ins", fn, self._esem(e, (n - 1) // CH)))
        for r in reads:
            r.readers[key] = val
        for w in writes:
            w.writers = {key: val}
            w.readers = {}

    def barrier(self, engines=None):
        for e in (engines or self.ENG):
            for st in self.streams:
                if st.count and st.key not in self.bg:
                    self._wait(e, st.key, st.count, (st.sem, st.count))
            for eng in self.ENG:
                if eng != e and self.cnt[eng]:
                    self._wait_tok(e, ("e", eng), self.cnt[eng])

    def emit(self):
        nc = self.nc
        import os
        if os.environ.get('DUMP'):
            for e in self.ENG:
                print('ENGINE', e)
                for it in self.prog[e][:int(os.environ['DUMP'])]:
                    if it[0] == 'wait':
                        print('   wait', it[1][0].name if hasattr(it[1][0], 'name') else it[1][0], it[1][1])
                    else:
                        print('  ', it[0], (it[2].name if hasattr(it[2], 'name') else it[2]), it[1].__code__.co_firstlineno)
        with nc.Block() as block:
            def mk(e):
                items = self.prog[e]

                def body(eng):
                    n = len(items)
                    for i, it in enumerate(items):
                        if it[0] == "wait":
                            if FUSE_WAIT and i + 1 < n and items[i + 1][0] != "wait":
                                continue
                            eng.wait_ge(it[1][0], it[1][1])
                        else:
                            ins = it[1](eng)
                            if FUSE_WAIT and i > 0 and items[i - 1][0] == "wait":
                                ins._wait_ge(items[i - 1][1][0], items[i - 1][1][1])
                            ins.then_inc(it[2], 16 if it[0] == "dma" else 1)
                return body

            for e in self.ENG:
                if self.prog[e]:
                    getattr(block, self.HANDLE[e])(mk(e))
        for e in self.ENG:
            self.total += len(self.prog[e])
            self.prog[e] = []


def _rope_table(pos, r, theta):
    half = r // 2
    inv = np.power(np.float32(theta), -np.arange(half, dtype=np.float32) * np.float32(2.0 / r)).astype(np.float32)
    ang = pos.astype(np.float32)[:, None] * inv[None, :]
    return np.concatenate([np.cos(ang), np.sin(ang)], axis=1).astype(np.float32)


def make_consts():
    pos = np.arange(S_LEN)
    c = {}
    c["c_ident_bf"] = np.eye(128, dtype=np.float32).astype(ml_dtypes.bfloat16)
    c["c_ident_f"] = np.eye(128, dtype=np.float32)
    sel = np.zeros((65, 64), np.float32)
    sel[64, :] = 1.0
    c["c_sel"] = sel
    c["c_rope0"] = _rope_table(pos, 16, 500000.0)
    c["c_ropec"] = _rope_table(pos, 32, 500000.0)
    c["c_roperow"] = _rope_table(pos // 64, 32, 10000.0)
    c["c_ropecol"] = _rope_table(pos % 64, 32, 10000.0)
    a = np.arange(128)[:, None]
    q = np.arange(128)[None, :]
    A = (a >= q).astype(np.float32)
    B = (a <= q).astype(np.float32)
    Ae = A * (a >= 64)
    Be = B * (a < 64)
    m = np.zeros((3, 128, 512), np.float32)
    m[0] = np.concatenate([A, B, A, B], 1)
    m[1] = np.concatenate([Ae, B, A, B], 1)
    m[2] = np.concatenate([A, B, A, Be], 1)
    c["c_mask"] = np.ascontiguousarray(m.transpose(1, 0, 2)).astype(ml_dtypes.bfloat16)
    return c


CONST_SPECS = {
    "c_ident_bf": ([128, 128], BF16), "c_ident_f": ([128, 128], F32), "c_sel": ([65, 64], F32),
    "c_rope0": ([S_LEN, 16], F32), "c_ropec": ([S_LEN, 32], F32), "c_roperow": ([S_LEN, 32], F32),
    "c_ropecol": ([S_LEN, 32], F32), "c_mask": ([128, 3, 512], BF16),
}

INPUT_SPECS = {
    "x": [S_LEN, D], "norm_mix": [2, D], "norm_ffn": [2, D], "even_w_in": [D, 2816],
    "even_gmlp_norm": [1, 256], "even_w_spatial": [4, 128, 128], "even_b_spatial": [4, 128],
    "even_w_out": [D, D], "odd_w_in": [D, 1184], "odd_cq_norm": [1, 256], "odd_w_cq_up": [256, 768],
    "odd_ckv_norm": [1, 128], "odd_w_ckv_up": [128, 1024], "odd_dq_norm": [1, 64], "odd_dk_norm": [1, 64],
    "odd_w_out": [D, D], "moe_w_router": [2, D, 16], "moe_w_gate": [2, 16, D, 512],
    "moe_w_up": [2, 16, D, 512], "moe_w_down": [2, 16, 512, D], "final_norm": [1, D],
}


class Builder:
    def __init__(self, dbg=(), upto="all"):
        self.nc = bass.Bass("TRN2", target_bir_lowering=False)
        self.dbg = set(dbg)
        self.upto = upto
        self.root = ExitStack()
        self.S = Sched(self.nc, self.root)
        self.inp = {}
        for k, shp in INPUT_SPECS.items():
            self.inp[k] = Buf(self.nc.dram_tensor(k, shp, F32, kind="ExternalInput").ap(), k, False)
        for k, (shp, dt) in CONST_SPECS.items():
            self.inp[k] = Buf(self.nc.dram_tensor(k, shp, dt, kind="ExternalInput").ap(), k, False)
        self.out = Buf(self.nc.dram_tensor("y", [S_LEN, D], F32, kind="ExternalOutput").ap(), "y", False)
        self.scr = {}
        self.phn = 0
        self.affall = self.sb(self.root, "affall", [128, NT, NEXP], F32)
        self.gw = self.sb(self.root, "gw", [128, NT, NEXP], F32)
        self.xa = self.dram("xa", [S_LEN, D], F32)
        self.xb = self.dram("xb", [S_LEN, D], F32)
        self.hTd = self.dram("hTd", [D, S_LEN], BF16)

    def dram(self, name, shape, dt):
        kind = "ExternalOutput" if name in self.dbg else "Internal"
        b = Buf(self.nc.dram_tensor(name, shape, dt, kind=kind).ap(), name, False)
        self.scr[name] = b
        return b

    def sb(self, ph, name, shape, dt):
        name = f"p{self.phn}_{name}"
        return Buf(ph.enter_context(self.nc.sbuf_tensor(name, shape, dt)), name)

    def ps(self, ph, name, shape, dt):
        name = f"p{self.phn}_{name}"
        return Buf(ph.enter_context(self.nc.psum_tensor(name, shape, dt)), name)

    def _op(self, e, fn, r, w):
        self.S.op(e, fn, reads=[b.r for b in r], writes=[b.r for b in w])

    def pe(self, fn, r, w):
        self._op("pe", fn, r, w)

    def act(self, fn, r, w):
        self._op("act", fn, r, w)

    def dve(self, fn, r, w):
        self._op("dve", fn, r, w)

    def pool(self, fn, r, w):
        self._op("pool", fn, r, w)

    def dma(self, q, out, in_, r, w, stream, **kw):
        self.S.op(q, lambda e: e.dma_start(out=out, in_=in_, **kw), reads=[b.r for b in r if b.track],
                  writes=[b.r for b in w if b.track], stream=stream.r)

    def phase_begin(self):
        self.phn += 1
        self.S.barrier()
        self.S.release_streams()
        return ExitStack()

    def phase_end(self, ph):
        self.S.emit()
        ph.close()

    def rmsnorm_rstd(self, src, srcbufs, junk, ssq, rstd, n, width=None):
        sc = float(n) ** -0.5
        self.pool(lambda e: e.memset(ssq.t[:, 0:1], 0.0), [], [ssq])
        self.act(lambda e: e.activation(out=junk, in_=src, func=AF.Square, scale=sc, accum_out=ssq.t[:, 0:1]),
                 srcbufs, [ssq])
        self.dve(lambda e: e.tensor_scalar_add(out=rstd.t[:, 0:1], in0=ssq.t[:, 0:1], scalar1=EPS), [ssq], [rstd])
        self.act(lambda e: e.activation(out=rstd.t[:, 0:1], in_=rstd.t[:, 0:1], func=AF.Sqrt), [rstd], [rstd])
        self.dve(lambda e: e.reciprocal(out=rstd.t[:, 0:1], in_=rstd.t[:, 0:1]), [rstd], [rstd])

    def build(self):
        with self.root:
            if self.upto in ("B1only", "B0only", "Eonly"):
                self.catT = self.dram("catT", [1024, S_LEN], BF16)
                if self.upto == "B1only":
                    self.QcT = self.dram("QcT", [768, S_LEN], BF16)
                    self.KcT = self.dram("KcT", [768, S_LEN], BF16)
                    self.Vc = self.dram("Vc", [S_LEN, 512], BF16)
                    self.QdT = self.dram("QdT", [512, S_LEN], BF16)
                    self.KdT = self.dram("KdT", [128, S_LEN], BF16)
                    self.Vd = self.dram("Vd", [S_LEN, 128], BF16)
                    self.layer1_B()
                elif self.upto == "B0only":
                    self.V0 = self.dram("V0", [PAD + S_LEN + PAD, 768], BF16)
                    self.QT0 = self.dram("QT0", [768, S_LEN], BF16)
                    self.KT0 = self.dram("KT0", [768, S_LEN], BF16)
                    self.layer0_B()
                else:
                    self.wg_bf = self.dram("wg_bf", [2, NEXP, D, 512], BF16)
                    self.wu_bf = self.dram("wu_bf", [2, NEXP, D, 512], BF16)
                    self.wd_bf = self.dram("wd_bf", [2, NEXP, 512, D], BF16)
                    self.phase_E(0, last=False)
                return self.finish()
            self.prologue()
            if self.upto == "pro":
                return self.finish()
            self.layer0_A()
            if self.upto == "A0":
                return self.finish()
            self.layer0_B()
            if self.upto == "B0":
                return self.finish()
            self.phase_C(0, self.inp["x"], self.inp["even_w_out"])
            if self.upto == "C0":
                return self.finish()
            self.phase_D()
            if self.upto == "D0":
                return self.finish()
            self.phase_E(0, last=False)
            if self.upto == "E0":
                return self.finish()
            self.layer1_A()
            if self.upto == "A1":
                return self.finish()
            self.layer1_B()
            if self.upto == "B1":
                return self.finish()
            self.phase_C(1, self.xb, self.inp["odd_w_out"])
            if self.upto == "C1":
                return self.finish()
            self.phase_D()
            self.phase_E(1, last=True)
            return self.finish()

    def finish(self):
        ph = self.phase_begin()
        self.phase_end(ph)
        return self.nc

    def prologue(self):
        self.wg_bf = self.dram("wg_bf", [2, NEXP, D, 512], BF16)
        self.wu_bf = self.dram("wu_bf", [2, NEXP, D, 512], BF16)
        self.wd_bf = self.dram("wd_bf", [2, NEXP, 512, D], BF16)

    def prologue_issue(self):
        for l in range(2):
            for e_ in range(NEXP):
                for src, dst in ((self.inp["moe_w_gate"], self.wg_bf), (self.inp["moe_w_up"], self.wu_bf),
                                 (self.inp["moe_w_down"], self.wd_bf)):
                    self.dma("pool", dst.t[l, e_], src.t[l, e_], [], [], dst)
        for dst in (self.wg_bf, self.wu_bf, self.wd_bf):
            self.S.mark_bg(dst.r)

    def layer0_A(self):
        nc = self.nc
        I = self.inp
        self.V0 = self.dram("V0", [PAD + S_LEN + PAD, 768], BF16)
        self.QT0 = self.dram("QT0", [768, S_LEN], BF16)
        self.KT0 = self.dram("KT0", [768, S_LEN], BF16)
        self.catT = self.dram("catT", [1024, S_LEN], BF16)
        ph = self.phase_begin()
        sb = lambda n, s, d: self.sb(ph, n, s, d)
        ps = lambda n, s, d: self.ps(ph, n, s, d)
        ident = sb("ident", [128, 128], BF16)
        win = sb("win", [128, 8, 2816], BF16)
        gmix = sb("gmix", [128, D], F32)
        gmn = sb("gmn", [128, 256], F32)
        wsf = sb("wsf", [128, 4, 128], F32)
        wsb = sb("wsb", [128, 4, 128], BF16)
        wsT = sb("wsT", [128, 4, 128], BF16)
        bsT = sb("bsT", [128, 4], F32)
        zero = sb("zero", [128, 768], BF16)
        xt = [sb(f"xt{i}", [128, D], F32) for i in range(2)]
        junk = sb("junk", [128, D], BF16)
        ssq = [sb(f"ssq{i}", [128, 1], F32) for i in range(2)]
        rstd = [sb(f"rstd{i}", [128, 1], F32) for i in range(2)]
        hn = [sb(f"hn{i}", [128, D], BF16) for i in range(2)]
        hT = [sb(f"hT{i}", [128, 8, 128], BF16) for i in range(2)]
        cs = [sb(f"cs{i}", [128, 16], F32) for i in range(2)]
        qk = [sb(f"qk{i}", [128, 1536], BF16) for i in range(2)]
        rt = [sb(f"rt{i}", [128, 24, 8], F32) for i in range(4)]
        vb = [sb(f"vb{i}", [128, 768], BF16) for i in range(2)]
        zgs = [sb(f"zg{i}", [128, 512], F32) for i in range(2)]
        qkfs = [sb(f"qkf{i}", [128, 1536], F32) for i in range(2)]
        sqv = sb("sqv", [128, 256], F32)
        ss4 = sb("ss4", [128, 4], F32)
        r4 = sb("r4", [128, 4], F32)
        vvn = sb("vvn", [128, 256], F32)
        vvb = sb("vvb", [128, 256], BF16)
        mxb = sb("mxb", [128, 256], F32)
        gout = sb("gout", [128, 256], BF16)
        stq = [sb(f"stq{i}", [128, 6, 512], BF16) for i in range(2)]
        stk = [sb(f"stk{i}", [128, 6, 512], BF16) for i in range(2)]
        stg = [sb(f"stg{i}", [64, 4, 512], BF16) for i in range(2)]
        T0 = ps("T0", [128, 1024], BF16)
        T1 = ps("T1", [128, 1024], BF16)
        pqk = ps("pqk", [128, 1536], F32)
        pvz = ps("pvz", [128, 1536], F32)

        self.dma("sp", ident.t[:], I["c_ident_bf"].t[:, :], [], [ident], ident)
        self.dma("pool", win.t[:], I["even_w_in"].t.rearrange("(c p) n -> p c n", p=128), [], [win], win)
        self.dma("sp", gmix.t[:], I["norm_mix"].t[0:1, :].partition_broadcast(128), [], [gmix], gmix)
        self.dma("sp", gmn.t[:], I["even_gmlp_norm"].t[0:1, :].partition_broadcast(128), [], [gmn], gmn)
        self.dma("sp", wsf.t[:], I["even_w_spatial"].t.rearrange("g p q -> p g q"), [], [wsf], wsf)
        self.dma("sp", bsT.t[:], I["even_b_spatial"].t.rearrange("g p -> p g"), [], [bsT], bsT,
                 allow_slow_non_contiguous=True)
        self.dve(lambda e: e.tensor_copy(out=wsb.t[:], in_=wsf.t[:]), [wsf], [wsb])
        for g in range(4):
            self.pe(lambda e, g=g: e.transpose(out=T0.t[:, g * 128:(g + 1) * 128], in_=wsb.t[:, g, :], identity=ident.t[:]),
                    [wsb, ident], [T0])
        self.act(lambda e: e.copy(out=wsT.t[:].rearrange("p g q -> p (g q)"), in_=T0.t[:, 0:512]), [T0], [wsT])
        self.pool(lambda e: e.memset(zero.t[:], 0.0), [], [zero])
        for side in range(2):
            for k in range(PAD // 128):
                r0 = (0 if side == 0 else PAD + S_LEN) + k * 128
                self.dma("sp", self.V0.t[r0:r0 + 128, :], zero.t[:], [zero], [self.V0], zero)

        import os
        NT0 = int(os.environ.get('NGRP', NT // 4)) * 4

        def stage1(ti):
            grp, tt = divmod(ti, 4)
            sq_, sk_, sg_ = stq[grp % 2], stk[grp % 2], stg[grp % 2]
            b = ti % 2
            x_, ssq_, rstd_, hn_, hT_, cs_, qk_, vb_, qkf, zg = xt[b], ssq[b], rstd[b], hn[b], hT[b], cs[b], qk[b], vb[b], qkfs[b], zgs[b]
            tok = slice(ti * 128, (ti + 1) * 128)
            self.dma("sp", x_.t[:], I["x"].t[tok, :], [I["x"]], [x_], x_)
            self.dma("sp", cs_.t[:], I["c_rope0"].t[tok, :], [], [cs_], cs_)
            self.rmsnorm_rstd(x_.t[:], [x_], junk.t[:], ssq_, rstd_, D)
            self.dve(lambda e, x_=x_, rstd_=rstd_, hn_=hn_: e.scalar_tensor_tensor(
                out=hn_.t[:], in0=x_.t[:], scalar=rstd_.t[:, 0:1], in1=gmix.t[:], op0=ALU.mult, op1=ALU.mult),
                [x_, rstd_, gmix], [hn_])
            for j in range(8):
                self.pe(lambda e, j=j, hn_=hn_: e.transpose(out=T0.t[:, j * 128:(j + 1) * 128],
                                                           in_=hn_.t[:, j * 128:(j + 1) * 128], identity=ident.t[:]),
                        [hn_, ident], [T0])
            self.act(lambda e, hT_=hT_: e.copy(out=hT_.t[:].rearrange("p c n -> p (c n)"), in_=T0.t[:]), [T0], [hT_])
            for bank in range(3):
                for j in range(8):
                    self.pe(lambda e, j=j, bank=bank, hT_=hT_: e.matmul(
                        pqk.t[:, bank * 512:(bank + 1) * 512], lhsT=hT_.t[:, j, :],
                        rhs=win.t[:, j, bank * 512:(bank + 1) * 512], start=(j == 0), stop=(j == 7)),
                        [hT_, win], [pqk])
            for (o0, n_, c0) in ((0, 512, 1536), (512, 256, 2048), (1024, 512, 2304)):
                for j in range(8):
                    self.pe(lambda e, j=j, o0=o0, n_=n_, c0=c0, hT_=hT_: e.matmul(
                        pvz.t[:, o0:o0 + n_], lhsT=hT_.t[:, j, :], rhs=win.t[:, j, c0:c0 + n_],
                        start=(j == 0), stop=(j == 7)), [hT_, win], [pvz])
            self.act(lambda e: e.copy(out=qkf.t[:], in_=pqk.t[:]), [pqk], [qkf])
            self.act(lambda e, vb_=vb_: e.copy(out=vb_.t[:], in_=pvz.t[:, 0:768]), [pvz], [vb_])
            self.dma("sp", self.V0.t[PAD + ti * 128:PAD + (ti + 1) * 128, :], vb_.t[:], [vb_], [self.V0], vb_)
            self.act(lambda e: e.activation(out=zg.t[:], in_=pvz.t[:, 1024:1536], func=AF.Gelu_apprx_tanh), [pvz], [zg])

        def stage2(ti):
            grp, tt = divmod(ti, 4)
            sq_, sk_, sg_ = stq[grp % 2], stk[grp % 2], stg[grp % 2]
            b = ti % 2
            x_, ssq_, rstd_, hn_, hT_, cs_, qk_, vb_, qkf, zg = xt[b], ssq[b], rstd[b], hn[b], hT[b], cs[b], qk[b], vb[b], qkfs[b], zgs[b]
            tok = slice(ti * 128, (ti + 1) * 128)
            self.pool(lambda e, qk_=qk_: e.tensor_copy(out=qk_.t[:], in_=qkf.t[:]), [qkf], [qk_])
            pv = qkf.t[:].rearrange("p (h c) -> p h c", c=64)
            qv = qk_.t[:].rearrange("p (h c) -> p h c", c=64)
            cosb = cs_.t[:, 0:8].unsqueeze(1).to_broadcast([128, 24, 8])
            sinb = cs_.t[:, 8:16].unsqueeze(1).to_broadcast([128, 24, 8])
            x1 = pv[:, :, 0:8]
            x2 = pv[:, :, 8:16]
            self.dve(lambda e, x1=x1, cosb=cosb: e.tensor_tensor(out=rt[0].t[:], in0=x1, in1=cosb, op=ALU.mult), [qkf, cs_], [rt[0]])
            self.dve(lambda e, x2=x2, sinb=sinb: e.tensor_tensor(out=rt[1].t[:], in0=x2, in1=sinb, op=ALU.mult), [qkf, cs_], [rt[1]])
            self.dve(lambda e, x1=x1, sinb=sinb: e.tensor_tensor(out=rt[2].t[:], in0=x1, in1=sinb, op=ALU.mult), [qkf, cs_], [rt[2]])
            self.dve(lambda e, x2=x2, cosb=cosb: e.tensor_tensor(out=rt[3].t[:], in0=x2, in1=cosb, op=ALU.mult), [qkf, cs_], [rt[3]])
            self.pool(lambda e, qv=qv: e.tensor_tensor(out=qv[:, :, 0:8], in0=rt[0].t[:], in1=rt[1].t[:], op=ALU.subtract),
                      [rt[0], rt[1]], [qk_])
            self.pool(lambda e, qv=qv: e.tensor_tensor(out=qv[:, :, 8:16], in0=rt[2].t[:], in1=rt[3].t[:], op=ALU.add),
                      [rt[2], rt[3]], [qk_])
            self.dve(lambda e: e.tensor_tensor(out=sqv.t[:], in0=zg.t[:, 256:512], in1=zg.t[:, 256:512], op=ALU.mult), [zg], [sqv])
            self.dve(lambda e: e.tensor_reduce(out=ss4.t[:], in_=sqv.t[:].rearrange("p (g c) -> p g c", c=64), axis=AX.X, op=ALU.add), [sqv], [ss4])
            self.dve(lambda e: e.tensor_scalar(out=r4.t[:], in0=ss4.t[:], scalar1=1.0 / 64, scalar2=EPS, op0=ALU.mult, op1=ALU.add), [ss4], [r4])
            self.act(lambda e: e.activation(out=r4.t[:], in_=r4.t[:], func=AF.Sqrt), [r4], [r4])
            self.dve(lambda e: e.reciprocal(out=r4.t[:], in_=r4.t[:]), [r4], [r4])
            self.dve(lambda e: e.tensor_tensor(out=vvn.t[:].rearrange("p (g c) -> p g c", c=64),
                                               in0=zg.t[:, 256:512].rearrange("p (g c) -> p g c", c=64),
                                               in1=r4.t[:].unsqueeze(2).to_broadcast([128, 4, 64]), op=ALU.mult), [zg, r4], [vvn])
            self.pool(lambda e: e.tensor_tensor(out=vvb.t[:], in0=vvn.t[:], in1=gmn.t[:], op=ALU.mult), [vvn, gmn], [vvb])
            for g in range(4):
                self.pe(lambda e, g=g: e.matmul(pvz.t[:, 768 + g * 64:768 + (g + 1) * 64], lhsT=wsT.t[:, g, :],
                                                rhs=vvb.t[:, g * 64:(g + 1) * 64], start=True, stop=True), [wsT, vvb], [pvz])
            self.act(lambda e: e.copy(out=sqv.t[:], in_=pvz.t[:, 768:1024]), [pvz], [sqv])
            self.dve(lambda e: e.tensor_tensor(out=mxb.t[:].rearrange("p (g c) -> p g c", c=64),
                                               in0=sqv.t[:].rearrange("p (g c) -> p g c", c=64),
                                               in1=bsT.t[:].unsqueeze(2).to_broadcast([128, 4, 64]), op=ALU.add), [sqv, bsT], [mxb])
            self.pool(lambda e: e.tensor_tensor(out=gout.t[:], in0=mxb.t[:], in1=zg.t[:, 0:256], op=ALU.mult), [mxb, zg], [gout])
            for half, st_ in ((0, sq_), (1, sk_)):
                for j in range(6):
                    c0 = half * 768 + j * 128
                    self.pe(lambda e, j=j, c0=c0, qk_=qk_: e.transpose(out=T1.t[:, j * 128:(j + 1) * 128], in_=qk_.t[:, c0:c0 + 128],
                                                                     identity=ident.t[:]), [qk_, ident], [T1])
                self.act(lambda e, st_=st_, tt=tt: e.copy(out=st_.t[:, :, tt * 128:(tt + 1) * 128],
                                                        in_=T1.t[:, 0:768].rearrange("p (j n) -> p j n", n=128)), [T1], [st_])
            for g in range(4):
                self.pe(lambda e, g=g: e.transpose(out=T1.t[0:64, g * 128:(g + 1) * 128], in_=gout.t[:, g * 64:(g + 1) * 64],
                                                   identity=ident.t[:]), [gout, ident], [T1])
            self.dve(lambda e, sg_=sg_, tt=tt: e.tensor_copy(out=sg_.t[:, :, tt * 128:(tt + 1) * 128],
                                                           in_=T1.t[0:64, 0:512].rearrange("p (j n) -> p j n", n=128)), [T1], [sg_])
            if tt == 3:
                gtok = slice(grp * 512, (grp + 1) * 512)
                self.dma("sp", self.QT0.t.rearrange("(j p) t -> p j t", p=128)[:, :, gtok], sq_.t[:], [sq_], [self.QT0], sq_)
                self.dma("sp", self.KT0.t.rearrange("(j p) t -> p j t", p=128)[:, :, gtok], sk_.t[:], [sk_], [self.KT0], sk_)
                self.dma("sp", self.catT.t[768:1024, :].rearrange("(j p) t -> p j t", p=64)[:, :, gtok], sg_.t[:], [sg_], [self.catT], sg_)

        for s_ in range(NT0 + 1):
            if s_ < NT0:
                stage1(s_)
            if s_ >= 1:
                stage2(s_ - 1)
        self.phase_end(ph)

    def layer0_B(self):
        I = self.inp
        ph = self.phase_begin()
        sb = lambda n, s, d: self.sb(ph, n, s, d)
        ps = lambda n, s, d: self.ps(ph, n, s, d)
        sel = sb("sel", [65, 64], F32)
        mask = sb("mask", [128, 3, 512], BF16)
        qT = [sb(f"qT{i}", [64, S_LEN], BF16) for i in range(2)]
        kT = [sb(f"kT{i}", [64, PAD + S_LEN + PAD], BF16) for i in range(2)]
        vA = [sb(f"vA{i}", [128, 80, 65], BF16) for i in range(2)]
        acc = sb("acc", [65, S_LEN], F32)
        aTs = sb("aTs", [64, S_LEN], BF16)
        rz = [sb(f"rz{i}", [64, 512], F32) for i in range(2)]
        Eb = [sb(f"Eb{i}", [128, 512], BF16) for i in range(4)]
        Pb = [sb(f"Pb{i}", [128, 512], BF16) for i in range(4)]
        Sp = [ps(f"Sp{i}", [128, 512], F32) for i in range(4)]
        Op = [ps(f"Op{i}", [65, 512], F32) for i in range(2)]
        bc = [ps(f"bc{i}", [64, 512], F32) for i in range(2)]
        self.dma("sp", sel.t[:], I["c_sel"].t[:, :], [], [sel], sel)
        self.dma("sp", mask.t[:], I["c_mask"].t[:, :, :], [], [mask], mask)
        for i in range(2):
            self.pool(lambda e, i=i: e.memset(kT[i].t[:, 0:PAD], 0.0), [], [kT[i]])
            self.pool(lambda e, i=i: e.memset(kT[i].t[:, PAD + S_LEN:], 0.0), [], [kT[i]])
            self.pool(lambda e, i=i: e.memset(vA[i].t[:, :, 64:65], 1.0), [], [vA[i]])
        if hasattr(self, "wg_bf") and self.upto != "B0only":
            self.prologue_issue()
        import os
        hp = [(h, pi, d) for h in range(12) for pi, d in enumerate((1, 4, 16))]

        def load_qk(h):
            q_, k_ = qT[h % 2], kT[h % 2]
            self.dma("sp", q_.t[:], self.QT0.t[h * 64:(h + 1) * 64, :], [], [q_], q_)
            self.dma("sp", k_.t[:, PAD:PAD + S_LEN], self.KT0.t[h * 64:(h + 1) * 64, :], [], [k_], k_)

        def load_v(k):
            h, pi, d = hp[k]
            ntile = S_LEN // d // 128 + 1
            v_ = vA[k % 2]
            r0 = PAD - 64 * d
            rows = ntile * 128 * d
            src = self.V0.t[r0:r0 + rows, h * 64:(h + 1) * 64].rearrange("(m a i) c -> a i m c", a=128, i=d)
            dstv = v_.t[:, 0:d * ntile, 0:64].rearrange("a (i m) c -> a i m c", i=d)
            for i in range(d):
                for m0 in range(0, ntile, 16):
                    m1 = min(ntile, m0 + 16)
                    self.dma("sp", dstv[:, i, m0:m1, :], src[:, i, m0:m1, :], [], [v_], v_)

        load_qk(0)
        load_v(0)
        cnt = {"it": 0, "og": 0}
        LA = 2
        for k, (h, pi, d) in enumerate(hp):
            q_, k_, v_ = qT[h % 2], kT[h % 2], vA[k % 2]
            if pi == 0 and h + 1 < 12:
                load_qk(h + 1)
            if k + 1 < len(hp):
                load_v(k + 1)
            nb = S_LEN // d // 128
            ntile = nb + 1
            pairs = [(i, gq, pr) for i in range(d) for gq in range(nb // 4) for pr in range(2)]
            bufs = {}
            obuf = {}

            def emit_qk(idx):
                i, gq, pr = pairs[idx]
                n0 = gq * 4
                b4 = cnt["it"] % 4
                cnt["it"] += 1
                S_, E_, P_ = Sp[b4], Eb[b4], Pb[b4]
                bufs[idx] = P_
                nA = n0 + 2 * pr
                for (m, qb, nq, c0) in ((nA, nA, 128, 0), (nA + 1, nA, 256, 128), (nA + 2, nA + 1, 128, 384)):
                    k0 = PAD - 64 * d + i + 128 * d * m
                    q0 = 128 * d * qb + i
                    self.pe(lambda e, S_=S_, c0=c0, k0=k0, q0=q0, nq=nq, d=d, k_=k_, q_=q_: e.matmul(
                        S_.t[:, c0:c0 + nq], lhsT=k_.t[:, k0:k0 + 127 * d + 1:d], rhs=q_.t[:, q0:q0 + (nq - 1) * d + 1:d],
                        start=True, stop=True), [k_, q_], [S_])
                self.act(lambda e, S_=S_, E_=E_: e.activation(out=E_.t[:], in_=S_.t[:], func=AF.Exp, scale=0.125), [S_], [E_])
                first = (nA == 0)
                last = (nA + 2 == nb)
                assert not (first and last)
                mi = 1 if first else (2 if last else 0)
                self.dve(lambda e, E_=E_, P_=P_, mi=mi: e.tensor_tensor(out=P_.t[:], in0=E_.t[:], in1=mask.t[:, mi, :], op=ALU.mult),
                         [E_, mask], [P_])

            def emit_pv(idx):
                i, gq, pr = pairs[idx]
                n0 = gq * 4
                P_ = bufs.pop(idx)
                if pr == 0:
                    obuf[(i, gq)] = Op[cnt["og"] % 2]
                    cnt["og"] += 1
                O_ = obuf[(i, gq)]
                for blk in range(2):
                    n = n0 + 2 * pr + blk
                    oc = (2 * pr + blk) * 128
                    for ab in range(2):
                        m = n + ab
                        c0 = (blk * 2 + ab) * 128
                        self.pe(lambda e, O_=O_, oc=oc, c0=c0, P_=P_, ti=i * ntile + m, ab=ab, v_=v_: e.matmul(
                            O_.t[:, oc:oc + 128], lhsT=v_.t[:, ti, :], rhs=P_.t[:, c0:c0 + 128],
                            start=(ab == 0), stop=(ab == 1)), [v_, P_], [O_])
                if pr == 1:
                    del obuf[(i, gq)]
                    a0 = 128 * d * n0 + i
                    av = acc.t[:, a0:a0 + 511 * d + 1:d]
                    if pi == 0:
                        self.dve(lambda e, av=av, O_=O_: e.tensor_copy(out=av, in_=O_.t[:]), [O_], [acc])
                    else:
                        self.dve(lambda e, av=av, O_=O_: e.tensor_tensor(out=av, in0=O_.t[:], in1=av, op=ALU.add), [O_, acc], [acc])

            npair = len(pairs)
            for idx in range(min(LA, npair)):
                emit_qk(idx)
            for idx in range(npair):
                if idx + LA < npair:
                    emit_qk(idx + LA)
                emit_pv(idx)
            if pi == 2:
                for c in range(S_LEN // 512):
                    cs_ = slice(c * 512, (c + 1) * 512)
                    b_ = bc[c % 2]
                    r_ = rz[c % 2]
                    self.pe(lambda e, b_=b_, cs_=cs_: e.matmul(b_.t[:], lhsT=sel.t[:], rhs=acc.t[:, cs_], start=True, stop=True), [sel, acc], [b_])
                    self.dve(lambda e, b_=b_, r_=r_: e.reciprocal(out=r_.t[:], in_=b_.t[:]), [b_], [r_])
                    self.dve(lambda e, r_=r_, cs_=cs_: e.tensor_tensor(out=aTs.t[:, cs_], in0=acc.t[0:64, cs_], in1=r_.t[:], op=ALU.mult), [acc, r_], [aTs])
                self.dma("sp", self.catT.t[h * 64:(h + 1) * 64, :], aTs.t[:], [aTs], [], aTs)
        self.phase_end(ph)


    def phase_C(self, l, xin, wout_in):
        I = self.inp
        ph = self.phase_begin()
        sb = lambda n, s, d: self.sb(ph, n, s, d)
        ps = lambda n, s, d: self.ps(ph, n, s, d)
        ident = sb("ident", [128, 128], BF16)
        identf = sb("identf", [128, 128], F32)
        wout = sb("wout", [64, 16, D], BF16)
        gffn = sb("gffn", [128, D], F32)
        wr32 = sb("wr32", [128, 8, NEXP], F32)
        catg = [sb(f"catg{i}", [64, 16, 512], BF16) for i in range(2)]
        xt = [sb(f"xt{i}", [128, D], F32) for i in range(2)]
        x1t = [sb(f"x1t{i}", [128, D], F32) for i in range(2)]
        junk = sb("junk", [128, D], BF16)
        ssq = [sb(f"ssq{i}", [128, 1], F32) for i in range(2)]
        rstd = [sb(f"rstd{i}", [128, 1], F32) for i in range(2)]
        hnf = [sb(f"hnf{i}", [128, D], F32) for i in range(2)]
        hnb = [sb(f"hnb{i}", [128, D], BF16) for i in range(2)]
        hTs = [sb(f"hTs{i}", [128, 8, 512], BF16) for i in range(2)]
        hT32 = [sb(f"hT32{i}", [128, 8, 128], F32) for i in range(2)]
        lgs = sb("lgs", [128, NEXP], F32)
        ex = sb("ex", [128, NEXP], F32)
        mx = sb("mx", [128, 1], F32)
        se = sb("se", [128, 1], F32)
        T0 = ps("T0", [128, 1024], BF16)
        T32 = ps("T32", [128, 1024], F32)
        pm = [ps(f"pm{i}", [128, 1024], F32) for i in range(2)]
        lg = ps("lg", [128, NEXP], F32)
        self.dma("sp", ident.t[:], I["c_ident_bf"].t[:, :], [], [ident], ident)
        self.dma("sp", identf.t[:], I["c_ident_f"].t[:, :], [], [identf], identf)
        self.dma("pool", wout.t[:], wout_in.t.rearrange("(c p) n -> p c n", p=64), [], [wout], wout)
        self.dma("sp", gffn.t[:], I["norm_ffn"].t[l:l + 1, :].partition_broadcast(128), [], [gffn], gffn)
        self.dma("sp", wr32.t[:], I["moe_w_router"].t[l].rearrange("(c p) e -> p c e", p=128), [], [wr32], wr32)
        def stage1(ti):
            grp, tt = divmod(ti, 4)
            cg = catg[grp % 2]
            b = ti % 2
            x_, pm_ = xt[b], pm[b]
            tok = slice(ti * 128, (ti + 1) * 128)
            if tt == 0:
                gtok = slice(grp * 512, (grp + 1) * 512)
                self.dma("sp", cg.t[:], self.catT.t.rearrange("(c p) t -> p c t", p=64)[:, :, gtok], [], [cg], cg)
            self.dma("sp", x_.t[:], xin.t[tok, :], [], [x_], x_)
            for half in range(2):
                for c in range(16):
                    self.pe(lambda e, half=half, c=c, cg=cg, tt=tt, pm_=pm_: e.matmul(
                        pm_.t[:, half * 512:(half + 1) * 512], lhsT=cg.t[:, c, tt * 128:(tt + 1) * 128],
                        rhs=wout.t[:, c, half * 512:(half + 1) * 512], start=(c == 0), stop=(c == 15)), [cg, wout], [pm_])

        def stage2(ti):
            b = ti % 2
            x_, x1_, ssq_, rstd_, hnf_, hnb_, pm_ = xt[b], x1t[b], ssq[b], rstd[b], hnf[b], hnb[b], pm[b]
            tok = slice(ti * 128, (ti + 1) * 128)
            for half in range(2):
                hs_ = slice(half * 512, (half + 1) * 512)
                self.dve(lambda e, hs_=hs_, x_=x_, x1_=x1_, pm_=pm_: e.tensor_tensor(out=x1_.t[:, hs_], in0=pm_.t[:, hs_], in1=x_.t[:, hs_], op=ALU.add),
                         [pm_, x_], [x1_])
            self.dma("sp", self.xa.t[tok, :], x1_.t[:], [x1_], [], x1_)
            self.rmsnorm_rstd(x1_.t[:], [x1_], junk.t[:], ssq_, rstd_, D)
            self.dve(lambda e, x1_=x1_, rstd_=rstd_, hnf_=hnf_: e.scalar_tensor_tensor(
                out=hnf_.t[:], in0=x1_.t[:], scalar=rstd_.t[:, 0:1], in1=gffn.t[:], op0=ALU.mult, op1=ALU.mult),
                [x1_, rstd_, gffn], [hnf_])
            self.pool(lambda e, hnf_=hnf_, hnb_=hnb_: e.tensor_copy(out=hnb_.t[:], in_=hnf_.t[:]), [hnf_], [hnb_])

        def stage3(ti):
            grp, tt = divmod(ti, 4)
            hs = hTs[grp % 2]
            b = ti % 2
            hnf_, hnb_, h32_ = hnf[b], hnb[b], hT32[b]
            for j in range(8):
                self.pe(lambda e, j=j, hnb_=hnb_: e.transpose(out=T0.t[:, j * 128:(j + 1) * 128], in_=hnb_.t[:, j * 128:(j + 1) * 128],
                                                            identity=ident.t[:]), [hnb_, ident], [T0])
            self.act(lambda e, hs=hs, tt=tt: e.copy(out=hs.t[:, :, tt * 128:(tt + 1) * 128],
                                                  in_=T0.t[:].rearrange("p (c n) -> p c n", n=128)), [T0], [hs])
            for j in range(8):
                self.pe(lambda e, j=j, hnf_=hnf_: e.transpose(out=T32.t[:, j * 128:(j + 1) * 128], in_=hnf_.t[:, j * 128:(j + 1) * 128],
                                                            identity=identf.t[:]), [hnf_, identf], [T32])
            for half in range(2):
                self.act(lambda e, half=half, h32_=h32_: e.copy(out=h32_.t[:, half * 4:(half + 1) * 4, :].rearrange("p c n -> p (c n)"),
                                                               in_=T32.t[:, half * 512:(half + 1) * 512]), [T32], [h32_])
            for j in range(8):
                self.pe(lambda e, j=j, h32_=h32_: e.matmul(lg.t[:, :], lhsT=h32_.t[:, j, :], rhs=wr32.t[:, j, :],
                                                         start=(j == 0), stop=(j == 7)), [h32_, wr32], [lg])
            self.act(lambda e: e.copy(out=lgs.t[:], in_=lg.t[:]), [lg], [lgs])
            self.dve(lambda e: e.reduce_max(out=mx.t[:], in_=lgs.t[:], axis=AX.X), [lgs], [mx])
            self.dve(lambda e: e.tensor_scalar_mul(out=mx.t[:], in0=mx.t[:], scalar1=-1.0), [mx], [mx])
            self.pool(lambda e: e.memset(se.t[:], 0.0), [], [se])
            self.act(lambda e: e.activation(out=ex.t[:], in_=lgs.t[:], func=AF.Exp, bias=mx.t[:, 0:1], scale=1.0, accum_out=se.t[:, 0:1]),
                     [lgs, mx], [ex, se])
            self.dve(lambda e: e.reciprocal(out=se.t[:], in_=se.t[:]), [se], [se])
            self.dve(lambda e, ti=ti: e.tensor_scalar_mul(out=self.affall.t[:, ti, :], in0=ex.t[:], scalar1=se.t[:, 0:1]), [ex, se], [self.affall])
            if tt == 3:
                gtok = slice(grp * 512, (grp + 1) * 512)
                self.dma("sp", self.hTd.t.rearrange("(c p) t -> p c t", p=128)[:, :, gtok], hs.t[:], [hs], [], hs)

        for s_ in range(NT + 2):
            if s_ < NT:
                stage1(s_)
            if 0 <= s_ - 1 < NT:
                stage2(s_ - 1)
            if 0 <= s_ - 2 < NT:
                stage3(s_ - 2)
        self.phase_end(ph)

    def phase_D(self):
        ph = self.phase_begin()
        sb = lambda n, s, d: self.sb(ph, n, s, d)
        ps = lambda n, s, d: self.ps(ph, n, s, d)
        ones = sb("ones", [128, 128], BF16)
        cmp_ = sb("cmp", [128, NT * NEXP], BF16)
        cnts = sb("cnts", [128, NT * NEXP], F32)
        cnt16 = sb("cnt16", [128, NEXP], F32)
        lo = sb("lo", [128, NEXP], F32)
        mid = sb("mid", [128, NEXP], F32)
        ge = sb("ge", [128, NEXP], F32)
        msk = sb("msk", [128, NT, NEXP], F32)
        cntp = ps("cntp", [128, NT * NEXP], F32)
        aff = self.affall
        self.pool(lambda e: e.memset(ones.t[:], 1.0), [], [ones])
        self.pool(lambda e: e.memset(lo.t[:], 0.0), [], [lo])
        for k in range(1, 29):
            c = 2.0 ** -k
            self.dve(lambda e, c=c: e.tensor_scalar_add(out=mid.t[:], in0=lo.t[:], scalar1=c), [lo], [mid])
            self.dve(lambda e: e.tensor_tensor(out=cmp_.t[:].rearrange("p (t x) -> p t x", x=NEXP), in0=aff.t[:],
                                               in1=mid.t[:].unsqueeze(1).to_broadcast([128, NT, NEXP]), op=ALU.is_ge), [aff, mid], [cmp_])
            for half in range(2):
                self.pe(lambda e, half=half: e.matmul(cntp.t[:, half * 512:(half + 1) * 512], lhsT=ones.t[:],
                                                      rhs=cmp_.t[:, half * 512:(half + 1) * 512], start=True, stop=True), [ones, cmp_], [cntp])
            self.act(lambda e: e.copy(out=cnts.t[:], in_=cntp.t[:]), [cntp], [cnts])
            self.dve(lambda e: e.tensor_reduce(out=cnt16.t[:], in_=cnts.t[:].rearrange("p (t x) -> p x t", x=NEXP), axis=AX.X, op=ALU.add),
                     [cnts], [cnt16])
            self.dve(lambda e, c=c: e.tensor_scalar(out=ge.t[:], in0=cnt16.t[:], scalar1=CAP - 0.5, scalar2=c, op0=ALU.is_ge, op1=ALU.mult),
                     [cnt16], [ge])
            self.dve(lambda e: e.tensor_tensor(out=lo.t[:], in0=lo.t[:], in1=ge.t[:], op=ALU.add), [lo, ge], [lo])
        self.dve(lambda e: e.tensor_tensor(out=msk.t[:], in0=aff.t[:], in1=lo.t[:].unsqueeze(1).to_broadcast([128, NT, NEXP]), op=ALU.is_ge),
                 [aff, lo], [msk])
        self.dve(lambda e: e.tensor_tensor(out=self.gw.t[:], in0=msk.t[:], in1=aff.t[:], op=ALU.mult), [msk, aff], [self.gw])
        self.phase_end(ph)

    def phase_E(self, l, last):
        I = self.inp
        self.S.bg_done()
        ph = self.phase_begin()
        sb = lambda n, s, d: self.sb(ph, n, s, d)
        ps = lambda n, s, d: self.ps(ph, n, s, d)
        SG = 1024
        NQ = SG // 512
        accbs = [sb(f"accb{i}", [128, SG // 128, D], F32) for i in range(2)]
        hss = [sb(f"hs{i}", [128, 8, SG], BF16) for i in range(2)]
        wgt = [sb(f"wgt{i}", [128, 8, 512], BF16) for i in range(2)]
        wut = [sb(f"wut{i}", [128, 8, 512], BF16) for i in range(2)]
        wdt = [sb(f"wdt{i}", [128, 4, D], BF16) for i in range(2)]
        sgt = [sb(f"sgt{i}", [128, 512], F32) for i in range(2)]
        actT = [sb(f"actT{i}", [128, 4, 512], BF16) for i in range(2)]
        pg = [ps(f"pg{i}", [128, 512], F32) for i in range(2)]
        pu = [ps(f"pu{i}", [128, 512], F32) for i in range(2)]
        py = [ps(f"py{i}", [128, 512], F32) for i in range(4)]
        if last:
            gfin = sb("gfin", [128, D], F32)
            junk = sb("junk", [128, D], BF16)
            ssq = [sb(f"ssq{i}", [128, 1], F32) for i in range(2)]
            rstd = [sb(f"rstd{i}", [128, 1], F32) for i in range(2)]
            yo = [sb(f"yo{i}", [128, D], F32) for i in range(2)]
            self.dma("sp", gfin.t[:], I["final_norm"].t[0:1, :].partition_broadcast(128), [], [gfin], gfin)
        wi = 0
        kf = 0
        ky = 0
        ka = 0
        import os
        ECUT = int(os.environ.get('ECUT', 9))
        NSG = int(os.environ.get('ESG', S_LEN // SG))

        def sg_loads(sgi):
            t0 = sgi * SG
            a_, h_ = accbs[sgi % 2], hss[sgi % 2]
            for q4 in range(NQ):
                self.dma("pool", a_.t[:, q4 * 4:(q4 + 1) * 4, :],
                         self.xa.t[t0 + q4 * 512:t0 + (q4 + 1) * 512, :].rearrange("(t p) d -> p t d", p=128), [], [a_], a_)
            self.dma("pool", h_.t[:], self.hTd.t.rearrange("(c p) t -> p c t", p=128)[:, :, t0:t0 + SG], [], [h_], h_)

        sg_loads(0)
        for sgi in range(NSG):
            t0 = sgi * SG
            accb, hs = accbs[sgi % 2], hss[sgi % 2]
            if sgi + 1 < NSG:
                sg_loads(sgi + 1)
            NE = int(os.environ.get('EEXP', NEXP))
            units = [(ex_, g4) for ex_ in range(NE) for g4 in range(SG // 512)]
            wbuf = {}
            ubuf = {}

            def emit_gu(u):
                nonlocal wi, ka, kf
                ex_, g4 = units[u]
                if g4 == 0:
                    wbuf[ex_] = (wgt[wi % 2], wut[wi % 2], wdt[wi % 2])
                    wi += 1
                    wg_, wu_, wd_ = wbuf[ex_]
                    self.dma("sp", wg_.t[:], self.wg_bf.t[l, ex_].rearrange("(c p) f -> p c f", p=128), [], [wg_], wg_)
                    self.dma("sp", wu_.t[:], self.wu_bf.t[l, ex_].rearrange("(c p) f -> p c f", p=128), [], [wu_], wu_)
                    self.dma("sp", wd_.t[:], self.wd_bf.t[l, ex_].rearrange("(c p) n -> p c n", p=128), [], [wd_], wd_)
                wg_, wu_, wd_ = wbuf[ex_]
                tk = slice(g4 * 512, (g4 + 1) * 512)
                aT_ = actT[ka % 2]
                ka += 1
                ubuf[u] = (aT_, wd_)
                for fc in range(4):
                    pg_, pu_, sg_ = pg[kf % 2], pu[kf % 2], sgt[kf % 2]
                    kf += 1
                    fs = slice(fc * 128, (fc + 1) * 128)
                    for j in range(8):
                        self.pe(lambda e, j=j, pg_=pg_, wg_=wg_, fs=fs, tk=tk, hs=hs: e.matmul(pg_.t[:], lhsT=wg_.t[:, j, fs], rhs=hs.t[:, j, tk],
                                                                                         start=(j == 0), stop=(j == 7)), [wg_, hs], [pg_])
                    for j in range(8):
                        self.pe(lambda e, j=j, pu_=pu_, wu_=wu_, fs=fs, tk=tk, hs=hs: e.matmul(pu_.t[:], lhsT=wu_.t[:, j, fs], rhs=hs.t[:, j, tk],
                                                                                         start=(j == 0), stop=(j == 7)), [wu_, hs], [pu_])
                    self.act(lambda e, pg_=pg_, sg_=sg_: e.activation(out=sg_.t[:], in_=pg_.t[:], func=AF.Silu), [pg_], [sg_])
                    self.dve(lambda e, pu_=pu_, sg_=sg_, aT_=aT_, fc=fc: e.tensor_tensor(out=aT_.t[:, fc, :], in0=pu_.t[:], in1=sg_.t[:], op=ALU.mult),
                             [pu_, sg_], [aT_])

            def emit_down(u):
                nonlocal ky
                ex_, g4 = units[u]
                aT_, wd_ = ubuf.pop(u)
                for tt in range(4):
                    tl = g4 * 4 + tt
                    tile = sgi * (SG // 128) + tl
                    for ch in range(2):
                        py_ = py[ky % 4]
                        ky += 1
                        for fc in range(4):
                            self.pe(lambda e, py_=py_, aT_=aT_, fc=fc, tt=tt, wd_=wd_, ch=ch: e.matmul(
                                py_.t[:], lhsT=aT_.t[:, fc, tt * 128:(tt + 1) * 128], rhs=wd_.t[:, fc, ch * 512:(ch + 1) * 512],
                                start=(fc == 0), stop=(fc == 3)), [aT_, wd_], [py_])
                        av = accb.t[:, tl, ch * 512:(ch + 1) * 512]
                        self.dve(lambda e, av=av, py_=py_, tile=tile, ex_=ex_: e.scalar_tensor_tensor(
                            out=av, in0=py_.t[:], scalar=self.gw.t[:, tile, ex_:ex_ + 1], in1=av, op0=ALU.mult, op1=ALU.add),
                            [py_, self.gw, accb], [accb])

            emit_gu(0)
            for u in range(len(units)):
                if u + 1 < len(units):
                    emit_gu(u + 1)
                emit_down(u)
            if not last:
                for q4 in range(NQ):
                    self.dma("pool", self.xb.t[t0 + q4 * 512:t0 + (q4 + 1) * 512, :].rearrange("(t p) d -> p t d", p=128),
                             accb.t[:, q4 * 4:(q4 + 1) * 4, :], [accb], [], accb)
            else:
                for tl in range(SG // 128):
                    b = tl % 2
                    self.rmsnorm_rstd(accb.t[:, tl, :], [accb], junk.t[:], ssq[b], rstd[b], D)
                    self.dve(lambda e, tl=tl, b=b, accb=accb: e.scalar_tensor_tensor(out=yo[b].t[:], in0=accb.t[:, tl, :], scalar=rstd[b].t[:, 0:1],
                                                                        in1=gfin.t[:], op0=ALU.mult, op1=ALU.mult), [accb, rstd[b], gfin], [yo[b]])
                    r0 = t0 + tl * 128
                    self.dma("sp", self.out.t[r0:r0 + 128, :], yo[b].t[:], [yo[b]], [], yo[b])
        self.phase_end(ph)


    def rope_apply(self, x1, x2, cosb, sinb, o1, o2, rt, shape, srcbufs, tabbufs, outbuf):
        v = [r.t[:, 0:shape[1], 0:shape[2]] for r in rt]
        self.dve(lambda e: e.tensor_tensor(out=v[0], in0=x1, in1=cosb, op=ALU.mult), srcbufs + tabbufs, [rt[0]])
        self.dve(lambda e: e.tensor_tensor(out=v[1], in0=x2, in1=sinb, op=ALU.mult), srcbufs + tabbufs, [rt[1]])
        self.dve(lambda e: e.tensor_tensor(out=v[2], in0=x1, in1=sinb, op=ALU.mult), srcbufs + tabbufs, [rt[2]])
        self.dve(lambda e: e.tensor_tensor(out=v[3], in0=x2, in1=cosb, op=ALU.mult), srcbufs + tabbufs, [rt[3]])
        self.pool(lambda e: e.tensor_tensor(out=o1, in0=v[0], in1=v[1], op=ALU.subtract), [rt[0], rt[1]], [outbuf])
        self.pool(lambda e: e.tensor_tensor(out=o2, in0=v[2], in1=v[3], op=ALU.add), [rt[2], rt[3]], [outbuf])

    def layer1_A(self):
        I = self.inp
        self.QcT = self.dram("QcT", [768, S_LEN], BF16)
        self.KcT = self.dram("KcT", [768, S_LEN], BF16)
        self.Vc = self.dram("Vc", [S_LEN, 512], BF16)
        self.QdT = self.dram("QdT", [512, S_LEN], BF16)
        self.KdT = self.dram("KdT", [128, S_LEN], BF16)
        self.Vd = self.dram("Vd", [S_LEN, 128], BF16)
        ph = self.phase_begin()
        sb = lambda n, s, d: self.sb(ph, n, s, d)
        ps = lambda n, s, d: self.ps(ph, n, s, d)
        ident = sb("ident", [128, 128], BF16)
        win = sb("win", [128, 8, 1184], BF16)
        wcq = sb("wcq", [128, 2, 768], BF16)
        wckv = sb("wckv", [128, 1024], BF16)
        gmix = sb("gmix", [128, D], F32)
        gcq = sb("gcq", [128, 256], F32)
        gckv = sb("gckv", [128, 128], F32)
        gdq = sb("gdq", [128, 64], F32)
        gdk = sb("gdk", [128, 64], F32)
        xt = [sb(f"xt{i}", [128, D], F32) for i in range(2)]
        junk = sb("junk", [128, D], BF16)
        ssq = [sb(f"ssq{i}", [128, 1], F32) for i in range(2)]
        rstd = [sb(f"rstd{i}", [128, 1], F32) for i in range(2)]
        ssq2 = sb("ssq2", [128, 1], F32)
        rstd2 = sb("rstd2", [128, 1], F32)
        ssq3 = sb("ssq3", [128, 1], F32)
        rstd3 = sb("rstd3", [128, 1], F32)
        hn = [sb(f"hn{i}", [128, D], BF16) for i in range(2)]
        hT = [sb(f"hT{i}", [128, 8, 128], BF16) for i in range(2)]
        tabs = [sb(f"tabs{i}", [128, 96], F32) for i in range(2)]
        pfs = [sb(f"pf{i}", [128, 1184], F32) for i in range(2)]
        cqn = sb("cqn", [128, 256], BF16)
        cqT = sb("cqT", [128, 2, 128], BF16)
        kvn = sb("kvn", [128, 128], BF16)
        kvT = sb("kvT", [128, 128], BF16)
        qcf = sb("qcf", [128, 768], F32)
        qcb = sb("qcb", [128, 768], BF16)
        kvf = sb("kvf", [128, 1024], F32)
        kcb = sb("kcb", [128, 768], BF16)
        vcb = [sb(f"vcb{i}", [128, 512], BF16) for i in range(2)]
        krf = sb("krf", [128, 1, 32], F32)
        rt = [sb(f"rt{i}", [128, 8, 16], F32) for i in range(4)]
        sq8 = sb("sq8", [128, 640], F32)
        ss10 = sb("ss10", [128, 10], F32)
        r10 = sb("r10", [128, 10], F32)
        dn = sb("dn", [128, 640], F32)
        dqb = sb("dqb", [128, 640], BF16)
        dvb = [sb(f"dvb{i}", [128, 128], BF16) for i in range(2)]
        stqc = [sb(f"stqc{i}", [96, 8, 512], BF16) for i in range(2)]
        stkc = [sb(f"stkc{i}", [96, 8, 512], BF16) for i in range(2)]
        stqd = [sb(f"stqd{i}", [128, 4, 512], BF16) for i in range(2)]
        stkd = [sb(f"stkd{i}", [128, 512], BF16) for i in range(2)]
        T0 = ps("T0", [128, 1024], BF16)
        T1 = ps("T1", [128, 1024], BF16)
        pq = ps("pq", [128, 1536], F32)
        pc = ps("pc", [128, 1024], F32)
        self.dma("sp", ident.t[:], I["c_ident_bf"].t[:, :], [], [ident], ident)
        self.dma("pool", win.t[:], I["odd_w_in"].t.rearrange("(c p) n -> p c n", p=128), [], [win], win)
        self.dma("pool", wcq.t[:], I["odd_w_cq_up"].t.rearrange("(c p) n -> p c n", p=128), [], [wcq], wcq)
        self.dma("pool", wckv.t[:], I["odd_w_ckv_up"].t[:, :], [], [wckv], wckv)
        self.dma("sp", gmix.t[:], I["norm_mix"].t[1:2, :].partition_broadcast(128), [], [gmix], gmix)
        self.dma("sp", gcq.t[:], I["odd_cq_norm"].t[0:1, :].partition_broadcast(128), [], [gcq], gcq)
        self.dma("sp", gckv.t[:], I["odd_ckv_norm"].t[0:1, :].partition_broadcast(128), [], [gckv], gckv)
        self.dma("sp", gdq.t[:], I["odd_dq_norm"].t[0:1, :].partition_broadcast(128), [], [gdq], gdq)
        self.dma("sp", gdk.t[:], I["odd_dk_norm"].t[0:1, :].partition_broadcast(128), [], [gdk], gdk)
        import os
        NT1 = int(os.environ.get('NGRP1', NT // 4)) * 4

        def stage1(ti):
            grp, tt = divmod(ti, 4)
            sqc, skc, sqd, skd = stqc[grp % 2], stkc[grp % 2], stqd[grp % 2], stkd[grp % 2]
            b = ti % 2
            x_, ssq_, rstd_, hn_, hT_, tb_, vcb_, dvb_, pf = xt[b], ssq[b], rstd[b], hn[b], hT[b], tabs[b], vcb[b], dvb[b], pfs[b]
            tok = slice(ti * 128, (ti + 1) * 128)
            tsl = slice(tt * 128, (tt + 1) * 128)
            self.dma("sp", x_.t[:], self.xb.t[tok, :], [], [x_], x_)
            self.dma("sp", tb_.t[:, 0:32], I["c_ropec"].t[tok, :], [], [tb_], tb_)
            self.dma("sp", tb_.t[:, 32:64], I["c_roperow"].t[tok, :], [], [tb_], tb_)
            self.dma("sp", tb_.t[:, 64:96], I["c_ropecol"].t[tok, :], [], [tb_], tb_)
            self.rmsnorm_rstd(x_.t[:], [x_], junk.t[:], ssq_, rstd_, D)
            self.dve(lambda e, x_=x_, rstd_=rstd_, hn_=hn_: e.scalar_tensor_tensor(
                out=hn_.t[:], in0=x_.t[:], scalar=rstd_.t[:, 0:1], in1=gmix.t[:], op0=ALU.mult, op1=ALU.mult),
                [x_, rstd_, gmix], [hn_])
            for j in range(8):
                self.pe(lambda e, j=j, hn_=hn_: e.transpose(out=T0.t[:, j * 128:(j + 1) * 128], in_=hn_.t[:, j * 128:(j + 1) * 128],
                                                           identity=ident.t[:]), [hn_, ident], [T0])
            self.act(lambda e, hT_=hT_: e.copy(out=hT_.t[:].rearrange("p c n -> p (c n)"), in_=T0.t[:]), [T0], [hT_])
            for (c0, n_) in ((0, 512), (512, 512), (1024, 160)):
                for j in range(8):
                    self.pe(lambda e, j=j, c0=c0, n_=n_, hT_=hT_: e.matmul(pq.t[:, c0:c0 + n_], lhsT=hT_.t[:, j, :], rhs=win.t[:, j, c0:c0 + n_],
                                                                        start=(j == 0), stop=(j == 7)), [hT_, win], [pq])
            self.act(lambda e: e.copy(out=pf.t[:], in_=pq.t[:, 0:1184]), [pq], [pf])

        def stage2(ti):
            grp, tt = divmod(ti, 4)
            sqc, skc, sqd, skd = stqc[grp % 2], stkc[grp % 2], stqd[grp % 2], stkd[grp % 2]
            b = ti % 2
            x_, ssq_, rstd_, hn_, hT_, tb_, vcb_, dvb_, pf = xt[b], ssq[b], rstd[b], hn[b], hT[b], tabs[b], vcb[b], dvb[b], pfs[b]
            tok = slice(ti * 128, (ti + 1) * 128)
            tsl = slice(tt * 128, (tt + 1) * 128)
            self.rmsnorm_rstd(pf.t[:, 0:256], [pf], junk.t[:, 0:256], ssq2, rstd2, 256)
            self.dve(lambda e: e.scalar_tensor_tensor(out=cqn.t[:], in0=pf.t[:, 0:256], scalar=rstd2.t[:, 0:1], in1=gcq.t[:],
                                                      op0=ALU.mult, op1=ALU.mult), [pf, rstd2, gcq], [cqn])
            for j in range(2):
                self.pe(lambda e, j=j: e.transpose(out=T1.t[:, j * 128:(j + 1) * 128], in_=cqn.t[:, j * 128:(j + 1) * 128], identity=ident.t[:]),
                        [cqn, ident], [T1])
            self.act(lambda e: e.copy(out=cqT.t[:].rearrange("p c n -> p (c n)"), in_=T1.t[:, 0:256]), [T1], [cqT])
            for (c0, n_) in ((0, 512), (512, 256)):
                for j in range(2):
                    self.pe(lambda e, j=j, c0=c0, n_=n_: e.matmul(pc.t[:, c0:c0 + n_], lhsT=cqT.t[:, j, :], rhs=wcq.t[:, j, c0:c0 + n_],
                                                                 start=(j == 0), stop=(j == 1)), [cqT, wcq], [pc])
            self.act(lambda e: e.copy(out=qcf.t[:], in_=pc.t[:, 0:768]), [pc], [qcf])
            self.pool(lambda e: e.tensor_copy(out=qcb.t[:], in_=qcf.t[:]), [qcf], [qcb])
            qv = qcf.t[:].rearrange("p (h c) -> p h c", c=96)
            qo = qcb.t[:].rearrange("p (h c) -> p h c", c=96)
            cb = tb_.t[:, 0:16].unsqueeze(1).to_broadcast([128, 8, 16])
            sn = tb_.t[:, 16:32].unsqueeze(1).to_broadcast([128, 8, 16])
            self.rope_apply(qv[:, :, 64:80], qv[:, :, 80:96], cb, sn, qo[:, :, 64:80], qo[:, :, 80:96], rt, [128, 8, 16], [qcf], [tb_], qcb)
            self.rmsnorm_rstd(pf.t[:, 256:384], [pf], junk.t[:, 0:128], ssq3, rstd3, 128)
            self.dve(lambda e: e.scalar_tensor_tensor(out=kvn.t[:], in0=pf.t[:, 256:384], scalar=rstd3.t[:, 0:1], in1=gckv.t[:],
                                                      op0=ALU.mult, op1=ALU.mult), [pf, rstd3, gckv], [kvn])
            self.pe(lambda e: e.transpose(out=T1.t[:, 0:128], in_=kvn.t[:], identity=ident.t[:]), [kvn, ident], [T1])
            self.act(lambda e: e.copy(out=kvT.t[:], in_=T1.t[:, 0:128]), [T1], [kvT])
            for c0 in (0, 512):
                self.pe(lambda e, c0=c0: e.matmul(pc.t[:, c0:c0 + 512], lhsT=kvT.t[:], rhs=wckv.t[:, c0:c0 + 512], start=True, stop=True),
                        [kvT, wckv], [pc])
            self.act(lambda e: e.copy(out=kvf.t[:], in_=pc.t[:]), [pc], [kvf])
            kv3 = kvf.t[:].rearrange("p (h c) -> p h c", c=128)
            ko = kcb.t[:].rearrange("p (h c) -> p h c", c=96)
            self.pool(lambda e: e.tensor_copy(out=ko[:, :, 0:64], in_=kv3[:, :, 0:64]), [kvf], [kcb])
            self.pool(lambda e, vcb_=vcb_: e.tensor_copy(out=vcb_.t[:].rearrange("p (h c) -> p h c", c=64), in_=kv3[:, :, 64:128]), [kvf], [vcb_])
            self.dma("sp", self.Vc.t[tok, :], vcb_.t[:], [vcb_], [], vcb_)
            kr = pf.t[:, 384:416].rearrange("p (o c) -> p o c", o=1)
            cb1 = tb_.t[:, 0:16].unsqueeze(1)
            sn1 = tb_.t[:, 16:32].unsqueeze(1)
            self.rope_apply(kr[:, :, 0:16], kr[:, :, 16:32], cb1, sn1, krf.t[:, :, 0:16], krf.t[:, :, 16:32], rt, [128, 1, 16], [pf], [tb_], krf)
            self.pool(lambda e: e.tensor_copy(out=ko[:, :, 64:96], in_=krf.t[:].to_broadcast([128, 8, 32])), [krf], [kcb])
            self.dve(lambda e: e.tensor_tensor(out=sq8.t[:], in0=pf.t[:, 416:1056], in1=pf.t[:, 416:1056], op=ALU.mult), [pf], [sq8])
            self.dve(lambda e: e.tensor_reduce(out=ss10.t[:], in_=sq8.t[:].rearrange("p (g c) -> p g c", c=64), axis=AX.X, op=ALU.add), [sq8], [ss10])
            self.dve(lambda e: e.tensor_scalar(out=r10.t[:], in0=ss10.t[:], scalar1=1.0 / 64, scalar2=EPS, op0=ALU.mult, op1=ALU.add), [ss10], [r10])
            self.act(lambda e: e.activation(out=r10.t[:], in_=r10.t[:], func=AF.Sqrt), [r10], [r10])
            self.dve(lambda e: e.reciprocal(out=r10.t[:], in_=r10.t[:]), [r10], [r10])
            self.dve(lambda e: e.tensor_tensor(out=dn.t[:].rearrange("p (g c) -> p g c", c=64),
                                               in0=pf.t[:, 416:1056].rearrange("p (g c) -> p g c", c=64),
                                               in1=r10.t[:].unsqueeze(2).to_broadcast([128, 10, 64]), op=ALU.mult), [pf, r10], [dn])
            self.pool(lambda e: e.tensor_tensor(out=dn.t[:, 0:512].rearrange("p (g c) -> p g c", c=64),
                                                in0=dn.t[:, 0:512].rearrange("p (g c) -> p g c", c=64),
                                                in1=gdq.t[:].unsqueeze(1).to_broadcast([128, 8, 64]), op=ALU.mult), [dn, gdq], [dn])
            self.pool(lambda e: e.tensor_tensor(out=dn.t[:, 512:640].rearrange("p (g c) -> p g c", c=64),
                                                in0=dn.t[:, 512:640].rearrange("p (g c) -> p g c", c=64),
                                                in1=gdk.t[:].unsqueeze(1).to_broadcast([128, 2, 64]), op=ALU.mult), [dn, gdk], [dn])
            for (g0, g1) in ((0, 8), (8, 10)):
                ng = g1 - g0
                dv_ = dn.t[:, g0 * 64:g1 * 64].rearrange("p (g c) -> p g c", c=64)
                do_ = dqb.t[:, g0 * 64:g1 * 64].rearrange("p (g c) -> p g c", c=64)
                for (tb0, d0) in ((32, 0), (64, 32)):
                    cbx = tb_.t[:, tb0:tb0 + 16].unsqueeze(1).to_broadcast([128, ng, 16])
                    snx = tb_.t[:, tb0 + 16:tb0 + 32].unsqueeze(1).to_broadcast([128, ng, 16])
                    self.rope_apply(dv_[:, :, d0:d0 + 16], dv_[:, :, d0 + 16:d0 + 32], cbx, snx,
                                    do_[:, :, d0:d0 + 16], do_[:, :, d0 + 16:d0 + 32], rt, [128, ng, 16], [dn], [tb_], dqb)
            self.pool(lambda e, dvb_=dvb_: e.tensor_copy(out=dvb_.t[:], in_=pf.t[:, 1056:1184]), [pf], [dvb_])
            self.dma("sp", self.Vd.t[tok, :], dvb_.t[:], [dvb_], [], dvb_)
            for h in range(8):
                self.pe(lambda e, h=h: e.transpose(out=T1.t[0:96, h * 128:(h + 1) * 128], in_=qcb.t[:, h * 96:(h + 1) * 96], identity=ident.t[:]),
                        [qcb, ident], [T1])
            self.act(lambda e, sqc=sqc, tsl=tsl: e.copy(out=sqc.t[:, :, tsl], in_=T1.t[0:96, :].rearrange("p (h n) -> p h n", n=128)), [T1], [sqc])
            for h in range(8):
                self.pe(lambda e, h=h: e.transpose(out=T0.t[0:96, h * 128:(h + 1) * 128], in_=kcb.t[:, h * 96:(h + 1) * 96], identity=ident.t[:]),
                        [kcb, ident], [T0])
            self.act(lambda e, skc=skc, tsl=tsl: e.copy(out=skc.t[:, :, tsl], in_=T0.t[0:96, :].rearrange("p (h n) -> p h n", n=128)), [T0], [skc])
            for j in range(5):
                self.pe(lambda e, j=j: e.transpose(out=T1.t[:, j * 128:(j + 1) * 128], in_=dqb.t[:, j * 128:(j + 1) * 128], identity=ident.t[:]),
                        [dqb, ident], [T1])
            self.act(lambda e, sqd=sqd, tsl=tsl: e.copy(out=sqd.t[:, :, tsl], in_=T1.t[:, 0:512].rearrange("p (h n) -> p h n", n=128)), [T1], [sqd])
            self.act(lambda e, skd=skd, tsl=tsl: e.copy(out=skd.t[:, tsl], in_=T1.t[:, 512:640]), [T1], [skd])
            if tt == 3:
                gtok = slice(grp * 512, (grp + 1) * 512)
                self.dma("sp", self.QcT.t.rearrange("(h p) t -> p h t", p=96)[:, :, gtok], sqc.t[:], [sqc], [], sqc)
                self.dma("sp", self.KcT.t.rearrange("(h p) t -> p h t", p=96)[:, :, gtok], skc.t[:], [skc], [], skc)
                self.dma("sp", self.QdT.t.rearrange("(j p) t -> p j t", p=128)[:, :, gtok], sqd.t[:], [sqd], [], sqd)
                self.dma("sp", self.KdT.t[:, gtok], skd.t[:], [skd], [], skd)

        for s_ in range(NT1 + 1):
            if s_ < NT1:
                stage1(s_)
            if s_ >= 1:
                stage2(s_ - 1)
        self.phase_end(ph)

    def layer1_B(self):
        I = self.inp
        ph = self.phase_begin()
        sb = lambda n, s, d: self.sb(ph, n, s, d)
        ps = lambda n, s, d: self.ps(ph, n, s, d)
        sel = sb("sel", [65, 64], F32)
        qT = [sb(f"qT{i}", [96, S_LEN], BF16) for i in range(2)]
        kT = [sb(f"kT{i}", [96, S_LEN], BF16) for i in range(2)]
        vA = [sb(f"vA{i}", [128, NT, 65], BF16) for i in range(2)]
        oTs = [sb(f"oTs{i}", [64, S_LEN], BF16) for i in range(2)]
        osb = [sb(f"osb{i}", [65, 512], F32) for i in range(2)]
        rz = [sb(f"rz{i}", [64, 512], F32) for i in range(2)]
        Eb = [sb(f"Eb{i}", [128, 512], BF16) for i in range(6)]
        Sp = [ps(f"Sp{i}", [128, 512], F32) for i in range(5)]
        Op = [ps(f"Op{i}", [65, 512], F32) for i in range(2)]
        bc = [ps(f"bc{i}", [64, 512], F32) for i in range(1)]
        self.dma("sp", sel.t[:], I["c_sel"].t[:, :], [], [sel], sel)
        for i in range(2):
            self.pool(lambda e, i=i: e.memset(vA[i].t[:, :, 64:65], 1.0), [], [vA[i]])
        import os
        NH = int(os.environ.get('NH1', 16))
        NQC = int(os.environ.get('NQC1', S_LEN // 512))
        LA = 3
        kvi = -1
        prev_kv = None
        gi = 0
        oc_i = 0
        pending = []
        for h in range(NH):
            if h < 8:
                dk, scale = 96, 96.0 ** -0.5
                qsrc = self.QcT.t[h * 96:(h + 1) * 96, :]
                ksrc = self.KcT.t[h * 96:(h + 1) * 96, :]
                vsrc = self.Vc.t[:, h * 64:(h + 1) * 64]
                kvkey = ("c", h)
            else:
                hd = h - 8
                kvh = hd // 4
                dk, scale = 64, 0.125
                qsrc = self.QdT.t[hd * 64:(hd + 1) * 64, :]
                ksrc = self.KdT.t[kvh * 64:(kvh + 1) * 64, :]
                vsrc = self.Vd.t[:, kvh * 64:(kvh + 1) * 64]
                kvkey = ("d", kvh)
            q_ = qT[h % 2]
            self.dma("sp", q_.t[0:dk, :], qsrc, [], [q_], q_)
            if kvkey != prev_kv:
                kvi += 1
                prev_kv = kvkey
                k_, v_ = kT[kvi % 2], vA[kvi % 2]
                self.dma("sp", k_.t[0:dk, :], ksrc, [], [k_], k_)
                vv = vsrc.rearrange("(m a) c -> a m c", a=128)
                for m0 in range(0, NT, 16):
                    self.dma("pool", v_.t[:, m0:m0 + 16, 0:64], vv[:, m0:m0 + 16, :], [], [v_], v_)
            o_ = oTs[h % 2]
            iters = [(qc, m) for qc in range(NQC) for m in range(NT)]
            n_it = len(iters)
            sbuf_of = {}

            def emit_qk(idx):
                nonlocal gi
                qc, m = iters[idx]
                S_ = Sp[gi % 5]
                E_ = Eb[gi % 6]
                gi += 1
                sbuf_of[idx] = (S_, E_)
                self.pe(lambda e, S_=S_, m=m, qc=qc, k_=k_, q_=q_, dk=dk: e.matmul(
                    S_.t[:], lhsT=k_.t[0:dk, m * 128:(m + 1) * 128], rhs=q_.t[0:dk, qc * 512:(qc + 1) * 512], start=True, stop=True),
                    [k_, q_], [S_])
                self.act(lambda e, S_=S_, E_=E_, scale=scale: e.activation(out=E_.t[:], in_=S_.t[:], func=AF.Exp, scale=scale), [S_], [E_])

            def flush_pending(force=False):
                keep = []
                for item in pending:
                    item[0] -= 1
                    if item[0] <= 0 or force:
                        item[1]()
                    else:
                        keep.append(item)
                pending[:] = keep

            for idx in range(min(LA, n_it)):
                emit_qk(idx)
            for idx in range(n_it):
                if idx + LA < n_it:
                    emit_qk(idx + LA)
                qc, m = iters[idx]
                if m == 0:
                    O_ = Op[oc_i % 2]
                    ob_, rz_, bc_ = osb[oc_i % 2], rz[oc_i % 2], bc[0]
                    oc_i += 1
                S_, E_ = sbuf_of.pop(idx)
                self.pe(lambda e, O_=O_, v_=v_, m=m, E_=E_: e.matmul(O_.t[:], lhsT=v_.t[:, m, :], rhs=E_.t[:], start=(m == 0), stop=(m == NT - 1)),
                        [v_, E_], [O_])
                flush_pending()
                if m == NT - 1:
                    self.dve(lambda e, O_=O_, ob_=ob_: e.tensor_copy(out=ob_.t[:], in_=O_.t[:]), [O_], [ob_])

                    def norm(ob_=ob_, rz_=rz_, bc_=bc_, qc=qc, o_=o_):
                        self.pe(lambda e: e.matmul(bc_.t[:], lhsT=sel.t[:], rhs=ob_.t[:], start=True, stop=True), [sel, ob_], [bc_])
                        self.dve(lambda e: e.reciprocal(out=rz_.t[:], in_=bc_.t[:]), [bc_], [rz_])
                        self.dve(lambda e: e.tensor_tensor(out=o_.t[:, qc * 512:(qc + 1) * 512], in0=ob_.t[0:64, :], in1=rz_.t[:], op=ALU.mult),
                                 [ob_, rz_], [o_])
                    pending.append([4, norm])
            flush_pending(force=True)
            self.dma("sp", self.catT.t[h * 64:(h + 1) * 64, :], o_.t[:], [o_], [], o_)
        self.phase_end(ph)


def prep_inputs(inputs, b):
    f = lambda a: np.ascontiguousarray(np.asarray(a, dtype=np.float32))
    m = {
        "x": f(inputs["x"][b]), "norm_mix": f(inputs["norm_mix"]), "norm_ffn": f(inputs["norm_ffn"]),
        "even_w_in": f(inputs["even_w_in"][0]), "even_gmlp_norm": f(inputs["even_gmlp_norm"][0]).reshape(1, 256),
        "even_w_spatial": f(inputs["even_w_spatial"][0]), "even_b_spatial": f(inputs["even_b_spatial"][0]),
        "even_w_out": f(inputs["even_w_out"][0]), "odd_w_in": f(inputs["odd_w_in"][0]),
        "odd_cq_norm": f(inputs["odd_cq_norm"]), "odd_w_cq_up": f(inputs["odd_w_cq_up"][0]),
        "odd_ckv_norm": f(inputs["odd_ckv_norm"]), "odd_w_ckv_up": f(inputs["odd_w_ckv_up"][0]),
        "odd_dq_norm": f(inputs["odd_dq_norm"]), "odd_dk_norm": f(inputs["odd_dk_norm"]),
        "odd_w_out": f(inputs["odd_w_out"][0]), "moe_w_router": f(inputs["moe_w_router"]),
        "moe_w_gate": f(inputs["moe_w_gate"]), "moe_w_up": f(inputs["moe_w_up"]), "moe_w_down": f(inputs["moe_w_down"]),
        "final_norm": f(inputs["final_norm"]).reshape(1, D),
    }
    return m


def kernel(**inputs):
    nb = inputs["x"].shape[0]
    nc = Builder().build()
    consts = make_consts()
    in_maps = []
    for b in range(nb):
        m = prep_inputs(inputs, b)
        m.update(consts)
        in_maps.append(m)
    res = run_bass_kernel_spmd(nc, in_maps, core_ids=list(range(nb)))
    return np.stack([np.asarray(r["y"]) for r in res.results], axis=0).astype(np.float32)
```
